# Optimizing a Trainium2 kernel written in Bass

```python
import math
import jax, jax.numpy as jnp
from jax import lax
import numpy as np

D_MODEL = 1024
BATCH = 8
SEQ = 4096
DEPTH = 2

GRID_W = 64
CTX_LEN = 256
N_BRANCH = 4
BRANCH_W = 256
HEAD_DIM = 64
N_HEADS = BRANCH_W // HEAD_DIM
S5_GROUP = 16
S5_GROUPS = BRANCH_W // S5_GROUP
S5_STATE = 64
NA_ROWS = 8
NA_COLS = 16
HG_CHUNK = 16
RET_CHUNK = 128
N_EXPERTS = 16
EC_CAPACITY = 2
D_EXPERT = 2816
ROPE_BASE = 10000.0
EPS = 1e-6
IN_SPLITS = (BRANCH_W, 3 * BRANCH_W, 5 * BRANCH_W, 4 * BRANCH_W, N_BRANCH * D_MODEL)
D_IN = sum(IN_SPLITS)

kernel_name = "hybrid_s5_natten_hgrn2_retnet_ec_dit"


def rms_norm(x, w=None):
    xf = x.astype(jnp.float32)
    y = xf * lax.rsqrt(jnp.mean(jnp.square(xf), axis=-1, keepdims=True) + EPS)
    if w is not None:
        y = y * w.astype(jnp.float32)
    return y.astype(x.dtype)


def modulate(h, shift, scale):
    return h * (1 + scale) + shift


def to_heads(t):
    b, l, _ = t.shape
    return jnp.transpose(t.reshape(b, l, N_HEADS, -1), (0, 2, 1, 3))


def from_heads(t):
    b, h, l, d = t.shape
    return jnp.transpose(t, (0, 2, 1, 3)).reshape(b, l, h * d)


def axial_rope(n_tokens):
    t = np.arange(n_tokens)
    quarter = HEAD_DIM // 4
    inv = ROPE_BASE ** (-np.arange(quarter) / quarter)
    ang = np.concatenate([(t // GRID_W)[:, None] * inv, (t % GRID_W)[:, None] * inv], axis=1)
    return jnp.asarray(np.cos(ang), jnp.float32), jnp.asarray(np.sin(ang), jnp.float32)


def apply_rope(x, cos, sin):
    half = x.shape[-1] // 2
    x1, x2 = x[..., :half], x[..., half:]
    cos = cos[None, :, None, :]
    sin = sin[None, :, None, :]
    return jnp.concatenate([x1 * cos - x2 * sin, x2 * cos + x1 * sin], axis=-1)


def s5_scan(u, lam_re, lam_im, log_step, b_re, b_im, s0_re, s0_im):
    dt = jnp.exp(log_step)[:, None]
    mag = jnp.exp(lam_re * dt)
    ar = mag * jnp.cos(lam_im * dt)
    ai = mag * jnp.sin(lam_im * dt)
    den = lam_re * lam_re + lam_im * lam_im
    zr = ((ar - 1.0) * lam_re + ai * lam_im) / den
    zi = (ai * lam_re - (ar - 1.0) * lam_im) / den
    bb_re = zr[..., None] * b_re - zi[..., None] * b_im
    bb_im = zr[..., None] * b_im + zi[..., None] * b_re
    bu_re = jnp.einsum('blgp,gnp->blgn', u, bb_re)
    bu_im = jnp.einsum('blgp,gnp->blgn', u, bb_im)
    bu_re = bu_re.at[:, 0].add(ar * s0_re - ai * s0_im)
    bu_im = bu_im.at[:, 0].add(ar * s0_im + ai * s0_re)
    a_re = jnp.broadcast_to(ar, bu_re.shape)
    a_im = jnp.broadcast_to(ai, bu_im.shape)

    def combine(e1, e2):
        a1r, a1i, b1r, b1i = e1
        a2r, a2i, b2r, b2i = e2
        return (a1r * a2r - a1i * a2i, a1r * a2i + a1i * a2r,
                a2r * b1r - a2i * b1i + b2r, a2r * b1i + a2i * b1r + b2i)

    _, _, x_re, x_im = lax.associative_scan(combine, (a_re, a_im, bu_re, bu_im), axis=1)
    return x_re, x_im


def s5_readout(x_re, x_im, c_re, c_im):
    return jnp.einsum('blgn,gpn->blgp', x_re, c_re) - jnp.einsum('blgn,gpn->blgp', x_im, c_im)


def s5_mixer(u_ctx, u_lat, lam_re, lam_im, log_step, b_re, b_im, c_re, c_im, d_skip, w_glu, with_ctx_out):
    f32 = jnp.float32
    out_dtype = u_lat.dtype
    uc, ul = u_ctx.astype(f32), u_lat.astype(f32)
    gc = uc.reshape(uc.shape[0], uc.shape[1], S5_GROUPS, S5_GROUP)
    gl = ul.reshape(ul.shape[0], ul.shape[1], S5_GROUPS, S5_GROUP)
    zero = jnp.zeros((ul.shape[0], S5_GROUPS, S5_STATE), f32)
    y_ctx, y_lat = 0.0, 0.0
    for d in range(2):
        rev = (lambda t: t) if d == 0 else (lambda t: jnp.flip(t, axis=1))
        prm = (lam_re[d].astype(f32), lam_im[d].astype(f32), log_step[d].astype(f32),
               b_re[d].astype(f32), b_im[d].astype(f32))
        cr, ci = c_re[d].astype(f32), c_im[d].astype(f32)
        xc_re, xc_im = s5_scan(rev(gc), *prm, zero, zero)
        xl_re, xl_im = s5_scan(rev(gl), *prm, xc_re[:, -1], xc_im[:, -1])
        y_lat = y_lat + rev(s5_readout(xl_re, xl_im, cr, ci))
        if with_ctx_out:
            y_ctx = y_ctx + rev(s5_readout(xc_re, xc_im, cr, ci))

    def finish(y, u):
        y = y.reshape(u.shape) + d_skip.astype(f32) * u
        z = jax.nn.gelu(y)
        return (z * jax.nn.sigmoid(z @ w_glu.astype(f32))).astype(out_dtype)

    return (finish(y_ctx, uc) if with_ctx_out else None), finish(y_lat, ul)


def neighbourhood_attention(qkv_ctx, qkv_lat, q_norm_w, k_norm_w, rpb, with_ctx_out):
    b, l, _ = qkv_lat.shape
    lc = qkv_ctx.shape[1]
    rows = l // GRID_W
    wr = min(NA_ROWS, rows)
    scale = HEAD_DIM ** -0.5

    def split(t, n):
        q, k, v = jnp.split(t, 3, axis=-1)
        q = rms_norm(q.reshape(b, n, N_HEADS, HEAD_DIM), q_norm_w)
        k = rms_norm(k.reshape(b, n, N_HEADS, HEAD_DIM), k_norm_w)
        return q, k, v.reshape(b, n, N_HEADS, HEAD_DIM)

    qc, kc, vc = split(qkv_ctx, lc)
    ql, kl, vl = split(qkv_lat, l)
    qg = ql.reshape(b, rows, GRID_W, N_HEADS, HEAD_DIM)
    kg = kl.reshape(b, rows, GRID_W, N_HEADS, HEAD_DIM)
    vg = vl.reshape(b, rows, GRID_W, N_HEADS, HEAD_DIM)
    r = np.arange(rows)
    row_idx = np.clip(r - wr // 2, 0, rows - wr)[:, None] + np.arange(wr)[None, :]
    kband = kg[:, row_idx]
    vband = vg[:, row_idx]
    col = np.arange(GRID_W)
    col_start = np.clip(col - NA_COLS // 2, 0, GRID_W - NA_COLS)
    col_ok = (col[None, :] >= col_start[:, None]) & (col[None, :] < col_start[:, None] + NA_COLS)
    dr_idx = (row_idx - r[:, None] + NA_ROWS - 1).reshape(rows, 1, wr, 1)
    dc_idx = np.clip(col[None, :] - col[:, None] + NA_COLS - 1, 0, 2 * NA_COLS - 2).reshape(1, GRID_W, 1, GRID_W)
    bias = rpb[:, dr_idx, dc_idx]
    s_loc = jnp.einsum('brihd,brajhd->bhriaj', qg, kband).astype(jnp.float32) * scale + bias[None].astype(jnp.float32)
    s_loc = jnp.where(jnp.asarray(col_ok).reshape(1, 1, 1, GRID_W, 1, GRID_W), s_loc, -jnp.inf)
    s_ctx = jnp.einsum('brihd,bchd->bhric', qg, kc).astype(jnp.float32) * scale
    n_loc = wr * GRID_W
    s_all = jnp.concatenate([s_loc.reshape(b, N_HEADS, rows, GRID_W, n_loc), s_ctx], axis=-1)
    p = jax.nn.softmax(s_all, axis=-1).astype(vl.dtype)
    p_loc = p[..., :n_loc].reshape(b, N_HEADS, rows, GRID_W, wr, GRID_W)
    p_ctx = p[..., n_loc:]
    o_lat = jnp.einsum('bhriaj,brajhd->brihd', p_loc, vband) + jnp.einsum('bhric,bchd->brihd', p_ctx, vc)
    o_lat = o_lat.reshape(b, l, BRANCH_W)
    o_ctx = None
    if with_ctx_out:
        sc = jnp.einsum('bqhd,bkhd->bhqk', qc, kc).astype(jnp.float32) * scale
        pc = jax.nn.softmax(sc, axis=-1).astype(vc.dtype)
        o_ctx = jnp.einsum('bhqk,bkhd->bqhd', pc, vc).reshape(b, lc, BRANCH_W)
    return o_ctx, o_lat


def bidir_prefix(run, ctx_dirs, lat_dirs, with_ctx_out):
    o_ctx, o_lat = None, None
    for d in range(2):
        rev = (lambda t: t) if d == 0 else (lambda t: jnp.flip(t, axis=2))
        oc, sc = run(d, [rev(t) for t in ctx_dirs[d]], None)
        ol, _ = run(d, [rev(t) for t in lat_dirs[d]], sc)
        o_lat = rev(ol) if o_lat is None else o_lat + rev(ol)
        if with_ctx_out:
            o_ctx = rev(oc) if o_ctx is None else o_ctx + rev(oc)
    return o_ctx, o_lat


def blocks(t, chunk):
    b, h, l, d = t.shape
    return jnp.moveaxis(t.reshape(b, h, l // chunk, chunk, d), 2, 0)


def unblocks(t):
    n, b, h, c, d = t.shape
    return jnp.moveaxis(t, 0, 2).reshape(b, h, n * c, d)


def gated_chunk_scan(q, k, v, log_f, s0, chunk):
    b, h, _, dk = q.shape
    dv = v.shape[-1]
    if s0 is None:
        s0 = jnp.zeros((b, h, dk, dv), jnp.float32)
    qb, kb, vb, gb = blocks(q, chunk), blocks(k, chunk), blocks(v, chunk), blocks(log_f, chunk)
    cum = jnp.cumsum(gb, axis=3)
    last = cum[:, :, :, -1:, :]
    q_dec = qb * jnp.exp(cum)
    k_dec = kb * jnp.exp(-cum)
    k_end = kb * jnp.exp(last - cum)
    causal = jnp.tril(jnp.ones((chunk, chunk), bool))
    att = jnp.where(causal, jnp.einsum('nbhid,nbhjd->nbhij', q_dec, k_dec), 0.0)
    intra = jnp.einsum('nbhij,nbhje->nbhie', att, vb)
    kv = jnp.einsum('nbhjd,nbhje->nbhde', k_end, vb)
    decay_end = jnp.exp(last[:, :, :, 0, :])

    def step(s, xs):
        kv_n, d_n = xs
        return d_n[..., None] * s + kv_n, s

    s_fin, s_prev = lax.scan(step, s0, (kv, decay_end))
    cross = jnp.einsum('nbhid,nbhde->nbhie', q_dec, s_prev)
    return unblocks(intra + cross), s_fin


def hgrn2_mixer(p_ctx, p_lat, lower_bound, norm_w, with_ctx_out):
    f32 = jnp.float32
    out_dtype = p_lat.dtype
    lb = lower_bound.astype(f32)

    def prep(p):
        q, f_fw, f_bw, i, g = jnp.split(p.astype(f32), 5, axis=-1)
        qh = to_heads(jax.nn.silu(q))
        vh = to_heads(i)
        dirs = []
        for f_logit in (f_fw, f_bw):
            fg = lb + (1.0 - lb) * jax.nn.sigmoid(f_logit)
            dirs.append((qh, to_heads(1.0 - fg), vh, to_heads(jnp.log(fg))))
        return dirs, g

    ctx_dirs, g_ctx = prep(p_ctx)
    lat_dirs, g_lat = prep(p_lat)
    run = lambda d, a, s0: gated_chunk_scan(a[0], a[1], a[2], a[3], s0, HG_CHUNK)
    o_ctx, o_lat = bidir_prefix(run, ctx_dirs, lat_dirs, with_ctx_out)

    def finish(o, g):
        return (from_heads(rms_norm(o, norm_w)) * jax.nn.silu(g)).astype(out_dtype)

    return (finish(o_ctx, g_ctx) if with_ctx_out else None), finish(o_lat, g_lat)


def retention_chunk_scan(q, k, v, log_gamma, s0, chunk):
    b, h, _, dk = q.shape
    dv = v.shape[-1]
    if s0 is None:
        s0 = jnp.zeros((b, h, dk, dv), jnp.float32)
    qb, kb, vb = blocks(q, chunk), blocks(k, chunk), blocks(v, chunk)
    idx = jnp.arange(chunk, dtype=jnp.float32)
    diff = idx[:, None] - idx[None, :]
    lg = log_gamma[:, None, None]
    decay = jnp.where(diff >= 0, jnp.exp(jnp.maximum(diff, 0.0) * lg), 0.0)
    xi = jnp.exp((idx + 1.0)[None, :] * log_gamma[:, None])[..., None]
    zeta = jnp.exp((chunk - 1.0 - idx)[None, :] * log_gamma[:, None])[..., None]
    gamma_c = jnp.exp(chunk * log_gamma)[:, None, None]
    intra = jnp.einsum('nbhij,nbhje->nbhie', jnp.einsum('nbhid,nbhjd->nbhij', qb, kb) * decay, vb)
    kv = jnp.einsum('nbhjd,nbhje->nbhde', kb * zeta, vb)

    def step(s, kv_n):
        return gamma_c * s + kv_n, s

    s_fin, s_prev = lax.scan(step, s0, kv)
    cross = jnp.einsum('nbhid,nbhde->nbhie', qb, s_prev) * xi
    return unblocks(intra + cross), s_fin


def retention_mixer(p_ctx, p_lat, decay_logit, with_ctx_out):
    f32 = jnp.float32
    out_dtype = p_lat.dtype
    cos, sin = axial_rope(p_lat.shape[1])

    def prep(p, rope):
        b, n, _ = p.shape
        q, k, v, g = jnp.split(p.astype(f32), 4, axis=-1)
        q = q.reshape(b, n, N_HEADS, HEAD_DIM)
        k = k.reshape(b, n, N_HEADS, HEAD_DIM)
        if rope:
            q, k = apply_rope(q, cos, sin), apply_rope(k, cos, sin)
        qh = jnp.transpose(q, (0, 2, 1, 3))
        kh = jnp.transpose(k, (0, 2, 1, 3)) * (HEAD_DIM ** -0.5)
        return (qh, kh, to_heads(v)), g

    qkv_c, g_ctx = prep(p_ctx, False)
    qkv_l, g_lat = prep(p_lat, True)
    log_gamma = jax.nn.log_sigmoid(decay_logit.astype(f32))
    run = lambda d, a, s0: retention_chunk_scan(a[0], a[1], a[2], log_gamma[d], s0, RET_CHUNK)
    o_ctx, o_lat = bidir_prefix(run, [qkv_c, qkv_c], [qkv_l, qkv_l], with_ctx_out)

    def finish(o, g):
        return (from_heads(rms_norm(o)) * jax.nn.silu(g)).astype(out_dtype)

    return (finish(o_ctx, g_ctx) if with_ctx_out else None), finish(o_lat, g_lat)


def merge_branches(outs, gate_logits, w_branch, w_out):
    gates = jnp.split(gate_logits, N_BRANCH, axis=-1)
    m = 0.0
    for br in range(N_BRANCH):
        m = m + jax.nn.sigmoid(gates[br]) * (outs[br] @ w_branch[br])
    return m @ w_out


def token_mixers(h_ctx, h_lat, w_in, s5_lam_re, s5_lam_im, s5_log_step, s5_b_re, s5_b_im, s5_c_re, s5_c_im,
                 s5_d, s5_glu_w, na_q_norm, na_k_norm, na_rpb, hg_lb, hg_norm_w, ret_decay_logit,
                 w_branch, w_out, with_ctx_out):
    cuts = [int(v) for v in np.cumsum(IN_SPLITS)[:-1]]
    pl = jnp.split(h_lat @ w_in, cuts, axis=-1)
    pc = jnp.split(h_ctx @ w_in, cuts, axis=-1)
    s5_c, s5_l = s5_mixer(pc[0], pl[0], s5_lam_re, s5_lam_im, s5_log_step, s5_b_re, s5_b_im,
                          s5_c_re, s5_c_im, s5_d, s5_glu_w, with_ctx_out)
    na_c, na_l = neighbourhood_attention(pc[1], pl[1], na_q_norm, na_k_norm, na_rpb, with_ctx_out)
    hg_c, hg_l = hgrn2_mixer(pc[2], pl[2], hg_lb, hg_norm_w, with_ctx_out)
    rt_c, rt_l = retention_mixer(pc[3], pl[3], ret_decay_logit, with_ctx_out)
    out_l = merge_branches((s5_l, na_l, hg_l, rt_l), pl[4], w_branch, w_out)
    out_c = merge_branches((s5_c, na_c, hg_c, rt_c), pc[4], w_branch, w_out) if with_ctx_out else None
    return out_c, out_l


def expert_choice_ffn(h, router_w, w_gate, w_up, w_down):
    b, n, _ = h.shape
    cap = EC_CAPACITY * n // N_EXPERTS
    aff = jax.nn.softmax(jnp.einsum('bnd,de->bne', h.astype(jnp.float32), router_w.astype(jnp.float32)), axis=-1)
    g, idx = lax.top_k(jnp.swapaxes(aff, 1, 2), cap)
    idx_e = jnp.moveaxis(idx, 1, 0)
    g_e = jnp.moveaxis(g, 1, 0)
    b_idx = jnp.arange(b)[:, None]

    def one_expert(args):
        wg, wu, wd, ie, ge = args
        xe = h[b_idx, ie]
        ye = (jax.nn.silu(xe @ wg) * (xe @ wu)) @ wd
        return ye * ge[..., None].astype(ye.dtype)

    y = lax.map(one_expert, (w_gate, w_up, w_down, idx_e, g_e))
    return jnp.zeros_like(h).at[jnp.arange(b)[None, :, None], idx_e].add(y.astype(h.dtype))


def setup_inputs(seed: int = 0) -> dict:
    key = jax.random.key(seed)
    ks = iter(jax.random.split(key, 40))
    f32 = jnp.float32

    def nrm(shape, s):
        return s * jax.random.normal(next(ks), shape, f32)

    D, H, G, N, P, E, F = D_MODEL, N_HEADS, S5_GROUPS, S5_STATE, S5_GROUP, N_EXPERTS, D_EXPERT
    lam_im_base = jnp.pi * jnp.arange(N, dtype=f32)
    ret_base = jnp.log(jnp.exp2(5.0 + jnp.arange(H, dtype=f32)) - 1.0)
    return {
        'x': nrm((BATCH, SEQ, D), 1.0),
        'c': nrm((BATCH, D), 1.0),
        'ctx': nrm((BATCH, CTX_LEN, D), 1.0),
        'c_ctx': nrm((D,), 1.0),
        'ada_w': nrm((DEPTH, D, 6 * D), 0.02),
        'ada_b': nrm((DEPTH, 6 * D), 0.01),
        'norm_mix_w': 1.0 + nrm((DEPTH, D), 0.02),
        'norm_ffn_w': 1.0 + nrm((DEPTH, D), 0.02),
        'w_in': nrm((DEPTH, D, D_IN), D ** -0.5),
        's5_lam_re': -0.5 + nrm((DEPTH, 2, G, N), 0.01),
        's5_lam_im': lam_im_base + nrm((DEPTH, 2, G, N), 0.01),
        's5_log_step': jax.random.uniform(next(ks), (DEPTH, 2, G), f32, math.log(1e-3), math.log(1e-1)),
        's5_b_re': nrm((DEPTH, 2, G, N, P), (2.0 * P) ** -0.5),
        's5_b_im': nrm((DEPTH, 2, G, N, P), (2.0 * P) ** -0.5),
        's5_c_re': nrm((DEPTH, 2, G, P, N), N ** -0.5),
        's5_c_im': nrm((DEPTH, 2, G, P, N), N ** -0.5),
        's5_d': nrm((DEPTH, BRANCH_W), 1.0),
        's5_glu_w': nrm((DEPTH, BRANCH_W, BRANCH_W), BRANCH_W ** -0.5),
        'na_q_norm': 1.0 + nrm((DEPTH, HEAD_DIM), 0.02),
        'na_k_norm': 1.0 + nrm((DEPTH, HEAD_DIM), 0.02),
        'na_rpb': nrm((DEPTH, H, 2 * NA_ROWS - 1, 2 * NA_COLS - 1), 0.02),
        'hg_lower_bounds': nrm((DEPTH, BRANCH_W), 0.1),
        'hg_norm_w': 1.0 + nrm((DEPTH, HEAD_DIM), 0.02),
        'ret_decay_logit': ret_base + nrm((DEPTH, 2, H), 0.01),
        'w_branch': nrm((DEPTH, N_BRANCH, BRANCH_W, D), BRANCH_W ** -0.5),
        'w_out': nrm((DEPTH, D, D), D ** -0.5),
        'router_w': nrm((DEPTH, D, E), D ** -0.5),
        'ex_w_gate': nrm((DEPTH, E, D, F), D ** -0.5),
        'ex_w_up': nrm((DEPTH, E, D, F), D ** -0.5),
        'ex_w_down': nrm((DEPTH, E, F, D), F ** -0.5),
    }


def reference(x, c, ctx, c_ctx, ada_w, ada_b, norm_mix_w, norm_ffn_w, w_in, s5_lam_re, s5_lam_im,
              s5_log_step, s5_b_re, s5_b_im, s5_c_re, s5_c_im, s5_d, s5_glu_w, na_q_norm, na_k_norm,
              na_rpb, hg_lower_bounds, hg_norm_w, ret_decay_logit, w_branch, w_out, router_w,
              ex_w_gate, ex_w_up, ex_w_down):
    lb_p = jax.nn.softmax(hg_lower_bounds.astype(jnp.float32), axis=0)
    lower_bounds = jnp.cumsum(lb_p, axis=0) - lb_p[0]
    cond_lat = jax.nn.silu(c)
    cond_ctx = jax.nn.silu(c_ctx)[None]
    xc = ctx
    for li in range(DEPTH):
        last = li == DEPTH - 1
        mod_l = [m[:, None] for m in jnp.split(cond_lat @ ada_w[li] + ada_b[li], 6, axis=-1)]
        mod_c = [m[:, None] for m in jnp.split(cond_ctx @ ada_w[li] + ada_b[li], 6, axis=-1)]
        sh1_l, sc1_l, g1_l, sh2_l, sc2_l, g2_l = mod_l
        sh1_c, sc1_c, g1_c, sh2_c, sc2_c, g2_c = mod_c
        h_l = modulate(rms_norm(x, norm_mix_w[li]), sh1_l, sc1_l)
        h_c = modulate(rms_norm(xc, norm_mix_w[li]), sh1_c, sc1_c)
        mix_c, mix_l = token_mixers(h_c, h_l, w_in[li], s5_lam_re[li], s5_lam_im[li], s5_log_step[li],
                                    s5_b_re[li], s5_b_im[li], s5_c_re[li], s5_c_im[li], s5_d[li], s5_glu_w[li],
                                    na_q_norm[li], na_k_norm[li], na_rpb[li], lower_bounds[li], hg_norm_w[li],
                                    ret_decay_logit[li], w_branch[li], w_out[li], not last)
        x = x + g1_l * mix_l
        h_l = modulate(rms_norm(x, norm_ffn_w[li]), sh2_l, sc2_l)
        x = x + g2_l * expert_choice_ffn(h_l, router_w[li], ex_w_gate[li], ex_w_up[li], ex_w_down[li])
        if not last:
            xc = xc + g1_c * mix_c
            h_c = modulate(rms_norm(xc, norm_ffn_w[li]), sh2_c, sc2_c)
            xc = xc + g2_c * expert_choice_ffn(h_c, router_w[li], ex_w_gate[li], ex_w_up[li], ex_w_down[li])
    return x
```

```python
import os
import numpy as np
import ml_dtypes
import concourse.bass as bass
import concourse.mybir as mybir
from concourse.bass_utils import run_bass_kernel_spmd
from contextlib import ExitStack

F32 = mybir.dt.float32
BF16 = mybir.dt.bfloat16
I32 = mybir.dt.int32
ACT = mybir.ActivationFunctionType
ALU = mybir.AluOpType
AX = mybir.AxisListType

D = 1024
SEQ = 4096
CTX = 256
T = SEQ + CTX
NT = T // 128
DEPTH = 2
D_IN = 7424
NE = 16
FE = 2816
EPS = 1e-6
LIMIT = int(os.environ.get("MK_LIMIT", "0"))
SAME_ENG_SYNC = bool(os.environ.get("MK_SAMESYNC"))
TB = [(i * 512, 512) for i in range(8)] + [(4096, 256)]


class Buf:
    __slots__ = ("name", "w", "r", "dma_sem", "dma_cnt", "pend_w", "pend_r")

    def __init__(self, name):
        self.name = name
        self.w = None
        self.r = {}
        self.dma_sem = None
        self.dma_cnt = 0
        self.pend_w = {}
        self.pend_r = {}


class _Eng:
    def __init__(self, name, h, sem):
        self.name = name
        self.h = h
        self.sem = sem
        self.cnt = 0
        self.seen = {}
        self.seen_dma = {}


class Sched:
    def __init__(self, nc, stack):
        self.nc = nc
        self.stack = stack
        hs = {"pe": nc.tensor, "act": nc.scalar, "dve": nc.vector, "pool": nc.gpsimd, "sp": nc.sync}
        self.eng = {}
        for k, h in hs.items():
            sem = stack.enter_context(nc.semaphore("s_" + k))
            self.eng[k] = _Eng(k, h, sem)
        self.dma_sems = []
        self.free_sems = []
        self.scope_ents = []
        self.n_ins = 0

    def _wait_eng(self, E, e2, s, force=False):
        if e2 == E.name and E.name == "pe" and not (force or SAME_ENG_SYNC):
            return
        if E.seen.get(e2, 0) >= s:
            return
        E.h.wait_ge(self.eng[e2].sem, s)
        E.seen[e2] = s

    def _wait_dma(self, E, sem, val):
        k = id(sem)
        if E.seen_dma.get(k, 0) >= val:
            return
        E.h.wait_ge(sem, val)
        E.seen_dma[k] = val

    def _sync(self, E, r, w, rs=()):
        for b in r:
            if b.w is not None:
                self._wait_eng(E, b.w[0], b.w[1])
            if b.dma_cnt:
                self._wait_dma(E, b.dma_sem[0], b.dma_cnt)
        for b in rs:
            if b.w is not None:
                self._wait_eng(E, b.w[0], b.w[1], force=True)
            if b.dma_cnt:
                self._wait_dma(E, b.dma_sem[0], b.dma_cnt)
        for b in w:
            if b.w is not None:
                self._wait_eng(E, b.w[0], b.w[1])
            for e2, s in b.r.items():
                self._wait_eng(E, e2, s)
            if b.dma_cnt:
                self._wait_dma(E, b.dma_sem[0], b.dma_cnt)

    def op(self, e, fn, r=(), w=(), rs=()):
        if LIMIT and self.n_ins >= LIMIT:
            return None
        E = self.eng[e]
        self._sync(E, r, w, rs)
        ins = fn(E.h)
        E.cnt += 1
        ins.then_inc(E.sem, 1)
        for b in r:
            b.r[e] = E.cnt
        for b in rs:
            b.r[e] = E.cnt
        for b in w:
            b.w = (e, E.cnt)
            b.r = {}
        self.n_ins += 1
        return ins

    def _get_dma_sem(self, sb):
        if sb.dma_sem is None:
            if self.free_sems:
                ent = self.free_sems.pop()
            else:
                ent = [self.stack.enter_context(self.nc.semaphore("d%d" % len(self.dma_sems))), 0]
                self.dma_sems.append(ent)
            self.scope_ents.append(ent)
            sb.dma_sem = ent
        return sb.dma_sem

    def release_phase_sems(self):
        self.free_sems.extend(self.scope_ents)
        self.scope_ents = []

    def dma(self, q, out, in_, sb, dr=None, load=True, **kw):
        if LIMIT and self.n_ins >= LIMIT:
            return None
        Q = self.eng[q]
        ent = self._get_dma_sem(sb)
        if load:
            self._sync(Q, (), (sb,))
        else:
            self._sync(Q, (sb,), ())
        if dr is not None:
            pend = dr.pend_w if load else dr.pend_r
            for sem, val in pend.values():
                self._wait_dma(Q, sem, val)
            if not load:
                for sem, val in dr.pend_w.values():
                    self._wait_dma(Q, sem, val)
        ins = Q.h.dma_start(out=out, in_=in_, **kw)
        ent[1] += 16
        sb.dma_cnt = ent[1]
        ins.then_inc(ent[0], 16)
        if dr is not None:
            (dr.pend_r if load else dr.pend_w)[id(ent[0])] = (ent[0], ent[1])
        self.n_ins += 1
        return ins

    def barrier(self):
        for E in self.eng.values():
            for E2 in self.eng.values():
                if E2 is not E and E2.cnt:
                    self._wait_eng(E, E2.name, E2.cnt)
            for sem, cnt in self.dma_sems:
                if cnt:
                    self._wait_dma(E, sem, cnt)


class Rot:
    def __init__(self, K, name, shape, dtype, n):
        self.t = [K.sb(f"{name}{i}", shape, dtype) for i in range(n)]
        self.b = [Buf(f"{name}{i}") for i in range(n)]
        self.i = 0

    def get(self):
        i = self.i
        self.i = (i + 1) % len(self.t)
        return self.t[i], self.b[i]


class Ctx:
    pass


def mk_ctx(nc, stack, debug):
    K = Ctx()
    K.nc = nc
    K.S = Sched(nc, stack)
    K.top = stack
    K.stack = stack
    K.debug = set(debug or ())
    K.inputs = {}
    K.dbuf = {}

    K.uid = 0

    def sb(name, shape, dtype=F32):
        K.uid += 1
        return K.stack.enter_context(nc.sbuf_tensor(f"{name}_{K.uid}", list(shape), dtype))
    K.sb = sb

    def inp(name, shape, dtype=F32):
        if name not in K.inputs:
            K.inputs[name] = nc.dram_tensor(name, list(shape), dtype, kind="ExternalInput").ap()
        return K.inputs[name]
    K.inp = inp

    def dram(name, shape, dtype):
        kind = "ExternalOutput" if name in K.debug else "Internal"
        ap = nc.dram_tensor(name, list(shape), dtype, kind=kind).ap()
        K.dbuf[name] = Buf(name)
        return ap, K.dbuf[name]
    K.dram = dram
    K.ps = [stack.enter_context(nc.psum_tensor(f"ps{i}", [128, 512], F32)) for i in range(8)]
    K.psb = [Buf(f"ps{i}") for i in range(8)]
    K.pi = 0

    def bank():
        i = K.pi
        K.pi = (i + 1) % 8
        return K.ps[i], K.psb[i]
    K.bank = bank
    K.evi = 0

    def ev():
        K.evi ^= 1
        return "act" if K.evi else "dve"
    K.ev = ev
    return K


class phase:
    def __init__(self, K):
        self.K = K

    def __enter__(self):
        self.prev = self.K.stack
        self.st = ExitStack()
        self.st.__enter__()
        self.K.stack = self.st
        return self

    def __exit__(self, *a):
        self.K.S.barrier()
        self.K.S.release_phase_sems()
        self.K.stack = self.prev
        return self.st.__exit__(*a)


def evac(K, eng, out, in_, r, w, func=None, scale=None, bias=None, rs=()):
    S = K.S
    if eng == "act":
        kw = {}
        if scale is not None:
            kw["scale"] = scale
        if bias is not None:
            kw["bias"] = bias
        f = func if func is not None else (ACT.Identity if bias is not None else ACT.Copy)
        S.op("act", lambda h: h.activation(out=out, in_=in_, func=f, **kw), r=r, w=w, rs=rs)
    else:
        assert func is None
        if scale is None and bias is None:
            S.op(eng, lambda h: h.tensor_copy(out=out, in_=in_), r=r, w=w)
        elif bias is None:
            S.op(eng, lambda h: h.tensor_scalar(out=out, in0=in_, scalar1=scale, scalar2=None, op0=ALU.mult), r=r, w=w, rs=rs)
        else:
            sc = 1.0 if scale is None else scale
            S.op(eng, lambda h: h.tensor_scalar(out=out, in0=in_, scalar1=sc, scalar2=bias, op0=ALU.mult, op1=ALU.add), r=r, w=w, rs=rs)


def dump(K, name, src_ap, src_b, shape, dtype):
    if name in K.debug:
        ap, b = K.dram(name, shape, dtype)
        K.S.dma("sp", ap, src_ap, src_b, dr=b, load=False)


def phase0(K, li):
    nc, S = K.nc, K.S
    ada_w = K.inp("ada_w", [DEPTH, D, 6 * D])
    ada_bT = K.inp("ada_bT", [DEPTH, 128, 48])
    nmw = K.inp("norm_mix_wT", [DEPTH, 128, 8])
    nfw = K.inp("norm_ffn_wT", [DEPTH, 128, 8])
    with phase(K):
        aw = Rot(K, "adaw", [128, 6 * D], F32, 2)
        adab = K.sb("adab", [128, 48]); adab_b = Buf("adab")
        nw = K.sb("nw", [128, 2, 8]); nw_b = Buf("nw")
        dg = Rot(K, "dg", [128, 128], F32, 3)
        S.dma("sp", adab[:], ada_bT[li], adab_b)
        S.dma("sp", nw[:, 0, :], nmw[li], nw_b)
        S.dma("sp", nw[:, 1, :], nfw[li], nw_b)
        pmA, pmAb = K.bank()
        pmB, pmBb = K.bank()
        for k in range(8):
            a, ab = aw.get()
            S.dma("sp", a[:], ada_w[li, k * 128:(k + 1) * 128, :], ab)
            pm, pmb = (pmA, pmAb) if k < 4 else (pmB, pmBb)
            c0 = (k % 4) * 96
            for j in range(48):
                S.op("pe", lambda h: h.matmul(pm[:, c0 + 2 * j:c0 + 2 * j + 2], lhsT=a[:, j * 128:(j + 1) * 128], rhs=K.cond[:, k, :],
                                              start=True, stop=True),
                     r=[ab, K.cond_b], w=[pmb])
        mv = K.mv
        acc = K.sb("p0acc", [128, 2, 96]); acc_b = Buf("p0acc")
        for i, (pm, pmb) in enumerate(((pmA, pmAb), (pmB, pmBb))):
            S.op("dve", lambda h: h.tensor_reduce(out=acc[:, i, :], in_=pm[:, 0:384].rearrange("p (k c) -> p c k", k=4), axis=AX.X, op=ALU.add),
                 r=[pmb], w=[acc_b])
        S.op("dve", lambda h: h.tensor_tensor(out=acc[:, 0, :], in0=acc[:, 0, :], in1=acc[:, 1, :], op=ALU.add), r=[acc_b], w=[acc_b])
        S.op("dve", lambda h: h.tensor_tensor(out=mv[:].rearrange("p v k s -> p (v k) s"),
                                              in0=acc[:, 0, :].rearrange("p (j s) -> p j s", s=2),
                                              in1=adab[:].unsqueeze(2).to_broadcast([128, 48, 2]), op=ALU.add),
             r=[acc_b, adab_b], w=[K.mv_b])
        for (Ai, vi, wi) in ((K.A1, 1, 0), (K.A2, 4, 1)):
            for s in range(2):
                S.op("dve", lambda h: h.scalar_tensor_tensor(out=Ai[:, :, s], in0=mv[:, vi, :, s], scalar=1.0, in1=nw[:, wi, :],
                                                             op0=ALU.add, op1=ALU.mult),
                     r=[K.mv_b, nw_b], w=[K.A_b])
        for gi, vi in enumerate((2, 5)):
            for s in range(2):
                for half in range(2):
                    pb, pbb = K.bank()
                    for q in range(4):
                        kk = half * 4 + q
                        d, db = dg.get()
                        S.op("dve", lambda h: h.tensor_scalar(out=d[:], in0=K.ident[:], scalar1=mv[:, vi, kk, s:s + 1], scalar2=None, op0=ALU.mult),
                             r=[K.ident_b], w=[db], rs=[K.mv_b])
                        S.op("pe", lambda h: h.matmul(pb[:, q * 128:(q + 1) * 128], lhsT=K.ones[:], rhs=d[:], start=True, stop=True),
                             r=[K.ones_b, db], w=[pbb])
                    evac(K, K.ev(), K.gbc[:, gi * 2 + s, half * 512:(half + 1) * 512], pb[:], r=[pbb], w=[K.gbc_b])
        dump(K, "dbg_mv", K.mv[:], K.mv_b, [128, 6, 8, 2], F32)
        dump(K, "dbg_gbc", K.gbc[:], K.gbc_b, [128, 4, D], F32)


def norm_to_fm(K, xsrc, xsrc_b, hT, hT_b, A, B_vi, xn_dram=None):
    nc, S = K.nc, K.S
    groups = [(0, 2, 1)] + [(2 + 4 * i, 4, 0) for i in range(8)]
    with phase(K):
        xr = Rot(K, "xr", [128, D], F32, 3)
        xnr = Rot(K, "xnr", [128, D], F32, 8)
        junk = K.sb("junk", [128, D], BF16); junk_b = Buf("junk")
        ssr = Rot(K, "ssr", [128, 4], F32, 4)
        xnb = Rot(K, "xnb", [128, D], BF16, 3) if xn_dram is not None else None
        for (t0, n, s) in groups:
            xns = []
            for i in range(n):
                t = t0 + i
                x, xb = xr.get()
                S.dma("sp", x[:], xsrc[t * 128:(t + 1) * 128, :], xb, dr=xsrc_b)
                ss, ssb = ssr.get()
                S.op("pool", lambda h: h.memset(ss[:], 0.0), w=[ssb])
                S.op("act", lambda h: h.activation(out=junk[:], in_=x[:], func=ACT.Square, accum_out=ss[:, 0:1]), r=[xb], w=[junk_b, ssb])
                S.op("act", lambda h: h.activation(out=ss[:, 1:2], in_=ss[:, 0:1], func=ACT.Sqrt, scale=1.0 / D, bias=K.epsc[:, 0:1]), r=[ssb, K.epsc_b], w=[ssb])
                S.op("dve", lambda h: h.reciprocal(out=ss[:, 2:3], in_=ss[:, 1:2]), r=[ssb], w=[ssb])
                xn, xnbuf = xnr.get()
                S.op("act", lambda h: h.activation(out=xn[:], in_=x[:], func=ACT.Copy, scale=ss[:, 2:3]), r=[xb], w=[xnbuf], rs=[ssb])
                xns.append((xn, xnbuf))
                if xn_dram is not None:
                    o, ob = xnb.get()
                    S.op("pool", lambda h: h.tensor_copy(out=o[:], in_=xn[:]), r=[xnbuf], w=[ob])
                    S.dma("sp", xn_dram[0][t * 128:(t + 1) * 128, :], o[:], ob, dr=xn_dram[1], load=False)
            for j in range(8):
                ps, psb = K.bank()
                for i, (xn, xnbuf) in enumerate(xns):
                    S.op("pe", lambda h: h.transpose(out=ps[:, i * 128:(i + 1) * 128], in_=xn[:, j * 128:(j + 1) * 128], identity=K.ident[:]),
                         r=[xnbuf, K.ident_b], w=[psb])
                evac(K, K.ev(), hT[:, j, t0 * 128:(t0 + n) * 128], ps[:, 0:n * 128], r=[psb, K.A_b, K.mv_b], w=[hT_b],
                     scale=A[:, j, s:s + 1], bias=K.mv[:, B_vi, j, s:s + 1])


BLOCKS = [
    ("s5u", "fm", None), ("naq", "fm", None), ("nak", "fm", None), ("nav", "tm", None),
    ("hgq", "fm", "silu"), ("hgff", "fm32", None), ("hgfb", "fm32", None), ("hgi", "tm", None), ("hgg", "fm", "silu"),
    ("rtq", "rope", 1.0), ("rtk", "rope", 0.125), ("rtv", "tm", None), ("rtg", "fm", "silu"),
] + [(f"gate{i}", "gate", i) for i in range(16)]


def alloc_proj_scratch(K, external=False):
    P = {}

    def mk(name, shape, dt):
        if external:
            return (K.inp(name, shape, dt), Buf(name))
        return K.dram(name, shape, dt)
    for name, kind, _ in BLOCKS:
        if kind == "tm":
            P[name] = mk("P_" + name, [T, 256], BF16)
        elif kind == "fm32":
            P[name] = mk("P_" + name, [256, T], F32)
        elif kind == "gate":
            continue
        else:
            P[name] = mk("P_" + name, [256, T], BF16)
    P["gate"] = mk("P_gate", [4096, T], BF16)
    return P


def phase1b(K, li, hT, hT_b, P):
    nc, S = K.nc, K.S
    w_in = K.inp("w_in", [DEPTH, D, D_IN])
    ropec = K.inp("rope_cos", [128, T], F32)
    ropes = K.inp("rope_sin", [128, T], F32)
    wv = w_in[li].rearrange("(k p) c -> p k c", p=128)
    with phase(K):
        wst = Rot(K, "wst", [128, 8, 256], F32, 2)
        wbf = Rot(K, "wbf", [128, 8, 256], BF16, 2)
        wsw = K.sb("wsw", [128, 8, 256], BF16); wsw_b = Buf("wsw")
        cosT = K.sb("cosT", [128, T]); sinT = K.sb("sinT", [128, T]); rope_b = Buf("rope")
        ob16 = Rot(K, "ob16", [128, 512], BF16, 4)
        of32 = Rot(K, "of32", [128, 512], F32, 3)
        tmp = Rot(K, "rtmp", [128, 512], F32, 4)
        S.dma("sp", cosT[:], ropec, rope_b)
        S.dma("sp", sinT[:], ropes, rope_b)
        nblk = len(BLOCKS)

        def load_w(cb):
            ws, wsb = wst.get()
            S.dma("sp", ws[:], wv[:, :, cb * 256:(cb + 1) * 256], wsb)
            return ws, wsb
        nxt = load_w(0)
        for cb in range(nblk):
            ws, wsb = nxt
            wb, wbb = wbf.get()
            S.op("pool", lambda h: h.tensor_copy(out=wb[:, 0:4, :], in_=ws[:, 0:4, :]), r=[wsb], w=[wbb])
            S.op("dve", lambda h: h.tensor_copy(out=wb[:, 4:8, :], in_=ws[:, 4:8, :]), r=[wsb], w=[wbb])
            if cb + 1 < nblk:
                nxt = load_w(cb + 1)
            name, kind, post = BLOCKS[cb]
            if kind == "tm":
                dst, dstb = P[name]
                for t in range(NT):
                    ps, psb = K.bank()
                    for k in range(8):
                        S.op("pe", lambda h: h.matmul(ps[:, 0:256], lhsT=hT[:, k, t * 128:(t + 1) * 128], rhs=wb[:, k, :],
                                                      start=(k == 0), stop=(k == 7)), r=[hT_b, wbb], w=[psb])
                    o, ob = ob16.get()
                    evac(K, K.ev(), o[:, 0:256], ps[:, 0:256], r=[psb], w=[ob])
                    S.dma("sp", dst[t * 128:(t + 1) * 128, :], o[:, 0:256], ob, dr=dstb, load=False)
                continue
            if kind == "rope":
                v_in = wb[:].rearrange("p k (h two i) -> p k h two i", h=4, two=2)
                v_out = wsw[:].rearrange("p k (h two i) -> p k h two i", h=4, two=2)
                for k in range(8):
                    S.op("act", lambda h: h.activation(out=v_out[:, k, :, 0, :], in_=v_in[:, k, :, 1, :], func=ACT.Copy, scale=-1.0), r=[wbb], w=[wsw_b])
                    S.op("pool", lambda h: h.tensor_copy(out=v_out[:, k, :, 1, :], in_=v_in[:, k, :, 0, :]), r=[wbb], w=[wsw_b])
            for cc in range(2):
                for (tk0, n) in TB:
                    ps, psb = K.bank()
                    for k in range(8):
                        S.op("pe", lambda h: h.matmul(ps[:, 0:n], lhsT=wb[:, k, cc * 128:(cc + 1) * 128], rhs=hT[:, k, tk0:tk0 + n],
                                                      start=(k == 0), stop=(k == 7)), r=[hT_b, wbb], w=[psb])
                    if kind == "rope":
                        ps2, psb2 = K.bank()
                        for k in range(8):
                            S.op("pe", lambda h: h.matmul(ps2[:, 0:n], lhsT=wsw[:, k, cc * 128:(cc + 1) * 128], rhs=hT[:, k, tk0:tk0 + n],
                                                          start=(k == 0), stop=(k == 7)), r=[hT_b, wsw_b], w=[psb2])
                        t1, t1b = tmp.get()
                        t2, t2b = tmp.get()
                        S.op("dve", lambda h: h.tensor_tensor(out=t1[:, 0:n], in0=ps[:, 0:n], in1=cosT[:, tk0:tk0 + n], op=ALU.mult), r=[psb, rope_b], w=[t1b])
                        S.op("dve", lambda h: h.tensor_tensor(out=t2[:, 0:n], in0=ps2[:, 0:n], in1=sinT[:, tk0:tk0 + n], op=ALU.mult), r=[psb2, rope_b], w=[t2b])
                        o, ob = ob16.get()
                        if post == 1.0:
                            S.op("pool", lambda h: h.tensor_tensor(out=o[:, 0:n], in0=t1[:, 0:n], in1=t2[:, 0:n], op=ALU.add), r=[t1b, t2b], w=[ob])
                        else:
                            S.op("pool", lambda h: h.tensor_tensor(out=t1[:, 0:n], in0=t1[:, 0:n], in1=t2[:, 0:n], op=ALU.add), r=[t1b, t2b], w=[t1b])
                            S.op("act", lambda h: h.activation(out=o[:, 0:n], in_=t1[:, 0:n], func=ACT.Copy, scale=float(post)), r=[t1b], w=[ob])
                        dst, dstb = P[name]
                        S.dma("sp", dst[cc * 128:(cc + 1) * 128, tk0:tk0 + n], o[:, 0:n], ob, dr=dstb, load=False)
                    elif kind == "fm32":
                        o, ob = of32.get()
                        evac(K, K.ev(), o[:, 0:n], ps[:, 0:n], r=[psb], w=[ob])
                        dst, dstb = P[name]
                        S.dma("sp", dst[cc * 128:(cc + 1) * 128, tk0:tk0 + n], o[:, 0:n], ob, dr=dstb, load=False)
                    else:
                        o, ob = ob16.get()
                        if kind == "gate":
                            evac(K, "act", o[:, 0:n], ps[:, 0:n], r=[psb], w=[ob], func=ACT.Sigmoid)
                            dst, dstb = P["gate"]
                            r0 = post * 256 + cc * 128
                        else:
                            if post == "silu":
                                evac(K, "act", o[:, 0:n], ps[:, 0:n], r=[psb], w=[ob], func=ACT.Silu)
                            else:
                                evac(K, K.ev(), o[:, 0:n], ps[:, 0:n], r=[psb], w=[ob])
                            dst, dstb = P[name]
                            r0 = cc * 128
                        S.dma("sp", dst[r0:r0 + 128, tk0:tk0 + n], o[:, 0:n], ob, dr=dstb, load=False)


def load_fm(K, tile, tb, src, srcb, nchunks=2):
    for cc in range(nchunks):
        K.S.dma("sp", tile[:, cc, :], src[cc * 128:(cc + 1) * 128, :], tb, dr=srcb)


def head_norm_finish(K, po, pob, n, gt, gtb, out, outb, sq_rot, rs_rot, wcol=None, wb=None):
    S = K.S
    sq, sqb = sq_rot.get()
    sqh = sq[:].bitcast(BF16)
    S.op("act", lambda h: h.activation(out=sqh[0:64, 0:n], in_=po[0:64, 0:n], func=ACT.Square), r=[pob], w=[sqb])
    pss, pssb = K.bank()
    S.op("pe", lambda h: h.matmul(pss[0:64, 0:n], lhsT=K.onesb[0:64, 0:64], rhs=sqh[0:64, 0:n], start=True, stop=True), r=[sqb, K.onesb_b], w=[pssb])
    rs, rsb = rs_rot.get()
    S.op("act", lambda h: h.activation(out=rs[0:64, 0:n], in_=pss[0:64, 0:n], func=ACT.Sqrt, scale=1.0 / 64, bias=K.epsc[0:64, 0:1]), r=[pssb, K.epsc_b], w=[rsb])
    S.op("dve", lambda h: h.reciprocal(out=rs[0:64, 0:n], in_=rs[0:64, 0:n]), r=[rsb], w=[rsb])
    if wcol is None:
        S.op("dve", lambda h: h.tensor_tensor(out=sq[0:64, 0:n], in0=po[0:64, 0:n], in1=rs[0:64, 0:n], op=ALU.mult), r=[pob, rsb], w=[sqb])
    else:
        S.op("dve", lambda h: h.scalar_tensor_tensor(out=sq[0:64, 0:n], in0=po[0:64, 0:n], scalar=wcol, in1=rs[0:64, 0:n], op0=ALU.mult, op1=ALU.mult),
             r=[pob, rsb], w=[sqb], rs=[wb])
    S.op("pool", lambda h: h.tensor_tensor(out=out, in0=sq[0:64, 0:n], in1=gt, op=ALU.mult), r=[sqb, gtb], w=[outb])


def mm_k64(K, ps, psb, c0, n, lhsT, rhs, r0, rb, start=True, stop=True):
    S = K.S
    if r0 == 0:
        S.op("pe", lambda h: h.matmul(ps[:, c0:c0 + n], lhsT=lhsT, rhs=rhs, start=start, stop=stop), r=rb, w=[psb])
    else:
        for hf in range(2):
            S.op("pe", lambda h: h.matmul(ps[hf * 64:(hf + 1) * 64, c0:c0 + n], lhsT=lhsT[:, hf * 64:(hf + 1) * 64], rhs=rhs, start=start, stop=stop), r=rb, w=[psb])


def load_tm(K, tile, tb, src, srcb, c0=0, cw=256):
    v = src.rearrange("(n p) c -> p n c", p=128)
    for n0 in range(0, NT, 6):
        n1 = min(NT, n0 + 6)
        K.S.dma("sp", tile[:, n0:n1, :], v[:, n0:n1, c0:c0 + cw], tb, dr=srcb)


def hv(dr):
    return dr.rearrange("(h v) t -> v h t", v=64)


def phase_ret(K, li, P, O):
    nc, S = K.nc, K.S
    dlin = K.inp("ret_bc", [DEPTH, 128, 8])
    cE0 = K.inp("c_epos0", [128, 128]); cE1 = K.inp("c_epos1", [128, 128])
    cM0 = K.inp("c_mk0", [128, 128]); cM1 = K.inp("c_mk1", [128, 128])
    cI1 = K.inp("c_iota1", [128, 128]); cIr = K.inp("c_iotar", [128, 128])
    cPc = K.inp("c_pcol", [128, 2])
    Od, Odb = O
    with phase(K):
        cst = K.sb("rt_cst", [128, 6, 128]); cst_b = Buf("rt_cst")
        for i, c in enumerate((cE0, cE1, cM0, cM1, cI1, cIr)):
            S.dma("sp", cst[:, i, :], c, cst_b)
        pcol = K.sb("rt_pcol", [128, 2]); pcol_b = Buf("rt_pcol")
        S.dma("sp", pcol[:], cPc, pcol_b)
        dl = K.sb("rt_dl", [128, 8]); dl_b = Buf("rt_dl")
        S.dma("sp", dl[:], dlin[li], dl_b)
        y = K.sb("rt_y", [128, 8]); ta = K.sb("rt_ta", [128, 8]); lg = K.sb("rt_lg", [128, 8]); lg_b = Buf("rt_lg")
        S.op("act", lambda h: h.activation(out=y[:], in_=dl[:], func=ACT.Exp, scale=-1.0), r=[dl_b], w=[lg_b])
        S.op("dve", lambda h: h.tensor_scalar(out=ta[:], in0=y[:], scalar1=-0.25, scalar2=1.0 / 3.0, op0=ALU.mult, op1=ALU.add), r=[lg_b], w=[lg_b])
        S.op("dve", lambda h: h.tensor_tensor(out=ta[:], in0=ta[:], in1=y[:], op=ALU.mult), r=[lg_b], w=[lg_b])
        S.op("dve", lambda h: h.tensor_scalar(out=ta[:], in0=ta[:], scalar1=-0.5, scalar2=None, op0=ALU.add), r=[lg_b], w=[lg_b])
        S.op("dve", lambda h: h.tensor_tensor(out=ta[:], in0=ta[:], in1=y[:], op=ALU.mult), r=[lg_b], w=[lg_b])
        S.op("dve", lambda h: h.tensor_scalar(out=ta[:], in0=ta[:], scalar1=1.0, scalar2=None, op0=ALU.add), r=[lg_b], w=[lg_b])
        S.op("dve", lambda h: h.tensor_tensor(out=ta[:], in0=ta[:], in1=y[:], op=ALU.mult), r=[lg_b], w=[lg_b])
        S.op("dve", lambda h: h.tensor_scalar(out=lg[:], in0=ta[:], scalar1=-1.0, scalar2=None, op0=ALU.mult), r=[lg_b], w=[lg_b])
        lgp = K.sb("rt_lgp", [128, 2, 2]); lgp_b = Buf("rt_lgp")
        for cc in range(2):
            for d in range(2):
                for hp in range(2):
                    col = d * 4 + 2 * cc + hp
                    S.op("dve", lambda h: h.tensor_copy(out=lgp[hp * 64:(hp + 1) * 64, cc, d:d + 1], in_=lg[hp * 64:(hp + 1) * 64, col:col + 1]), r=[lg_b], w=[lgp_b])
        M = K.sb("rt_M", [128, 4, 128]); M_b = Buf("rt_M")
        e0 = K.sb("rt_e0", [128, 128]); e1 = K.sb("rt_e1", [128, 128]); e_b = Buf("rt_e")
        for hh in range(4):
            S.op("act", lambda h: h.activation(out=e0[:], in_=cst[:, 0, :], func=ACT.Exp, scale=lg[:, hh:hh + 1]), r=[cst_b], w=[e_b], rs=[lg_b])
            S.op("act", lambda h: h.activation(out=e1[:], in_=cst[:, 1, :], func=ACT.Exp, scale=lg[:, 4 + hh:5 + hh]), r=[cst_b], w=[e_b], rs=[lg_b])
            S.op("dve", lambda h: h.tensor_tensor(out=e0[:], in0=e0[:], in1=cst[:, 2, :], op=ALU.mult), r=[e_b, cst_b], w=[e_b])
            S.op("dve", lambda h: h.tensor_tensor(out=e1[:], in0=e1[:], in1=cst[:, 3, :], op=ALU.mult), r=[e_b, cst_b], w=[e_b])
            S.op("dve", lambda h: h.tensor_tensor(out=M[:, hh, :], in0=e0[:], in1=e1[:], op=ALU.add), r=[e_b], w=[M_b])
        XI = K.sb("rt_XI", [128, 2, 2, 128]); XI_b = Buf("rt_XI")
        for d in range(2):
            for cc in range(2):
                S.op("act", lambda h: h.activation(out=XI[:, d, cc, :], in_=cst[:, 4 + d, :], func=ACT.Exp, scale=lgp[:, cc, d:d + 1]), r=[cst_b], w=[XI_b], rs=[lgp_b])
        ZT = K.sb("rt_ZT", [128, 2, 4]); ZT_b = Buf("rt_ZT")
        for d in range(2):
            S.op("act", lambda h: h.activation(out=ZT[:, d, :], in_=lg[:, d * 4:(d + 1) * 4], func=ACT.Exp, scale=pcol[:, d:d + 1]), r=[lg_b, pcol_b], w=[ZT_b], rs=[pcol_b])
        G128 = K.sb("rt_G", [128, 2, 2]); G_b = Buf("rt_G")
        S.op("act", lambda h: h.activation(out=G128[:], in_=lgp[:], func=ACT.Exp, scale=128.0), r=[lgp_b], w=[G_b])
        qT = K.sb("rt_qT", [128, 2, T], BF16); qT_b = Buf("rt_qT")
        kT = K.sb("rt_kT", [128, 2, T], BF16); kT_b = Buf("rt_kT")
        load_fm(K, qT, qT_b, *P["rtq"])
        load_fm(K, kT, kT_b, *P["rtk"])
        vall = K.sb("rt_v", [128, NT, 256], BF16); v_b = Buf("rt_v")
        load_tm(K, vall, v_b, *P["rtv"])
        qx = K.sb("rt_qx", [128, 2, 2, T], BF16); qx_b = Buf("rt_qx")
        for d in range(2):
            for cc in range(2):
                eng = "dve" if cc == 0 else "pool"
                S.op(eng, lambda h: h.tensor_tensor(out=qx[:, d, cc, :].rearrange("p (n a) -> p n a", a=128),
                                                    in0=qT[:, cc, :].rearrange("p (n a) -> p n a", a=128),
                                                    in1=XI[:, d, cc, :].unsqueeze(1).to_broadcast([128, NT, 128]), op=ALU.mult),
                     r=[qT_b, XI_b], w=[qx_b])
        kz = K.sb("rt_kz", [128, 2, NT, 256], BF16); kz_b = Buf("rt_kz")
        for t in range(NT):
            ps, psb = K.bank()
            pv = ps[:].bitcast(BF16)
            for cc in range(2):
                S.op("pe", lambda h: h.transpose(out=pv[:, cc * 128:(cc + 1) * 128], in_=kT[:, cc, t * 128:(t + 1) * 128], identity=K.identb[:]),
                     r=[kT_b, K.identb_b], w=[psb])
            for d in range(2):
                eng = "dve" if d == 0 else "pool"
                if eng == "pool":
                    eng = "dve"
                S.op(eng, lambda h: h.tensor_tensor(out=kz[:, d, t, :].rearrange("p (h k) -> p h k", h=4),
                                                    in0=pv[:, 0:256].rearrange("p (h k) -> p h k", h=4),
                                                    in1=ZT[:, d, :].unsqueeze(2).to_broadcast([128, 4, 64]), op=ALU.mult),
                     r=[psb, ZT_b], w=[kz_b])
        Sst = K.sb("rt_S", [128, 2, 2, 64]); Sst_b = [Buf("rt_S0"), Buf("rt_S1")]
        Sall = K.sb("rt_Sall", [128, 2, NT, 2, 64], BF16); Sall_b = Buf("rt_Sall")
        S.op("dve", lambda h: h.memset(Sst[:], 0.0), w=Sst_b)
        order = [list(range(NT)), [1, 0] + list(range(NT - 1, 1, -1))]
        for step in range(NT):
            for d in range(2):
                t = order[d][step]
                pk, pkb = K.bank()
                for hh in range(4):
                    cc = hh // 2
                    S.op("pe", lambda h: h.matmul(pk[:, hh * 64:(hh + 1) * 64], lhsT=kz[:, d, t, cc * 128:(cc + 1) * 128], rhs=vall[:, t, hh * 64:(hh + 1) * 64],
                                                  start=True, stop=True), r=[kz_b, v_b], w=[pkb])
                S.op("act", lambda h: h.activation(out=Sall[:, d, t, :, :], in_=Sst[:, d, :, :], func=ACT.Copy), r=[Sst_b[d]], w=[Sall_b])
                S.op("dve", lambda h: h.tensor_tensor(out=Sst[:, d, :, :], in0=Sst[:, d, :, :], in1=G128[:, :, d:d + 1].to_broadcast([128, 2, 64]), op=ALU.mult),
                     r=[Sst_b[d], G_b], w=[Sst_b[d]])
                pkv = pk[:, 0:256].rearrange("p (c q v) -> p c q v", c=2, q=2)
                for hp in range(2):
                    S.op("dve", lambda h: h.tensor_tensor(out=Sst[hp * 64:(hp + 1) * 64, d, :, :], in0=Sst[hp * 64:(hp + 1) * 64, d, :, :],
                                                          in1=pkv[hp * 64:(hp + 1) * 64, :, hp, :], op=ALU.add), r=[Sst_b[d], pkb], w=[Sst_b[d]])
        A_r = Rot(K, "rt_A", [128, 4, 128], BF16, 3)
        sq_r = Rot(K, "rt_sq", [64, 512], F32, 2)
        rs_r = Rot(K, "rt_rs", [64, 512], F32, 2)
        g_r = Rot(K, "rt_g", [64, 4, 128], BF16, 3)
        o_r = Rot(K, "rt_o", [64, 4, 128], BF16, 3)
        gsrc, gsrcb = P["rtg"]
        for t in range(NT):
            tk = slice(t * 128, (t + 1) * 128)
            pa, pab = K.bank()
            for hh in range(4):
                cc, r0 = hh // 2, (hh % 2) * 64
                mm_k64(K, pa, pab, hh * 128, 128, kT[r0:r0 + 64, cc, tk], qT[r0:r0 + 64, cc, tk], r0, [kT_b, qT_b])
            A, Ab = A_r.get()
            S.op("dve", lambda h: h.tensor_tensor(out=A[:], in0=pa[:].rearrange("p (h t) -> p h t", h=4), in1=M[:], op=ALU.mult), r=[pab, M_b], w=[Ab])
            po, pob = K.bank()
            for hh in range(4):
                cc, r0 = hh // 2, (hh % 2) * 64
                oo = po[0:64, hh * 128:(hh + 1) * 128]
                S.op("pe", lambda h: h.matmul(oo, lhsT=vall[:, t, hh * 64:(hh + 1) * 64], rhs=A[:, hh, :], start=True, stop=False), r=[v_b, Ab], w=[pob])
                for d in range(2):
                    S.op("pe", lambda h: h.matmul(oo, lhsT=Sall[r0:r0 + 64, d, t, cc, :], rhs=qx[r0:r0 + 64, d, cc, tk], start=False, stop=(d == 1)),
                         r=[Sall_b, qx_b], w=[pob])
            g, gb = g_r.get()
            S.dma("sp", g[:], hv(gsrc)[:, :, tk], gb, dr=gsrcb)
            o, ob = o_r.get()
            head_norm_finish(K, po, pob, 512, g[:].rearrange("p h t -> p (h t)"), gb, o[:].rearrange("p h t -> p (h t)"), ob, sq_r, rs_r)
            S.dma("sp", hv(Od)[:, :, tk], o[:], ob, dr=Odb, load=False)


def _band_start(r):
    return min(max(r - 4, 0), 56)


def phase_na(K, li, P, O):
    nc, S = K.nc, K.S
    rpb = K.inp("na_rpbT2", [DEPTH, 128, 4, 15, 64])
    nmask = K.inp("c_namask", [128, 64])
    nw = K.inp("na_qk_normT", [DEPTH, 128, 2])
    blk64 = K.inp("c_blk64", [128, 128])
    Od, Odb = O
    NEG = -240000.0
    with phase(K):
        qT = K.sb("na_qT", [128, 2, T], BF16); qT_b = Buf("na_qT")
        kT = K.sb("na_kT", [128, 2, T], BF16); kT_b = Buf("na_kT")
        load_fm(K, qT, qT_b, *P["naq"])
        load_fm(K, kT, kT_b, *P["nak"])
        vall = K.sb("na_v", [128, NT, 256], BF16); v_b = Buf("na_v")
        load_tm(K, vall, v_b, *P["nav"])
        wq = K.sb("na_w", [128, 2]); wq_b = Buf("na_w")
        S.dma("sp", wq[:], nw[li], wq_b)
        b64 = K.sb("na_b64", [128, 128]); b64_b = Buf("na_b64")
        S.dma("sp", b64[:], blk64, b64_b)
        sq_r = Rot(K, "na_sq", [128, 512], F32, 2)
        rs_r = Rot(K, "na_rs", [128, 512], F32, 2)
        for wi, (X, Xb) in enumerate(((qT, qT_b), (kT, kT_b))):
            for cc in range(2):
                for (t0, n) in TB:
                    xs = X[:, cc, t0:t0 + n]
                    sq, sqb = sq_r.get()
                    S.op("act", lambda h: h.activation(out=sq[:, 0:n], in_=xs, func=ACT.Square), r=[Xb], w=[sqb])
                    ps, psb = K.bank()
                    S.op("pe", lambda h: h.matmul(ps[:, 0:n], lhsT=b64[:], rhs=sq[:, 0:n], start=True, stop=True), r=[b64_b, sqb], w=[psb])
                    rs, rsb = rs_r.get()
                    S.op("act", lambda h: h.activation(out=rs[:, 0:n], in_=ps[:, 0:n], func=ACT.Sqrt, scale=1.0 / 64, bias=K.epsc[:, 0:1]), r=[psb, K.epsc_b], w=[rsb])
                    S.op("dve", lambda h: h.reciprocal(out=rs[:, 0:n], in_=rs[:, 0:n]), r=[rsb], w=[rsb])
                    S.op("dve", lambda h: h.scalar_tensor_tensor(out=xs, in0=xs, scalar=wq[:, wi:wi + 1], in1=rs[:, 0:n], op0=ALU.mult, op1=ALU.mult),
                         r=[Xb, rsb], w=[Xb], rs=[wq_b])
        rp = K.sb("na_rp", [128, 4, 15, 64]); rp_b = Buf("na_rp")
        S.dma("sp", rp[:], rpb[li], rp_b)
        mk = K.sb("na_mk", [128, 64]); mk_b = Buf("na_mk")
        S.dma("sp", mk[:], nmask, mk_b)
        Bt = K.sb("na_Bt", [128, 4, 15, 64], BF16); Bt_b = Buf("na_Bt")
        S.op("dve", lambda h: h.scalar_tensor_tensor(out=Bt[:].rearrange("p h r q -> p (h r) q"), in0=rp[:].rearrange("p h r q -> p (h r) q"), scalar=8.0,
                                                     in1=mk[:].unsqueeze(1).to_broadcast([128, 60, 64]), op0=ALU.mult, op1=ALU.add),
             r=[rp_b, mk_b], w=[Bt_b])
        NPAT = 30
        pat_t = K.sb("na_pat", [128, NPAT, 4, 128], BF16)
        pat_b = [Buf(f"na_pat{i}") for i in range(NPAT)]
        pats = {}

        def bias_tile(tq, m):
            key = []
            for kr in range(2):
                for qr in range(2):
                    krow, qrow = 2 * m + kr, 2 * tq + qr
                    bs = _band_start(qrow)
                    key.append(krow - qrow if bs <= krow < bs + 8 else None)
            key = tuple(key)
            if key not in pats:
                idx = len(pats)
                assert idx < NPAT
                pats[key] = idx
                i = 0
                for kr in range(2):
                    for qr in range(2):
                        dr = key[i]; i += 1
                        dst = pat_t[kr * 64:(kr + 1) * 64, idx, :, qr * 64:(qr + 1) * 64]
                        if dr is None:
                            S.op("pool", lambda h: h.memset(dst, NEG), w=[pat_b[idx]])
                        else:
                            S.op("pool", lambda h: h.tensor_copy(out=dst, in_=Bt[kr * 64:(kr + 1) * 64, :, dr + 7, :]), r=[Bt_b], w=[pat_b[idx]])
            idx = pats[key]
            return pat_t[:, idx, :, :], pat_b[idx]

        ones64 = K.sb("na_ones", [128, 64], BF16); ones64_b = Buf("na_ones")
        S.op("dve", lambda h: h.memset(ones64[:], 1.0), w=[ones64_b])
        pm_r = Rot(K, "na_pm", [128, 7, 512], BF16, 2)
        rd_r = Rot(K, "na_rd", [64, 512], F32, 2)
        o_r = Rot(K, "na_o", [64, 4, 128], BF16, 3)
        for tt in range(NT):
            tk = slice(tt * 128, (tt + 1) * 128)
            if tt < 2:
                keys = [(0, None), (1, None)]
            else:
                tq = tt - 2
                m0 = _band_start(2 * tq) // 2
                m1 = (_band_start(2 * tq + 1) + 7) // 2
                keys = [(2 + m, m) for m in range(m0, m1 + 1)] + [(0, None), (1, None)]
            pm, pmb = pm_r.get()
            for ki, (kt, m) in enumerate(keys):
                kk = slice(kt * 128, (kt + 1) * 128)
                ps, psb = K.bank()
                if m is not None:
                    bt, btb = bias_tile(tt - 2, m)
                for hh in range(4):
                    cc, r0 = hh // 2, (hh % 2) * 64
                    mm_k64(K, ps, psb, hh * 128, 128, kT[r0:r0 + 64, cc, kk], qT[r0:r0 + 64, cc, tk], r0, [kT_b, qT_b], start=True, stop=(m is None))
                    if m is not None:
                        S.op("pe", lambda h: h.matmul(ps[:, hh * 128:(hh + 1) * 128], lhsT=K.identb[:], rhs=bt[:, hh, :], start=False, stop=True),
                             r=[K.identb_b, btb], w=[psb])
                S.op("act", lambda h: h.activation(out=pm[:, ki, :], in_=ps[:], func=ACT.Exp, scale=0.125), r=[psb], w=[pmb])
            nk = len(keys)
            po, pob = K.bank()
            for hh in range(4):
                for ki, (kt, m) in enumerate(keys):
                    S.op("pe", lambda h: h.matmul(po[0:64, hh * 128:(hh + 1) * 128], lhsT=vall[:, kt, hh * 64:(hh + 1) * 64], rhs=pm[:, ki, hh * 128:(hh + 1) * 128],
                                                  start=(ki == 0), stop=(ki == nk - 1)), r=[v_b, pmb], w=[pob])
            pd, pdb = K.bank()
            for ki in range(nk):
                S.op("pe", lambda h: h.matmul(pd[0:64, :], lhsT=ones64[:], rhs=pm[:, ki, :], start=(ki == 0), stop=(ki == nk - 1)), r=[ones64_b, pmb], w=[pdb])
            rd, rdb = rd_r.get()
            S.op("dve", lambda h: h.reciprocal(out=rd[:], in_=pd[0:64, :]), r=[pdb], w=[rdb])
            o, ob = o_r.get()
            S.op("dve", lambda h: h.tensor_tensor(out=o[:].rearrange("p h t -> p (h t)"), in0=po[0:64, :], in1=rd[:], op=ALU.mult), r=[pob, rdb], w=[ob])
            S.dma("sp", hv(Od)[:, :, tk], o[:], ob, dr=Odb, load=False)


NCH = T // 16
HB = 256


def _hg_spos(d, n):
    if d == 0:
        return n
    return 15 - n if n < 16 else 16 + (271 - n)


def phase_hg(K, li, P, O):
    nc, S = K.nc, K.S
    lbin = K.inp("hg_lbT", [128, 2, 2])
    nwin = K.inp("hg_norm_wT", [DEPTH, 64, 1])
    crst = K.inp("c_rst", [128, HB])
    cm = [K.inp("c_hgm0", [128, 128]), K.inp("c_hgm1", [128, 128])]
    cbm = K.inp("c_blkm", [128, 2, 8])
    Od, Odb = O
    NC16 = HB // 16
    with phase(K):
        lbr = K.sb("hg_lbr", [128, 2, 2]); lb_b = Buf("hg_lb")
        S.dma("sp", lbr[:], lbin, lb_b)
        lb = K.sb("hg_lbv", [128, 2]); oml = K.sb("hg_oml", [128, 2])
        if li == 0:
            S.op("dve", lambda h: h.memset(lb[:], 0.0), w=[lb_b])
        else:
            S.op("dve", lambda h: h.tensor_tensor(out=lb[:], in0=lbr[:, 1, :], in1=lbr[:, 0, :], op=ALU.subtract), r=[lb_b], w=[lb_b])
            S.op("act", lambda h: h.activation(out=lb[:], in_=lb[:], func=ACT.Sigmoid), r=[lb_b], w=[lb_b])
        S.op("dve", lambda h: h.tensor_scalar(out=oml[:], in0=lb[:], scalar1=-1.0, scalar2=1.0, op0=ALU.mult, op1=ALU.add), r=[lb_b], w=[lb_b])
        nwt = K.sb("hg_nw", [64, 1]); nw_b = Buf("hg_nw")
        S.dma("sp", nwt[:], nwin[li], nw_b)
        rst = K.sb("hg_rst", [128, HB]); rst_b = Buf("hg_rst")
        S.dma("sp", rst[:], crst, rst_b)
        msk = K.sb("hg_msk", [128, 2, 128]); msk_b = Buf("hg_msk")
        for d in range(2):
            S.dma("sp", msk[:, d, :], cm[d], msk_b)
        bm = K.sb("hg_bm", [128, 2, 8]); bm_b = Buf("hg_bm")
        S.dma("sp", bm[:], cbm, bm_b)
        vall = K.sb("hg_v", [128, NT, 128], BF16); v_b = Buf("hg_v")
        qd = K.sb("hg_qd", [128, T], BF16); kd = K.sb("hg_kd", [128, T], BF16)
        qd_b, kd_b = Buf("hg_qd"), Buf("hg_kd")
        dec = K.sb("hg_dec", [128, NCH]); dec_b = Buf("hg_dec")
        decs = K.sb("hg_decs", [128, NCH]); decs_b = Buf("hg_decs")
        kvb = K.sb("hg_kvb", [128, NCH, 64], BF16); kvb_b = Buf("hg_kvb")
        Sb = K.sb("hg_Sb", [128, NCH, 64], BF16); Sb_b = Buf("hg_Sb")
        Oacc = K.sb("hg_Oacc", [64, 2, T]); Oacc_b = Buf("hg_Oacc")
        fl_r = Rot(K, "hg_fl", [128, HB], F32, 2)
        qs_r = Rot(K, "hg_qs", [128, HB], BF16, 2)
        ex_r = Rot(K, "hg_ex", [128, HB], F32, 3)
        ke_r = Rot(K, "hg_ke", [128, HB], BF16, 2)
        tA = K.sb("hg_tA", [128, HB]); tB = K.sb("hg_tB", [128, HB]); tC = K.sb("hg_tC", [128, HB]); tD = K.sb("hg_tD", [128, HB])
        tmp_b = Buf("hg_tmp")
        keT_r = Rot(K, "hg_keT", [128, 128], BF16, 3)
        vb_r = Rot(K, "hg_vb", [128, 2, 8, 64], BF16, 3)
        A_r = Rot(K, "hg_A", [128, 2, 128], BF16, 3)
        sq_r = Rot(K, "hg_sq", [64, 512], F32, 2)
        rs_r = Rot(K, "hg_rs", [64, 512], F32, 2)
        g_r = Rot(K, "hg_g", [64, 2, 256], BF16, 2)
        o_r = Rot(K, "hg_o", [64, 2, 256], BF16, 2)
        fsrc = [P["hgff"], P["hgfb"]]
        c3 = lambda ap: ap.rearrange("p (n c) -> p n c", c=16)
        for cc in range(2):
            load_tm(K, vall, v_b, *P["hgi"], c0=cc * 128, cw=128)
            for d in range(2):
                for bi in range(T // HB):
                    tb = slice(bi * HB, (bi + 1) * HB)
                    cb = slice(bi * NC16, (bi + 1) * NC16)
                    fl, flb = fl_r.get()
                    S.dma("sp", fl[:], fsrc[d][0][cc * 128:(cc + 1) * 128, tb], flb, dr=fsrc[d][1])
                    qs, qsb = qs_r.get()
                    S.dma("sp", qs[:], P["hgq"][0][cc * 128:(cc + 1) * 128, tb], qsb, dr=P["hgq"][1])
                    S.op("act", lambda h: h.activation(out=tA[:], in_=fl[:], func=ACT.Sigmoid), r=[flb], w=[tmp_b])
                    S.op("dve", lambda h: h.tensor_scalar(out=tA[:], in0=tA[:], scalar1=oml[:, cc:cc + 1], scalar2=lb[:, cc:cc + 1], op0=ALU.mult, op1=ALU.add),
                         r=[tmp_b], w=[tmp_b], rs=[lb_b])
                    S.op("act", lambda h: h.activation(out=tB[:], in_=tA[:], func=ACT.Ln), r=[tmp_b], w=[tmp_b])
                    S.op("pool", lambda h: h.tensor_scalar(out=tC[:], in0=tA[:], scalar1=-1.0, scalar2=1.0, op0=ALU.mult, op1=ALU.add), r=[tmp_b], w=[tmp_b])
                    S.op("dve", lambda h: h.tensor_tensor_scan(out=tA[:], data0=rst[:], data1=tB[:], initial=0.0, op0=ALU.mult, op1=ALU.add),
                         r=[tmp_b, rst_b], w=[tmp_b])
                    Pv = c3(tA[:])
                    totb = Pv[:, :, 15:16].to_broadcast([128, NC16, 16])
                    if d == 0:
                        Eq = tA
                        S.op("dve", lambda h: h.tensor_tensor(out=c3(tD[:]), in0=totb, in1=Pv, op=ALU.subtract), r=[tmp_b], w=[tmp_b])
                    else:
                        S.op("dve", lambda h: h.tensor_tensor(out=tD[:], in0=tA[:], in1=tB[:], op=ALU.subtract), r=[tmp_b], w=[tmp_b])
                        S.op("dve", lambda h: h.tensor_tensor(out=c3(tB[:]), in0=totb, in1=c3(tD[:]), op=ALU.subtract), r=[tmp_b], w=[tmp_b])
                        Eq = tB
                    S.op("act", lambda h: h.activation(out=dec[:, cb], in_=Pv[:, :, 15], func=ACT.Exp), r=[tmp_b], w=[dec_b])
                    e1, e1b = ex_r.get()
                    S.op("act", lambda h: h.activation(out=e1[:], in_=Eq[:], func=ACT.Exp), r=[tmp_b], w=[e1b])
                    S.op("dve", lambda h: h.tensor_tensor(out=qd[:, tb], in0=qs[:], in1=e1[:], op=ALU.mult), r=[qsb, e1b], w=[qd_b])
                    e2, e2b = ex_r.get()
                    S.op("act", lambda h: h.activation(out=e2[:], in_=Eq[:], func=ACT.Exp, scale=-1.0), r=[tmp_b], w=[e2b])
                    S.op("pool", lambda h: h.tensor_tensor(out=kd[:, tb], in0=tC[:], in1=e2[:], op=ALU.mult), r=[tmp_b, e2b], w=[kd_b])
                    e3, e3b = ex_r.get()
                    S.op("act", lambda h: h.activation(out=e3[:], in_=tD[:], func=ACT.Exp), r=[tmp_b], w=[e3b])
                    ke, keb = ke_r.get()
                    S.op("dve", lambda h: h.tensor_tensor(out=ke[:], in0=tC[:], in1=e3[:], op=ALU.mult), r=[tmp_b, e3b], w=[keb])
                    for tl in range(HB // 128):
                        t = bi * (HB // 128) + tl
                        ps, psb = K.bank()
                        pv = ps[:].bitcast(BF16)
                        S.op("pe", lambda h: h.transpose(out=pv[:, 0:128], in_=ke[:, tl * 128:(tl + 1) * 128], identity=K.identb[:]), r=[keb, K.identb_b], w=[psb])
                        keT, keTb = keT_r.get()
                        S.op("act", lambda h: h.activation(out=keT[:], in_=pv[:, 0:128], func=ACT.Copy), r=[psb], w=[keTb])
                        vb, vbb = vb_r.get()
                        S.op("pool", lambda h: h.tensor_tensor(out=vb[:], in0=vall[:, t, :].rearrange("p (h v) -> p h v", h=2).unsqueeze(2).to_broadcast([128, 2, 8, 64]),
                                                               in1=bm[:, d, :].unsqueeze(1).unsqueeze(3).to_broadcast([128, 2, 8, 64]), op=ALU.mult), r=[v_b, bm_b], w=[vbb])
                        if d == 0:
                            s0 = 8 * t
                        else:
                            s0 = 8 * (1 - t) if t < 2 else 280 - 8 * t
                        for hp in range(2):
                            pk, pkb = K.bank()
                            S.op("pe", lambda h: h.matmul(pk[:], lhsT=keT[:], rhs=vb[:, hp, :, :].rearrange("p c v -> p (c v)"), start=True, stop=True),
                                 r=[keTb, vbb], w=[pkb])
                            evac(K, K.ev(), kvb[hp * 64:(hp + 1) * 64, s0:s0 + 8, :], pk[hp * 64:(hp + 1) * 64, :].rearrange("p (c v) -> p c v", c=8), r=[pkb], w=[kvb_b])
                if d == 0:
                    S.op("dve", lambda h: h.tensor_copy(out=decs[:], in_=dec[:]), r=[dec_b], w=[decs_b])
                else:
                    da = dec[:]
                    r1 = bass.AP(da.tensor, da.offset + 15, [list(da.ap[0]), [-1, 16]])
                    r2 = bass.AP(da.tensor, da.offset + 271, [list(da.ap[0]), [-1, 256]])
                    S.op("dve", lambda h: h.tensor_copy(out=decs[:, 0:16], in_=r1), r=[dec_b], w=[decs_b])
                    S.op("dve", lambda h: h.tensor_copy(out=decs[:, 16:272], in_=r2), r=[dec_b], w=[decs_b])
                for v in range(64):
                    S.op("dve", lambda h: h.tensor_tensor_scan(out=Sb[:, :, v], data0=decs[:], data1=kvb[:, :, v], initial=0.0, op0=ALU.mult, op1=ALU.add),
                         r=[decs_b, kvb_b], w=[Sb_b])
                for t in range(NT):
                    tk = slice(t * 128, (t + 1) * 128)
                    pa, pab = K.bank()
                    for hp in range(2):
                        r0 = hp * 64
                        mm_k64(K, pa, pab, hp * 128, 128, kd[r0:r0 + 64, tk], qd[r0:r0 + 64, tk], r0, [kd_b, qd_b])
                    A, Ab = A_r.get()
                    S.op("dve", lambda h: h.tensor_tensor(out=A[:], in0=pa[:, 0:256].rearrange("p (h t) -> p h t", h=2),
                                                          in1=msk[:, d, :].unsqueeze(1).to_broadcast([128, 2, 128]), op=ALU.mult), r=[pab, msk_b], w=[Ab])
                    po, pob = K.bank()
                    for hp in range(2):
                        r0 = hp * 64
                        mm = []
                        for c in range(8):
                            sp = _hg_spos(d, 8 * t + c)
                            if sp >= 1:
                                mm.append((c, sp))
                        S.op("pe", lambda h: h.matmul(po[0:64, hp * 128:(hp + 1) * 128], lhsT=vall[:, t, hp * 64:(hp + 1) * 64], rhs=A[:, hp, :], start=True, stop=(len(mm) == 0)),
                             r=[v_b, Ab], w=[pob])
                        for i, (c, sp) in enumerate(mm):
                            S.op("pe", lambda h: h.matmul(po[0:64, hp * 128 + 16 * c:hp * 128 + 16 * c + 16], lhsT=Sb[r0:r0 + 64, sp - 1, :],
                                                          rhs=qd[r0:r0 + 64, t * 128 + 16 * c:t * 128 + 16 * c + 16], start=False, stop=(i == len(mm) - 1)),
                                 r=[Sb_b, qd_b], w=[pob])
                    ov = Oacc[:, :, tk]
                    pov = po[0:64, 0:256].rearrange("p (h t) -> p h t", h=2)
                    if d == 0:
                        S.op("act", lambda h: h.activation(out=ov, in_=pov, func=ACT.Copy), r=[pob], w=[Oacc_b])
                    else:
                        S.op("dve", lambda h: h.tensor_tensor(out=ov, in0=ov, in1=pov, op=ALU.add), r=[pob, Oacc_b], w=[Oacc_b])
            gsrc, gsrcb = P["hgg"]
            for t2 in range(T // 256):
                tk = slice(t2 * 256, (t2 + 1) * 256)
                g, gb = g_r.get()
                S.dma("sp", g[:], hv(gsrc)[:, 2 * cc:2 * cc + 2, tk], gb, dr=gsrcb)
                o, ob = o_r.get()
                sq, sqb = sq_r.get()
                h2 = lambda ap: ap.rearrange("p (h t) -> p h t", h=2)
                sqh = sq[:].bitcast(BF16)
                S.op("act", lambda h: h.activation(out=h2(sqh[:, 0:512]), in_=Oacc[:, :, tk], func=ACT.Square), r=[Oacc_b], w=[sqb])
                pss, pssb = K.bank()
                S.op("pe", lambda h: h.matmul(pss[0:64, :], lhsT=K.onesb[0:64, 0:64], rhs=sqh[:, 0:512], start=True, stop=True), r=[sqb, K.onesb_b], w=[pssb])
                rs, rsb = rs_r.get()
                S.op("act", lambda h: h.activation(out=rs[:], in_=pss[0:64, :], func=ACT.Sqrt, scale=1.0 / 64, bias=K.epsc[0:64, 0:1]), r=[pssb, K.epsc_b], w=[rsb])
                S.op("dve", lambda h: h.reciprocal(out=rs[:], in_=rs[:]), r=[rsb], w=[rsb])
                S.op("dve", lambda h: h.scalar_tensor_tensor(out=h2(sq[:]), in0=Oacc[:, :, tk], scalar=nwt[:, 0:1], in1=h2(rs[:]), op0=ALU.mult, op1=ALU.mult),
                     r=[Oacc_b, rsb], w=[sqb], rs=[nw_b])
                S.op("pool", lambda h: h.tensor_tensor(out=o[:], in0=h2(sq[:]), in1=g[:], op=ALU.mult), r=[sqb, gb], w=[ob])
                S.dma("sp", hv(Od)[:, 2 * cc:2 * cc + 2, tk], o[:], ob, dr=Odb, load=False)


TWO_PI_LO = 6.28318


def _s5_spos(m):
    return 15 - m if m < 16 else 287 - m


def phase_s5(K, li, P, O):
    nc, S = K.nc, K.S
    lamin = K.inp("s5_lamT", [DEPTH, 128, 2, 32])
    stepin = K.inp("s5_stepT", [DEPTH, 128, 32])
    Bin = K.inp("s5_BT", [DEPTH, 128, 2, 32, 16])
    Cin = K.inp("s5_CT", [DEPTH, 128, 2, 32, 16])
    din = K.inp("s5_dT", [DEPTH, 128, 2])
    gluin = K.inp("s5_glu_w", [DEPTH, 256, 256])
    selin = K.inp("c_sel", [128, 64, 128], BF16)
    selTin = K.inp("c_selT", [128, 64, 128], BF16)
    m01in = [K.inp("c_s5m0", [128, 2, 256]), K.inp("c_s5m1", [128, 2, 256])]
    kin = [K.inp("c_kA", [128, 32]), K.inp("c_kB", [128, 32])]
    sel3in = K.inp("c_sel3", [128, 3])
    Od, Odb = O
    with phase(K):
        Gl = K.sb("s5_Gl", [128, 32, 2, 128], BF16); Gl_b = Buf("s5_Gl")
        Aw = K.sb("s5_Aw", [128, 16, 2, 256], BF16); Aw_b = Buf("s5_Aw")
        Hs = K.sb("s5_H", [128, 2, 16, 256], BF16); Hs_b = Buf("s5_H")
        MUA = K.sb("s5_MUA", [128, 2, 16]); MUB = K.sb("s5_MUB", [128, 2, 16]); MU_b = Buf("s5_MU")
        with phase(K):
            lam = K.sb("s5_lam", [128, 2, 32]); lam_b = Buf("s5_lam")
            stp = K.sb("s5_stp", [128, 32]); stp_b = Buf("s5_stp")
            Bt = K.sb("s5_Bt", [128, 2, 32, 16]); Bt_b = Buf("s5_Bt")
            Ct = K.sb("s5_Ct", [128, 2, 32, 16]); Ct_b = Buf("s5_Ct")
            kk = K.sb("s5_kk", [128, 2, 32]); kk_b = Buf("s5_kk")
            sel3 = K.sb("s5_sel3", [128, 3]); sel3_b = Buf("s5_sel3")
            m01 = K.sb("s5_m01", [128, 2, 2, 256]); m01_b = Buf("s5_m01")
            S.dma("sp", lam[:], lamin[li], lam_b)
            S.dma("sp", stp[:], stepin[li], stp_b)
            S.dma("sp", Bt[:], Bin[li], Bt_b)
            S.dma("sp", Ct[:], Cin[li], Ct_b)
            for i in range(2):
                S.dma("sp", kk[:, i, :], kin[i], kk_b)
                S.dma("sp", m01[:, i, :, :], m01in[i], m01_b)
            S.dma("sp", sel3[:], sel3in, sel3_b)
            sm = K.sb("s5_sm", [128, 12, 32]); sm_b = Buf("s5_sm")
            DT, EA, TH, DEN, AM1, ZR, ZI, T1, T2 = [sm[:, i, :] for i in range(9)]
            lr, lim = lam[:, 0, :], lam[:, 1, :]
            dv = lambda f, r=(), w=(), rs=(): S.op("dve", f, r=list(r) + [sm_b], w=list(w) + [sm_b], rs=rs)
            S.op("act", lambda h: h.activation(out=DT, in_=stp[:], func=ACT.Exp), r=[stp_b], w=[sm_b])
            dv(lambda h: h.tensor_tensor(out=EA, in0=lr, in1=DT, op=ALU.mult), r=[lam_b])
            dv(lambda h: h.tensor_tensor(out=TH, in0=lim, in1=DT, op=ALU.mult), r=[lam_b])
            PW = K.sb("s5_PW", [128, 2, 2, 32, 32]); PW_b = Buf("s5_PW")
            big = K.sb("s5_big", [128, 4, 32, 32]); big_b = Buf("s5_big")
            bigi = K.sb("s5_bigi", [128, 32, 32], I32)
            bg = lambda f, r=(), w=(): S.op("dve", f, r=list(r) + [big_b, sm_b], w=list(w) + [big_b])
            for ab in range(2):
                kb3 = kk[:, ab, :].unsqueeze(1).to_broadcast([128, 32, 32])
                bg(lambda h: h.tensor_tensor(out=big[:, 0], in0=EA.unsqueeze(2).to_broadcast([128, 32, 32]), in1=kb3, op=ALU.mult), r=[kk_b])
                S.op("act", lambda h: h.activation(out=big[:, 1], in_=big[:, 0], func=ACT.Exp), r=[big_b], w=[big_b])
                bg(lambda h: h.tensor_tensor(out=big[:, 0], in0=TH.unsqueeze(2).to_broadcast([128, 32, 32]), in1=kb3, op=ALU.mult), r=[kk_b])
                for ri in range(2):
                    bg(lambda h: h.tensor_scalar(out=big[:, 2], in0=big[:, 0], scalar1=1.0 / (2 * np.pi), scalar2=(0.25 if ri == 0 else 0.0), op0=ALU.mult, op1=ALU.add))
                    bg(lambda h: h.tensor_copy(out=bigi[:], in_=big[:, 2]))
                    bg(lambda h: h.tensor_copy(out=big[:, 3], in_=bigi[:]))
                    bg(lambda h: h.tensor_tensor(out=big[:, 2], in0=big[:, 2], in1=big[:, 3], op=ALU.subtract))
                    bg(lambda h: h.tensor_scalar(out=big[:, 3], in0=big[:, 2], scalar1=0.5, scalar2=None, op0=ALU.is_gt))
                    bg(lambda h: h.tensor_tensor(out=big[:, 2], in0=big[:, 2], in1=big[:, 3], op=ALU.subtract))
                    bg(lambda h: h.tensor_scalar(out=big[:, 3], in0=big[:, 2], scalar1=-0.5, scalar2=None, op0=ALU.is_lt))
                    bg(lambda h: h.tensor_tensor(out=big[:, 2], in0=big[:, 2], in1=big[:, 3], op=ALU.add))
                    S.op("act", lambda h: h.activation(out=big[:, 3], in_=big[:, 2], func=ACT.Sin, scale=TWO_PI_LO), r=[big_b], w=[big_b])
                    bg(lambda h: h.tensor_tensor(out=PW[:, ab, ri], in0=big[:, 3], in1=big[:, 1], op=ALU.mult), w=[PW_b])
            AR, AI = PW[:, 0, 0, :, 16], PW[:, 0, 1, :, 16]
            dv(lambda h: h.tensor_tensor(out=DEN, in0=lr, in1=lr, op=ALU.mult), r=[lam_b])
            dv(lambda h: h.tensor_tensor(out=T1, in0=lim, in1=lim, op=ALU.mult), r=[lam_b])
            dv(lambda h: h.tensor_tensor(out=DEN, in0=DEN, in1=T1, op=ALU.add))
            dv(lambda h: h.reciprocal(out=DEN, in_=DEN))
            dv(lambda h: h.tensor_scalar(out=AM1, in0=AR, scalar1=-1.0, scalar2=None, op0=ALU.add), r=[PW_b])
            dv(lambda h: h.tensor_tensor(out=T1, in0=AM1, in1=lr, op=ALU.mult), r=[lam_b])
            dv(lambda h: h.tensor_tensor(out=T2, in0=AI, in1=lim, op=ALU.mult), r=[lam_b, PW_b])
            dv(lambda h: h.tensor_tensor(out=ZR, in0=T1, in1=T2, op=ALU.add))
            dv(lambda h: h.tensor_tensor(out=ZR, in0=ZR, in1=DEN, op=ALU.mult))
            dv(lambda h: h.tensor_tensor(out=T1, in0=AI, in1=lr, op=ALU.mult), r=[lam_b, PW_b])
            dv(lambda h: h.tensor_tensor(out=T2, in0=AM1, in1=lim, op=ALU.mult), r=[lam_b])
            dv(lambda h: h.tensor_tensor(out=ZI, in0=T1, in1=T2, op=ALU.subtract))
            dv(lambda h: h.tensor_tensor(out=ZI, in0=ZI, in1=DEN, op=ALU.mult))
            BB = K.sb("s5_BB", [128, 2, 32, 16]); BB_b = Buf("s5_BB")
            tq = K.sb("s5_tq", [128, 2, 32, 16]); tq_b = Buf("s5_tq")
            zr3 = ZR.unsqueeze(2).to_broadcast([128, 32, 16]); zi3 = ZI.unsqueeze(2).to_broadcast([128, 32, 16])
            bq = lambda f: S.op("dve", f, r=[sm_b, Bt_b, tq_b, BB_b], w=[tq_b, BB_b])
            bq(lambda h: h.tensor_tensor(out=tq[:, 0], in0=Bt[:, 0], in1=zr3, op=ALU.mult))
            bq(lambda h: h.tensor_tensor(out=tq[:, 1], in0=Bt[:, 1], in1=zi3, op=ALU.mult))
            bq(lambda h: h.tensor_tensor(out=BB[:, 0], in0=tq[:, 0], in1=tq[:, 1], op=ALU.subtract))
            bq(lambda h: h.tensor_tensor(out=tq[:, 0], in0=Bt[:, 1], in1=zr3, op=ALU.mult))
            bq(lambda h: h.tensor_tensor(out=tq[:, 1], in0=Bt[:, 0], in1=zi3, op=ALU.mult))
            bq(lambda h: h.tensor_tensor(out=BB[:, 1], in0=tq[:, 0], in1=tq[:, 1], op=ALU.add))
            for d in range(2):
                rows = slice(d * 64, (d + 1) * 64)
                mur = PW[rows, 0, 0, d * 16:(d + 1) * 16, 31]; mui = PW[rows, 0, 1, d * 16:(d + 1) * 16, 31]
                S.op("dve", lambda h: h.tensor_copy(out=MUA[rows, 0, :], in_=mur), r=[PW_b], w=[MU_b])
                S.op("dve", lambda h: h.tensor_copy(out=MUA[rows, 1, :], in_=mur), r=[PW_b], w=[MU_b])
                S.op("dve", lambda h: h.tensor_scalar(out=MUB[rows, 0, :], in0=mui, scalar1=-1.0, scalar2=None, op0=ALU.mult), r=[PW_b], w=[MU_b])
                S.op("dve", lambda h: h.tensor_copy(out=MUB[rows, 1, :], in_=mui), r=[PW_b], w=[MU_b])
            cp = K.sb("s5_cp", [128, 6, 8, 256]); cp_b = Buf("s5_cp")
            XS = K.sb("s5_XS", [128, 2, 8, 256], BF16); YS = K.sb("s5_YS", [128, 2, 8, 256], BF16); XY_b = Buf("s5_XY")
            GTs = K.sb("s5_GTs", [128, 8, 256], BF16); GTs_b = Buf("s5_GTs")
            atmp = Rot(K, "s5_at", [128, 2, 256], F32, 2)
            v4 = lambda ap: ap.rearrange("p g (a b) -> p g a b", a=16)

            def cprod(ab, k0, coef, coef_b, dgs):
                pr = PW[:, ab, 0, dgs, k0:k0 + 16].unsqueeze(3).to_broadcast([128, 8, 16, 16])
                pi = PW[:, ab, 1, dgs, k0:k0 + 16].unsqueeze(3).to_broadcast([128, 8, 16, 16])
                cr = coef[:, 0, dgs, :].unsqueeze(2).to_broadcast([128, 8, 16, 16])
                ci = coef[:, 1, dgs, :].unsqueeze(2).to_broadcast([128, 8, 16, 16])
                rr = [PW_b, coef_b, cp_b]
                S.op("dve", lambda h: h.tensor_tensor(out=v4(cp[:, 0]), in0=pr, in1=cr, op=ALU.mult), r=rr, w=[cp_b])
                S.op("pool", lambda h: h.tensor_tensor(out=v4(cp[:, 1]), in0=pi, in1=ci, op=ALU.mult), r=rr, w=[cp_b])
                S.op("dve", lambda h: h.tensor_tensor(out=v4(cp[:, 2]), in0=pr, in1=ci, op=ALU.mult), r=rr, w=[cp_b])
                S.op("pool", lambda h: h.tensor_tensor(out=v4(cp[:, 3]), in0=pi, in1=cr, op=ALU.mult), r=rr, w=[cp_b])
                S.op("dve", lambda h: h.tensor_tensor(out=cp[:, 4], in0=cp[:, 0], in1=cp[:, 1], op=ALU.subtract), r=[cp_b], w=[cp_b])
                S.op("pool", lambda h: h.tensor_tensor(out=cp[:, 5], in0=cp[:, 2], in1=cp[:, 3], op=ALU.add), r=[cp_b], w=[cp_b])

            def stack(out, outb, imcol):
                S.op("dve", lambda h: h.tensor_scalar(out=cp[:, 0], in0=cp[:, 4], scalar1=sel3[:, 0:1], scalar2=None, op0=ALU.mult), r=[cp_b, sel3_b], w=[cp_b])
                S.op("dve", lambda h: h.scalar_tensor_tensor(out=out, in0=cp[:, 5], scalar=sel3[:, imcol:imcol + 1], in1=cp[:, 0], op0=ALU.mult, op1=ALU.add),
                     r=[cp_b, sel3_b], w=[outb])

            for gb in range(2):
                for d in range(2):
                    dgs = slice(d * 16 + gb * 8, d * 16 + gb * 8 + 8)
                    cprod(1 if d == 0 else 0, 1 if d == 0 else 15, BB, BB_b, dgs)
                    stack(GTs[:], GTs_b, 1)
                    if d == 1:
                        S.op("pool", lambda h: h.tensor_copy(out=XS[:, 1], in_=GTs[:]), r=[GTs_b], w=[XY_b])
                    for gl in range(8):
                        g = gb * 8 + gl
                        for jh in range(2):
                            ps, psb = K.bank()
                            pv = ps[:].bitcast(BF16)
                            S.op("pe", lambda h: h.transpose(out=pv[:, 0:128], in_=GTs[:, gl, jh * 128:(jh + 1) * 128], identity=K.identb[:]), r=[GTs_b, K.identb_b], w=[psb])
                            evac(K, K.ev(), Gl[:, d * 16 + g, jh, :], pv[:, 0:128], r=[psb], w=[Gl_b])
                    if d == 0:
                        cprod(1, 16, BB, BB_b, dgs)
                        stack(XS[:, 0], XY_b, 1)
                    cprod(0 if d == 0 else 1, 15 if d == 0 else 16, Ct, Ct_b, dgs)
                    stack(YS[:, d], XY_b, 2)
                    cprod(0 if d == 0 else 1, 16 if d == 0 else 0, Ct, Ct_b, dgs)
                    rows = slice(d * 64, (d + 1) * 64)
                    S.op("act", lambda h: h.activation(out=Hs[rows, 0, gb * 8:gb * 8 + 8, :], in_=cp[rows, 4], func=ACT.Copy), r=[cp_b], w=[Hs_b])
                    S.op("act", lambda h: h.activation(out=Hs[rows, 1, gb * 8:gb * 8 + 8, :], in_=cp[rows, 5], func=ACT.Copy, scale=-1.0), r=[cp_b], w=[Hs_b])
                for gl in range(8):
                    g = gb * 8 + gl
                    for jh in range(2):
                        pss = []
                        for d in range(2):
                            ps, psb = K.bank()
                            S.op("pe", lambda h: h.matmul(ps[:, 0:256], lhsT=XS[:, d, gl, jh * 128:(jh + 1) * 128], rhs=YS[:, d, gl, :], start=True, stop=True), r=[XY_b], w=[psb])
                            pss.append((ps, psb))
                        at, atb = atmp.get()
                        for d in range(2):
                            S.op("dve", lambda h: h.tensor_tensor(out=at[:, d, :], in0=pss[d][0][:, 0:256], in1=m01[:, d, jh, :], op=ALU.mult), r=[pss[d][1], m01_b], w=[atb])
                        S.op("pool", lambda h: h.tensor_tensor(out=Aw[:, g, jh, :], in0=at[:, 0, :], in1=at[:, 1, :], op=ALU.add), r=[atb], w=[Aw_b])
        with phase(K):
            uT = K.sb("s5_uT", [128, 2, T], BF16); uT_b = Buf("s5_uT")
            load_fm(K, uT, uT_b, *P["s5u"])
            U = K.sb("s5_U", [128, 16, 2, NCH], BF16); U_b = Buf("s5_U")
            SN = K.sb("s5_SN", [128, 2, 16, NCH], BF16); SN_b = Buf("s5_SN")
            with phase(K):
                sel = K.sb("s5_sel", [128, 64, 128], BF16); sel_b = Buf("s5_sel")
                for q4 in range(4):
                    S.dma("sp", sel[:, q4 * 16:(q4 + 1) * 16, :], selin[:, q4 * 16:(q4 + 1) * 16, :], sel_b)
                E = K.sb("s5_E", [128, 2, 16, NCH]); E_b = Buf("s5_E")
                ua = uT[:]
                for g in range(16):
                    cc, gl = g // 8, g % 8
                    for jh in range(2):
                        ps, psb = K.bank()
                        for jl in range(8):
                            rhs = bass.AP(ua.tensor, ua.offset + cc * T + 8 * jh + jl, [list(ua.ap[0]), [16, NCH]])
                            S.op("pe", lambda h: h.matmul(ps[:, 0:NCH], lhsT=sel[:, gl * 8 + jl, :], rhs=rhs, start=(jl == 0), stop=(jl == 7)), r=[sel_b, uT_b], w=[psb])
                        evac(K, K.ev(), U[:, g, jh, :], ps[:, 0:NCH], r=[psb], w=[U_b])
                for g in range(16):
                    for ri in range(2):
                        ps, psb = K.bank()
                        for d in range(2):
                            for jh in range(2):
                                S.op("pe", lambda h: h.matmul(ps[d * 64:(d + 1) * 64, 0:NCH], lhsT=Gl[:, d * 16 + g, jh, ri * 64:(ri + 1) * 64], rhs=U[:, g, jh, :],
                                                              start=(jh == 0), stop=(jh == 1)), r=[Gl_b, U_b], w=[psb])
                        evac(K, K.ev(), E[0:64, ri, g, :], ps[0:64, 0:NCH], r=[psb], w=[E_b])
                        pa = ps[64:128, 0:NCH]
                        r1 = bass.AP(pa.tensor, pa.offset + 15, [list(pa.ap[0]), [-1, 16]])
                        r2 = bass.AP(pa.tensor, pa.offset + 271, [list(pa.ap[0]), [-1, 256]])
                        S.op("dve", lambda h: h.tensor_copy(out=E[64:128, ri, g, 0:16], in_=r1), r=[psb], w=[E_b])
                        S.op("dve", lambda h: h.tensor_copy(out=E[64:128, ri, g, 16:NCH], in_=r2), r=[psb], w=[E_b])
                w1 = K.sb("s5_w1", [128, 2, 16]); w2 = K.sb("s5_w2", [128, 2, 16]); w_b = Buf("s5_w")
                ea_ = E[:]
                rstride = 16 * NCH
                for pos in range(1, NCH):
                    prev = bass.AP(ea_.tensor, ea_.offset + pos - 1, [list(ea_.ap[0]), [rstride, 2], [NCH, 16]])
                    prev_sw = bass.AP(ea_.tensor, ea_.offset + rstride + pos - 1, [list(ea_.ap[0]), [-rstride, 2], [NCH, 16]])
                    cur = bass.AP(ea_.tensor, ea_.offset + pos, [list(ea_.ap[0]), [rstride, 2], [NCH, 16]])
                    S.op("dve", lambda h: h.tensor_tensor(out=w1[:], in0=prev, in1=MUA[:], op=ALU.mult), r=[E_b, MU_b], w=[w_b])
                    S.op("dve", lambda h: h.tensor_tensor(out=w2[:], in0=prev_sw, in1=MUB[:], op=ALU.mult), r=[E_b, MU_b], w=[w_b])
                    S.op("dve", lambda h: h.tensor_tensor(out=w1[:], in0=w1[:], in1=w2[:], op=ALU.add), r=[w_b], w=[w_b])
                    S.op("dve", lambda h: h.tensor_tensor(out=cur, in0=cur, in1=w1[:], op=ALU.add), r=[w_b, E_b], w=[E_b])
                S.op("pool", lambda h: h.memset(SN[:], 0.0), w=[SN_b])
                S.op("act", lambda h: h.activation(out=SN[0:64, :, :, 1:NCH], in_=E[0:64, :, :, 0:NCH - 1], func=ACT.Copy), r=[E_b], w=[SN_b])
                eh = E[64:128, :, :, :]
                rv1 = bass.AP(eh.tensor, eh.offset + 14, [list(eh.ap[0]), [rstride, 2], [NCH, 16], [-1, 15]])
                rv2 = bass.AP(eh.tensor, eh.offset + 270, [list(eh.ap[0]), [rstride, 2], [NCH, 16], [-1, 256]])
                S.op("dve", lambda h: h.tensor_copy(out=SN[64:128, :, :, 0:15], in_=rv1), r=[E_b], w=[SN_b])
                S.op("dve", lambda h: h.tensor_copy(out=SN[64:128, :, :, 16:NCH], in_=rv2), r=[E_b], w=[SN_b])
            with phase(K):
                selT = K.sb("s5_selT", [128, 64, 128], BF16); selT_b = Buf("s5_selT")
                for q4 in range(4):
                    S.dma("sp", selT[:, q4 * 16:(q4 + 1) * 16, :], selTin[:, q4 * 16:(q4 + 1) * 16, :], selT_b)
                Ysb = K.sb("s5_Ysb", [128, 16, 2, NCH], BF16); Ysb_b = Buf("s5_Ysb")
                yT = K.sb("s5_yT", [128, T]); yT_b = Buf("s5_yT")
                dcol = K.sb("s5_dcol", [128, 2]); dcol_b = Buf("s5_dcol")
                S.dma("sp", dcol[:], din[li], dcol_b)
                wgs = K.sb("s5_wgs", [128, 2, 256]); wgs_b = Buf("s5_wgs")
                S.dma("sp", wgs[:], gluin[li].rearrange("(c p) n -> p c n", p=128), wgs_b)
                wg = K.sb("s5_wg", [128, 2, 256], BF16); wg_b = Buf("s5_wg")
                S.op("pool", lambda h: h.tensor_copy(out=wg[:], in_=wgs[:]), r=[wgs_b], w=[wg_b])
                for g in range(16):
                    for th in range(2):
                        ps, psb = K.bank()
                        cols = slice(th * 128, (th + 1) * 128)
                        for jh in range(2):
                            S.op("pe", lambda h: h.matmul(ps[:, 0:NCH], lhsT=Aw[:, g, jh, cols], rhs=U[:, g, jh, :], start=(jh == 0), stop=False), r=[Aw_b, U_b], w=[psb])
                        for ri in range(2):
                            S.op("pe", lambda h: h.matmul(ps[:, 0:NCH], lhsT=Hs[0:64, ri, g, cols], rhs=SN[0:64, ri, g, :], start=False, stop=False), r=[Hs_b, SN_b], w=[psb])
                        for ri in range(2):
                            for hf in range(2):
                                c2 = slice(th * 128 + hf * 64, th * 128 + (hf + 1) * 64)
                                S.op("pe", lambda h: h.matmul(ps[hf * 64:(hf + 1) * 64, 0:NCH], lhsT=Hs[64:128, ri, g, c2], rhs=SN[64:128, ri, g, :], start=False, stop=(ri == 1)),
                                     r=[Hs_b, SN_b], w=[psb])
                        evac(K, K.ev(), Ysb[:, g, th, :], ps[:, 0:NCH], r=[psb], w=[Ysb_b])
                zT = K.sb("s5_zT", [128, 2, T], BF16); zT_b = Buf("s5_zT")
                ft = Rot(K, "s5_ft", [128, 512], F32, 3)
                ya = yT[:]
                for cc in range(2):
                    for t in range(16):
                        th, tl = t // 8, t % 8
                        ps, psb = K.bank()
                        for gl in range(8):
                            S.op("pe", lambda h: h.matmul(ps[:, 0:NCH], lhsT=selT[:, gl * 8 + tl, :], rhs=Ysb[:, cc * 8 + gl, th, :], start=(gl == 0), stop=(gl == 7)),
                                 r=[selT_b, Ysb_b], w=[psb])
                        dst = bass.AP(ya.tensor, ya.offset + t, [list(ya.ap[0]), [16, NCH]])
                        evac(K, K.ev(), dst, ps[:, 0:NCH], r=[psb], w=[yT_b])
                    for (t0, n) in TB:
                        yv = yT[:, t0:t0 + n]
                        S.op("dve", lambda h: h.scalar_tensor_tensor(out=yv, in0=uT[:, cc, t0:t0 + n], scalar=dcol[:, cc:cc + 1], in1=yv, op0=ALU.mult, op1=ALU.add),
                             r=[uT_b, yT_b], w=[yT_b], rs=[dcol_b])
                        a, ab_ = ft.get()
                        S.op("pool", lambda h: h.tensor_tensor(out=a[:, 0:n], in0=yv, in1=yv, op=ALU.mult), r=[yT_b], w=[ab_])
                        S.op("dve", lambda h: h.tensor_scalar(out=a[:, 0:n], in0=a[:, 0:n], scalar1=0.044715, scalar2=1.0, op0=ALU.mult, op1=ALU.add), r=[ab_], w=[ab_])
                        S.op("pool", lambda h: h.tensor_tensor(out=a[:, 0:n], in0=a[:, 0:n], in1=yv, op=ALU.mult), r=[ab_, yT_b], w=[ab_])
                        S.op("act", lambda h: h.activation(out=a[:, 0:n], in_=a[:, 0:n], func=ACT.Sigmoid, scale=1.5957691216), r=[ab_], w=[ab_])
                        S.op("dve", lambda h: h.tensor_tensor(out=zT[:, cc, t0:t0 + n], in0=a[:, 0:n], in1=yv, op=ALU.mult), r=[ab_, yT_b], w=[zT_b])
                ob = Rot(K, "s5_ob", [128, 512], BF16, 3)
                for co in range(2):
                    for (t0, n) in TB:
                        ps, psb = K.bank()
                        for ci in range(2):
                            S.op("pe", lambda h: h.matmul(ps[:, 0:n], lhsT=wg[:, ci, co * 128:(co + 1) * 128], rhs=zT[:, ci, t0:t0 + n], start=(ci == 0), stop=(ci == 1)),
                                 r=[wg_b, zT_b], w=[psb])
                        a, ab_ = ft.get()
                        S.op("act", lambda h: h.activation(out=a[:, 0:n], in_=ps[:, 0:n], func=ACT.Sigmoid), r=[psb], w=[ab_])
                        o, obb = ob.get()
                        S.op("dve", lambda h: h.tensor_tensor(out=o[:, 0:n], in0=a[:, 0:n], in1=zT[:, co, t0:t0 + n], op=ALU.mult), r=[ab_, zT_b], w=[obb])
                        S.dma("sp", Od[co * 128:(co + 1) * 128, t0:t0 + n], o[:, 0:n], obb, dr=Odb, load=False)


def phase_merge(K, li, P, O, xsrc, xsrc_b, xdst, xdst_b, last):
    nc, S = K.nc, K.S
    wbr_in = K.inp("w_branch", [DEPTH, 4, 256, D])
    wout_in = K.inp("w_out", [DEPTH, D, D])
    with phase(K):
        stg = Rot(K, "mg_stg", [128, D], F32, 2)
        wbr = K.sb("mg_wbr", [128, 4, 2, D], BF16); wbr_b = Buf("mg_wbr")
        wog = K.sb("mg_wog", [128, 2, 8, D], BF16); wog_b = Buf("mg_wog")
        for br in range(4):
            for cc in range(2):
                st, stb = stg.get()
                S.dma("sp", st[:], wbr_in[li, br, cc * 128:(cc + 1) * 128, :], stb)
                S.op("pool", lambda h: h.tensor_copy(out=wbr[:, br, cc, :], in_=st[:]), r=[stb], w=[wbr_b])
        for kc in range(8):
            st, stb = stg.get()
            S.dma("sp", st[:], wout_in[li, kc * 128:(kc + 1) * 128, :], stb)
            for s_ in range(2):
                S.op("dve", lambda h: h.tensor_tensor(out=wog[:, s_, kc, :], in0=st[:], in1=K.gbc[:, s_, :], op=ALU.mult), r=[stb, K.gbc_b], w=[wog_b])
        ob_r = Rot(K, "mg_ob", [128, 4, 2, 512], BF16, 2)
        gt_r = Rot(K, "mg_gt", [128, 32, 512], BF16, 2)
        tm_r = Rot(K, "mg_tm", [128, 4, 512], F32, 2)
        mT_r = Rot(K, "mg_mT", [128, 8, 512], BF16, 2)
        x_r = Rot(K, "mg_x", [128, D], F32, 3)
        names = ("s5", "na", "hg", "rt")
        gsrc, gsrcb = P["gate"]
        gview = gsrc.rearrange("(j p) t -> p j t", p=128)
        for (t0, n) in TB:
            ob, obb = ob_r.get()
            for br in range(4):
                for cc in range(2):
                    S.dma("sp", ob[:, br, cc, 0:n], O[names[br]][0][cc * 128:(cc + 1) * 128, t0:t0 + n], obb, dr=O[names[br]][1])
            gt, gtb = gt_r.get()
            for br in range(4):
                S.dma("sp", gt[:, br * 8:(br + 1) * 8, 0:n], gview[:, br * 8:(br + 1) * 8, t0:t0 + n], gtb, dr=gsrcb)
            mT, mTb = mT_r.get()
            for dc in range(8):
                tm, tmb = tm_r.get()
                for br in range(4):
                    ps, psb = K.bank()
                    for cc in range(2):
                        S.op("pe", lambda h: h.matmul(ps[:, 0:n], lhsT=wbr[:, br, cc, dc * 128:(dc + 1) * 128], rhs=ob[:, br, cc, 0:n], start=(cc == 0), stop=(cc == 1)),
                             r=[wbr_b, obb], w=[psb])
                    S.op("dve", lambda h: h.tensor_tensor(out=tm[:, br, 0:n], in0=ps[:, 0:n], in1=gt[:, br * 8 + dc, 0:n], op=ALU.mult), r=[psb, gtb], w=[tmb])
                S.op("pool", lambda h: h.tensor_tensor(out=tm[:, 0, 0:n], in0=tm[:, 0, 0:n], in1=tm[:, 1, 0:n], op=ALU.add), r=[tmb], w=[tmb])
                S.op("pool", lambda h: h.tensor_tensor(out=tm[:, 2, 0:n], in0=tm[:, 2, 0:n], in1=tm[:, 3, 0:n], op=ALU.add), r=[tmb], w=[tmb])
                S.op("pool", lambda h: h.tensor_tensor(out=mT[:, dc, 0:n], in0=tm[:, 0, 0:n], in1=tm[:, 2, 0:n], op=ALU.add), r=[tmb], w=[mTb])
            for i in range(n // 128):
                t = t0 // 128 + i
                s_ = 1 if t < 2 else 0
                if last and t < 2:
                    continue
                x, xb = x_r.get()
                S.dma("sp", x[:], xsrc[t * 128:(t + 1) * 128, :], xb, dr=xsrc_b)
                for half in range(2):
                    ps, psb = K.bank()
                    for kc in range(8):
                        S.op("pe", lambda h: h.matmul(ps[:], lhsT=mT[:, kc, i * 128:(i + 1) * 128], rhs=wog[:, s_, kc, half * 512:(half + 1) * 512], start=(kc == 0), stop=(kc == 7)),
                             r=[mTb, wog_b], w=[psb])
                    S.op("dve", lambda h: h.tensor_tensor(out=x[:, half * 512:(half + 1) * 512], in0=x[:, half * 512:(half + 1) * 512], in1=ps[:], op=ALU.add), r=[psb, xb], w=[xb])
                r0 = t * 128 - (CTX if last else 0)
                S.dma("sp", xdst[r0:r0 + 128, :], x[:], xb, dr=xdst_b, load=False)


def phase_route(K, li, xsrc, xsrc_b, row_off, xn2, xn2_b, R, with_ctx):
    nc, S = K.nc, K.S
    rwin = K.inp("router_wT", [DEPTH, 128, 8, 16])
    groups = ([(0, 2, 1)] if with_ctx else []) + [(2 + 4 * i, 4, 0) for i in range(8)]
    with phase(K):
        rw = K.sb("rt_rw", [128, 8, 16]); rw_b = Buf("rt_rw")
        S.dma("sp", rw[:], rwin[li], rw_b)
        xr = Rot(K, "r_xr", [128, D], F32, 3)
        xnr = Rot(K, "r_xnr", [128, D], F32, 8)
        junk = K.sb("r_junk", [128, D], BF16); junk_b = Buf("r_junk")
        ssr = Rot(K, "r_ssr", [128, 8], F32, 4)
        xnb = Rot(K, "r_xnb", [128, D], BF16, 3)
        hg_r = Rot(K, "r_hg", [128, 8, 512], F32, 2)
        affT = K.sb("r_affT", [16, T]); affT_b = Buf("r_affT")
        codeT = K.sb("r_codeT", [16, T]); codeT_b = Buf("r_codeT")
        S.op("pool", lambda h: h.memset(R.aff[:], 0.0), w=[R.aff_b])
        S.op("pool", lambda h: h.memset(R.code[:], -1.0), w=[R.code_b])
        for (t0, n, s) in groups:
            xns = []
            for i in range(n):
                t = t0 + i
                x, xb = xr.get()
                S.dma("sp", x[:], xsrc[t * 128 - row_off:(t + 1) * 128 - row_off, :], xb, dr=xsrc_b)
                ss, ssb = ssr.get()
                S.op("pool", lambda h: h.memset(ss[:], 0.0), w=[ssb])
                S.op("act", lambda h: h.activation(out=junk[:], in_=x[:], func=ACT.Square, accum_out=ss[:, 0:1]), r=[xb], w=[junk_b, ssb])
                S.op("act", lambda h: h.activation(out=ss[:, 1:2], in_=ss[:, 0:1], func=ACT.Sqrt, scale=1.0 / D, bias=K.epsc[:, 0:1]), r=[ssb, K.epsc_b], w=[ssb])
                S.op("dve", lambda h: h.reciprocal(out=ss[:, 2:3], in_=ss[:, 1:2]), r=[ssb], w=[ssb])
                xn, xnbuf = xnr.get()
                S.op("act", lambda h: h.activation(out=xn[:], in_=x[:], func=ACT.Copy, scale=ss[:, 2:3]), r=[xb], w=[xnbuf], rs=[ssb])
                xns.append((xn, xnbuf))
                o, ob = xnb.get()
                S.op("pool", lambda h: h.tensor_copy(out=o[:], in_=xn[:]), r=[xnbuf], w=[ob])
                S.dma("sp", xn2[t * 128:(t + 1) * 128, :], o[:], ob, dr=xn2_b, load=False)
            hgt, hgb = hg_r.get()
            for j in range(8):
                ps, psb = K.bank()
                for i, (xn, xnbuf) in enumerate(xns):
                    S.op("pe", lambda h: h.transpose(out=ps[:, i * 128:(i + 1) * 128], in_=xn[:, j * 128:(j + 1) * 128], identity=K.ident[:]),
                         r=[xnbuf, K.ident_b], w=[psb])
                evac(K, K.ev(), hgt[:, j, 0:n * 128], ps[:, 0:n * 128], r=[psb, K.A_b, K.mv_b], w=[hgb],
                     scale=K.A2[:, j, s:s + 1], bias=K.mv[:, 3, j, s:s + 1])
            for i in range(n):
                t = t0 + i
                pl, plb = K.bank()
                for j in range(8):
                    S.op("pe", lambda h: h.matmul(pl[:, 0:16], lhsT=hgt[:, j, i * 128:(i + 1) * 128], rhs=rw[:, j, :], start=(j == 0), stop=(j == 7)),
                         r=[hgb, rw_b], w=[plb])
                ss, ssb = ssr.get()
                S.op("dve", lambda h: h.reduce_max(out=ss[:, 0:1], in_=pl[:, 0:16], axis=AX.X), r=[plb], w=[ssb])
                S.op("dve", lambda h: h.tensor_scalar(out=ss[:, 1:2], in0=ss[:, 0:1], scalar1=-1.0, scalar2=None, op0=ALU.mult), r=[ssb], w=[ssb])
                S.op("pool", lambda h: h.memset(ss[:, 2:3], 0.0), w=[ssb])
                S.op("act", lambda h: h.activation(out=R.aff[:, t, :], in_=pl[:, 0:16], func=ACT.Exp, bias=ss[:, 1:2], accum_out=ss[:, 2:3]), r=[plb], w=[R.aff_b, ssb], rs=[ssb])
                S.op("dve", lambda h: h.reciprocal(out=ss[:, 3:4], in_=ss[:, 2:3]), r=[ssb], w=[ssb])
                S.op("dve", lambda h: h.tensor_scalar(out=R.aff[:, t, :], in0=R.aff[:, t, :], scalar1=ss[:, 3:4], scalar2=None, op0=ALU.mult), r=[R.aff_b], w=[R.aff_b], rs=[ssb])
                pt, ptb = K.bank()
                S.op("pe", lambda h: h.transpose(out=pt[0:16, 0:128], in_=R.aff[:, t, :], identity=K.ident[:]), r=[R.aff_b, K.ident_b], w=[ptb])
                evac(K, K.ev(), affT[:, t * 128:(t + 1) * 128], pt[0:16, 0:128], r=[ptb], w=[affT_b])
        bs = K.sb("r_bs", [16, 8]); bs_b = Buf("r_bs")
        cj = K.sb("r_cj", [16, SEQ]); cj_b = Buf("r_cj")
        onesT = K.sb("r_onesT", [16, SEQ]); onesT_b = Buf("r_onesT")
        S.op("pool", lambda h: h.memset(onesT[:], 1.0), w=[onesT_b])
        sets = ([(0, CTX, 2 * CTX // NE)] if with_ctx else []) + [(CTX, SEQ, 2 * SEQ // NE)]
        for (c0, n, cap) in sets:
            av = affT[:, c0:c0 + n]
            LO, HI, MID, CNT, GE, D1 = [bs[:, i:i + 1] for i in range(6)]
            b1 = lambda f, rs_=True: S.op("dve", f, r=[bs_b], w=[bs_b], rs=[bs_b] if rs_ else [])
            b1(lambda h: h.memset(bs[:], 0.0), False)
            b1(lambda h: h.memset(HI, 1.0), False)
            for it in range(34):
                b1(lambda h: h.tensor_tensor(out=MID, in0=LO, in1=HI, op=ALU.add), False)
                b1(lambda h: h.tensor_scalar(out=MID, in0=MID, scalar1=0.5, scalar2=None, op0=ALU.mult), False)
                S.op("dve", lambda h: h.tensor_scalar(out=cj[:, 0:n], in0=av, scalar1=MID, scalar2=0.0, op0=ALU.is_ge, op1=ALU.add, accum_out=CNT),
                     r=[affT_b], w=[cj_b, bs_b], rs=[bs_b])
                b1(lambda h: h.tensor_scalar(out=GE, in0=CNT, scalar1=float(cap) - 0.5, scalar2=None, op0=ALU.is_ge), False)
                b1(lambda h: h.tensor_tensor(out=D1, in0=MID, in1=LO, op=ALU.subtract), False)
                b1(lambda h: h.scalar_tensor_tensor(out=LO, in0=D1, scalar=GE, in1=LO, op0=ALU.mult, op1=ALU.add))
                b1(lambda h: h.tensor_tensor(out=D1, in0=HI, in1=MID, op=ALU.subtract), False)
                b1(lambda h: h.scalar_tensor_tensor(out=HI, in0=D1, scalar=GE, in1=MID, op0=ALU.mult, op1=ALU.add))
            S.op("dve", lambda h: h.tensor_scalar(out=cj[:, 0:n], in0=av, scalar1=LO, scalar2=None, op0=ALU.is_ge), r=[affT_b], w=[cj_b], rs=[bs_b])
            S.op("dve", lambda h: h.tensor_tensor_scan(out=codeT[:, c0:c0 + n], data0=onesT[:, 0:n], data1=cj[:, 0:n], initial=0.0, op0=ALU.mult, op1=ALU.add),
                 r=[cj_b, onesT_b], w=[codeT_b])
            S.op("dve", lambda h: h.tensor_tensor(out=codeT[:, c0:c0 + n], in0=codeT[:, c0:c0 + n], in1=cj[:, 0:n], op=ALU.mult), r=[cj_b, codeT_b], w=[codeT_b])
            S.op("dve", lambda h: h.tensor_scalar(out=codeT[:, c0:c0 + n], in0=codeT[:, c0:c0 + n], scalar1=-1.0, scalar2=None, op0=ALU.add), r=[codeT_b], w=[codeT_b])
            for t in range(c0 // 128, (c0 + n) // 128):
                pt, ptb = K.bank()
                S.op("pe", lambda h: h.transpose(out=pt[:, 0:16], in_=codeT[0:16, t * 128:(t + 1) * 128], identity=K.ident[0:16, 0:16]), r=[codeT_b, K.ident_b], w=[ptb])
                evac(K, K.ev(), R.code[:, t, :], pt[:, 0:16], r=[ptb], w=[R.code_b])
        dump(K, "dbg_aff", R.aff[:], R.aff_b, [128, NT, 16], F32)
        dump(K, "dbg_code", R.code[:], R.code_b, [128, NT, 16], F32)


NF = FE // 128


def phase_moe(K, li, xn2, xn2_b, R, xacc, xacc_b, row_off, with_ctx):
    nc, S = K.nc, K.S
    wg_in = K.inp("ex_w_gate", [DEPTH, NE, D, FE])
    wu_in = K.inp("ex_w_up", [DEPTH, NE, D, FE])
    wd_in = K.inp("ex_w_down", [DEPTH, NE, FE, D])
    iot_in = K.inp("c_iota512", [128, 512])
    NS = 544 if with_ctx else 512
    CAPC = 2 * CTX // NE
    with phase(K):
        xn = K.sb("mo_xn", [128, NT, D], BF16); xn_b = Buf("mo_xn")
        vx = xn2.rearrange("(n p) c -> p n c", p=128)
        for n0 in range(0, NT, 2):
            S.dma("sp", xn[:, n0:n0 + 2, :], vx[:, n0:n0 + 2, :], xn_b, dr=xn2_b)
        iot = K.sb("mo_iot", [128, 512]); iot_b = Buf("mo_iot")
        S.dma("sp", iot[:], iot_in, iot_b)
        sel_r = Rot(K, "mo_sel", [128, 512], BF16, 3)
        xeT = K.sb("mo_xeT", [128, 8, NS], BF16); xeT_b = Buf("mo_xeT")
        actT = K.sb("mo_actT", [128, NF, NS], BF16); actT_b = Buf("mo_actT")
        wst = Rot(K, "mo_wst", [128, 8, 128], F32, 4)
        wbf = Rot(K, "mo_wbf", [128, 8, 128], BF16, 4)
        dst_r = Rot(K, "mo_dst", [128, 512], F32, 2)
        dbf = Rot(K, "mo_dbf", [128, 2, 512], BF16, 2)
        sl_r = Rot(K, "mo_sl", [128, 512], F32, 2)
        ye = K.sb("mo_ye", [128, 5, D], BF16); ye_b = Buf("mo_ye")
        sg_r = Rot(K, "mo_sg", [128, 4, 128], BF16, 3)
        oo_r = Rot(K, "mo_oo", [128, 512], F32, 2)
        xt_b = {}
        nst = 5 if with_ctx else 4
        for e in range(NE):
            for pas in range(2):
                pgs = [K.bank() for _ in range(4)]
                for t in range(2, NT):
                    sel, selb = sel_r.get()
                    S.op("dve", lambda h: h.tensor_scalar(out=sel[:], in0=iot[:], scalar1=R.code[:, t, e:e + 1], scalar2=None, op0=ALU.is_equal), r=[iot_b], w=[selb], rs=[R.code_b])
                    for q in range(4):
                        kc = pas * 4 + q
                        S.op("pe", lambda h: h.matmul(pgs[q][0][:], lhsT=xn[:, t, kc * 128:(kc + 1) * 128], rhs=sel[:], start=(t == 2), stop=(t == NT - 1)),
                             r=[xn_b, selb], w=[pgs[q][1]])
                for q in range(4):
                    kc = pas * 4 + q
                    evac(K, "act", xeT[:, kc, 0:512], pgs[q][0][:], r=[pgs[q][1], K.A_b, K.mv_b], w=[xeT_b], scale=K.A2[:, kc, 0:1], bias=K.mv[:, 3, kc, 0:1])
            if with_ctx:
                pc, pcb = K.bank()
                sels = []
                for t in range(2):
                    sel, selb = sel_r.get()
                    S.op("dve", lambda h: h.tensor_scalar(out=sel[:, 0:CAPC], in0=iot[:, 0:CAPC], scalar1=R.code[:, t, e:e + 1], scalar2=None, op0=ALU.is_equal), r=[iot_b], w=[selb], rs=[R.code_b])
                    sels.append((sel, selb))
                for kc in range(8):
                    for t in range(2):
                        S.op("pe", lambda h: h.matmul(pc[:, kc * CAPC:(kc + 1) * CAPC], lhsT=xn[:, t, kc * 128:(kc + 1) * 128], rhs=sels[t][0][:, 0:CAPC], start=(t == 0), stop=(t == 1)),
                             r=[xn_b, sels[t][1]], w=[pcb])
                for kc in range(8):
                    evac(K, "act", xeT[:, kc, 512:NS], pc[:, kc * CAPC:(kc + 1) * CAPC], r=[pcb, K.A_b, K.mv_b], w=[xeT_b], scale=K.A2[:, kc, 1:2], bias=K.mv[:, 3, kc, 1:2])
            def load_gu(f):
                res = []
                for w_in in (wg_in, wu_in):
                    ws, wsb = wst.get()
                    S.dma("sp", ws[:], w_in[li, e].rearrange("(k p) f -> p k f", p=128)[:, :, f * 128:(f + 1) * 128], wsb)
                    res.append((ws, wsb))
                return res
            nxt = load_gu(0)
            for f in range(NF):
                cur = nxt
                wbs = []
                for i, (ws, wsb) in enumerate(cur):
                    wb, wbb = wbf.get()
                    S.op("pool" if i == 0 else "dve", lambda h: h.tensor_copy(out=wb[:], in_=ws[:]), r=[wsb], w=[wbb])
                    wbs.append((wb, wbb))
                if f + 1 < NF:
                    nxt = load_gu(f + 1)
                segs = [(0, 512)] + ([(512, NS)] if with_ctx else [])
                for (a0, a1) in segs:
                    pg, pgb = K.bank()
                    pu, pub = K.bank()
                    for (pp, ppb, (wb, wbb)) in ((pg, pgb, wbs[0]), (pu, pub, wbs[1])):
                        for kc in range(8):
                            S.op("pe", lambda h: h.matmul(pp[:, 0:a1 - a0], lhsT=wb[:, kc, :], rhs=xeT[:, kc, a0:a1], start=(kc == 0), stop=(kc == 7)), r=[wbb, xeT_b], w=[ppb])
                    sl, slb = sl_r.get()
                    S.op("act", lambda h: h.activation(out=sl[:, 0:a1 - a0], in_=pg[:, 0:a1 - a0], func=ACT.Silu), r=[pgb], w=[slb])
                    S.op("dve", lambda h: h.tensor_tensor(out=actT[:, f, a0:a1], in0=sl[:, 0:a1 - a0], in1=pu[:, 0:a1 - a0], op=ALU.mult), r=[slb, pub], w=[actT_b])
            for half in range(2):
                hs = slice(half * 512, (half + 1) * 512)
                pys = [K.bank() for _ in range(nst)]
                for f in range(NF):
                    ds_, dsb = dst_r.get()
                    S.dma("sp", ds_[:], wd_in[li, e, f * 128:(f + 1) * 128, hs], dsb)
                    db, dbb = dbf.get()
                    S.op("dve", lambda h: h.tensor_tensor(out=db[:, 0, :], in0=ds_[:], in1=K.gbc[:, 2, hs], op=ALU.mult), r=[dsb, K.gbc_b], w=[dbb])
                    if with_ctx:
                        S.op("pool", lambda h: h.tensor_tensor(out=db[:, 1, :], in0=ds_[:], in1=K.gbc[:, 3, hs], op=ALU.mult), r=[dsb, K.gbc_b], w=[dbb])
                    for st in range(nst):
                        ns_ = 128 if st < 4 else CAPC
                        S.op("pe", lambda h: h.matmul(pys[st][0][0:ns_, :], lhsT=actT[:, f, st * 128:st * 128 + ns_], rhs=db[:, 1 if st == 4 else 0, :], start=(f == 0), stop=(f == NF - 1)),
                             r=[actT_b, dbb], w=[pys[st][1]])
                for st in range(nst):
                    ns_ = 128 if st < 4 else CAPC
                    evac(K, K.ev(), ye[0:ns_, st, hs], pys[st][0][0:ns_, :], r=[pys[st][1]], w=[ye_b])
            for t in range(0 if with_ctx else 2, NT):
                isctx = t < 2
                ncol = CAPC if isctx else 512
                sel, selb = sel_r.get()
                S.op("dve", lambda h: h.tensor_scalar(out=sel[:, 0:ncol], in0=iot[:, 0:ncol], scalar1=R.code[:, t, e:e + 1], scalar2=R.aff[:, t, e:e + 1],
                                                      op0=ALU.is_equal, op1=ALU.mult), r=[iot_b], w=[selb], rs=[R.code_b, R.aff_b])
                pt, ptb = K.bank()
                pv = pt[:].bitcast(BF16)
                sts = [4] if isctx else [0, 1, 2, 3]
                for st in sts:
                    ns_ = CAPC if isctx else 128
                    q = st % 4
                    S.op("pe", lambda h: h.transpose(out=pv[0:ns_, q * 128:(q + 1) * 128], in_=sel[:, q * 128:q * 128 + ns_], identity=K.identb[:]), r=[selb, K.identb_b], w=[ptb])
                sg, sgb = sg_r.get()
                if isctx:
                    evac(K, K.ev(), sg[0:CAPC, 0, :], pv[0:CAPC, 0:128], r=[ptb], w=[sgb])
                else:
                    evac(K, K.ev(), sg[:].rearrange("p s t -> p (s t)"), pv[:, 0:512], r=[ptb], w=[sgb])
                for half in range(2):
                    hs = slice(half * 512, (half + 1) * 512)
                    po, pob = K.bank()
                    for si, st in enumerate(sts):
                        ns_ = 128 if st < 4 else CAPC
                        S.op("pe", lambda h: h.matmul(po[:], lhsT=sg[0:ns_, st % 4, :], rhs=ye[0:ns_, st, hs], start=(si == 0), stop=(si == len(sts) - 1)),
                             r=[sgb, ye_b], w=[pob])
                    oo, oob = oo_r.get()
                    evac(K, K.ev(), oo[:], po[:], r=[pob], w=[oob])
                    r0 = t * 128 - row_off
                    tb_ = xt_b.setdefault((t, half), Buf("xacc_t"))
                    S.dma("pool", xacc[r0:r0 + 128, hs], oo[:], oob, dr=tb_, load=False, accum_op=ALU.add)


def setup_consts(K):
    nc, S = K.nc, K.S
    K.ident = K.sb("ident", [128, 128]); K.ident_b = Buf("ident")
    K.ones = K.sb("ones", [128, 128]); K.ones_b = Buf("ones")
    K.epsc = K.sb("epsc", [128, 1]); K.epsc_b = Buf("epsc")
    K.cond = K.sb("cond", [128, 8, 2]); K.cond_b = Buf("cond")
    K.mv = K.sb("mv", [128, 6, 8, 2]); K.mv_b = Buf("mv")
    K.A1 = K.sb("A1", [128, 8, 2]); K.A2 = K.sb("A2", [128, 8, 2]); K.A_b = Buf("A")
    K.gbc = K.sb("gbc", [128, 4, D]); K.gbc_b = Buf("gbc")
    S.dma("sp", K.ident[:], K.inp("c_ident", [128, 128]), K.ident_b)
    K.identb = K.sb("identb", [128, 128], BF16); K.identb_b = Buf("identb")
    S.op("pool", lambda h: h.tensor_copy(out=K.identb[:], in_=K.ident[:]), r=[K.ident_b], w=[K.identb_b])
    S.op("dve", lambda h: h.memset(K.ones[:], 1.0), w=[K.ones_b])
    K.onesb = K.sb("onesb", [128, 128], BF16); K.onesb_b = Buf("onesb")
    S.op("dve", lambda h: h.memset(K.onesb[:], 1.0), w=[K.onesb_b])
    S.op("dve", lambda h: h.memset(K.epsc[:], EPS), w=[K.epsc_b])
    craw = K.sb("craw", [128, 2, 8]); craw_b = Buf("craw")
    S.dma("sp", craw[:, 0, :], K.inp("cT", [128, 8]), craw_b)
    S.dma("sp", craw[:, 1, :], K.inp("c_ctxT", [128, 8]), craw_b)
    for s in range(2):
        S.op("act", lambda h: h.activation(out=K.cond[:, :, s], in_=craw[:, s, :], func=ACT.Silu), r=[craw_b], w=[K.cond_b])
    S.scope_ents = []


def build_program(debug=None, upto="all", only=None, p_external=False, o_external=False, layers=DEPTH):
    nc = bass.Bass("TRN2", target_bir_lowering=False)
    with ExitStack() as stack:
        K = mk_ctx(nc, stack, debug)
        setup_consts(K)
        R = Ctx()
        R.aff = K.sb("R_aff", [128, NT, 16]); R.aff_b = Buf("R_aff")
        R.code = K.sb("R_code", [128, NT, 16]); R.code_b = Buf("R_code")
        xs0 = K.inp("xs0", [T, D]); xs0_b = Buf("xs0")
        kind1 = "ExternalOutput" if "xs1" in K.debug else "Internal"
        xs1 = nc.dram_tensor("xs1", [T, D], F32, kind=kind1).ap(); xs1_b = Buf("xs1")
        out = nc.dram_tensor("out", [SEQ, D], F32, kind="ExternalOutput").ap(); out_b = Buf("out")
        xn2, xn2_b = K.dram("xn2", [T, D], BF16)
        P = alloc_proj_scratch(K, p_external)
        if o_external:
            O = {n: (K.inp("O_" + n, [256, T], BF16), Buf("O_" + n)) for n in ("s5", "na", "hg", "rt")}
        else:
            O = {n: K.dram("O_" + n, [256, T], BF16) for n in ("s5", "na", "hg", "rt")}
        xcur, xcur_b = xs0, xs0_b
        for li in range(layers):
            last = li == DEPTH - 1
            if not p_external or upto in ("all", "merge", "route"):
                phase0(K, li)
            if not p_external:
                with phase(K):
                    hT = K.sb("hT", [128, 8, T], BF16); hT_b = Buf("hT")
                    norm_to_fm(K, xcur, xcur_b, hT, hT_b, K.A1, 0)
                    dump(K, "dbg_hT", hT[:], hT_b, [128, 8, T], BF16)
                    phase1b(K, li, hT, hT_b, P)
            if upto == "p1":
                break
            if not o_external:
                if only is None or "rt" in only:
                    phase_ret(K, li, P, O["rt"])
                if only is None or "na" in only:
                    phase_na(K, li, P, O["na"])
                if only is None or "hg" in only:
                    phase_hg(K, li, P, O["hg"])
                if only is None or "s5" in only:
                    phase_s5(K, li, P, O["s5"])
            if upto == "mix":
                break
            xnext, xnext_b = (out, out_b) if last else (xs1, xs1_b)
            phase_merge(K, li, P, O, xcur, xcur_b, xnext, xnext_b, last)
            if upto == "merge":
                break
            phase_route(K, li, xnext, xnext_b, CTX if last else 0, xn2, xn2_b, R, not last)
            if upto == "route":
                break
            phase_moe(K, li, xn2, xn2_b, R, xnext, xnext_b, CTX if last else 0, not last)
            xcur, xcur_b = xnext, xnext_b
        K.S.barrier()
    return nc, K


def _pk(v):
    v = np.asarray(v, np.float32)
    return np.ascontiguousarray(v.reshape(-1, 128).T)


def rope_tables():
    t = np.arange(SEQ)
    quarter = 16
    inv = 10000.0 ** (-np.arange(quarter) / quarter)
    ang = np.concatenate([(t // 64)[:, None] * inv, (t % 64)[:, None] * inv], axis=1)
    cos = np.ones((128, T), np.float32)
    sin = np.zeros((128, T), np.float32)
    for p in range(128):
        i = p % 32
        cos[p, CTX:] = np.cos(ang[:, i])
        sin[p, CTX:] = np.sin(ang[:, i])
    return cos, sin


def host_consts():
    g = {}
    g["c_ident"] = np.eye(128, dtype=np.float32)
    j = np.arange(128)[:, None]; t = np.arange(128)[None, :]
    g["c_epos0"] = np.maximum(t - j, 0).astype(np.float32)
    g["c_epos1"] = np.maximum(j - t, 0).astype(np.float32)
    g["c_mk0"] = (t >= j).astype(np.float32)
    g["c_mk1"] = (j >= t).astype(np.float32)
    g["c_iota1"] = np.broadcast_to((np.arange(128) + 1.0)[None, :], (128, 128)).astype(np.float32)
    g["c_iotar"] = np.broadcast_to((128.0 - np.arange(128))[None, :], (128, 128)).astype(np.float32)
    g["c_pcol"] = np.stack([127.0 - np.arange(128), np.arange(128) * 1.0], axis=1).astype(np.float32)
    g["c_blk64"] = ((j // 64) == (t // 64)).astype(np.float32)
    same = (j // 16) == (t // 16)
    g["c_hgm0"] = (same & (j <= t)).astype(np.float32)
    g["c_hgm1"] = (same & (j >= t)).astype(np.float32)
    c = np.arange(8)[None, :]
    tt = np.arange(128)[:, None]
    g["c_blkm"] = np.stack([(tt // 16 == c), (tt // 16 == 7 - c)], axis=1).astype(np.float32)
    rst = np.ones((128, HB), np.float32); rst[:, ::16] = 0.0
    g["c_rst"] = rst
    kcol = np.arange(64)[:, None]; qcol = np.arange(64)[None, :]
    cs = np.clip(qcol - 8, 0, 48)
    ok = (kcol >= cs) & (kcol < cs + 16)
    m = np.where(ok, 0.0, -240000.0).astype(np.float32)
    g["c_namask"] = np.concatenate([m, m], axis=0)
    cos, sin = rope_tables()
    g["rope_cos"] = cos
    g["rope_sin"] = sin
    sel = np.zeros((128, 8, 8, 128), np.float32)
    for gl in range(8):
        for jl in range(8):
            for q in range(16):
                sel[16 * gl + q, gl, jl, 16 * jl + q] = 1.0
    g["c_sel"] = sel.reshape(128, 64, 128).astype(ml_dtypes.bfloat16)
    g["c_selT"] = np.ascontiguousarray(np.transpose(sel, (3, 1, 2, 0))).reshape(128, 64, 128).astype(ml_dtypes.bfloat16)
    jj = (np.arange(2)[None, :, None] * 8 + (np.arange(128) // 16)[:, None, None])
    tt2 = (np.arange(256) // 16)[None, None, :]
    g["c_s5m0"] = (jj <= tt2).astype(np.float32)
    g["c_s5m1"] = (jj >= tt2).astype(np.float32)
    g["c_kA"] = np.broadcast_to((np.arange(32) - 15.0)[None, :], (128, 32)).astype(np.float32)
    g["c_kB"] = np.broadcast_to((16.0 - np.arange(32))[None, :], (128, 32)).astype(np.float32)
    g["c_iota512"] = np.broadcast_to(np.arange(512, dtype=np.float32)[None, :], (128, 512))
    g["c_iotap"] = (np.arange(128)[:, None] + 128.0 * np.arange(4)[None, :]).astype(np.float32)
    e16 = np.zeros((128, 32, 128), np.float32)
    for j in range(16):
        e16[j, j, :] = 1.0
        e16[32 + j, 16 + j, :] = 1.0
    g["c_e16"] = e16
    p = np.arange(128)
    g["c_sel3"] = np.stack([(p < 64) * 1.0, (p >= 64) * 1.0, (p >= 64) * -1.0], axis=1).astype(np.float32)
    return g


def prep_core_inputs(inputs, b, names):
    g = host_consts()
    f32 = lambda a: np.asarray(a, np.float32)
    g["xs0"] = np.concatenate([inputs["ctx"][b], inputs["x"][b]], axis=0).astype(np.float32)
    g["cT"] = _pk(inputs["c"][b])
    g["c_ctxT"] = _pk(inputs["c_ctx"])
    g["ada_w"] = inputs["ada_w"]
    g["ada_bT"] = np.stack([_pk(inputs["ada_b"][l]) for l in range(DEPTH)])
    g["norm_mix_wT"] = np.stack([_pk(inputs["norm_mix_w"][l]) for l in range(DEPTH)])
    g["norm_ffn_wT"] = np.stack([_pk(inputs["norm_ffn_w"][l]) for l in range(DEPTH)])
    g["w_in"] = inputs["w_in"]
    g["ret_bc"] = np.broadcast_to(f32(inputs["ret_decay_logit"]).reshape(DEPTH, 1, 8), (DEPTH, 128, 8))
    qn, kn = f32(inputs["na_q_norm"]), f32(inputs["na_k_norm"])
    g["na_qk_normT"] = np.stack([np.stack([np.tile(qn[l], 2), np.tile(kn[l], 2)], axis=1) for l in range(DEPTH)])
    kcol = np.arange(64)[:, None]; qcol = np.arange(64)[None, :]
    dc = np.clip(kcol - qcol + 15, 0, 30)
    rp = f32(inputs["na_rpb"])[:, :, :, dc]
    rp = np.transpose(rp, (0, 3, 1, 2, 4))
    g["na_rpbT2"] = np.concatenate([rp, rp], axis=1)
    g["w_branch"] = inputs["w_branch"]
    g["w_out"] = inputs["w_out"]
    g["router_wT"] = np.ascontiguousarray(np.transpose(f32(inputs["router_w"]).reshape(DEPTH, 8, 128, NE), (0, 2, 1, 3)))
    g["ex_w_gate"] = inputs["ex_w_gate"]
    g["ex_w_up"] = inputs["ex_w_up"]
    g["ex_w_down"] = inputs["ex_w_down"]
    def nfirst(a):
        a = f32(a)
        a = np.moveaxis(a, 3, 1)
        a = a.reshape(a.shape[0], 64, 32, *a.shape[4:])
        return np.concatenate([a, a], axis=1)
    g["s5_lamT"] = np.stack([nfirst(inputs["s5_lam_re"]), nfirst(inputs["s5_lam_im"])], axis=2)
    g["s5_stepT"] = np.broadcast_to(f32(inputs["s5_log_step"]).reshape(DEPTH, 1, 32), (DEPTH, 128, 32))
    g["s5_BT"] = np.stack([nfirst(inputs["s5_b_re"]), nfirst(inputs["s5_b_im"])], axis=2)
    cre = np.swapaxes(f32(inputs["s5_c_re"]), 3, 4); cim = np.swapaxes(f32(inputs["s5_c_im"]), 3, 4)
    g["s5_CT"] = np.stack([nfirst(cre), nfirst(cim)], axis=2)
    g["s5_dT"] = np.ascontiguousarray(np.transpose(f32(inputs["s5_d"]).reshape(DEPTH, 2, 128), (0, 2, 1)))
    g["s5_glu_w"] = f32(inputs["s5_glu_w"])
    lbp = f32(inputs["hg_lower_bounds"])
    g["hg_lbT"] = np.ascontiguousarray(np.transpose(lbp.reshape(DEPTH, 2, 128), (2, 0, 1)))
    g["hg_norm_wT"] = f32(inputs["hg_norm_w"]).reshape(DEPTH, 64, 1)
    return {k: np.ascontiguousarray(v) for k, v in g.items() if k in names}


def kernel(**inputs):
    nc, K = build_program()
    names = set(K.inputs.keys())
    in_maps = [prep_core_inputs(inputs, b, names) for b in range(8)]
    res = run_bass_kernel_spmd(nc, in_maps, core_ids=list(range(8)))
    return np.stack([np.asarray(r["out"], np.float32) for r in res.results], axis=0)
```

```python
import os
import numpy as np
import ml_dtypes
import concourse.bass as bass
import concourse.mybir as mybir
from concourse.bass_utils import run_bass_kernel_spmd
from contextlib import ExitStack

F32 = mybir.dt.float32
BF16 = mybir.dt.bfloat16
I32 = mybir.dt.int32
ACT = mybir.ActivationFunctionType
ALU = mybir.AluOpType
AX = mybir.AxisListType

D = 1024
SEQ = 4096
CTX = 256
T = SEQ + CTX
NT = T // 128
DEPTH = 2
D_IN = 7424
NE = 16
FE = 2816
EPS = 1e-6
LIMIT = int(os.environ.get("MK_LIMIT", "0"))
SAME_ENG_SYNC = bool(os.environ.get("MK_SAMESYNC"))
TB = [(i * 512, 512) for i in range(8)] + [(4096, 256)]


class Buf:
    __slots__ = ("name", "w", "r", "dma_sem", "dma_cnt", "pend_w", "pend_r")

    def __init__(self, name):
        self.name = name
        self.w = None
        self.r = {}
        self.dma_sem = None
        self.dma_cnt = 0
        self.pend_w = {}
        self.pend_r = {}


class _Eng:
    def __init__(self, name, h, sem):
        self.name = name
        self.h = h
        self.sem = sem
        self.cnt = 0
        self.seen = {}
        self.seen_dma = {}


class Sched:
    def __init__(self, nc, stack):
        self.nc = nc
        self.stack = stack
        hs = {"pe": nc.tensor, "act": nc.scalar, "dve": nc.vector, "pool": nc.gpsimd, "sp": nc.sync}
        self.eng = {}
        for k, h in hs.items():
            sem = stack.enter_context(nc.semaphore("s_" + k))
            self.eng[k] = _Eng(k, h, sem)
        self.dma_sems = []
        self.free_sems = []
        self.scope_ents = []
        self.n_ins = 0

    def _wait_eng(self, E, e2, s, force=False):
        if e2 == E.name and E.name == "pe" and not (force or SAME_ENG_SYNC):
            return
        if E.seen.get(e2, 0) >= s:
            return
        E.h.wait_ge(self.eng[e2].sem, s)
        E.seen[e2] = s

    def _wait_dma(self, E, sem, val):
        k = id(sem)
        if E.seen_dma.get(k, 0) >= val:
            return
        E.h.wait_ge(sem, val)
        E.seen_dma[k] = val

    def _sync(self, E, r, w, rs=()):
        for b in r:
            if b.w is not None:
                self._wait_eng(E, b.w[0], b.w[1])
            if b.dma_cnt:
                self._wait_dma(E, b.dma_sem[0], b.dma_cnt)
        for b in rs:
            if b.w is not None:
                self._wait_eng(E, b.w[0], b.w[1], force=True)
            if b.dma_cnt:
                self._wait_dma(E, b.dma_sem[0], b.dma_cnt)
        for b in w:
            if b.w is not None:
                self._wait_eng(E, b.w[0], b.w[1])
            for e2, s in b.r.items():
                self._wait_eng(E, e2, s)
            if b.dma_cnt:
                self._wait_dma(E, b.dma_sem[0], b.dma_cnt)

    def op(self, e, fn, r=(), w=(), rs=()):
        if LIMIT and self.n_ins >= LIMIT:
            return None
        E = self.eng[e]
        self._sync(E, r, w, rs)
        ins = fn(E.h)
        E.cnt += 1
        ins.then_inc(E.sem, 1)
        for b in r:
            b.r[e] = E.cnt
        for b in rs:
            b.r[e] = E.cnt
        for b in w:
            b.w = (e, E.cnt)
            b.r = {}
        self.n_ins += 1
        return ins

    def _get_dma_sem(self, sb):
        if sb.dma_sem is None:
            if self.free_sems:
                ent = self.free_sems.pop()
            else:
                ent = [self.stack.enter_context(self.nc.semaphore("d%d" % len(self.dma_sems))), 0]
                self.dma_sems.append(ent)
            self.scope_ents.append(ent)
            sb.dma_sem = ent
        return sb.dma_sem

    def release_phase_sems(self):
        self.free_sems.extend(self.scope_ents)
        self.scope_ents = []

    def dma(self, q, out, in_, sb, dr=None, load=True, **kw):
        if LIMIT and self.n_ins >= LIMIT:
            return None
        Q = self.eng[q]
        ent = self._get_dma_sem(sb)
        if load:
            self._sync(Q, (), (sb,))
        else:
            self._sync(Q, (sb,), ())
        if dr is not None:
            pend = dr.pend_w if load else dr.pend_r
            for sem, val in pend.values():
                self._wait_dma(Q, sem, val)
            if not load:
                for sem, val in dr.pend_w.values():
                    self._wait_dma(Q, sem, val)
        ins = Q.h.dma_start(out=out, in_=in_, **kw)
        ent[1] += 16
        sb.dma_cnt = ent[1]
        ins.then_inc(ent[0], 16)
        if dr is not None:
            (dr.pend_r if load else dr.pend_w)[id(ent[0])] = (ent[0], ent[1])
        self.n_ins += 1
        return ins

    def idma(self, out, in_, sb, idx_b, out_off=None, in_off=None, dr=None, gather=True, **kw):
        if LIMIT and self.n_ins >= LIMIT:
            return None
        Q = self.eng["pool"]
        ent = self._get_dma_sem(sb)
        if gather:
            self._sync(Q, (idx_b,), (sb,))
        else:
            self._sync(Q, (idx_b, sb), ())
        if dr is not None:
            for sem, val in dr.pend_w.values():
                self._wait_dma(Q, sem, val)
            if not gather:
                for sem, val in dr.pend_r.values():
                    self._wait_dma(Q, sem, val)
        ins = Q.h.indirect_dma_start(out=out, out_offset=out_off, in_=in_, in_offset=in_off, **kw)
        ent[1] += 16
        sb.dma_cnt = ent[1]
        ins.then_inc(ent[0], 16)
        if dr is not None:
            (dr.pend_r if gather else dr.pend_w)[id(ent[0])] = (ent[0], ent[1])
        self.n_ins += 1
        return ins

    def barrier(self):
        for E in self.eng.values():
            for E2 in self.eng.values():
                if E2 is not E and E2.cnt:
                    self._wait_eng(E, E2.name, E2.cnt)
            for sem, cnt in self.dma_sems:
                if cnt:
                    self._wait_dma(E, sem, cnt)


class Rot:
    def __init__(self, K, name, shape, dtype, n):
        self.t = [K.sb(f"{name}{i}", shape, dtype) for i in range(n)]
        self.b = [Buf(f"{name}{i}") for i in range(n)]
        self.i = 0

    def get(self):
        i = self.i
        self.i = (i + 1) % len(self.t)
        return self.t[i], self.b[i]


class Ctx:
    pass


def mk_ctx(nc, stack, debug):
    K = Ctx()
    K.nc = nc
    K.S = Sched(nc, stack)
    K.top = stack
    K.stack = stack
    K.debug = set(debug or ())
    K.inputs = {}
    K.dbuf = {}

    K.uid = 0

    def sb(name, shape, dtype=F32):
        K.uid += 1
        return K.stack.enter_context(nc.sbuf_tensor(f"{name}_{K.uid}", list(shape), dtype))
    K.sb = sb

    def inp(name, shape, dtype=F32):
        if name not in K.inputs:
            K.inputs[name] = nc.dram_tensor(name, list(shape), dtype, kind="ExternalInput").ap()
        return K.inputs[name]
    K.inp = inp

    def dram(name, shape, dtype):
        kind = "ExternalOutput" if name in K.debug else "Internal"
        ap = nc.dram_tensor(name, list(shape), dtype, kind=kind).ap()
        K.dbuf[name] = Buf(name)
        return ap, K.dbuf[name]
    K.dram = dram
    K.ps = [stack.enter_context(nc.psum_tensor(f"ps{i}", [128, 512], F32)) for i in range(8)]
    K.psb = [Buf(f"ps{i}") for i in range(8)]
    K.pi = 0

    def bank():
        i = K.pi
        K.pi = (i + 1) % 8
        return K.ps[i], K.psb[i]
    K.bank = bank
    K.evi = 0

    def ev():
        K.evi ^= 1
        return "act" if K.evi else "dve"
    K.ev = ev
    return K


class phase:
    def __init__(self, K):
        self.K = K

    def __enter__(self):
        self.prev = self.K.stack
        self.st = ExitStack()
        self.st.__enter__()
        self.K.stack = self.st
        return self

    def __exit__(self, *a):
        self.K.S.barrier()
        self.K.S.release_phase_sems()
        self.K.stack = self.prev
        return self.st.__exit__(*a)


def evac(K, eng, out, in_, r, w, func=None, scale=None, bias=None, rs=()):
    S = K.S
    if eng == "act":
        kw = {}
        if scale is not None:
            kw["scale"] = scale
        if bias is not None:
            kw["bias"] = bias
        f = func if func is not None else (ACT.Identity if bias is not None else ACT.Copy)
        S.op("act", lambda h: h.activation(out=out, in_=in_, func=f, **kw), r=r, w=w, rs=rs)
    else:
        assert func is None
        if scale is None and bias is None:
            S.op(eng, lambda h: h.tensor_copy(out=out, in_=in_), r=r, w=w)
        elif bias is None:
            S.op(eng, lambda h: h.tensor_scalar(out=out, in0=in_, scalar1=scale, scalar2=None, op0=ALU.mult), r=r, w=w, rs=rs)
        else:
            sc = 1.0 if scale is None else scale
            S.op(eng, lambda h: h.tensor_scalar(out=out, in0=in_, scalar1=sc, scalar2=bias, op0=ALU.mult, op1=ALU.add), r=r, w=w, rs=rs)


def dump(K, name, src_ap, src_b, shape, dtype):
    if name in K.debug:
        ap, b = K.dram(name, shape, dtype)
        K.S.dma("sp", ap, src_ap, src_b, dr=b, load=False)


def phase0(K, li):
    nc, S = K.nc, K.S
    ada_w = K.inp("ada_w", [DEPTH, D, 6 * D])
    ada_bT = K.inp("ada_bT", [DEPTH, 128, 48])
    nmw = K.inp("norm_mix_wT", [DEPTH, 128, 8])
    nfw = K.inp("norm_ffn_wT", [DEPTH, 128, 8])
    with phase(K):
        aw = Rot(K, "adaw", [128, 6 * D], F32, 2)
        adab = K.sb("adab", [128, 48]); adab_b = Buf("adab")
        nw = K.sb("nw", [128, 2, 8]); nw_b = Buf("nw")
        dg = Rot(K, "dg", [128, 128], F32, 3)
        S.dma("sp", adab[:], ada_bT[li], adab_b)
        S.dma("sp", nw[:, 0, :], nmw[li], nw_b)
        S.dma("sp", nw[:, 1, :], nfw[li], nw_b)
        pmA, pmAb = K.bank()
        pmB, pmBb = K.bank()
        for k in range(8):
            a, ab = aw.get()
            S.dma("sp", a[:], ada_w[li, k * 128:(k + 1) * 128, :], ab)
            pm, pmb = (pmA, pmAb) if k < 4 else (pmB, pmBb)
            c0 = (k % 4) * 96
            for j in range(48):
                S.op("pe", lambda h: h.matmul(pm[:, c0 + 2 * j:c0 + 2 * j + 2], lhsT=a[:, j * 128:(j + 1) * 128], rhs=K.cond[:, k, :],
                                              start=True, stop=True),
                     r=[ab, K.cond_b], w=[pmb])
        mv = K.mv
        acc = K.sb("p0acc", [128, 2, 96]); acc_b = Buf("p0acc")
        for i, (pm, pmb) in enumerate(((pmA, pmAb), (pmB, pmBb))):
            S.op("dve", lambda h: h.tensor_reduce(out=acc[:, i, :], in_=pm[:, 0:384].rearrange("p (k c) -> p c k", k=4), axis=AX.X, op=ALU.add),
                 r=[pmb], w=[acc_b])
        S.op("dve", lambda h: h.tensor_tensor(out=acc[:, 0, :], in0=acc[:, 0, :], in1=acc[:, 1, :], op=ALU.add), r=[acc_b], w=[acc_b])
        S.op("dve", lambda h: h.tensor_tensor(out=mv[:].rearrange("p v k s -> p (v k) s"),
                                              in0=acc[:, 0, :].rearrange("p (j s) -> p j s", s=2),
                                              in1=adab[:].unsqueeze(2).to_broadcast([128, 48, 2]), op=ALU.add),
             r=[acc_b, adab_b], w=[K.mv_b])
        for (Ai, vi, wi) in ((K.A1, 1, 0), (K.A2, 4, 1)):
            for s in range(2):
                S.op("dve", lambda h: h.scalar_tensor_tensor(out=Ai[:, :, s], in0=mv[:, vi, :, s], scalar=1.0, in1=nw[:, wi, :],
                                                             op0=ALU.add, op1=ALU.mult),
                     r=[K.mv_b, nw_b], w=[K.A_b])
        for gi, vi in enumerate((2, 5)):
            for s in range(2):
                for half in range(2):
                    pb, pbb = K.bank()
                    for q in range(4):
                        kk = half * 4 + q
                        d, db = dg.get()
                        S.op("dve", lambda h: h.tensor_scalar(out=d[:], in0=K.ident[:], scalar1=mv[:, vi, kk, s:s + 1], scalar2=None, op0=ALU.mult),
                             r=[K.ident_b], w=[db], rs=[K.mv_b])
                        S.op("pe", lambda h: h.matmul(pb[:, q * 128:(q + 1) * 128], lhsT=K.ones[:], rhs=d[:], start=True, stop=True),
                             r=[K.ones_b, db], w=[pbb])
                    evac(K, K.ev(), K.gbc[:, gi * 2 + s, half * 512:(half + 1) * 512], pb[:], r=[pbb], w=[K.gbc_b])
        dump(K, "dbg_mv", K.mv[:], K.mv_b, [128, 6, 8, 2], F32)
        dump(K, "dbg_gbc", K.gbc[:], K.gbc_b, [128, 4, D], F32)


def norm_to_fm(K, xsrc, xsrc_b, hT, hT_b, A, B_vi, xn_dram=None):
    nc, S = K.nc, K.S
    groups = [(0, 2, 1)] + [(2 + 4 * i, 4, 0) for i in range(8)]
    with phase(K):
        xr = Rot(K, "xr", [128, D], F32, 3)
        xnr = Rot(K, "xnr", [128, D], F32, 8)
        junk = K.sb("junk", [128, D], BF16); junk_b = Buf("junk")
        ssr = Rot(K, "ssr", [128, 4], F32, 4)
        xnb = Rot(K, "xnb", [128, D], BF16, 3) if xn_dram is not None else None
        for (t0, n, s) in groups:
            xns = []
            for i in range(n):
                t = t0 + i
                x, xb = xr.get()
                S.dma("sp", x[:], xsrc[t * 128:(t + 1) * 128, :], xb, dr=xsrc_b)
                ss, ssb = ssr.get()
                S.op("pool", lambda h: h.memset(ss[:], 0.0), w=[ssb])
                S.op("act", lambda h: h.activation(out=junk[:], in_=x[:], func=ACT.Square, accum_out=ss[:, 0:1]), r=[xb], w=[junk_b, ssb])
                S.op("act", lambda h: h.activation(out=ss[:, 1:2], in_=ss[:, 0:1], func=ACT.Sqrt, scale=1.0 / D, bias=K.epsc[:, 0:1]), r=[ssb, K.epsc_b], w=[ssb])
                S.op("dve", lambda h: h.reciprocal(out=ss[:, 2:3], in_=ss[:, 1:2]), r=[ssb], w=[ssb])
                xn, xnbuf = xnr.get()
                S.op("act", lambda h: h.activation(out=xn[:], in_=x[:], func=ACT.Copy, scale=ss[:, 2:3]), r=[xb], w=[xnbuf], rs=[ssb])
                xns.append((xn, xnbuf))
                if xn_dram is not None:
                    o, ob = xnb.get()
                    S.op("pool", lambda h: h.tensor_copy(out=o[:], in_=xn[:]), r=[xnbuf], w=[ob])
                    S.dma("sp", xn_dram[0][t * 128:(t + 1) * 128, :], o[:], ob, dr=xn_dram[1], load=False)
            for j in range(8):
                ps, psb = K.bank()
                for i, (xn, xnbuf) in enumerate(xns):
                    S.op("pe", lambda h: h.transpose(out=ps[:, i * 128:(i + 1) * 128], in_=xn[:, j * 128:(j + 1) * 128], identity=K.ident[:]),
                         r=[xnbuf, K.ident_b], w=[psb])
                evac(K, K.ev(), hT[:, j, t0 * 128:(t0 + n) * 128], ps[:, 0:n * 128], r=[psb, K.A_b, K.mv_b], w=[hT_b],
                     scale=A[:, j, s:s + 1], bias=K.mv[:, B_vi, j, s:s + 1])


BLOCKS = [
    ("s5u", "fm", None), ("naq", "fm", None), ("nak", "fm", None), ("nav", "tm", None),
    ("hgq", "fm", "silu"), ("hgff", "fm32", None), ("hgfb", "fm32", None), ("hgi", "tm", None), ("hgg", "fm", "silu"),
    ("rtq", "rope", 1.0), ("rtk", "rope", 0.125), ("rtv", "tm", None), ("rtg", "fm", "silu"),
] + [(f"gate{i}", "gate", i) for i in range(16)]


def alloc_proj_scratch(K, external=False):
    P = {}

    def mk(name, shape, dt):
        if external:
            return (K.inp(name, shape, dt), Buf(name))
        return K.dram(name, shape, dt)
    for name, kind, _ in BLOCKS:
        if kind == "tm":
            P[name] = mk("P_" + name, [T, 256], BF16)
        elif kind == "fm32":
            P[name] = mk("P_" + name, [256, T], F32)
        elif kind == "gate":
            continue
        else:
            P[name] = mk("P_" + name, [256, T], BF16)
    P["gate"] = mk("P_gate", [4096, T], BF16)
    return P


def phase1b(K, li, hT, hT_b, P):
    nc, S = K.nc, K.S
    w_in = K.inp("w_in", [DEPTH, D, D_IN])
    ropec = K.inp("rope_cos", [128, T], F32)
    ropes = K.inp("rope_sin", [128, T], F32)
    wv = w_in[li].rearrange("(k p) c -> p k c", p=128)
    with phase(K):
        wst = Rot(K, "wst", [128, 8, 256], F32, 2)
        wbf = Rot(K, "wbf", [128, 8, 256], BF16, 2)
        wsw = K.sb("wsw", [128, 8, 256], BF16); wsw_b = Buf("wsw")
        cosT = K.sb("cosT", [128, T]); sinT = K.sb("sinT", [128, T]); rope_b = Buf("rope")
        ob16 = Rot(K, "ob16", [128, 512], BF16, 4)
        of32 = Rot(K, "of32", [128, 512], F32, 3)
        tmp = Rot(K, "rtmp", [128, 512], F32, 4)
        S.dma("sp", cosT[:], ropec, rope_b)
        S.dma("sp", sinT[:], ropes, rope_b)
        nblk = len(BLOCKS)

        def load_w(cb):
            ws, wsb = wst.get()
            S.dma("sp", ws[:], wv[:, :, cb * 256:(cb + 1) * 256], wsb)
            return ws, wsb
        nxt = load_w(0)
        for cb in range(nblk):
            ws, wsb = nxt
            wb, wbb = wbf.get()
            S.op("pool", lambda h: h.tensor_copy(out=wb[:, 0:4, :], in_=ws[:, 0:4, :]), r=[wsb], w=[wbb])
            S.op("dve", lambda h: h.tensor_copy(out=wb[:, 4:8, :], in_=ws[:, 4:8, :]), r=[wsb], w=[wbb])
            if cb + 1 < nblk:
                nxt = load_w(cb + 1)
            name, kind, post = BLOCKS[cb]
            if kind == "tm":
                dst, dstb = P[name]
                for t in range(NT):
                    ps, psb = K.bank()
                    for k in range(8):
                        S.op("pe", lambda h: h.matmul(ps[:, 0:256], lhsT=hT[:, k, t * 128:(t + 1) * 128], rhs=wb[:, k, :],
                                                      start=(k == 0), stop=(k == 7)), r=[hT_b, wbb], w=[psb])
                    o, ob = ob16.get()
                    evac(K, K.ev(), o[:, 0:256], ps[:, 0:256], r=[psb], w=[ob])
                    S.dma("sp", dst[t * 128:(t + 1) * 128, :], o[:, 0:256], ob, dr=dstb, load=False)
                continue
            if kind == "rope":
                v_in = wb[:].rearrange("p k (h two i) -> p k h two i", h=4, two=2)
                v_out = wsw[:].rearrange("p k (h two i) -> p k h two i", h=4, two=2)
                for k in range(8):
                    S.op("act", lambda h: h.activation(out=v_out[:, k, :, 0, :], in_=v_in[:, k, :, 1, :], func=ACT.Copy, scale=-1.0), r=[wbb], w=[wsw_b])
                    S.op("pool", lambda h: h.tensor_copy(out=v_out[:, k, :, 1, :], in_=v_in[:, k, :, 0, :]), r=[wbb], w=[wsw_b])
            for cc in range(2):
                for (tk0, n) in TB:
                    ps, psb = K.bank()
                    for k in range(8):
                        S.op("pe", lambda h: h.matmul(ps[:, 0:n], lhsT=wb[:, k, cc * 128:(cc + 1) * 128], rhs=hT[:, k, tk0:tk0 + n],
                                                      start=(k == 0), stop=(k == 7)), r=[hT_b, wbb], w=[psb])
                    if kind == "rope":
                        ps2, psb2 = K.bank()
                        for k in range(8):
                            S.op("pe", lambda h: h.matmul(ps2[:, 0:n], lhsT=wsw[:, k, cc * 128:(cc + 1) * 128], rhs=hT[:, k, tk0:tk0 + n],
                                                          start=(k == 0), stop=(k == 7)), r=[hT_b, wsw_b], w=[psb2])
                        t1, t1b = tmp.get()
                        t2, t2b = tmp.get()
                        S.op("dve", lambda h: h.tensor_tensor(out=t1[:, 0:n], in0=ps[:, 0:n], in1=cosT[:, tk0:tk0 + n], op=ALU.mult), r=[psb, rope_b], w=[t1b])
                        S.op("dve", lambda h: h.tensor_tensor(out=t2[:, 0:n], in0=ps2[:, 0:n], in1=sinT[:, tk0:tk0 + n], op=ALU.mult), r=[psb2, rope_b], w=[t2b])
                        o, ob = ob16.get()
                        if post == 1.0:
                            S.op("pool", lambda h: h.tensor_tensor(out=o[:, 0:n], in0=t1[:, 0:n], in1=t2[:, 0:n], op=ALU.add), r=[t1b, t2b], w=[ob])
                        else:
                            S.op("pool", lambda h: h.tensor_tensor(out=t1[:, 0:n], in0=t1[:, 0:n], in1=t2[:, 0:n], op=ALU.add), r=[t1b, t2b], w=[t1b])
                            S.op("act", lambda h: h.activation(out=o[:, 0:n], in_=t1[:, 0:n], func=ACT.Copy, scale=float(post)), r=[t1b], w=[ob])
                        dst, dstb = P[name]
                        S.dma("sp", dst[cc * 128:(cc + 1) * 128, tk0:tk0 + n], o[:, 0:n], ob, dr=dstb, load=False)
                    elif kind == "fm32":
                        o, ob = of32.get()
                        evac(K, K.ev(), o[:, 0:n], ps[:, 0:n], r=[psb], w=[ob])
                        dst, dstb = P[name]
                        S.dma("sp", dst[cc * 128:(cc + 1) * 128, tk0:tk0 + n], o[:, 0:n], ob, dr=dstb, load=False)
                    else:
                        o, ob = ob16.get()
                        if kind == "gate":
                            evac(K, "act", o[:, 0:n], ps[:, 0:n], r=[psb], w=[ob], func=ACT.Sigmoid)
                            dst, dstb = P["gate"]
                            r0 = post * 256 + cc * 128
                        else:
                            if post == "silu":
                                evac(K, "act", o[:, 0:n], ps[:, 0:n], r=[psb], w=[ob], func=ACT.Silu)
                            else:
                                evac(K, K.ev(), o[:, 0:n], ps[:, 0:n], r=[psb], w=[ob])
                            dst, dstb = P[name]
                            r0 = cc * 128
                        S.dma("sp", dst[r0:r0 + 128, tk0:tk0 + n], o[:, 0:n], ob, dr=dstb, load=False)


def load_fm(K, tile, tb, src, srcb, nchunks=2):
    for cc in range(nchunks):
        K.S.dma("sp", tile[:, cc, :], src[cc * 128:(cc + 1) * 128, :], tb, dr=srcb)


def head_norm_finish(K, po, pob, n, gt, gtb, out, outb, sq_rot, rs_rot, wcol=None, wb=None):
    S = K.S
    sq, sqb = sq_rot.get()
    sqh = sq[:].bitcast(BF16)
    S.op("act", lambda h: h.activation(out=sqh[0:64, 0:n], in_=po[0:64, 0:n], func=ACT.Square), r=[pob], w=[sqb])
    pss, pssb = K.bank()
    S.op("pe", lambda h: h.matmul(pss[0:64, 0:n], lhsT=K.onesb[0:64, 0:64], rhs=sqh[0:64, 0:n], start=True, stop=True), r=[sqb, K.onesb_b], w=[pssb])
    rs, rsb = rs_rot.get()
    S.op("act", lambda h: h.activation(out=rs[0:64, 0:n], in_=pss[0:64, 0:n], func=ACT.Sqrt, scale=1.0 / 64, bias=K.epsc[0:64, 0:1]), r=[pssb, K.epsc_b], w=[rsb])
    S.op("dve", lambda h: h.reciprocal(out=rs[0:64, 0:n], in_=rs[0:64, 0:n]), r=[rsb], w=[rsb])
    if wcol is None:
        S.op("dve", lambda h: h.tensor_tensor(out=sq[0:64, 0:n], in0=po[0:64, 0:n], in1=rs[0:64, 0:n], op=ALU.mult), r=[pob, rsb], w=[sqb])
    else:
        S.op("dve", lambda h: h.scalar_tensor_tensor(out=sq[0:64, 0:n], in0=po[0:64, 0:n], scalar=wcol, in1=rs[0:64, 0:n], op0=ALU.mult, op1=ALU.mult),
             r=[pob, rsb], w=[sqb], rs=[wb])
    S.op("pool", lambda h: h.tensor_tensor(out=out, in0=sq[0:64, 0:n], in1=gt, op=ALU.mult), r=[sqb, gtb], w=[outb])


def mm_k64(K, ps, psb, c0, n, lhsT, rhs, r0, rb, start=True, stop=True):
    S = K.S
    if r0 == 0:
        S.op("pe", lambda h: h.matmul(ps[:, c0:c0 + n], lhsT=lhsT, rhs=rhs, start=start, stop=stop), r=rb, w=[psb])
    else:
        for hf in range(2):
            S.op("pe", lambda h: h.matmul(ps[hf * 64:(hf + 1) * 64, c0:c0 + n], lhsT=lhsT[:, hf * 64:(hf + 1) * 64], rhs=rhs, start=start, stop=stop), r=rb, w=[psb])


def load_tm(K, tile, tb, src, srcb, c0=0, cw=256):
    v = src.rearrange("(n p) c -> p n c", p=128)
    for n0 in range(0, NT, 6):
        n1 = min(NT, n0 + 6)
        K.S.dma("sp", tile[:, n0:n1, :], v[:, n0:n1, c0:c0 + cw], tb, dr=srcb)


def hv(dr):
    return dr.rearrange("(h v) t -> v h t", v=64)


def phase_ret(K, li, P, O):
    nc, S = K.nc, K.S
    dlin = K.inp("ret_bc", [DEPTH, 128, 8])
    cE0 = K.inp("c_epos0", [128, 128]); cE1 = K.inp("c_epos1", [128, 128])
    cM0 = K.inp("c_mk0", [128, 128]); cM1 = K.inp("c_mk1", [128, 128])
    cI1 = K.inp("c_iota1", [128, 128]); cIr = K.inp("c_iotar", [128, 128])
    cPc = K.inp("c_pcol", [128, 2])
    Od, Odb = O
    with phase(K):
        cst = K.sb("rt_cst", [128, 6, 128]); cst_b = Buf("rt_cst")
        for i, c in enumerate((cE0, cE1, cM0, cM1, cI1, cIr)):
            S.dma("sp", cst[:, i, :], c, cst_b)
        pcol = K.sb("rt_pcol", [128, 2]); pcol_b = Buf("rt_pcol")
        S.dma("sp", pcol[:], cPc, pcol_b)
        dl = K.sb("rt_dl", [128, 8]); dl_b = Buf("rt_dl")
        S.dma("sp", dl[:], dlin[li], dl_b)
        y = K.sb("rt_y", [128, 8]); ta = K.sb("rt_ta", [128, 8]); lg = K.sb("rt_lg", [128, 8]); lg_b = Buf("rt_lg")
        S.op("act", lambda h: h.activation(out=y[:], in_=dl[:], func=ACT.Exp, scale=-1.0), r=[dl_b], w=[lg_b])
        S.op("dve", lambda h: h.tensor_scalar(out=ta[:], in0=y[:], scalar1=-0.25, scalar2=1.0 / 3.0, op0=ALU.mult, op1=ALU.add), r=[lg_b], w=[lg_b])
        S.op("dve", lambda h: h.tensor_tensor(out=ta[:], in0=ta[:], in1=y[:], op=ALU.mult), r=[lg_b], w=[lg_b])
        S.op("dve", lambda h: h.tensor_scalar(out=ta[:], in0=ta[:], scalar1=-0.5, scalar2=None, op0=ALU.add), r=[lg_b], w=[lg_b])
        S.op("dve", lambda h: h.tensor_tensor(out=ta[:], in0=ta[:], in1=y[:], op=ALU.mult), r=[lg_b], w=[lg_b])
        S.op("dve", lambda h: h.tensor_scalar(out=ta[:], in0=ta[:], scalar1=1.0, scalar2=None, op0=ALU.add), r=[lg_b], w=[lg_b])
        S.op("dve", lambda h: h.tensor_tensor(out=ta[:], in0=ta[:], in1=y[:], op=ALU.mult), r=[lg_b], w=[lg_b])
        S.op("dve", lambda h: h.tensor_scalar(out=lg[:], in0=ta[:], scalar1=-1.0, scalar2=None, op0=ALU.mult), r=[lg_b], w=[lg_b])
        lgp = K.sb("rt_lgp", [128, 2, 2]); lgp_b = Buf("rt_lgp")
        for cc in range(2):
            for d in range(2):
                for hp in range(2):
                    col = d * 4 + 2 * cc + hp
                    S.op("dve", lambda h: h.tensor_copy(out=lgp[hp * 64:(hp + 1) * 64, cc, d:d + 1], in_=lg[hp * 64:(hp + 1) * 64, col:col + 1]), r=[lg_b], w=[lgp_b])
        M = K.sb("rt_M", [128, 4, 128]); M_b = Buf("rt_M")
        e0 = K.sb("rt_e0", [128, 128]); e1 = K.sb("rt_e1", [128, 128]); e_b = Buf("rt_e")
        for hh in range(4):
            S.op("act", lambda h: h.activation(out=e0[:], in_=cst[:, 0, :], func=ACT.Exp, scale=lg[:, hh:hh + 1]), r=[cst_b], w=[e_b], rs=[lg_b])
            S.op("act", lambda h: h.activation(out=e1[:], in_=cst[:, 1, :], func=ACT.Exp, scale=lg[:, 4 + hh:5 + hh]), r=[cst_b], w=[e_b], rs=[lg_b])
            S.op("dve", lambda h: h.tensor_tensor(out=e0[:], in0=e0[:], in1=cst[:, 2, :], op=ALU.mult), r=[e_b, cst_b], w=[e_b])
            S.op("dve", lambda h: h.tensor_tensor(out=e1[:], in0=e1[:], in1=cst[:, 3, :], op=ALU.mult), r=[e_b, cst_b], w=[e_b])
            S.op("dve", lambda h: h.tensor_tensor(out=M[:, hh, :], in0=e0[:], in1=e1[:], op=ALU.add), r=[e_b], w=[M_b])
        XI = K.sb("rt_XI", [128, 2, 2, 128]); XI_b = Buf("rt_XI")
        for d in range(2):
            for cc in range(2):
                S.op("act", lambda h: h.activation(out=XI[:, d, cc, :], in_=cst[:, 4 + d, :], func=ACT.Exp, scale=lgp[:, cc, d:d + 1]), r=[cst_b], w=[XI_b], rs=[lgp_b])
        ZT = K.sb("rt_ZT", [128, 2, 4]); ZT_b = Buf("rt_ZT")
        for d in range(2):
            S.op("act", lambda h: h.activation(out=ZT[:, d, :], in_=lg[:, d * 4:(d + 1) * 4], func=ACT.Exp, scale=pcol[:, d:d + 1]), r=[lg_b, pcol_b], w=[ZT_b], rs=[pcol_b])
        G128 = K.sb("rt_G", [128, 2, 2]); G_b = Buf("rt_G")
        S.op("act", lambda h: h.activation(out=G128[:], in_=lgp[:], func=ACT.Exp, scale=128.0), r=[lgp_b], w=[G_b])
        qT = K.sb("rt_qT", [128, 2, T], BF16); qT_b = Buf("rt_qT")
        kT = K.sb("rt_kT", [128, 2, T], BF16); kT_b = Buf("rt_kT")
        load_fm(K, qT, qT_b, *P["rtq"])
        load_fm(K, kT, kT_b, *P["rtk"])
        vall = K.sb("rt_v", [128, NT, 256], BF16); v_b = Buf("rt_v")
        load_tm(K, vall, v_b, *P["rtv"])
        qx = K.sb("rt_qx", [128, 2, 2, T], BF16); qx_b = Buf("rt_qx")
        for d in range(2):
            for cc in range(2):
                eng = "dve" if cc == 0 else "pool"
                S.op(eng, lambda h: h.tensor_tensor(out=qx[:, d, cc, :].rearrange("p (n a) -> p n a", a=128),
                                                    in0=qT[:, cc, :].rearrange("p (n a) -> p n a", a=128),
                                                    in1=XI[:, d, cc, :].unsqueeze(1).to_broadcast([128, NT, 128]), op=ALU.mult),
                     r=[qT_b, XI_b], w=[qx_b])
        kz = K.sb("rt_kz", [128, 2, NT, 256], BF16); kz_b = Buf("rt_kz")
        for t in range(NT):
            ps, psb = K.bank()
            pv = ps[:].bitcast(BF16)
            for cc in range(2):
                S.op("pe", lambda h: h.transpose(out=pv[:, cc * 128:(cc + 1) * 128], in_=kT[:, cc, t * 128:(t + 1) * 128], identity=K.identb[:]),
                     r=[kT_b, K.identb_b], w=[psb])
            for d in range(2):
                eng = "dve" if d == 0 else "pool"
                if eng == "pool":
                    eng = "dve"
                S.op(eng, lambda h: h.tensor_tensor(out=kz[:, d, t, :].rearrange("p (h k) -> p h k", h=4),
                                                    in0=pv[:, 0:256].rearrange("p (h k) -> p h k", h=4),
                                                    in1=ZT[:, d, :].unsqueeze(2).to_broadcast([128, 4, 64]), op=ALU.mult),
                     r=[psb, ZT_b], w=[kz_b])
        Sst = K.sb("rt_S", [128, 2, 2, 64]); Sst_b = [Buf("rt_S0"), Buf("rt_S1")]
        Sall = K.sb("rt_Sall", [128, 2, NT, 2, 64], BF16); Sall_b = Buf("rt_Sall")
        S.op("dve", lambda h: h.memset(Sst[:], 0.0), w=Sst_b)
        order = [list(range(NT)), [1, 0] + list(range(NT - 1, 1, -1))]
        for step in range(NT):
            for d in range(2):
                t = order[d][step]
                pk, pkb = K.bank()
                for hh in range(4):
                    cc = hh // 2
                    S.op("pe", lambda h: h.matmul(pk[:, hh * 64:(hh + 1) * 64], lhsT=kz[:, d, t, cc * 128:(cc + 1) * 128], rhs=vall[:, t, hh * 64:(hh + 1) * 64],
                                                  start=True, stop=True), r=[kz_b, v_b], w=[pkb])
                S.op("act", lambda h: h.activation(out=Sall[:, d, t, :, :], in_=Sst[:, d, :, :], func=ACT.Copy), r=[Sst_b[d]], w=[Sall_b])
                S.op("dve", lambda h: h.tensor_tensor(out=Sst[:, d, :, :], in0=Sst[:, d, :, :], in1=G128[:, :, d:d + 1].to_broadcast([128, 2, 64]), op=ALU.mult),
                     r=[Sst_b[d], G_b], w=[Sst_b[d]])
                pkv = pk[:, 0:256].rearrange("p (c q v) -> p c q v", c=2, q=2)
                for hp in range(2):
                    S.op("dve", lambda h: h.tensor_tensor(out=Sst[hp * 64:(hp + 1) * 64, d, :, :], in0=Sst[hp * 64:(hp + 1) * 64, d, :, :],
                                                          in1=pkv[hp * 64:(hp + 1) * 64, :, hp, :], op=ALU.add), r=[Sst_b[d], pkb], w=[Sst_b[d]])
        A_r = Rot(K, "rt_A", [128, 4, 128], BF16, 3)
        sq_r = Rot(K, "rt_sq", [64, 512], F32, 2)
        rs_r = Rot(K, "rt_rs", [64, 512], F32, 2)
        g_r = Rot(K, "rt_g", [64, 4, 128], BF16, 3)
        o_r = Rot(K, "rt_o", [64, 4, 128], BF16, 3)
        gsrc, gsrcb = P["rtg"]
        for t in range(NT):
            tk = slice(t * 128, (t + 1) * 128)
            pa, pab = K.bank()
            for hh in range(4):
                cc, r0 = hh // 2, (hh % 2) * 64
                mm_k64(K, pa, pab, hh * 128, 128, kT[r0:r0 + 64, cc, tk], qT[r0:r0 + 64, cc, tk], r0, [kT_b, qT_b])
            A, Ab = A_r.get()
            S.op("dve", lambda h: h.tensor_tensor(out=A[:], in0=pa[:].rearrange("p (h t) -> p h t", h=4), in1=M[:], op=ALU.mult), r=[pab, M_b], w=[Ab])
            po, pob = K.bank()
            for hh in range(4):
                cc, r0 = hh // 2, (hh % 2) * 64
                oo = po[0:64, hh * 128:(hh + 1) * 128]
                S.op("pe", lambda h: h.matmul(oo, lhsT=vall[:, t, hh * 64:(hh + 1) * 64], rhs=A[:, hh, :], start=True, stop=False), r=[v_b, Ab], w=[pob])
                for d in range(2):
                    S.op("pe", lambda h: h.matmul(oo, lhsT=Sall[r0:r0 + 64, d, t, cc, :], rhs=qx[r0:r0 + 64, d, cc, tk], start=False, stop=(d == 1)),
                         r=[Sall_b, qx_b], w=[pob])
            g, gb = g_r.get()
            S.dma("sp", g[:], hv(gsrc)[:, :, tk], gb, dr=gsrcb)
            o, ob = o_r.get()
            head_norm_finish(K, po, pob, 512, g[:].rearrange("p h t -> p (h t)"), gb, o[:].rearrange("p h t -> p (h t)"), ob, sq_r, rs_r)
            S.dma("sp", hv(Od)[:, :, tk], o[:], ob, dr=Odb, load=False)


def _band_start(r):
    return min(max(r - 4, 0), 56)


def phase_na(K, li, P, O):
    nc, S = K.nc, K.S
    rpb = K.inp("na_rpbT2", [DEPTH, 128, 4, 15, 64])
    nmask = K.inp("c_namask", [128, 64])
    nw = K.inp("na_qk_normT", [DEPTH, 128, 2])
    blk64 = K.inp("c_blk64", [128, 128])
    Od, Odb = O
    NEG = -240000.0
    with phase(K):
        qT = K.sb("na_qT", [128, 2, T], BF16); qT_b = Buf("na_qT")
        kT = K.sb("na_kT", [128, 2, T], BF16); kT_b = Buf("na_kT")
        load_fm(K, qT, qT_b, *P["naq"])
        load_fm(K, kT, kT_b, *P["nak"])
        vall = K.sb("na_v", [128, NT, 256], BF16); v_b = Buf("na_v")
        load_tm(K, vall, v_b, *P["nav"])
        wq = K.sb("na_w", [128, 2]); wq_b = Buf("na_w")
        S.dma("sp", wq[:], nw[li], wq_b)
        b64 = K.sb("na_b64", [128, 128]); b64_b = Buf("na_b64")
        S.dma("sp", b64[:], blk64, b64_b)
        sq_r = Rot(K, "na_sq", [128, 512], F32, 2)
        rs_r = Rot(K, "na_rs", [128, 512], F32, 2)
        for wi, (X, Xb) in enumerate(((qT, qT_b), (kT, kT_b))):
            for cc in range(2):
                for (t0, n) in TB:
                    xs = X[:, cc, t0:t0 + n]
                    sq, sqb = sq_r.get()
                    S.op("act", lambda h: h.activation(out=sq[:, 0:n], in_=xs, func=ACT.Square), r=[Xb], w=[sqb])
                    ps, psb = K.bank()
                    S.op("pe", lambda h: h.matmul(ps[:, 0:n], lhsT=b64[:], rhs=sq[:, 0:n], start=True, stop=True), r=[b64_b, sqb], w=[psb])
                    rs, rsb = rs_r.get()
                    S.op("act", lambda h: h.activation(out=rs[:, 0:n], in_=ps[:, 0:n], func=ACT.Sqrt, scale=1.0 / 64, bias=K.epsc[:, 0:1]), r=[psb, K.epsc_b], w=[rsb])
                    S.op("dve", lambda h: h.reciprocal(out=rs[:, 0:n], in_=rs[:, 0:n]), r=[rsb], w=[rsb])
                    S.op("dve", lambda h: h.scalar_tensor_tensor(out=xs, in0=xs, scalar=wq[:, wi:wi + 1], in1=rs[:, 0:n], op0=ALU.mult, op1=ALU.mult),
                         r=[Xb, rsb], w=[Xb], rs=[wq_b])
        rp = K.sb("na_rp", [128, 4, 15, 64]); rp_b = Buf("na_rp")
        S.dma("sp", rp[:], rpb[li], rp_b)
        mk = K.sb("na_mk", [128, 64]); mk_b = Buf("na_mk")
        S.dma("sp", mk[:], nmask, mk_b)
        Bt = K.sb("na_Bt", [128, 4, 15, 64], BF16); Bt_b = Buf("na_Bt")
        S.op("dve", lambda h: h.scalar_tensor_tensor(out=Bt[:].rearrange("p h r q -> p (h r) q"), in0=rp[:].rearrange("p h r q -> p (h r) q"), scalar=8.0,
                                                     in1=mk[:].unsqueeze(1).to_broadcast([128, 60, 64]), op0=ALU.mult, op1=ALU.add),
             r=[rp_b, mk_b], w=[Bt_b])
        NPAT = 30
        pat_t = K.sb("na_pat", [128, NPAT, 4, 128], BF16)
        pat_b = [Buf(f"na_pat{i}") for i in range(NPAT)]
        pats = {}

        def bias_tile(tq, m):
            key = []
            for kr in range(2):
                for qr in range(2):
                    krow, qrow = 2 * m + kr, 2 * tq + qr
                    bs = _band_start(qrow)
                    key.append(krow - qrow if bs <= krow < bs + 8 else None)
            key = tuple(key)
            if key not in pats:
                idx = len(pats)
                assert idx < NPAT
                pats[key] = idx
                i = 0
                for kr in range(2):
                    for qr in range(2):
                        dr = key[i]; i += 1
                        dst = pat_t[kr * 64:(kr + 1) * 64, idx, :, qr * 64:(qr + 1) * 64]
                        if dr is None:
                            S.op("pool", lambda h: h.memset(dst, NEG), w=[pat_b[idx]])
                        else:
                            S.op("pool", lambda h: h.tensor_copy(out=dst, in_=Bt[kr * 64:(kr + 1) * 64, :, dr + 7, :]), r=[Bt_b], w=[pat_b[idx]])
            idx = pats[key]
            return pat_t[:, idx, :, :], pat_b[idx]

        ones64 = K.sb("na_ones", [128, 64], BF16); ones64_b = Buf("na_ones")
        S.op("dve", lambda h: h.memset(ones64[:], 1.0), w=[ones64_b])
        pm_r = Rot(K, "na_pm", [128, 7, 512], BF16, 2)
        rd_r = Rot(K, "na_rd", [64, 512], F32, 2)
        o_r = Rot(K, "na_o", [64, 4, 128], BF16, 3)
        for tt in range(NT):
            tk = slice(tt * 128, (tt + 1) * 128)
            if tt < 2:
                keys = [(0, None), (1, None)]
            else:
                tq = tt - 2
                m0 = _band_start(2 * tq) // 2
                m1 = (_band_start(2 * tq + 1) + 7) // 2
                keys = [(2 + m, m) for m in range(m0, m1 + 1)] + [(0, None), (1, None)]
            pm, pmb = pm_r.get()
            for ki, (kt, m) in enumerate(keys):
                kk = slice(kt * 128, (kt + 1) * 128)
                ps, psb = K.bank()
                if m is not None:
                    bt, btb = bias_tile(tt - 2, m)
                for hh in range(4):
                    cc, r0 = hh // 2, (hh % 2) * 64
                    mm_k64(K, ps, psb, hh * 128, 128, kT[r0:r0 + 64, cc, kk], qT[r0:r0 + 64, cc, tk], r0, [kT_b, qT_b], start=True, stop=(m is None))
                    if m is not None:
                        S.op("pe", lambda h: h.matmul(ps[:, hh * 128:(hh + 1) * 128], lhsT=K.identb[:], rhs=bt[:, hh, :], start=False, stop=True),
                             r=[K.identb_b, btb], w=[psb])
                S.op("act", lambda h: h.activation(out=pm[:, ki, :], in_=ps[:], func=ACT.Exp, scale=0.125), r=[psb], w=[pmb])
            nk = len(keys)
            po, pob = K.bank()
            for hh in range(4):
                for ki, (kt, m) in enumerate(keys):
                    S.op("pe", lambda h: h.matmul(po[0:64, hh * 128:(hh + 1) * 128], lhsT=vall[:, kt, hh * 64:(hh + 1) * 64], rhs=pm[:, ki, hh * 128:(hh + 1) * 128],
                                                  start=(ki == 0), stop=(ki == nk - 1)), r=[v_b, pmb], w=[pob])
            pd, pdb = K.bank()
            for ki in range(nk):
                S.op("pe", lambda h: h.matmul(pd[0:64, :], lhsT=ones64[:], rhs=pm[:, ki, :], start=(ki == 0), stop=(ki == nk - 1)), r=[ones64_b, pmb], w=[pdb])
            rd, rdb = rd_r.get()
            S.op("dve", lambda h: h.reciprocal(out=rd[:], in_=pd[0:64, :]), r=[pdb], w=[rdb])
            o, ob = o_r.get()
            S.op("dve", lambda h: h.tensor_tensor(out=o[:].rearrange("p h t -> p (h t)"), in0=po[0:64, :], in1=rd[:], op=ALU.mult), r=[pob, rdb], w=[ob])
            S.dma("sp", hv(Od)[:, :, tk], o[:], ob, dr=Odb, load=False)


NCH = T // 16
HB = 256


def _hg_spos(d, n):
    if d == 0:
        return n
    return 15 - n if n < 16 else 16 + (271 - n)


def phase_hg(K, li, P, O):
    nc, S = K.nc, K.S
    lbin = K.inp("hg_lbT", [128, 2, 2])
    nwin = K.inp("hg_norm_wT", [DEPTH, 64, 1])
    crst = K.inp("c_rst", [128, HB])
    cm = [K.inp("c_hgm0", [128, 128]), K.inp("c_hgm1", [128, 128])]
    cbm = K.inp("c_blkm", [128, 2, 8])
    Od, Odb = O
    NC16 = HB // 16
    with phase(K):
        lbr = K.sb("hg_lbr", [128, 2, 2]); lb_b = Buf("hg_lb")
        S.dma("sp", lbr[:], lbin, lb_b)
        lb = K.sb("hg_lbv", [128, 2]); oml = K.sb("hg_oml", [128, 2])
        if li == 0:
            S.op("dve", lambda h: h.memset(lb[:], 0.0), w=[lb_b])
        else:
            S.op("dve", lambda h: h.tensor_tensor(out=lb[:], in0=lbr[:, 1, :], in1=lbr[:, 0, :], op=ALU.subtract), r=[lb_b], w=[lb_b])
            S.op("act", lambda h: h.activation(out=lb[:], in_=lb[:], func=ACT.Sigmoid), r=[lb_b], w=[lb_b])
        S.op("dve", lambda h: h.tensor_scalar(out=oml[:], in0=lb[:], scalar1=-1.0, scalar2=1.0, op0=ALU.mult, op1=ALU.add), r=[lb_b], w=[lb_b])
        nwt = K.sb("hg_nw", [64, 1]); nw_b = Buf("hg_nw")
        S.dma("sp", nwt[:], nwin[li], nw_b)
        rst = K.sb("hg_rst", [128, HB]); rst_b = Buf("hg_rst")
        S.dma("sp", rst[:], crst, rst_b)
        msk = K.sb("hg_msk", [128, 2, 128]); msk_b = Buf("hg_msk")
        for d in range(2):
            S.dma("sp", msk[:, d, :], cm[d], msk_b)
        bm = K.sb("hg_bm", [128, 2, 8]); bm_b = Buf("hg_bm")
        S.dma("sp", bm[:], cbm, bm_b)
        vall = K.sb("hg_v", [128, NT, 128], BF16); v_b = Buf("hg_v")
        qd = K.sb("hg_qd", [128, T], BF16); kd = K.sb("hg_kd", [128, T], BF16)
        qd_b, kd_b = Buf("hg_qd"), Buf("hg_kd")
        dec = K.sb("hg_dec", [128, NCH]); dec_b = Buf("hg_dec")
        decs = K.sb("hg_decs", [128, NCH]); decs_b = Buf("hg_decs")
        kvb = K.sb("hg_kvb", [128, NCH, 64], BF16); kvb_b = Buf("hg_kvb")
        Sb = K.sb("hg_Sb", [128, NCH, 64], BF16); Sb_b = Buf("hg_Sb")
        Oacc = K.sb("hg_Oacc", [64, 2, T]); Oacc_b = Buf("hg_Oacc")
        fl_r = Rot(K, "hg_fl", [128, HB], F32, 2)
        qs_r = Rot(K, "hg_qs", [128, HB], BF16, 2)
        ex_r = Rot(K, "hg_ex", [128, HB], F32, 3)
        ke_r = Rot(K, "hg_ke", [128, HB], BF16, 2)
        tA = K.sb("hg_tA", [128, HB]); tB = K.sb("hg_tB", [128, HB]); tC = K.sb("hg_tC", [128, HB]); tD = K.sb("hg_tD", [128, HB])
        tmp_b = Buf("hg_tmp")
        keT_r = Rot(K, "hg_keT", [128, 128], BF16, 3)
        vb_r = Rot(K, "hg_vb", [128, 2, 8, 64], BF16, 3)
        A_r = Rot(K, "hg_A", [128, 2, 128], BF16, 3)
        sq_r = Rot(K, "hg_sq", [64, 512], F32, 2)
        rs_r = Rot(K, "hg_rs", [64, 512], F32, 2)
        g_r = Rot(K, "hg_g", [64, 2, 256], BF16, 2)
        o_r = Rot(K, "hg_o", [64, 2, 256], BF16, 2)
        fsrc = [P["hgff"], P["hgfb"]]
        c3 = lambda ap: ap.rearrange("p (n c) -> p n c", c=16)
        for cc in range(2):
            load_tm(K, vall, v_b, *P["hgi"], c0=cc * 128, cw=128)
            for d in range(2):
                for bi in range(T // HB):
                    tb = slice(bi * HB, (bi + 1) * HB)
                    cb = slice(bi * NC16, (bi + 1) * NC16)
                    fl, flb = fl_r.get()
                    S.dma("sp", fl[:], fsrc[d][0][cc * 128:(cc + 1) * 128, tb], flb, dr=fsrc[d][1])
                    qs, qsb = qs_r.get()
                    S.dma("sp", qs[:], P["hgq"][0][cc * 128:(cc + 1) * 128, tb], qsb, dr=P["hgq"][1])
                    S.op("act", lambda h: h.activation(out=tA[:], in_=fl[:], func=ACT.Sigmoid), r=[flb], w=[tmp_b])
                    S.op("dve", lambda h: h.tensor_scalar(out=tA[:], in0=tA[:], scalar1=oml[:, cc:cc + 1], scalar2=lb[:, cc:cc + 1], op0=ALU.mult, op1=ALU.add),
                         r=[tmp_b], w=[tmp_b], rs=[lb_b])
                    S.op("act", lambda h: h.activation(out=tB[:], in_=tA[:], func=ACT.Ln), r=[tmp_b], w=[tmp_b])
                    S.op("pool", lambda h: h.tensor_scalar(out=tC[:], in0=tA[:], scalar1=-1.0, scalar2=1.0, op0=ALU.mult, op1=ALU.add), r=[tmp_b], w=[tmp_b])
                    S.op("dve", lambda h: h.tensor_tensor_scan(out=tA[:], data0=rst[:], data1=tB[:], initial=0.0, op0=ALU.mult, op1=ALU.add),
                         r=[tmp_b, rst_b], w=[tmp_b])
                    Pv = c3(tA[:])
                    totb = Pv[:, :, 15:16].to_broadcast([128, NC16, 16])
                    if d == 0:
                        Eq = tA
                        S.op("dve", lambda h: h.tensor_tensor(out=c3(tD[:]), in0=totb, in1=Pv, op=ALU.subtract), r=[tmp_b], w=[tmp_b])
                    else:
                        S.op("dve", lambda h: h.tensor_tensor(out=tD[:], in0=tA[:], in1=tB[:], op=ALU.subtract), r=[tmp_b], w=[tmp_b])
                        S.op("dve", lambda h: h.tensor_tensor(out=c3(tB[:]), in0=totb, in1=c3(tD[:]), op=ALU.subtract), r=[tmp_b], w=[tmp_b])
                        Eq = tB
                    S.op("act", lambda h: h.activation(out=dec[:, cb], in_=Pv[:, :, 15], func=ACT.Exp), r=[tmp_b], w=[dec_b])
                    e1, e1b = ex_r.get()
                    S.op("act", lambda h: h.activation(out=e1[:], in_=Eq[:], func=ACT.Exp), r=[tmp_b], w=[e1b])
                    S.op("dve", lambda h: h.tensor_tensor(out=qd[:, tb], in0=qs[:], in1=e1[:], op=ALU.mult), r=[qsb, e1b], w=[qd_b])
                    e2, e2b = ex_r.get()
                    S.op("act", lambda h: h.activation(out=e2[:], in_=Eq[:], func=ACT.Exp, scale=-1.0), r=[tmp_b], w=[e2b])
                    S.op("pool", lambda h: h.tensor_tensor(out=kd[:, tb], in0=tC[:], in1=e2[:], op=ALU.mult), r=[tmp_b, e2b], w=[kd_b])
                    e3, e3b = ex_r.get()
                    S.op("act", lambda h: h.activation(out=e3[:], in_=tD[:], func=ACT.Exp), r=[tmp_b], w=[e3b])
                    ke, keb = ke_r.get()
                    S.op("dve", lambda h: h.tensor_tensor(out=ke[:], in0=tC[:], in1=e3[:], op=ALU.mult), r=[tmp_b, e3b], w=[keb])
                    for tl in range(HB // 128):
                        t = bi * (HB // 128) + tl
                        ps, psb = K.bank()
                        pv = ps[:].bitcast(BF16)
                        S.op("pe", lambda h: h.transpose(out=pv[:, 0:128], in_=ke[:, tl * 128:(tl + 1) * 128], identity=K.identb[:]), r=[keb, K.identb_b], w=[psb])
                        keT, keTb = keT_r.get()
                        S.op("act", lambda h: h.activation(out=keT[:], in_=pv[:, 0:128], func=ACT.Copy), r=[psb], w=[keTb])
                        vb, vbb = vb_r.get()
                        S.op("pool", lambda h: h.tensor_tensor(out=vb[:], in0=vall[:, t, :].rearrange("p (h v) -> p h v", h=2).unsqueeze(2).to_broadcast([128, 2, 8, 64]),
                                                               in1=bm[:, d, :].unsqueeze(1).unsqueeze(3).to_broadcast([128, 2, 8, 64]), op=ALU.mult), r=[v_b, bm_b], w=[vbb])
                        if d == 0:
                            s0 = 8 * t
                        else:
                            s0 = 8 * (1 - t) if t < 2 else 280 - 8 * t
                        for hp in range(2):
                            pk, pkb = K.bank()
                            S.op("pe", lambda h: h.matmul(pk[:], lhsT=keT[:], rhs=vb[:, hp, :, :].rearrange("p c v -> p (c v)"), start=True, stop=True),
                                 r=[keTb, vbb], w=[pkb])
                            evac(K, K.ev(), kvb[hp * 64:(hp + 1) * 64, s0:s0 + 8, :], pk[hp * 64:(hp + 1) * 64, :].rearrange("p (c v) -> p c v", c=8), r=[pkb], w=[kvb_b])
                if d == 0:
                    S.op("dve", lambda h: h.tensor_copy(out=decs[:], in_=dec[:]), r=[dec_b], w=[decs_b])
                else:
                    da = dec[:]
                    r1 = bass.AP(da.tensor, da.offset + 15, [list(da.ap[0]), [-1, 16]])
                    r2 = bass.AP(da.tensor, da.offset + 271, [list(da.ap[0]), [-1, 256]])
                    S.op("dve", lambda h: h.tensor_copy(out=decs[:, 0:16], in_=r1), r=[dec_b], w=[decs_b])
                    S.op("dve", lambda h: h.tensor_copy(out=decs[:, 16:272], in_=r2), r=[dec_b], w=[decs_b])
                for v in range(64):
                    S.op("dve", lambda h: h.tensor_tensor_scan(out=Sb[:, :, v], data0=decs[:], data1=kvb[:, :, v], initial=0.0, op0=ALU.mult, op1=ALU.add),
                         r=[decs_b, kvb_b], w=[Sb_b])
                for t in range(NT):
                    tk = slice(t * 128, (t + 1) * 128)
                    pa, pab = K.bank()
                    for hp in range(2):
                        r0 = hp * 64
                        mm_k64(K, pa, pab, hp * 128, 128, kd[r0:r0 + 64, tk], qd[r0:r0 + 64, tk], r0, [kd_b, qd_b])
                    A, Ab = A_r.get()
                    S.op("dve", lambda h: h.tensor_tensor(out=A[:], in0=pa[:, 0:256].rearrange("p (h t) -> p h t", h=2),
                                                          in1=msk[:, d, :].unsqueeze(1).to_broadcast([128, 2, 128]), op=ALU.mult), r=[pab, msk_b], w=[Ab])
                    po, pob = K.bank()
                    for hp in range(2):
                        r0 = hp * 64
                        mm = []
                        for c in range(8):
                            sp = _hg_spos(d, 8 * t + c)
                            if sp >= 1:
                                mm.append((c, sp))
                        S.op("pe", lambda h: h.matmul(po[0:64, hp * 128:(hp + 1) * 128], lhsT=vall[:, t, hp * 64:(hp + 1) * 64], rhs=A[:, hp, :], start=True, stop=(len(mm) == 0)),
                             r=[v_b, Ab], w=[pob])
                        for i, (c, sp) in enumerate(mm):
                            S.op("pe", lambda h: h.matmul(po[0:64, hp * 128 + 16 * c:hp * 128 + 16 * c + 16], lhsT=Sb[r0:r0 + 64, sp - 1, :],
                                                          rhs=qd[r0:r0 + 64, t * 128 + 16 * c:t * 128 + 16 * c + 16], start=False, stop=(i == len(mm) - 1)),
                                 r=[Sb_b, qd_b], w=[pob])
                    ov = Oacc[:, :, tk]
                    pov = po[0:64, 0:256].rearrange("p (h t) -> p h t", h=2)
                    if d == 0:
                        S.op("act", lambda h: h.activation(out=ov, in_=pov, func=ACT.Copy), r=[pob], w=[Oacc_b])
                    else:
                        S.op("dve", lambda h: h.tensor_tensor(out=ov, in0=ov, in1=pov, op=ALU.add), r=[pob, Oacc_b], w=[Oacc_b])
            gsrc, gsrcb = P["hgg"]
            for t2 in range(T // 256):
                tk = slice(t2 * 256, (t2 + 1) * 256)
                g, gb = g_r.get()
                S.dma("sp", g[:], hv(gsrc)[:, 2 * cc:2 * cc + 2, tk], gb, dr=gsrcb)
                o, ob = o_r.get()
                sq, sqb = sq_r.get()
                h2 = lambda ap: ap.rearrange("p (h t) -> p h t", h=2)
                sqh = sq[:].bitcast(BF16)
                S.op("act", lambda h: h.activation(out=h2(sqh[:, 0:512]), in_=Oacc[:, :, tk], func=ACT.Square), r=[Oacc_b], w=[sqb])
                pss, pssb = K.bank()
                S.op("pe", lambda h: h.matmul(pss[0:64, :], lhsT=K.onesb[0:64, 0:64], rhs=sqh[:, 0:512], start=True, stop=True), r=[sqb, K.onesb_b], w=[pssb])
                rs, rsb = rs_r.get()
                S.op("act", lambda h: h.activation(out=rs[:], in_=pss[0:64, :], func=ACT.Sqrt, scale=1.0 / 64, bias=K.epsc[0:64, 0:1]), r=[pssb, K.epsc_b], w=[rsb])
                S.op("dve", lambda h: h.reciprocal(out=rs[:], in_=rs[:]), r=[rsb], w=[rsb])
                S.op("dve", lambda h: h.scalar_tensor_tensor(out=h2(sq[:]), in0=Oacc[:, :, tk], scalar=nwt[:, 0:1], in1=h2(rs[:]), op0=ALU.mult, op1=ALU.mult),
                     r=[Oacc_b, rsb], w=[sqb], rs=[nw_b])
                S.op("pool", lambda h: h.tensor_tensor(out=o[:], in0=h2(sq[:]), in1=g[:], op=ALU.mult), r=[sqb, gb], w=[ob])
                S.dma("sp", hv(Od)[:, 2 * cc:2 * cc + 2, tk], o[:], ob, dr=Odb, load=False)


TWO_PI_LO = 6.28318


def _s5_spos(m):
    return 15 - m if m < 16 else 287 - m


def phase_s5(K, li, P, O):
    nc, S = K.nc, K.S
    lamin = K.inp("s5_lamT", [DEPTH, 128, 2, 32])
    stepin = K.inp("s5_stepT", [DEPTH, 128, 32])
    Bin = K.inp("s5_BT", [DEPTH, 128, 2, 32, 16])
    Cin = K.inp("s5_CT", [DEPTH, 128, 2, 32, 16])
    din = K.inp("s5_dT", [DEPTH, 128, 2])
    gluin = K.inp("s5_glu_w", [DEPTH, 256, 256])
    selin = K.inp("c_sel", [128, 64, 128], BF16)
    selTin = K.inp("c_selT", [128, 64, 128], BF16)
    m01in = [K.inp("c_s5m0", [128, 2, 256]), K.inp("c_s5m1", [128, 2, 256])]
    kin = [K.inp("c_kA", [128, 32]), K.inp("c_kB", [128, 32])]
    sel3in = K.inp("c_sel3", [128, 3])
    Od, Odb = O
    with phase(K):
        Gl = K.sb("s5_Gl", [128, 32, 2, 128], BF16); Gl_b = Buf("s5_Gl")
        Aw = K.sb("s5_Aw", [128, 16, 2, 256], BF16); Aw_b = Buf("s5_Aw")
        Hs = K.sb("s5_H", [128, 2, 16, 256], BF16); Hs_b = Buf("s5_H")
        MUA = K.sb("s5_MUA", [128, 2, 16]); MUB = K.sb("s5_MUB", [128, 2, 16]); MU_b = Buf("s5_MU")
        with phase(K):
            lam = K.sb("s5_lam", [128, 2, 32]); lam_b = Buf("s5_lam")
            stp = K.sb("s5_stp", [128, 32]); stp_b = Buf("s5_stp")
            Bt = K.sb("s5_Bt", [128, 2, 32, 16]); Bt_b = Buf("s5_Bt")
            Ct = K.sb("s5_Ct", [128, 2, 32, 16]); Ct_b = Buf("s5_Ct")
            kk = K.sb("s5_kk", [128, 2, 32]); kk_b = Buf("s5_kk")
            sel3 = K.sb("s5_sel3", [128, 3]); sel3_b = Buf("s5_sel3")
            m01 = K.sb("s5_m01", [128, 2, 2, 256]); m01_b = Buf("s5_m01")
            S.dma("sp", lam[:], lamin[li], lam_b)
            S.dma("sp", stp[:], stepin[li], stp_b)
            S.dma("sp", Bt[:], Bin[li], Bt_b)
            S.dma("sp", Ct[:], Cin[li], Ct_b)
            for i in range(2):
                S.dma("sp", kk[:, i, :], kin[i], kk_b)
                S.dma("sp", m01[:, i, :, :], m01in[i], m01_b)
            S.dma("sp", sel3[:], sel3in, sel3_b)
            sm = K.sb("s5_sm", [128, 12, 32]); sm_b = Buf("s5_sm")
            DT, EA, TH, DEN, AM1, ZR, ZI, T1, T2 = [sm[:, i, :] for i in range(9)]
            lr, lim = lam[:, 0, :], lam[:, 1, :]
            dv = lambda f, r=(), w=(), rs=(): S.op("dve", f, r=list(r) + [sm_b], w=list(w) + [sm_b], rs=rs)
            S.op("act", lambda h: h.activation(out=DT, in_=stp[:], func=ACT.Exp), r=[stp_b], w=[sm_b])
            dv(lambda h: h.tensor_tensor(out=EA, in0=lr, in1=DT, op=ALU.mult), r=[lam_b])
            dv(lambda h: h.tensor_tensor(out=TH, in0=lim, in1=DT, op=ALU.mult), r=[lam_b])
            PW = K.sb("s5_PW", [128, 2, 2, 32, 32]); PW_b = Buf("s5_PW")
            big = K.sb("s5_big", [128, 4, 32, 32]); big_b = Buf("s5_big")
            bigi = K.sb("s5_bigi", [128, 32, 32], I32)
            bg = lambda f, r=(), w=(): S.op("dve", f, r=list(r) + [big_b, sm_b], w=list(w) + [big_b])
            for ab in range(2):
                kb3 = kk[:, ab, :].unsqueeze(1).to_broadcast([128, 32, 32])
                bg(lambda h: h.tensor_tensor(out=big[:, 0], in0=EA.unsqueeze(2).to_broadcast([128, 32, 32]), in1=kb3, op=ALU.mult), r=[kk_b])
                S.op("act", lambda h: h.activation(out=big[:, 1], in_=big[:, 0], func=ACT.Exp), r=[big_b], w=[big_b])
                bg(lambda h: h.tensor_tensor(out=big[:, 0], in0=TH.unsqueeze(2).to_broadcast([128, 32, 32]), in1=kb3, op=ALU.mult), r=[kk_b])
                for ri in range(2):
                    bg(lambda h: h.tensor_scalar(out=big[:, 2], in0=big[:, 0], scalar1=1.0 / (2 * np.pi), scalar2=(0.25 if ri == 0 else 0.0), op0=ALU.mult, op1=ALU.add))
                    bg(lambda h: h.tensor_copy(out=bigi[:], in_=big[:, 2]))
                    bg(lambda h: h.tensor_copy(out=big[:, 3], in_=bigi[:]))
                    bg(lambda h: h.tensor_tensor(out=big[:, 2], in0=big[:, 2], in1=big[:, 3], op=ALU.subtract))
                    bg(lambda h: h.tensor_scalar(out=big[:, 3], in0=big[:, 2], scalar1=0.5, scalar2=None, op0=ALU.is_gt))
                    bg(lambda h: h.tensor_tensor(out=big[:, 2], in0=big[:, 2], in1=big[:, 3], op=ALU.subtract))
                    bg(lambda h: h.tensor_scalar(out=big[:, 3], in0=big[:, 2], scalar1=-0.5, scalar2=None, op0=ALU.is_lt))
                    bg(lambda h: h.tensor_tensor(out=big[:, 2], in0=big[:, 2], in1=big[:, 3], op=ALU.add))
                    S.op("act", lambda h: h.activation(out=big[:, 3], in_=big[:, 2], func=ACT.Sin, scale=TWO_PI_LO), r=[big_b], w=[big_b])
                    bg(lambda h: h.tensor_tensor(out=PW[:, ab, ri], in0=big[:, 3], in1=big[:, 1], op=ALU.mult), w=[PW_b])
            AR, AI = PW[:, 0, 0, :, 16], PW[:, 0, 1, :, 16]
            dv(lambda h: h.tensor_tensor(out=DEN, in0=lr, in1=lr, op=ALU.mult), r=[lam_b])
            dv(lambda h: h.tensor_tensor(out=T1, in0=lim, in1=lim, op=ALU.mult), r=[lam_b])
            dv(lambda h: h.tensor_tensor(out=DEN, in0=DEN, in1=T1, op=ALU.add))
            dv(lambda h: h.reciprocal(out=DEN, in_=DEN))
            dv(lambda h: h.tensor_scalar(out=AM1, in0=AR, scalar1=-1.0, scalar2=None, op0=ALU.add), r=[PW_b])
            dv(lambda h: h.tensor_tensor(out=T1, in0=AM1, in1=lr, op=ALU.mult), r=[lam_b])
            dv(lambda h: h.tensor_tensor(out=T2, in0=AI, in1=lim, op=ALU.mult), r=[lam_b, PW_b])
            dv(lambda h: h.tensor_tensor(out=ZR, in0=T1, in1=T2, op=ALU.add))
            dv(lambda h: h.tensor_tensor(out=ZR, in0=ZR, in1=DEN, op=ALU.mult))
            dv(lambda h: h.tensor_tensor(out=T1, in0=AI, in1=lr, op=ALU.mult), r=[lam_b, PW_b])
            dv(lambda h: h.tensor_tensor(out=T2, in0=AM1, in1=lim, op=ALU.mult), r=[lam_b])
            dv(lambda h: h.tensor_tensor(out=ZI, in0=T1, in1=T2, op=ALU.subtract))
            dv(lambda h: h.tensor_tensor(out=ZI, in0=ZI, in1=DEN, op=ALU.mult))
            BB = K.sb("s5_BB", [128, 2, 32, 16]); BB_b = Buf("s5_BB")
            tq = K.sb("s5_tq", [128, 2, 32, 16]); tq_b = Buf("s5_tq")
            zr3 = ZR.unsqueeze(2).to_broadcast([128, 32, 16]); zi3 = ZI.unsqueeze(2).to_broadcast([128, 32, 16])
            bq = lambda f: S.op("dve", f, r=[sm_b, Bt_b, tq_b, BB_b], w=[tq_b, BB_b])
            bq(lambda h: h.tensor_tensor(out=tq[:, 0], in0=Bt[:, 0], in1=zr3, op=ALU.mult))
            bq(lambda h: h.tensor_tensor(out=tq[:, 1], in0=Bt[:, 1], in1=zi3, op=ALU.mult))
            bq(lambda h: h.tensor_tensor(out=BB[:, 0], in0=tq[:, 0], in1=tq[:, 1], op=ALU.subtract))
            bq(lambda h: h.tensor_tensor(out=tq[:, 0], in0=Bt[:, 1], in1=zr3, op=ALU.mult))
            bq(lambda h: h.tensor_tensor(out=tq[:, 1], in0=Bt[:, 0], in1=zi3, op=ALU.mult))
            bq(lambda h: h.tensor_tensor(out=BB[:, 1], in0=tq[:, 0], in1=tq[:, 1], op=ALU.add))
            for d in range(2):
                rows = slice(d * 64, (d + 1) * 64)
                mur = PW[rows, 0, 0, d * 16:(d + 1) * 16, 31]; mui = PW[rows, 0, 1, d * 16:(d + 1) * 16, 31]
                S.op("dve", lambda h: h.tensor_copy(out=MUA[rows, 0, :], in_=mur), r=[PW_b], w=[MU_b])
                S.op("dve", lambda h: h.tensor_copy(out=MUA[rows, 1, :], in_=mur), r=[PW_b], w=[MU_b])
                S.op("dve", lambda h: h.tensor_scalar(out=MUB[rows, 0, :], in0=mui, scalar1=-1.0, scalar2=None, op0=ALU.mult), r=[PW_b], w=[MU_b])
                S.op("dve", lambda h: h.tensor_copy(out=MUB[rows, 1, :], in_=mui), r=[PW_b], w=[MU_b])
            cp = K.sb("s5_cp", [128, 6, 8, 256]); cp_b = Buf("s5_cp")
            XS = K.sb("s5_XS", [128, 2, 8, 256], BF16); YS = K.sb("s5_YS", [128, 2, 8, 256], BF16); XY_b = Buf("s5_XY")
            GTs = K.sb("s5_GTs", [128, 8, 256], BF16); GTs_b = Buf("s5_GTs")
            atmp = Rot(K, "s5_at", [128, 2, 256], F32, 2)
            v4 = lambda ap: ap.rearrange("p g (a b) -> p g a b", a=16)

            def cprod(ab, k0, coef, coef_b, dgs):
                pr = PW[:, ab, 0, dgs, k0:k0 + 16].unsqueeze(3).to_broadcast([128, 8, 16, 16])
                pi = PW[:, ab, 1, dgs, k0:k0 + 16].unsqueeze(3).to_broadcast([128, 8, 16, 16])
                cr = coef[:, 0, dgs, :].unsqueeze(2).to_broadcast([128, 8, 16, 16])
                ci = coef[:, 1, dgs, :].unsqueeze(2).to_broadcast([128, 8, 16, 16])
                rr = [PW_b, coef_b, cp_b]
                S.op("dve", lambda h: h.tensor_tensor(out=v4(cp[:, 0]), in0=pr, in1=cr, op=ALU.mult), r=rr, w=[cp_b])
                S.op("pool", lambda h: h.tensor_tensor(out=v4(cp[:, 1]), in0=pi, in1=ci, op=ALU.mult), r=rr, w=[cp_b])
                S.op("dve", lambda h: h.tensor_tensor(out=v4(cp[:, 2]), in0=pr, in1=ci, op=ALU.mult), r=rr, w=[cp_b])
                S.op("pool", lambda h: h.tensor_tensor(out=v4(cp[:, 3]), in0=pi, in1=cr, op=ALU.mult), r=rr, w=[cp_b])
                S.op("dve", lambda h: h.tensor_tensor(out=cp[:, 4], in0=cp[:, 0], in1=cp[:, 1], op=ALU.subtract), r=[cp_b], w=[cp_b])
                S.op("pool", lambda h: h.tensor_tensor(out=cp[:, 5], in0=cp[:, 2], in1=cp[:, 3], op=ALU.add), r=[cp_b], w=[cp_b])

            def stack(out, outb, imcol):
                S.op("dve", lambda h: h.tensor_scalar(out=cp[:, 0], in0=cp[:, 4], scalar1=sel3[:, 0:1], scalar2=None, op0=ALU.mult), r=[cp_b, sel3_b], w=[cp_b])
                S.op("dve", lambda h: h.scalar_tensor_tensor(out=out, in0=cp[:, 5], scalar=sel3[:, imcol:imcol + 1], in1=cp[:, 0], op0=ALU.mult, op1=ALU.add),
                     r=[cp_b, sel3_b], w=[outb])

            for gb in range(2):
                for d in range(2):
                    dgs = slice(d * 16 + gb * 8, d * 16 + gb * 8 + 8)
                    cprod(1 if d == 0 else 0, 1 if d == 0 else 15, BB, BB_b, dgs)
                    stack(GTs[:], GTs_b, 1)
                    if d == 1:
                        S.op("pool", lambda h: h.tensor_copy(out=XS[:, 1], in_=GTs[:]), r=[GTs_b], w=[XY_b])
                    for gl in range(8):
                        g = gb * 8 + gl
                        for jh in range(2):
                            ps, psb = K.bank()
                            pv = ps[:].bitcast(BF16)
                            S.op("pe", lambda h: h.transpose(out=pv[:, 0:128], in_=GTs[:, gl, jh * 128:(jh + 1) * 128], identity=K.identb[:]), r=[GTs_b, K.identb_b], w=[psb])
                            evac(K, K.ev(), Gl[:, d * 16 + g, jh, :], pv[:, 0:128], r=[psb], w=[Gl_b])
                    if d == 0:
                        cprod(1, 16, BB, BB_b, dgs)
                        stack(XS[:, 0], XY_b, 1)
                    cprod(0 if d == 0 else 1, 15 if d == 0 else 16, Ct, Ct_b, dgs)
                    stack(YS[:, d], XY_b, 2)
                    cprod(0 if d == 0 else 1, 16 if d == 0 else 0, Ct, Ct_b, dgs)
                    rows = slice(d * 64, (d + 1) * 64)
                    S.op("act", lambda h: h.activation(out=Hs[rows, 0, gb * 8:gb * 8 + 8, :], in_=cp[rows, 4], func=ACT.Copy), r=[cp_b], w=[Hs_b])
                    S.op("act", lambda h: h.activation(out=Hs[rows, 1, gb * 8:gb * 8 + 8, :], in_=cp[rows, 5], func=ACT.Copy, scale=-1.0), r=[cp_b], w=[Hs_b])
                for gl in range(8):
                    g = gb * 8 + gl
                    for jh in range(2):
                        pss = []
                        for d in range(2):
                            ps, psb = K.bank()
                            S.op("pe", lambda h: h.matmul(ps[:, 0:256], lhsT=XS[:, d, gl, jh * 128:(jh + 1) * 128], rhs=YS[:, d, gl, :], start=True, stop=True), r=[XY_b], w=[psb])
                            pss.append((ps, psb))
                        at, atb = atmp.get()
                        for d in range(2):
                            S.op("dve", lambda h: h.tensor_tensor(out=at[:, d, :], in0=pss[d][0][:, 0:256], in1=m01[:, d, jh, :], op=ALU.mult), r=[pss[d][1], m01_b], w=[atb])
                        S.op("pool", lambda h: h.tensor_tensor(out=Aw[:, g, jh, :], in0=at[:, 0, :], in1=at[:, 1, :], op=ALU.add), r=[atb], w=[Aw_b])
        with phase(K):
            uT = K.sb("s5_uT", [128, 2, T], BF16); uT_b = Buf("s5_uT")
            load_fm(K, uT, uT_b, *P["s5u"])
            U = K.sb("s5_U", [128, 16, 2, NCH], BF16); U_b = Buf("s5_U")
            SN = K.sb("s5_SN", [128, 2, 16, NCH], BF16); SN_b = Buf("s5_SN")
            with phase(K):
                sel = K.sb("s5_sel", [128, 64, 128], BF16); sel_b = Buf("s5_sel")
                for q4 in range(4):
                    S.dma("sp", sel[:, q4 * 16:(q4 + 1) * 16, :], selin[:, q4 * 16:(q4 + 1) * 16, :], sel_b)
                E = K.sb("s5_E", [128, 2, 16, NCH]); E_b = Buf("s5_E")
                ua = uT[:]
                for g in range(16):
                    cc, gl = g // 8, g % 8
                    for jh in range(2):
                        ps, psb = K.bank()
                        for jl in range(8):
                            rhs = bass.AP(ua.tensor, ua.offset + cc * T + 8 * jh + jl, [list(ua.ap[0]), [16, NCH]])
                            S.op("pe", lambda h: h.matmul(ps[:, 0:NCH], lhsT=sel[:, gl * 8 + jl, :], rhs=rhs, start=(jl == 0), stop=(jl == 7)), r=[sel_b, uT_b], w=[psb])
                        evac(K, K.ev(), U[:, g, jh, :], ps[:, 0:NCH], r=[psb], w=[U_b])
                for g in range(16):
                    for ri in range(2):
                        ps, psb = K.bank()
                        for d in range(2):
                            for jh in range(2):
                                S.op("pe", lambda h: h.matmul(ps[d * 64:(d + 1) * 64, 0:NCH], lhsT=Gl[:, d * 16 + g, jh, ri * 64:(ri + 1) * 64], rhs=U[:, g, jh, :],
                                                              start=(jh == 0), stop=(jh == 1)), r=[Gl_b, U_b], w=[psb])
                        evac(K, K.ev(), E[0:64, ri, g, :], ps[0:64, 0:NCH], r=[psb], w=[E_b])
                        pa = ps[64:128, 0:NCH]
                        r1 = bass.AP(pa.tensor, pa.offset + 15, [list(pa.ap[0]), [-1, 16]])
                        r2 = bass.AP(pa.tensor, pa.offset + 271, [list(pa.ap[0]), [-1, 256]])
                        S.op("dve", lambda h: h.tensor_copy(out=E[64:128, ri, g, 0:16], in_=r1), r=[psb], w=[E_b])
                        S.op("dve", lambda h: h.tensor_copy(out=E[64:128, ri, g, 16:NCH], in_=r2), r=[psb], w=[E_b])
                w1 = K.sb("s5_w1", [128, 2, 16]); w2 = K.sb("s5_w2", [128, 2, 16]); w_b = Buf("s5_w")
                ea_ = E[:]
                rstride = 16 * NCH
                for pos in range(1, NCH):
                    prev = bass.AP(ea_.tensor, ea_.offset + pos - 1, [list(ea_.ap[0]), [rstride, 2], [NCH, 16]])
                    prev_sw = bass.AP(ea_.tensor, ea_.offset + rstride + pos - 1, [list(ea_.ap[0]), [-rstride, 2], [NCH, 16]])
                    cur = bass.AP(ea_.tensor, ea_.offset + pos, [list(ea_.ap[0]), [rstride, 2], [NCH, 16]])
                    S.op("dve", lambda h: h.tensor_tensor(out=w1[:], in0=prev, in1=MUA[:], op=ALU.mult), r=[E_b, MU_b], w=[w_b])
                    S.op("dve", lambda h: h.tensor_tensor(out=w2[:], in0=prev_sw, in1=MUB[:], op=ALU.mult), r=[E_b, MU_b], w=[w_b])
                    S.op("dve", lambda h: h.tensor_tensor(out=w1[:], in0=w1[:], in1=w2[:], op=ALU.add), r=[w_b], w=[w_b])
                    S.op("dve", lambda h: h.tensor_tensor(out=cur, in0=cur, in1=w1[:], op=ALU.add), r=[w_b, E_b], w=[E_b])
                S.op("pool", lambda h: h.memset(SN[:], 0.0), w=[SN_b])
                S.op("act", lambda h: h.activation(out=SN[0:64, :, :, 1:NCH], in_=E[0:64, :, :, 0:NCH - 1], func=ACT.Copy), r=[E_b], w=[SN_b])
                eh = E[64:128, :, :, :]
                rv1 = bass.AP(eh.tensor, eh.offset + 14, [list(eh.ap[0]), [rstride, 2], [NCH, 16], [-1, 15]])
                rv2 = bass.AP(eh.tensor, eh.offset + 270, [list(eh.ap[0]), [rstride, 2], [NCH, 16], [-1, 256]])
                S.op("dve", lambda h: h.tensor_copy(out=SN[64:128, :, :, 0:15], in_=rv1), r=[E_b], w=[SN_b])
                S.op("dve", lambda h: h.tensor_copy(out=SN[64:128, :, :, 16:NCH], in_=rv2), r=[E_b], w=[SN_b])
            with phase(K):
                selT = K.sb("s5_selT", [128, 64, 128], BF16); selT_b = Buf("s5_selT")
                for q4 in range(4):
                    S.dma("sp", selT[:, q4 * 16:(q4 + 1) * 16, :], selTin[:, q4 * 16:(q4 + 1) * 16, :], selT_b)
                Ysb = K.sb("s5_Ysb", [128, 16, 2, NCH], BF16); Ysb_b = Buf("s5_Ysb")
                yT = K.sb("s5_yT", [128, T]); yT_b = Buf("s5_yT")
                dcol = K.sb("s5_dcol", [128, 2]); dcol_b = Buf("s5_dcol")
                S.dma("sp", dcol[:], din[li], dcol_b)
                wgs = K.sb("s5_wgs", [128, 2, 256]); wgs_b = Buf("s5_wgs")
                S.dma("sp", wgs[:], gluin[li].rearrange("(c p) n -> p c n", p=128), wgs_b)
                wg = K.sb("s5_wg", [128, 2, 256], BF16); wg_b = Buf("s5_wg")
                S.op("pool", lambda h: h.tensor_copy(out=wg[:], in_=wgs[:]), r=[wgs_b], w=[wg_b])
                for g in range(16):
                    for th in range(2):
                        ps, psb = K.bank()
                        cols = slice(th * 128, (th + 1) * 128)
                        for jh in range(2):
                            S.op("pe", lambda h: h.matmul(ps[:, 0:NCH], lhsT=Aw[:, g, jh, cols], rhs=U[:, g, jh, :], start=(jh == 0), stop=False), r=[Aw_b, U_b], w=[psb])
                        for ri in range(2):
                            S.op("pe", lambda h: h.matmul(ps[:, 0:NCH], lhsT=Hs[0:64, ri, g, cols], rhs=SN[0:64, ri, g, :], start=False, stop=False), r=[Hs_b, SN_b], w=[psb])
                        for ri in range(2):
                            for hf in range(2):
                                c2 = slice(th * 128 + hf * 64, th * 128 + (hf + 1) * 64)
                                S.op("pe", lambda h: h.matmul(ps[hf * 64:(hf + 1) * 64, 0:NCH], lhsT=Hs[64:128, ri, g, c2], rhs=SN[64:128, ri, g, :], start=False, stop=(ri == 1)),
                                     r=[Hs_b, SN_b], w=[psb])
                        evac(K, K.ev(), Ysb[:, g, th, :], ps[:, 0:NCH], r=[psb], w=[Ysb_b])
                zT = K.sb("s5_zT", [128, 2, T], BF16); zT_b = Buf("s5_zT")
                ft = Rot(K, "s5_ft", [128, 512], F32, 3)
                ya = yT[:]
                for cc in range(2):
                    for t in range(16):
                        th, tl = t // 8, t % 8
                        ps, psb = K.bank()
                        for gl in range(8):
                            S.op("pe", lambda h: h.matmul(ps[:, 0:NCH], lhsT=selT[:, gl * 8 + tl, :], rhs=Ysb[:, cc * 8 + gl, th, :], start=(gl == 0), stop=(gl == 7)),
                                 r=[selT_b, Ysb_b], w=[psb])
                        dst = bass.AP(ya.tensor, ya.offset + t, [list(ya.ap[0]), [16, NCH]])
                        evac(K, K.ev(), dst, ps[:, 0:NCH], r=[psb], w=[yT_b])
                    for (t0, n) in TB:
                        yv = yT[:, t0:t0 + n]
                        S.op("dve", lambda h: h.scalar_tensor_tensor(out=yv, in0=uT[:, cc, t0:t0 + n], scalar=dcol[:, cc:cc + 1], in1=yv, op0=ALU.mult, op1=ALU.add),
                             r=[uT_b, yT_b], w=[yT_b], rs=[dcol_b])
                        a, ab_ = ft.get()
                        S.op("pool", lambda h: h.tensor_tensor(out=a[:, 0:n], in0=yv, in1=yv, op=ALU.mult), r=[yT_b], w=[ab_])
                        S.op("dve", lambda h: h.tensor_scalar(out=a[:, 0:n], in0=a[:, 0:n], scalar1=0.044715, scalar2=1.0, op0=ALU.mult, op1=ALU.add), r=[ab_], w=[ab_])
                        S.op("pool", lambda h: h.tensor_tensor(out=a[:, 0:n], in0=a[:, 0:n], in1=yv, op=ALU.mult), r=[ab_, yT_b], w=[ab_])
                        S.op("act", lambda h: h.activation(out=a[:, 0:n], in_=a[:, 0:n], func=ACT.Sigmoid, scale=1.5957691216), r=[ab_], w=[ab_])
                        S.op("dve", lambda h: h.tensor_tensor(out=zT[:, cc, t0:t0 + n], in0=a[:, 0:n], in1=yv, op=ALU.mult), r=[ab_, yT_b], w=[zT_b])
                ob = Rot(K, "s5_ob", [128, 512], BF16, 3)
                for co in range(2):
                    for (t0, n) in TB:
                        ps, psb = K.bank()
                        for ci in range(2):
                            S.op("pe", lambda h: h.matmul(ps[:, 0:n], lhsT=wg[:, ci, co * 128:(co + 1) * 128], rhs=zT[:, ci, t0:t0 + n], start=(ci == 0), stop=(ci == 1)),
                                 r=[wg_b, zT_b], w=[psb])
                        a, ab_ = ft.get()
                        S.op("act", lambda h: h.activation(out=a[:, 0:n], in_=ps[:, 0:n], func=ACT.Sigmoid), r=[psb], w=[ab_])
                        o, obb = ob.get()
                        S.op("dve", lambda h: h.tensor_tensor(out=o[:, 0:n], in0=a[:, 0:n], in1=zT[:, co, t0:t0 + n], op=ALU.mult), r=[ab_, zT_b], w=[obb])
                        S.dma("sp", Od[co * 128:(co + 1) * 128, t0:t0 + n], o[:, 0:n], obb, dr=Odb, load=False)


def phase_merge(K, li, P, O, xsrc, xsrc_b, xdst, xdst_b, last):
    nc, S = K.nc, K.S
    wbr_in = K.inp("w_branch", [DEPTH, 4, 256, D])
    wout_in = K.inp("w_out", [DEPTH, D, D])
    with phase(K):
        stg = Rot(K, "mg_stg", [128, D], F32, 2)
        wbr = K.sb("mg_wbr", [128, 4, 2, D], BF16); wbr_b = Buf("mg_wbr")
        wog = K.sb("mg_wog", [128, 2, 8, D], BF16); wog_b = Buf("mg_wog")
        for br in range(4):
            for cc in range(2):
                st, stb = stg.get()
                S.dma("sp", st[:], wbr_in[li, br, cc * 128:(cc + 1) * 128, :], stb)
                S.op("pool", lambda h: h.tensor_copy(out=wbr[:, br, cc, :], in_=st[:]), r=[stb], w=[wbr_b])
        for kc in range(8):
            st, stb = stg.get()
            S.dma("sp", st[:], wout_in[li, kc * 128:(kc + 1) * 128, :], stb)
            for s_ in range(2):
                S.op("dve", lambda h: h.tensor_tensor(out=wog[:, s_, kc, :], in0=st[:], in1=K.gbc[:, s_, :], op=ALU.mult), r=[stb, K.gbc_b], w=[wog_b])
        ob_r = Rot(K, "mg_ob", [128, 4, 2, 512], BF16, 2)
        gt_r = Rot(K, "mg_gt", [128, 32, 512], BF16, 2)
        tm_r = Rot(K, "mg_tm", [128, 4, 512], F32, 2)
        mT_r = Rot(K, "mg_mT", [128, 8, 512], BF16, 2)
        x_r = Rot(K, "mg_x", [128, D], F32, 3)
        names = ("s5", "na", "hg", "rt")
        gsrc, gsrcb = P["gate"]
        gview = gsrc.rearrange("(j p) t -> p j t", p=128)
        for (t0, n) in TB:
            ob, obb = ob_r.get()
            for br in range(4):
                for cc in range(2):
                    S.dma("sp", ob[:, br, cc, 0:n], O[names[br]][0][cc * 128:(cc + 1) * 128, t0:t0 + n], obb, dr=O[names[br]][1])
            gt, gtb = gt_r.get()
            for br in range(4):
                S.dma("sp", gt[:, br * 8:(br + 1) * 8, 0:n], gview[:, br * 8:(br + 1) * 8, t0:t0 + n], gtb, dr=gsrcb)
            mT, mTb = mT_r.get()
            for dc in range(8):
                tm, tmb = tm_r.get()
                for br in range(4):
                    ps, psb = K.bank()
                    for cc in range(2):
                        S.op("pe", lambda h: h.matmul(ps[:, 0:n], lhsT=wbr[:, br, cc, dc * 128:(dc + 1) * 128], rhs=ob[:, br, cc, 0:n], start=(cc == 0), stop=(cc == 1)),
                             r=[wbr_b, obb], w=[psb])
                    S.op("dve", lambda h: h.tensor_tensor(out=tm[:, br, 0:n], in0=ps[:, 0:n], in1=gt[:, br * 8 + dc, 0:n], op=ALU.mult), r=[psb, gtb], w=[tmb])
                S.op("pool", lambda h: h.tensor_tensor(out=tm[:, 0, 0:n], in0=tm[:, 0, 0:n], in1=tm[:, 1, 0:n], op=ALU.add), r=[tmb], w=[tmb])
                S.op("pool", lambda h: h.tensor_tensor(out=tm[:, 2, 0:n], in0=tm[:, 2, 0:n], in1=tm[:, 3, 0:n], op=ALU.add), r=[tmb], w=[tmb])
                S.op("pool", lambda h: h.tensor_tensor(out=mT[:, dc, 0:n], in0=tm[:, 0, 0:n], in1=tm[:, 2, 0:n], op=ALU.add), r=[tmb], w=[mTb])
            for i in range(n // 128):
                t = t0 // 128 + i
                s_ = 1 if t < 2 else 0
                if last and t < 2:
                    continue
                x, xb = x_r.get()
                S.dma("sp", x[:], xsrc[t * 128:(t + 1) * 128, :], xb, dr=xsrc_b)
                for half in range(2):
                    ps, psb = K.bank()
                    for kc in range(8):
                        S.op("pe", lambda h: h.matmul(ps[:], lhsT=mT[:, kc, i * 128:(i + 1) * 128], rhs=wog[:, s_, kc, half * 512:(half + 1) * 512], start=(kc == 0), stop=(kc == 7)),
                             r=[mTb, wog_b], w=[psb])
                    S.op("dve", lambda h: h.tensor_tensor(out=x[:, half * 512:(half + 1) * 512], in0=x[:, half * 512:(half + 1) * 512], in1=ps[:], op=ALU.add), r=[psb, xb], w=[xb])
                r0 = t * 128 - (CTX if last else 0)
                S.dma("sp", xdst[r0:r0 + 128, :], x[:], xb, dr=xdst_b, load=False)


def phase_route(K, li, xsrc, xsrc_b, row_off, xn2, xn2_b, R, with_ctx):
    nc, S = K.nc, K.S
    rwin = K.inp("router_wT", [DEPTH, 128, 8, 16])
    groups = ([(0, 2, 1)] if with_ctx else []) + [(2 + 4 * i, 4, 0) for i in range(8)]
    with phase(K):
        rw = K.sb("rt_rw", [128, 8, 16]); rw_b = Buf("rt_rw")
        S.dma("sp", rw[:], rwin[li], rw_b)
        xr = Rot(K, "r_xr", [128, D], F32, 3)
        xnr = Rot(K, "r_xnr", [128, D], F32, 8)
        junk = K.sb("r_junk", [128, D], BF16); junk_b = Buf("r_junk")
        ssr = Rot(K, "r_ssr", [128, 8], F32, 4)
        xnb = Rot(K, "r_xnb", [128, D], BF16, 3)
        hg_r = Rot(K, "r_hg", [128, 8, 512], F32, 2)
        affT = K.sb("r_affT", [16, T]); affT_b = Buf("r_affT")
        codeT = K.sb("r_codeT", [16, T]); codeT_b = Buf("r_codeT")
        S.op("pool", lambda h: h.memset(R.aff[:], 0.0), w=[R.aff_b])
        S.op("pool", lambda h: h.memset(R.code[:], -1.0), w=[R.code_b])
        for (t0, n, s) in groups:
            xns = []
            for i in range(n):
                t = t0 + i
                x, xb = xr.get()
                S.dma("sp", x[:], xsrc[t * 128 - row_off:(t + 1) * 128 - row_off, :], xb, dr=xsrc_b)
                ss, ssb = ssr.get()
                S.op("pool", lambda h: h.memset(ss[:], 0.0), w=[ssb])
                S.op("act", lambda h: h.activation(out=junk[:], in_=x[:], func=ACT.Square, accum_out=ss[:, 0:1]), r=[xb], w=[junk_b, ssb])
                S.op("act", lambda h: h.activation(out=ss[:, 1:2], in_=ss[:, 0:1], func=ACT.Sqrt, scale=1.0 / D, bias=K.epsc[:, 0:1]), r=[ssb, K.epsc_b], w=[ssb])
                S.op("dve", lambda h: h.reciprocal(out=ss[:, 2:3], in_=ss[:, 1:2]), r=[ssb], w=[ssb])
                xn, xnbuf = xnr.get()
                S.op("act", lambda h: h.activation(out=xn[:], in_=x[:], func=ACT.Copy, scale=ss[:, 2:3]), r=[xb], w=[xnbuf], rs=[ssb])
                xns.append((xn, xnbuf))
                o, ob = xnb.get()
                S.op("pool", lambda h: h.tensor_copy(out=o[:], in_=xn[:]), r=[xnbuf], w=[ob])
                S.dma("sp", xn2[t * 128:(t + 1) * 128, :], o[:], ob, dr=xn2_b, load=False)
            hgt, hgb = hg_r.get()
            for j in range(8):
                ps, psb = K.bank()
                for i, (xn, xnbuf) in enumerate(xns):
                    S.op("pe", lambda h: h.transpose(out=ps[:, i * 128:(i + 1) * 128], in_=xn[:, j * 128:(j + 1) * 128], identity=K.ident[:]),
                         r=[xnbuf, K.ident_b], w=[psb])
                evac(K, K.ev(), hgt[:, j, 0:n * 128], ps[:, 0:n * 128], r=[psb, K.A_b, K.mv_b], w=[hgb],
                     scale=K.A2[:, j, s:s + 1], bias=K.mv[:, 3, j, s:s + 1])
            for i in range(n):
                t = t0 + i
                pl, plb = K.bank()
                for j in range(8):
                    S.op("pe", lambda h: h.matmul(pl[:, 0:16], lhsT=hgt[:, j, i * 128:(i + 1) * 128], rhs=rw[:, j, :], start=(j == 0), stop=(j == 7)),
                         r=[hgb, rw_b], w=[plb])
                ss, ssb = ssr.get()
                S.op("dve", lambda h: h.reduce_max(out=ss[:, 0:1], in_=pl[:, 0:16], axis=AX.X), r=[plb], w=[ssb])
                S.op("dve", lambda h: h.tensor_scalar(out=ss[:, 1:2], in0=ss[:, 0:1], scalar1=-1.0, scalar2=None, op0=ALU.mult), r=[ssb], w=[ssb])
                S.op("pool", lambda h: h.memset(ss[:, 2:3], 0.0), w=[ssb])
                S.op("act", lambda h: h.activation(out=R.aff[:, t, :], in_=pl[:, 0:16], func=ACT.Exp, bias=ss[:, 1:2], accum_out=ss[:, 2:3]), r=[plb], w=[R.aff_b, ssb], rs=[ssb])
                S.op("dve", lambda h: h.reciprocal(out=ss[:, 3:4], in_=ss[:, 2:3]), r=[ssb], w=[ssb])
                S.op("dve", lambda h: h.tensor_scalar(out=R.aff[:, t, :], in0=R.aff[:, t, :], scalar1=ss[:, 3:4], scalar2=None, op0=ALU.mult), r=[R.aff_b], w=[R.aff_b], rs=[ssb])
                pt, ptb = K.bank()
                S.op("pe", lambda h: h.transpose(out=pt[0:16, 0:128], in_=R.aff[:, t, :], identity=K.ident[:]), r=[R.aff_b, K.ident_b], w=[ptb])
                evac(K, K.ev(), affT[:, t * 128:(t + 1) * 128], pt[0:16, 0:128], r=[ptb], w=[affT_b])
        bs = K.sb("r_bs", [16, 8]); bs_b = Buf("r_bs")
        cj = K.sb("r_cj", [16, SEQ]); cj_b = Buf("r_cj")
        onesT = K.sb("r_onesT", [16, SEQ]); onesT_b = Buf("r_onesT")
        S.op("pool", lambda h: h.memset(onesT[:], 1.0), w=[onesT_b])
        sets = ([(0, CTX, 2 * CTX // NE)] if with_ctx else []) + [(CTX, SEQ, 2 * SEQ // NE)]
        for (c0, n, cap) in sets:
            av = affT[:, c0:c0 + n]
            LO, HI, MID, CNT, GE, D1 = [bs[:, i:i + 1] for i in range(6)]
            b1 = lambda f, rs_=True: S.op("dve", f, r=[bs_b], w=[bs_b], rs=[bs_b] if rs_ else [])
            b1(lambda h: h.memset(bs[:], 0.0), False)
            b1(lambda h: h.memset(HI, 1.0), False)
            for it in range(34):
                b1(lambda h: h.tensor_tensor(out=MID, in0=LO, in1=HI, op=ALU.add), False)
                b1(lambda h: h.tensor_scalar(out=MID, in0=MID, scalar1=0.5, scalar2=None, op0=ALU.mult), False)
                S.op("dve", lambda h: h.tensor_scalar(out=cj[:, 0:n], in0=av, scalar1=MID, scalar2=0.0, op0=ALU.is_ge, op1=ALU.add, accum_out=CNT),
                     r=[affT_b], w=[cj_b, bs_b], rs=[bs_b])
                b1(lambda h: h.tensor_scalar(out=GE, in0=CNT, scalar1=float(cap) - 0.5, scalar2=None, op0=ALU.is_ge), False)
                b1(lambda h: h.tensor_tensor(out=D1, in0=MID, in1=LO, op=ALU.subtract), False)
                b1(lambda h: h.scalar_tensor_tensor(out=LO, in0=D1, scalar=GE, in1=LO, op0=ALU.mult, op1=ALU.add))
                b1(lambda h: h.tensor_tensor(out=D1, in0=HI, in1=MID, op=ALU.subtract), False)
                b1(lambda h: h.scalar_tensor_tensor(out=HI, in0=D1, scalar=GE, in1=MID, op0=ALU.mult, op1=ALU.add))
            S.op("dve", lambda h: h.tensor_scalar(out=cj[:, 0:n], in0=av, scalar1=LO, scalar2=None, op0=ALU.is_ge), r=[affT_b], w=[cj_b], rs=[bs_b])
            S.op("dve", lambda h: h.tensor_tensor_scan(out=codeT[:, c0:c0 + n], data0=onesT[:, 0:n], data1=cj[:, 0:n], initial=0.0, op0=ALU.mult, op1=ALU.add),
                 r=[cj_b, onesT_b], w=[codeT_b])
            S.op("dve", lambda h: h.tensor_tensor(out=codeT[:, c0:c0 + n], in0=codeT[:, c0:c0 + n], in1=cj[:, 0:n], op=ALU.mult), r=[cj_b, codeT_b], w=[codeT_b])
            S.op("dve", lambda h: h.tensor_scalar(out=codeT[:, c0:c0 + n], in0=codeT[:, c0:c0 + n], scalar1=-1.0, scalar2=None, op0=ALU.add), r=[codeT_b], w=[codeT_b])
            for t in range(c0 // 128, (c0 + n) // 128):
                pt, ptb = K.bank()
                S.op("pe", lambda h: h.transpose(out=pt[:, 0:16], in_=codeT[0:16, t * 128:(t + 1) * 128], identity=K.ident[0:16, 0:16]), r=[codeT_b, K.ident_b], w=[ptb])
                evac(K, K.ev(), R.code[:, t, :], pt[:, 0:16], r=[ptb], w=[R.code_b])
        dump(K, "dbg_aff", R.aff[:], R.aff_b, [128, NT, 16], F32)
        dump(K, "dbg_code", R.code[:], R.code_b, [128, NT, 16], F32)


NF = FE // 128


def phase_moe(K, li, xn2, xn2_b, R, xacc, xacc_b, row_off, with_ctx):
    nc, S = K.nc, K.S
    wg_in = K.inp("ex_w_gate", [DEPTH, NE, D, FE])
    wu_in = K.inp("ex_w_up", [DEPTH, NE, D, FE])
    wd_in = K.inp("ex_w_down", [DEPTH, NE, FE, D])
    iot_in = K.inp("c_iota512", [128, 512])
    pt_in = K.inp("c_ptidx", [128, 1 + NT])
    NS = 544 if with_ctx else 512
    CAPC = 2 * CTX // NE
    nst = 5 if with_ctx else 4
    with phase(K):
        iot = K.sb("mo_iot", [128, 512]); iot_b = Buf("mo_iot")
        S.dma("sp", iot[:], iot_in, iot_b)
        ptx = K.sb("mo_ptx", [128, 1 + NT]); ptx_b = Buf("mo_ptx")
        S.dma("sp", ptx[:], pt_in, ptx_b)
        TA = K.sb("mo_TA", [128, NT, NE, 4], BF16); TA_b = Buf("mo_TA")
        tf = K.sb("mo_tf", [128, NT, NE]); tf_b = Buf("mo_tf")
        S.op("dve", lambda h: h.tensor_copy(out=TA[:, :, :, 0], in_=ptx[:, 0:1].unsqueeze(2).to_broadcast([128, NT, NE])), r=[ptx_b], w=[TA_b])
        S.op("dve", lambda h: h.tensor_copy(out=TA[:, :, :, 1], in_=ptx[:, 1:1 + NT].unsqueeze(2).to_broadcast([128, NT, NE])), r=[ptx_b], w=[TA_b])
        S.op("dve", lambda h: h.tensor_copy(out=TA[:, :, :, 2], in_=R.aff[:]), r=[R.aff_b], w=[TA_b])
        S.op("dve", lambda h: h.tensor_tensor(out=tf[:], in0=R.aff[:], in1=TA[:, :, :, 2], op=ALU.subtract), r=[R.aff_b, TA_b], w=[tf_b])
        S.op("dve", lambda h: h.tensor_copy(out=TA[:, :, :, 3], in_=tf[:]), r=[tf_b], w=[TA_b])
        sel_r = Rot(K, "mo_sel", [128, 512], BF16, 4)
        ix_r = Rot(K, "mo_ix", [128, 5, 8], F32, 2)
        idx_r = Rot(K, "mo_idx", [128, 2, 8], I32, 2)
        xe_r = Rot(K, "mo_xe", [128, 5, D], BF16, 2)
        xeT = K.sb("mo_xeT", [128, 8, NS], BF16); xeT_b = Buf("mo_xeT")
        actT = K.sb("mo_actT", [128, NF, NS], BF16); actT_b = Buf("mo_actT")
        wst = Rot(K, "mo_wst", [128, 8, 128], F32, 6)
        wbf = Rot(K, "mo_wbf", [128, 8, 128], BF16, 4)
        dst_r = Rot(K, "mo_dst", [128, 512], F32, 3)
        dbf = Rot(K, "mo_dbf", [128, 2, 512], BF16, 3)
        sl_r = Rot(K, "mo_sl", [128, 512], F32, 3)
        ye_r = Rot(K, "mo_ye", [128, 5, D], F32, 2)
        xsc_b = Buf("xacc_sc")
        for e in range(NE):
            pidx = [K.bank() for _ in range(4)]
            for t in range(2, NT):
                sel, selb = sel_r.get()
                S.op("dve", lambda h: h.tensor_scalar(out=sel[:], in0=iot[:], scalar1=R.code[:, t, e:e + 1], scalar2=None, op0=ALU.is_equal), r=[iot_b], w=[selb], rs=[R.code_b])
                for st in range(4):
                    S.op("pe", lambda h: h.matmul(pidx[st][0][:, 0:4], lhsT=sel[:, st * 128:(st + 1) * 128], rhs=TA[:, t, e, :], start=(t == 2), stop=(t == NT - 1)),
                         r=[selb, TA_b], w=[pidx[st][1]])
            ix, ixb = ix_r.get()
            for st in range(4):
                evac(K, K.ev(), ix[:, st, 0:4], pidx[st][0][:, 0:4], r=[pidx[st][1]], w=[ixb])
            if with_ctx:
                pic, picb = K.bank()
                for t in range(2):
                    sel, selb = sel_r.get()
                    S.op("dve", lambda h: h.tensor_scalar(out=sel[:, 0:CAPC], in0=iot[:, 0:CAPC], scalar1=R.code[:, t, e:e + 1], scalar2=None, op0=ALU.is_equal), r=[iot_b], w=[selb], rs=[R.code_b])
                    S.op("pe", lambda h: h.matmul(pic[0:CAPC, 0:4], lhsT=sel[:, 0:CAPC], rhs=TA[:, t, e, :], start=(t == 0), stop=(t == 1)), r=[selb, TA_b], w=[picb])
                S.op("dve", lambda h: h.memset(ix[:, 4, :], 0.0), w=[ixb])
                evac(K, K.ev(), ix[0:CAPC, 4, 0:4], pic[0:CAPC, 0:4], r=[picb], w=[ixb])
            S.op("dve", lambda h: h.scalar_tensor_tensor(out=ix[:, :, 4], in0=ix[:, :, 1], scalar=128.0, in1=ix[:, :, 0], op0=ALU.mult, op1=ALU.add), r=[ixb], w=[ixb])
            S.op("dve", lambda h: h.tensor_tensor(out=ix[:, :, 5], in0=ix[:, :, 2], in1=ix[:, :, 3], op=ALU.add), r=[ixb], w=[ixb])
            S.op("dve", lambda h: h.tensor_scalar(out=ix[:, :, 6], in0=ix[:, :, 4], scalar1=-float(row_off), scalar2=None, op0=ALU.add), r=[ixb], w=[ixb])
            idx, idxb = idx_r.get()
            S.op("dve", lambda h: h.tensor_copy(out=idx[:, 0, 0:5], in_=ix[:, :, 4]), r=[ixb], w=[idxb])
            S.op("dve", lambda h: h.tensor_copy(out=idx[:, 1, 0:5], in_=ix[:, :, 6]), r=[ixb], w=[idxb])
            xe, xeb = xe_r.get()
            for st in range(nst):
                ns_ = 128 if st < 4 else CAPC
                S.idma(xe[0:ns_, st, :], xn2[:, :], xeb, idxb, in_off=bass.IndirectOffsetOnAxis(ap=idx[0:ns_, 0, st:st + 1], axis=0), dr=xn2_b, gather=True)
            for kc in range(8):
                pt, ptb = K.bank()
                pv = pt[:].bitcast(BF16)
                for st in range(nst):
                    ns_ = 128 if st < 4 else CAPC
                    S.op("pe", lambda h: h.transpose(out=pv[:, st * 128:st * 128 + ns_], in_=xe[0:ns_, st, kc * 128:(kc + 1) * 128], identity=K.identb[0:ns_, 0:ns_]),
                         r=[xeb, K.identb_b], w=[ptb])
                evac(K, "act", xeT[:, kc, 0:512], pv[:, 0:512], r=[ptb, K.A_b, K.mv_b], w=[xeT_b], scale=K.A2[:, kc, 0:1], bias=K.mv[:, 3, kc, 0:1])
                if with_ctx:
                    evac(K, "dve", xeT[:, kc, 512:NS], pv[:, 512:NS], r=[ptb, K.A_b, K.mv_b], w=[xeT_b], scale=K.A2[:, kc, 1:2], bias=K.mv[:, 3, kc, 1:2])
            def load_gu(f):
                res = []
                for w_in in (wg_in, wu_in):
                    ws, wsb = wst.get()
                    S.dma("sp", ws[:], w_in[li, e].rearrange("(k p) f -> p k f", p=128)[:, :, f * 128:(f + 1) * 128], wsb)
                    res.append((ws, wsb))
                return res
            pre = [load_gu(0), load_gu(1)]
            for f in range(NF):
                cur = pre.pop(0)
                wbs = []
                for i, (ws, wsb) in enumerate(cur):
                    wb, wbb = wbf.get()
                    if i == 0:
                        S.op("act", lambda h: h.activation(out=wb[:], in_=ws[:], func=ACT.Copy), r=[wsb], w=[wbb])
                    else:
                        S.op("dve", lambda h: h.tensor_copy(out=wb[:], in_=ws[:]), r=[wsb], w=[wbb])
                    wbs.append((wb, wbb))
                if f + 2 < NF:
                    pre.append(load_gu(f + 2))
                segs = [(0, 512)] + ([(512, NS)] if with_ctx else [])
                for (a0, a1) in segs:
                    pg, pgb = K.bank()
                    pu, pub = K.bank()
                    for (pp, ppb, (wb, wbb)) in ((pg, pgb, wbs[0]), (pu, pub, wbs[1])):
                        for kc in range(8):
                            S.op("pe", lambda h: h.matmul(pp[:, 0:a1 - a0], lhsT=wb[:, kc, :], rhs=xeT[:, kc, a0:a1], start=(kc == 0), stop=(kc == 7)), r=[wbb, xeT_b], w=[ppb])
                    sl, slb = sl_r.get()
                    S.op("act", lambda h: h.activation(out=sl[:, 0:a1 - a0], in_=pg[:, 0:a1 - a0], func=ACT.Silu), r=[pgb], w=[slb])
                    S.op("dve", lambda h: h.tensor_tensor(out=actT[:, f, a0:a1], in0=sl[:, 0:a1 - a0], in1=pu[:, 0:a1 - a0], op=ALU.mult), r=[slb, pub], w=[actT_b])
            ye, yeb = ye_r.get()
            for half in range(2):
                hs = slice(half * 512, (half + 1) * 512)
                pys = [K.bank() for _ in range(nst)]
                for f in range(NF):
                    ds_, dsb = dst_r.get()
                    S.dma("sp", ds_[:], wd_in[li, e, f * 128:(f + 1) * 128, hs], dsb)
                    db, dbb = dbf.get()
                    S.op("dve", lambda h: h.tensor_tensor(out=db[:, 0, :], in0=ds_[:], in1=K.gbc[:, 2, hs], op=ALU.mult), r=[dsb, K.gbc_b], w=[dbb])
                    if with_ctx:
                        S.op("dve", lambda h: h.tensor_tensor(out=db[:, 1, :], in0=ds_[:], in1=K.gbc[:, 3, hs], op=ALU.mult), r=[dsb, K.gbc_b], w=[dbb])
                    for st in range(nst):
                        ns_ = 128 if st < 4 else CAPC
                        S.op("pe", lambda h: h.matmul(pys[st][0][0:ns_, :], lhsT=actT[:, f, st * 128:st * 128 + ns_], rhs=db[:, 1 if st == 4 else 0, :], start=(f == 0), stop=(f == NF - 1)),
                             r=[actT_b, dbb], w=[pys[st][1]])
                for st in range(nst):
                    ns_ = 128 if st < 4 else CAPC
                    evac(K, "act", ye[0:ns_, st, hs], pys[st][0][0:ns_, :], r=[pys[st][1]], w=[yeb], scale=ix[0:ns_, st, 5:6], rs=[ixb])
            for st in range(nst):
                ns_ = 128 if st < 4 else CAPC
                S.idma(xacc[:, :], ye[0:ns_, st, :], yeb, idxb, out_off=bass.IndirectOffsetOnAxis(ap=idx[0:ns_, 1, st:st + 1], axis=0), dr=xsc_b, gather=False,
                       compute_op=ALU.add)


def setup_consts(K):
    nc, S = K.nc, K.S
    K.ident = K.sb("ident", [128, 128]); K.ident_b = Buf("ident")
    K.ones = K.sb("ones", [128, 128]); K.ones_b = Buf("ones")
    K.epsc = K.sb("epsc", [128, 1]); K.epsc_b = Buf("epsc")
    K.cond = K.sb("cond", [128, 8, 2]); K.cond_b = Buf("cond")
    K.mv = K.sb("mv", [128, 6, 8, 2]); K.mv_b = Buf("mv")
    K.A1 = K.sb("A1", [128, 8, 2]); K.A2 = K.sb("A2", [128, 8, 2]); K.A_b = Buf("A")
    K.gbc = K.sb("gbc", [128, 4, D]); K.gbc_b = Buf("gbc")
    S.dma("sp", K.ident[:], K.inp("c_ident", [128, 128]), K.ident_b)
    K.identb = K.sb("identb", [128, 128], BF16); K.identb_b = Buf("identb")
    S.op("pool", lambda h: h.tensor_copy(out=K.identb[:], in_=K.ident[:]), r=[K.ident_b], w=[K.identb_b])
    S.op("dve", lambda h: h.memset(K.ones[:], 1.0), w=[K.ones_b])
    K.onesb = K.sb("onesb", [128, 128], BF16); K.onesb_b = Buf("onesb")
    S.op("dve", lambda h: h.memset(K.onesb[:], 1.0), w=[K.onesb_b])
    S.op("dve", lambda h: h.memset(K.epsc[:], EPS), w=[K.epsc_b])
    craw = K.sb("craw", [128, 2, 8]); craw_b = Buf("craw")
    S.dma("sp", craw[:, 0, :], K.inp("cT", [128, 8]), craw_b)
    S.dma("sp", craw[:, 1, :], K.inp("c_ctxT", [128, 8]), craw_b)
    for s in range(2):
        S.op("act", lambda h: h.activation(out=K.cond[:, :, s], in_=craw[:, s, :], func=ACT.Silu), r=[craw_b], w=[K.cond_b])
    S.scope_ents = []


def build_program(debug=None, upto="all", only=None, p_external=False, o_external=False, layers=DEPTH):
    nc = bass.Bass("TRN2", target_bir_lowering=False)
    with ExitStack() as stack:
        K = mk_ctx(nc, stack, debug)
        setup_consts(K)
        R = Ctx()
        R.aff = K.sb("R_aff", [128, NT, 16]); R.aff_b = Buf("R_aff")
        R.code = K.sb("R_code", [128, NT, 16]); R.code_b = Buf("R_code")
        xs0 = K.inp("xs0", [T, D]); xs0_b = Buf("xs0")
        kind1 = "ExternalOutput" if "xs1" in K.debug else "Internal"
        xs1 = nc.dram_tensor("xs1", [T, D], F32, kind=kind1).ap(); xs1_b = Buf("xs1")
        out = nc.dram_tensor("out", [SEQ, D], F32, kind="ExternalOutput").ap(); out_b = Buf("out")
        xn2, xn2_b = K.dram("xn2", [T, D], BF16)
        P = alloc_proj_scratch(K, p_external)
        if o_external:
            O = {n: (K.inp("O_" + n, [256, T], BF16), Buf("O_" + n)) for n in ("s5", "na", "hg", "rt")}
        else:
            O = {n: K.dram("O_" + n, [256, T], BF16) for n in ("s5", "na", "hg", "rt")}
        xcur, xcur_b = xs0, xs0_b
        for li in range(layers):
            last = li == DEPTH - 1
            if not p_external or upto in ("all", "merge", "route"):
                phase0(K, li)
            if not p_external:
                with phase(K):
                    hT = K.sb("hT", [128, 8, T], BF16); hT_b = Buf("hT")
                    norm_to_fm(K, xcur, xcur_b, hT, hT_b, K.A1, 0)
                    dump(K, "dbg_hT", hT[:], hT_b, [128, 8, T], BF16)
                    phase1b(K, li, hT, hT_b, P)
            if upto == "p1":
                break
            if not o_external:
                if only is None or "rt" in only:
                    phase_ret(K, li, P, O["rt"])
                if only is None or "na" in only:
                    phase_na(K, li, P, O["na"])
                if only is None or "hg" in only:
                    phase_hg(K, li, P, O["hg"])
                if only is None or "s5" in only:
                    phase_s5(K, li, P, O["s5"])
            if upto == "mix":
                break
            xnext, xnext_b = (out, out_b) if last else (xs1, xs1_b)
            phase_merge(K, li, P, O, xcur, xcur_b, xnext, xnext_b, last)
            if upto == "merge":
                break
            phase_route(K, li, xnext, xnext_b, CTX if last else 0, xn2, xn2_b, R, not last)
            if upto == "route":
                break
            phase_moe(K, li, xn2, xn2_b, R, xnext, xnext_b, CTX if last else 0, not last)
            xcur, xcur_b = xnext, xnext_b
        K.S.barrier()
    return nc, K


def _pk(v):
    v = np.asarray(v, np.float32)
    return np.ascontiguousarray(v.reshape(-1, 128).T)


def rope_tables():
    t = np.arange(SEQ)
    quarter = 16
    inv = 10000.0 ** (-np.arange(quarter) / quarter)
    ang = np.concatenate([(t // 64)[:, None] * inv, (t % 64)[:, None] * inv], axis=1)
    cos = np.ones((128, T), np.float32)
    sin = np.zeros((128, T), np.float32)
    for p in range(128):
        i = p % 32
        cos[p, CTX:] = np.cos(ang[:, i])
        sin[p, CTX:] = np.sin(ang[:, i])
    return cos, sin


def host_consts():
    g = {}
    g["c_ident"] = np.eye(128, dtype=np.float32)
    j = np.arange(128)[:, None]; t = np.arange(128)[None, :]
    g["c_epos0"] = np.maximum(t - j, 0).astype(np.float32)
    g["c_epos1"] = np.maximum(j - t, 0).astype(np.float32)
    g["c_mk0"] = (t >= j).astype(np.float32)
    g["c_mk1"] = (j >= t).astype(np.float32)
    g["c_iota1"] = np.broadcast_to((np.arange(128) + 1.0)[None, :], (128, 128)).astype(np.float32)
    g["c_iotar"] = np.broadcast_to((128.0 - np.arange(128))[None, :], (128, 128)).astype(np.float32)
    g["c_pcol"] = np.stack([127.0 - np.arange(128), np.arange(128) * 1.0], axis=1).astype(np.float32)
    g["c_blk64"] = ((j // 64) == (t // 64)).astype(np.float32)
    same = (j // 16) == (t // 16)
    g["c_hgm0"] = (same & (j <= t)).astype(np.float32)
    g["c_hgm1"] = (same & (j >= t)).astype(np.float32)
    c = np.arange(8)[None, :]
    tt = np.arange(128)[:, None]
    g["c_blkm"] = np.stack([(tt // 16 == c), (tt // 16 == 7 - c)], axis=1).astype(np.float32)
    rst = np.ones((128, HB), np.float32); rst[:, ::16] = 0.0
    g["c_rst"] = rst
    kcol = np.arange(64)[:, None]; qcol = np.arange(64)[None, :]
    cs = np.clip(qcol - 8, 0, 48)
    ok = (kcol >= cs) & (kcol < cs + 16)
    m = np.where(ok, 0.0, -240000.0).astype(np.float32)
    g["c_namask"] = np.concatenate([m, m], axis=0)
    cos, sin = rope_tables()
    g["rope_cos"] = cos
    g["rope_sin"] = sin
    sel = np.zeros((128, 8, 8, 128), np.float32)
    for gl in range(8):
        for jl in range(8):
            for q in range(16):
                sel[16 * gl + q, gl, jl, 16 * jl + q] = 1.0
    g["c_sel"] = sel.reshape(128, 64, 128).astype(ml_dtypes.bfloat16)
    g["c_selT"] = np.ascontiguousarray(np.transpose(sel, (3, 1, 2, 0))).reshape(128, 64, 128).astype(ml_dtypes.bfloat16)
    jj = (np.arange(2)[None, :, None] * 8 + (np.arange(128) // 16)[:, None, None])
    tt2 = (np.arange(256) // 16)[None, None, :]
    g["c_s5m0"] = (jj <= tt2).astype(np.float32)
    g["c_s5m1"] = (jj >= tt2).astype(np.float32)
    g["c_kA"] = np.broadcast_to((np.arange(32) - 15.0)[None, :], (128, 32)).astype(np.float32)
    g["c_kB"] = np.broadcast_to((16.0 - np.arange(32))[None, :], (128, 32)).astype(np.float32)
    g["c_iota512"] = np.broadcast_to(np.arange(512, dtype=np.float32)[None, :], (128, 512))
    g["c_ptidx"] = np.concatenate([np.arange(128, dtype=np.float32)[:, None], np.broadcast_to(np.arange(NT, dtype=np.float32)[None, :], (128, NT))], axis=1)
    g["c_iotap"] = (np.arange(128)[:, None] + 128.0 * np.arange(4)[None, :]).astype(np.float32)
    e16 = np.zeros((128, 32, 128), np.float32)
    for j in range(16):
        e16[j, j, :] = 1.0
        e16[32 + j, 16 + j, :] = 1.0
    g["c_e16"] = e16
    p = np.arange(128)
    g["c_sel3"] = np.stack([(p < 64) * 1.0, (p >= 64) * 1.0, (p >= 64) * -1.0], axis=1).astype(np.float32)
    return g


def prep_core_inputs(inputs, b, names):
    g = host_consts()
    f32 = lambda a: np.asarray(a, np.float32)
    g["xs0"] = np.concatenate([inputs["ctx"][b], inputs["x"][b]], axis=0).astype(np.float32)
    g["cT"] = _pk(inputs["c"][b])
    g["c_ctxT"] = _pk(inputs["c_ctx"])
    g["ada_w"] = inputs["ada_w"]
    g["ada_bT"] = np.stack([_pk(inputs["ada_b"][l]) for l in range(DEPTH)])
    g["norm_mix_wT"] = np.stack([_pk(inputs["norm_mix_w"][l]) for l in range(DEPTH)])
    g["norm_ffn_wT"] = np.stack([_pk(inputs["norm_ffn_w"][l]) for l in range(DEPTH)])
    g["w_in"] = inputs["w_in"]
    g["ret_bc"] = np.broadcast_to(f32(inputs["ret_decay_logit"]).reshape(DEPTH, 1, 8), (DEPTH, 128, 8))
    qn, kn = f32(inputs["na_q_norm"]), f32(inputs["na_k_norm"])
    g["na_qk_normT"] = np.stack([np.stack([np.tile(qn[l], 2), np.tile(kn[l], 2)], axis=1) for l in range(DEPTH)])
    kcol = np.arange(64)[:, None]; qcol = np.arange(64)[None, :]
    dc = np.clip(kcol - qcol + 15, 0, 30)
    rp = f32(inputs["na_rpb"])[:, :, :, dc]
    rp = np.transpose(rp, (0, 3, 1, 2, 4))
    g["na_rpbT2"] = np.concatenate([rp, rp], axis=1)
    g["w_branch"] = inputs["w_branch"]
    g["w_out"] = inputs["w_out"]
    g["router_wT"] = np.ascontiguousarray(np.transpose(f32(inputs["router_w"]).reshape(DEPTH, 8, 128, NE), (0, 2, 1, 3)))
    g["ex_w_gate"] = inputs["ex_w_gate"]
    g["ex_w_up"] = inputs["ex_w_up"]
    g["ex_w_down"] = inputs["ex_w_down"]
    def nfirst(a):
        a = f32(a)
        a = np.moveaxis(a, 3, 1)
        a = a.reshape(a.shape[0], 64, 32, *a.shape[4:])
        return np.concatenate([a, a], axis=1)
    g["s5_lamT"] = np.stack([nfirst(inputs["s5_lam_re"]), nfirst(inputs["s5_lam_im"])], axis=2)
    g["s5_stepT"] = np.broadcast_to(f32(inputs["s5_log_step"]).reshape(DEPTH, 1, 32), (DEPTH, 128, 32))
    g["s5_BT"] = np.stack([nfirst(inputs["s5_b_re"]), nfirst(inputs["s5_b_im"])], axis=2)
    cre = np.swapaxes(f32(inputs["s5_c_re"]), 3, 4); cim = np.swapaxes(f32(inputs["s5_c_im"]), 3, 4)
    g["s5_CT"] = np.stack([nfirst(cre), nfirst(cim)], axis=2)
    g["s5_dT"] = np.ascontiguousarray(np.transpose(f32(inputs["s5_d"]).reshape(DEPTH, 2, 128), (0, 2, 1)))
    g["s5_glu_w"] = f32(inputs["s5_glu_w"])
    lbp = f32(inputs["hg_lower_bounds"])
    g["hg_lbT"] = np.ascontiguousarray(np.transpose(lbp.reshape(DEPTH, 2, 128), (2, 0, 1)))
    g["hg_norm_wT"] = f32(inputs["hg_norm_w"]).reshape(DEPTH, 64, 1)
    return {k: np.ascontiguousarray(v) for k, v in g.items() if k in names}


def kernel(**inputs):
    nc, K = build_program()
    names = set(K.inputs.keys())
    in_maps = [prep_core_inputs(inputs, b, names) for b in range(8)]
    res = run_bass_kernel_spmd(nc, in_maps, core_ids=list(range(8)))
    return np.stack([np.asarray(r["out"], np.float32) for r in res.results], axis=0)
```

```python
import os
import numpy as np
import ml_dtypes
import concourse.bass as bass
import concourse.mybir as mybir
from concourse.bass_utils import run_bass_kernel_spmd
from contextlib import ExitStack

F32 = mybir.dt.float32
BF16 = mybir.dt.bfloat16
I32 = mybir.dt.int32
ACT = mybir.ActivationFunctionType
ALU = mybir.AluOpType
AX = mybir.AxisListType

D = 1024
SEQ = 4096
CTX = 256
T = SEQ + CTX
NT = T // 128
DEPTH = 2
D_IN = 7424
NE = 16
FE = 2816
EPS = 1e-6
LIMIT = int(os.environ.get("MK_LIMIT", "0"))
SAME_ENG_SYNC = bool(os.environ.get("MK_SAMESYNC"))
TB = [(i * 512, 512) for i in range(8)] + [(4096, 256)]


class Buf:
    __slots__ = ("name", "w", "r", "dma_sem", "dma_cnt", "pend_w", "pend_r")

    def __init__(self, name):
        self.name = name
        self.w = None
        self.r = {}
        self.dma_sem = None
        self.dma_cnt = 0
        self.pend_w = {}
        self.pend_r = {}


class _Eng:
    def __init__(self, name, h, sem):
        self.name = name
        self.h = h
        self.sem = sem
        self.cnt = 0
        self.seen = {}
        self.seen_dma = {}


class Sched:
    def __init__(self, nc, stack):
        self.nc = nc
        self.stack = stack
        hs = {"pe": nc.tensor, "act": nc.scalar, "dve": nc.vector, "pool": nc.gpsimd, "sp": nc.sync}
        self.eng = {}
        for k, h in hs.items():
            sem = stack.enter_context(nc.semaphore("s_" + k))
            self.eng[k] = _Eng(k, h, sem)
        self.dma_sems = []
        self.free_sems = []
        self.scope_ents = []
        self.n_ins = 0

    def _wait_eng(self, E, e2, s, force=False):
        if e2 == E.name and E.name == "pe" and not (force or SAME_ENG_SYNC):
            return
        if E.seen.get(e2, 0) >= s:
            return
        E.h.wait_ge(self.eng[e2].sem, s)
        E.seen[e2] = s

    def _wait_dma(self, E, sem, val):
        k = id(sem)
        if E.seen_dma.get(k, 0) >= val:
            return
        E.h.wait_ge(sem, val)
        E.seen_dma[k] = val

    def _sync(self, E, r, w, rs=()):
        for b in r:
            if b.w is not None:
                self._wait_eng(E, b.w[0], b.w[1])
            if b.dma_cnt:
                self._wait_dma(E, b.dma_sem[0], b.dma_cnt)
        for b in rs:
            if b.w is not None:
                self._wait_eng(E, b.w[0], b.w[1], force=True)
            if b.dma_cnt:
                self._wait_dma(E, b.dma_sem[0], b.dma_cnt)
        for b in w:
            if b.w is not None:
                self._wait_eng(E, b.w[0], b.w[1])
            for e2, s in b.r.items():
                self._wait_eng(E, e2, s)
            if b.dma_cnt:
                self._wait_dma(E, b.dma_sem[0], b.dma_cnt)

    def op(self, e, fn, r=(), w=(), rs=()):
        if LIMIT and self.n_ins >= LIMIT:
            return None
        E = self.eng[e]
        self._sync(E, r, w, rs)
        ins = fn(E.h)
        E.cnt += 1
        ins.then_inc(E.sem, 1)
        for b in r:
            b.r[e] = E.cnt
        for b in rs:
            b.r[e] = E.cnt
        for b in w:
            b.w = (e, E.cnt)
            b.r = {}
        self.n_ins += 1
        return ins

    def _get_dma_sem(self, sb):
        if sb.dma_sem is None:
            if self.free_sems:
                ent = self.free_sems.pop()
            else:
                ent = [self.stack.enter_context(self.nc.semaphore("d%d" % len(self.dma_sems))), 0]
                self.dma_sems.append(ent)
            self.scope_ents.append(ent)
            sb.dma_sem = ent
        return sb.dma_sem

    def release_phase_sems(self):
        self.free_sems.extend(self.scope_ents)
        self.scope_ents = []

    def dma(self, q, out, in_, sb, dr=None, load=True, **kw):
        if LIMIT and self.n_ins >= LIMIT:
            return None
        Q = self.eng[q]
        ent = self._get_dma_sem(sb)
        if load:
            self._sync(Q, (), (sb,))
        else:
            self._sync(Q, (sb,), ())
        if dr is not None:
            pend = dr.pend_w if load else dr.pend_r
            for sem, val in pend.values():
                self._wait_dma(Q, sem, val)
            if not load:
                for sem, val in dr.pend_w.values():
                    self._wait_dma(Q, sem, val)
        ins = Q.h.dma_start(out=out, in_=in_, **kw)
        ent[1] += 16
        sb.dma_cnt = ent[1]
        ins.then_inc(ent[0], 16)
        if dr is not None:
            (dr.pend_r if load else dr.pend_w)[id(ent[0])] = (ent[0], ent[1])
        self.n_ins += 1
        return ins

    def idma(self, out, in_, sb, idx_b, out_off=None, in_off=None, dr=None, gather=True, **kw):
        if LIMIT and self.n_ins >= LIMIT:
            return None
        Q = self.eng["pool"]
        ent = self._get_dma_sem(sb)
        if gather:
            self._sync(Q, (idx_b,), (sb,))
        else:
            self._sync(Q, (idx_b, sb), ())
        if dr is not None:
            for sem, val in dr.pend_w.values():
                self._wait_dma(Q, sem, val)
            if not gather:
                for sem, val in dr.pend_r.values():
                    self._wait_dma(Q, sem, val)
        ins = Q.h.indirect_dma_start(out=out, out_offset=out_off, in_=in_, in_offset=in_off, **kw)
        ent[1] += 16
        sb.dma_cnt = ent[1]
        ins.then_inc(ent[0], 16)
        if dr is not None:
            (dr.pend_r if gather else dr.pend_w)[id(ent[0])] = (ent[0], ent[1])
        self.n_ins += 1
        return ins

    def barrier(self):
        for E in self.eng.values():
            for E2 in self.eng.values():
                if E2 is not E and E2.cnt:
                    self._wait_eng(E, E2.name, E2.cnt)
            for sem, cnt in self.dma_sems:
                if cnt:
                    self._wait_dma(E, sem, cnt)


class Rot:
    def __init__(self, K, name, shape, dtype, n):
        self.t = [K.sb(f"{name}{i}", shape, dtype) for i in range(n)]
        self.b = [Buf(f"{name}{i}") for i in range(n)]
        self.i = 0

    def get(self):
        i = self.i
        self.i = (i + 1) % len(self.t)
        return self.t[i], self.b[i]


class Ctx:
    pass


def mk_ctx(nc, stack, debug):
    K = Ctx()
    K.nc = nc
    K.S = Sched(nc, stack)
    K.top = stack
    K.stack = stack
    K.debug = set(debug or ())
    K.inputs = {}
    K.dbuf = {}

    K.uid = 0

    def sb(name, shape, dtype=F32):
        K.uid += 1
        return K.stack.enter_context(nc.sbuf_tensor(f"{name}_{K.uid}", list(shape), dtype))
    K.sb = sb

    def inp(name, shape, dtype=F32):
        if name not in K.inputs:
            K.inputs[name] = nc.dram_tensor(name, list(shape), dtype, kind="ExternalInput").ap()
        return K.inputs[name]
    K.inp = inp

    def dram(name, shape, dtype):
        kind = "ExternalOutput" if name in K.debug else "Internal"
        ap = nc.dram_tensor(name, list(shape), dtype, kind=kind).ap()
        K.dbuf[name] = Buf(name)
        return ap, K.dbuf[name]
    K.dram = dram
    K.ps = [stack.enter_context(nc.psum_tensor(f"ps{i}", [128, 512], F32)) for i in range(8)]
    K.psb = [Buf(f"ps{i}") for i in range(8)]
    K.pi = 0

    def bank():
        i = K.pi
        K.pi = (i + 1) % 8
        return K.ps[i], K.psb[i]
    K.bank = bank
    K.evi = 0

    def ev():
        K.evi ^= 1
        return "act" if K.evi else "dve"
    K.ev = ev
    return K


class phase:
    def __init__(self, K):
        self.K = K

    def __enter__(self):
        self.prev = self.K.stack
        self.st = ExitStack()
        self.st.__enter__()
        self.K.stack = self.st
        return self

    def __exit__(self, *a):
        self.K.S.barrier()
        self.K.S.release_phase_sems()
        self.K.stack = self.prev
        return self.st.__exit__(*a)


def evac(K, eng, out, in_, r, w, func=None, scale=None, bias=None, rs=()):
    S = K.S
    if eng == "act":
        kw = {}
        if scale is not None:
            kw["scale"] = scale
        if bias is not None:
            kw["bias"] = bias
        f = func if func is not None else (ACT.Identity if bias is not None else ACT.Copy)
        S.op("act", lambda h: h.activation(out=out, in_=in_, func=f, **kw), r=r, w=w, rs=rs)
    else:
        assert func is None
        if scale is None and bias is None:
            S.op(eng, lambda h: h.tensor_copy(out=out, in_=in_), r=r, w=w)
        elif bias is None:
            S.op(eng, lambda h: h.tensor_scalar(out=out, in0=in_, scalar1=scale, scalar2=None, op0=ALU.mult), r=r, w=w, rs=rs)
        else:
            sc = 1.0 if scale is None else scale
            S.op(eng, lambda h: h.tensor_scalar(out=out, in0=in_, scalar1=sc, scalar2=bias, op0=ALU.mult, op1=ALU.add), r=r, w=w, rs=rs)


def dump(K, name, src_ap, src_b, shape, dtype):
    if name in K.debug:
        ap, b = K.dram(name, shape, dtype)
        K.S.dma("sp", ap, src_ap, src_b, dr=b, load=False)


def phase0(K, li):
    nc, S = K.nc, K.S
    ada_w = K.inp("ada_w", [DEPTH, D, 6 * D])
    ada_bT = K.inp("ada_bT", [DEPTH, 128, 48])
    nmw = K.inp("norm_mix_wT", [DEPTH, 128, 8])
    nfw = K.inp("norm_ffn_wT", [DEPTH, 128, 8])
    with phase(K):
        aw = Rot(K, "adaw", [128, 6 * D], F32, 2)
        adab = K.sb("adab", [128, 48]); adab_b = Buf("adab")
        nw = K.sb("nw", [128, 2, 8]); nw_b = Buf("nw")
        dg = Rot(K, "dg", [128, 128], F32, 3)
        S.dma("sp", adab[:], ada_bT[li], adab_b)
        S.dma("sp", nw[:, 0, :], nmw[li], nw_b)
        S.dma("sp", nw[:, 1, :], nfw[li], nw_b)
        pmA, pmAb = K.bank()
        pmB, pmBb = K.bank()
        for k in range(8):
            a, ab = aw.get()
            S.dma("sp", a[:], ada_w[li, k * 128:(k + 1) * 128, :], ab)
            pm, pmb = (pmA, pmAb) if k < 4 else (pmB, pmBb)
            c0 = (k % 4) * 96
            for j in range(48):
                S.op("pe", lambda h: h.matmul(pm[:, c0 + 2 * j:c0 + 2 * j + 2], lhsT=a[:, j * 128:(j + 1) * 128], rhs=K.cond[:, k, :],
                                              start=True, stop=True),
                     r=[ab, K.cond_b], w=[pmb])
        mv = K.mv
        acc = K.sb("p0acc", [128, 2, 96]); acc_b = Buf("p0acc")
        for i, (pm, pmb) in enumerate(((pmA, pmAb), (pmB, pmBb))):
            S.op("dve", lambda h: h.tensor_reduce(out=acc[:, i, :], in_=pm[:, 0:384].rearrange("p (k c) -> p c k", k=4), axis=AX.X, op=ALU.add),
                 r=[pmb], w=[acc_b])
        S.op("dve", lambda h: h.tensor_tensor(out=acc[:, 0, :], in0=acc[:, 0, :], in1=acc[:, 1, :], op=ALU.add), r=[acc_b], w=[acc_b])
        S.op("dve", lambda h: h.tensor_tensor(out=mv[:].rearrange("p v k s -> p (v k) s"),
                                              in0=acc[:, 0, :].rearrange("p (j s) -> p j s", s=2),
                                              in1=adab[:].unsqueeze(2).to_broadcast([128, 48, 2]), op=ALU.add),
             r=[acc_b, adab_b], w=[K.mv_b])
        for (Ai, vi, wi) in ((K.A1, 1, 0), (K.A2, 4, 1)):
            for s in range(2):
                S.op("dve", lambda h: h.scalar_tensor_tensor(out=Ai[:, :, s], in0=mv[:, vi, :, s], scalar=1.0, in1=nw[:, wi, :],
                                                             op0=ALU.add, op1=ALU.mult),
                     r=[K.mv_b, nw_b], w=[K.A_b])
        for gi, vi in enumerate((2, 5)):
            for s in range(2):
                for half in range(2):
                    pb, pbb = K.bank()
                    for q in range(4):
                        kk = half * 4 + q
                        d, db = dg.get()
                        S.op("dve", lambda h: h.tensor_scalar(out=d[:], in0=K.ident[:], scalar1=mv[:, vi, kk, s:s + 1], scalar2=None, op0=ALU.mult),
                             r=[K.ident_b], w=[db], rs=[K.mv_b])
                        S.op("pe", lambda h: h.matmul(pb[:, q * 128:(q + 1) * 128], lhsT=K.ones[:], rhs=d[:], start=True, stop=True),
                             r=[K.ones_b, db], w=[pbb])
                    evac(K, K.ev(), K.gbc[:, gi * 2 + s, half * 512:(half + 1) * 512], pb[:], r=[pbb], w=[K.gbc_b])
        dump(K, "dbg_mv", K.mv[:], K.mv_b, [128, 6, 8, 2], F32)
        dump(K, "dbg_gbc", K.gbc[:], K.gbc_b, [128, 4, D], F32)


def norm_to_fm(K, xsrc, xsrc_b, hT, hT_b, A, B_vi, xn_dram=None):
    nc, S = K.nc, K.S
    groups = [(0, 2, 1)] + [(2 + 4 * i, 4, 0) for i in range(8)]
    with phase(K):
        xr = Rot(K, "xr", [128, D], F32, 3)
        xnr = Rot(K, "xnr", [128, D], F32, 8)
        junk = K.sb("junk", [128, D], BF16); junk_b = Buf("junk")
        ssr = Rot(K, "ssr", [128, 4], F32, 4)
        xnb = Rot(K, "xnb", [128, D], BF16, 3) if xn_dram is not None else None
        for (t0, n, s) in groups:
            xns = []
            for i in range(n):
                t = t0 + i
                x, xb = xr.get()
                S.dma("sp", x[:], xsrc[t * 128:(t + 1) * 128, :], xb, dr=xsrc_b)
                ss, ssb = ssr.get()
                S.op("pool", lambda h: h.memset(ss[:], 0.0), w=[ssb])
                S.op("act", lambda h: h.activation(out=junk[:], in_=x[:], func=ACT.Square, accum_out=ss[:, 0:1]), r=[xb], w=[junk_b, ssb])
                S.op("act", lambda h: h.activation(out=ss[:, 1:2], in_=ss[:, 0:1], func=ACT.Sqrt, scale=1.0 / D, bias=K.epsc[:, 0:1]), r=[ssb, K.epsc_b], w=[ssb])
                S.op("dve", lambda h: h.reciprocal(out=ss[:, 2:3], in_=ss[:, 1:2]), r=[ssb], w=[ssb])
                xn, xnbuf = xnr.get()
                S.op("act", lambda h: h.activation(out=xn[:], in_=x[:], func=ACT.Copy, scale=ss[:, 2:3]), r=[xb], w=[xnbuf], rs=[ssb])
                xns.append((xn, xnbuf))
                if xn_dram is not None:
                    o, ob = xnb.get()
                    S.op("pool", lambda h: h.tensor_copy(out=o[:], in_=xn[:]), r=[xnbuf], w=[ob])
                    S.dma("sp", xn_dram[0][t * 128:(t + 1) * 128, :], o[:], ob, dr=xn_dram[1], load=False)
            for j in range(8):
                ps, psb = K.bank()
                for i, (xn, xnbuf) in enumerate(xns):
                    S.op("pe", lambda h: h.transpose(out=ps[:, i * 128:(i + 1) * 128], in_=xn[:, j * 128:(j + 1) * 128], identity=K.ident[:]),
                         r=[xnbuf, K.ident_b], w=[psb])
                evac(K, K.ev(), hT[:, j, t0 * 128:(t0 + n) * 128], ps[:, 0:n * 128], r=[psb, K.A_b, K.mv_b], w=[hT_b],
                     scale=A[:, j, s:s + 1], bias=K.mv[:, B_vi, j, s:s + 1])


BLOCKS = [
    ("s5u", "fm", None), ("naq", "fm", None), ("nak", "fm", None), ("nav", "tm", None),
    ("hgq", "fm", "silu"), ("hgff", "fm32", None), ("hgfb", "fm32", None), ("hgi", "tm", None), ("hgg", "fm", "silu"),
    ("rtq", "rope", 1.0), ("rtk", "rope", 0.125), ("rtv", "tm", None), ("rtg", "fm", "silu"),
] + [(f"gate{i}", "gate", i) for i in range(16)]


def alloc_proj_scratch(K, external=False):
    P = {}

    def mk(name, shape, dt):
        if external:
            return (K.inp(name, shape, dt), Buf(name))
        return K.dram(name, shape, dt)
    for name, kind, _ in BLOCKS:
        if kind == "tm":
            P[name] = mk("P_" + name, [T, 256], BF16)
        elif kind == "fm32":
            P[name] = mk("P_" + name, [256, T], F32)
        elif kind == "gate":
            continue
        else:
            P[name] = mk("P_" + name, [256, T], BF16)
    P["gate"] = mk("P_gate", [4096, T], BF16)
    return P


def phase1b(K, li, hT, hT_b, P):
    nc, S = K.nc, K.S
    w_in = K.inp("w_in", [DEPTH, D, D_IN])
    ropec = K.inp("rope_cos", [128, T], F32)
    ropes = K.inp("rope_sin", [128, T], F32)
    wv = w_in[li].rearrange("(k p) c -> p k c", p=128)
    with phase(K):
        wst = Rot(K, "wst", [128, 8, 256], F32, 2)
        wbf = Rot(K, "wbf", [128, 8, 256], BF16, 2)
        wsw = K.sb("wsw", [128, 8, 256], BF16); wsw_b = Buf("wsw")
        cosT = K.sb("cosT", [128, T]); sinT = K.sb("sinT", [128, T]); rope_b = Buf("rope")
        ob16 = Rot(K, "ob16", [128, 512], BF16, 4)
        of32 = Rot(K, "of32", [128, 512], F32, 3)
        tmp = Rot(K, "rtmp", [128, 512], F32, 4)
        S.dma("sp", cosT[:], ropec, rope_b)
        S.dma("sp", sinT[:], ropes, rope_b)
        nblk = len(BLOCKS)

        def load_w(cb):
            ws, wsb = wst.get()
            S.dma("sp", ws[:], wv[:, :, cb * 256:(cb + 1) * 256], wsb)
            return ws, wsb
        nxt = load_w(0)
        for cb in range(nblk):
            ws, wsb = nxt
            wb, wbb = wbf.get()
            S.op("pool", lambda h: h.tensor_copy(out=wb[:, 0:4, :], in_=ws[:, 0:4, :]), r=[wsb], w=[wbb])
            S.op("dve", lambda h: h.tensor_copy(out=wb[:, 4:8, :], in_=ws[:, 4:8, :]), r=[wsb], w=[wbb])
            if cb + 1 < nblk:
                nxt = load_w(cb + 1)
            name, kind, post = BLOCKS[cb]
            if kind == "tm":
                dst, dstb = P[name]
                for t in range(NT):
                    ps, psb = K.bank()
                    for k in range(8):
                        S.op("pe", lambda h: h.matmul(ps[:, 0:256], lhsT=hT[:, k, t * 128:(t + 1) * 128], rhs=wb[:, k, :],
                                                      start=(k == 0), stop=(k == 7)), r=[hT_b, wbb], w=[psb])
                    o, ob = ob16.get()
                    evac(K, K.ev(), o[:, 0:256], ps[:, 0:256], r=[psb], w=[ob])
                    S.dma("sp", dst[t * 128:(t + 1) * 128, :], o[:, 0:256], ob, dr=dstb, load=False)
                continue
            if kind == "rope":
                v_in = wb[:].rearrange("p k (h two i) -> p k h two i", h=4, two=2)
                v_out = wsw[:].rearrange("p k (h two i) -> p k h two i", h=4, two=2)
                for k in range(8):
                    S.op("act", lambda h: h.activation(out=v_out[:, k, :, 0, :], in_=v_in[:, k, :, 1, :], func=ACT.Copy, scale=-1.0), r=[wbb], w=[wsw_b])
                    S.op("pool", lambda h: h.tensor_copy(out=v_out[:, k, :, 1, :], in_=v_in[:, k, :, 0, :]), r=[wbb], w=[wsw_b])
            for cc in range(2):
                for (tk0, n) in TB:
                    ps, psb = K.bank()
                    for k in range(8):
                        S.op("pe", lambda h: h.matmul(ps[:, 0:n], lhsT=wb[:, k, cc * 128:(cc + 1) * 128], rhs=hT[:, k, tk0:tk0 + n],
                                                      start=(k == 0), stop=(k == 7)), r=[hT_b, wbb], w=[psb])
                    if kind == "rope":
                        ps2, psb2 = K.bank()
                        for k in range(8):
                            S.op("pe", lambda h: h.matmul(ps2[:, 0:n], lhsT=wsw[:, k, cc * 128:(cc + 1) * 128], rhs=hT[:, k, tk0:tk0 + n],
                                                          start=(k == 0), stop=(k == 7)), r=[hT_b, wsw_b], w=[psb2])
                        t1, t1b = tmp.get()
                        t2, t2b = tmp.get()
                        S.op("dve", lambda h: h.tensor_tensor(out=t1[:, 0:n], in0=ps[:, 0:n], in1=cosT[:, tk0:tk0 + n], op=ALU.mult), r=[psb, rope_b], w=[t1b])
                        S.op("dve", lambda h: h.tensor_tensor(out=t2[:, 0:n], in0=ps2[:, 0:n], in1=sinT[:, tk0:tk0 + n], op=ALU.mult), r=[psb2, rope_b], w=[t2b])
                        o, ob = ob16.get()
                        if post == 1.0:
                            S.op("pool", lambda h: h.tensor_tensor(out=o[:, 0:n], in0=t1[:, 0:n], in1=t2[:, 0:n], op=ALU.add), r=[t1b, t2b], w=[ob])
                        else:
                            S.op("pool", lambda h: h.tensor_tensor(out=t1[:, 0:n], in0=t1[:, 0:n], in1=t2[:, 0:n], op=ALU.add), r=[t1b, t2b], w=[t1b])
                            S.op("act", lambda h: h.activation(out=o[:, 0:n], in_=t1[:, 0:n], func=ACT.Copy, scale=float(post)), r=[t1b], w=[ob])
                        dst, dstb = P[name]
                        S.dma("sp", dst[cc * 128:(cc + 1) * 128, tk0:tk0 + n], o[:, 0:n], ob, dr=dstb, load=False)
                    elif kind == "fm32":
                        o, ob = of32.get()
                        evac(K, K.ev(), o[:, 0:n], ps[:, 0:n], r=[psb], w=[ob])
                        dst, dstb = P[name]
                        S.dma("sp", dst[cc * 128:(cc + 1) * 128, tk0:tk0 + n], o[:, 0:n], ob, dr=dstb, load=False)
                    else:
                        o, ob = ob16.get()
                        if kind == "gate":
                            evac(K, "act", o[:, 0:n], ps[:, 0:n], r=[psb], w=[ob], func=ACT.Sigmoid)
                            dst, dstb = P["gate"]
                            r0 = post * 256 + cc * 128
                        else:
                            if post == "silu":
                                evac(K, "act", o[:, 0:n], ps[:, 0:n], r=[psb], w=[ob], func=ACT.Silu)
                            else:
                                evac(K, K.ev(), o[:, 0:n], ps[:, 0:n], r=[psb], w=[ob])
                            dst, dstb = P[name]
                            r0 = cc * 128
                        S.dma("sp", dst[r0:r0 + 128, tk0:tk0 + n], o[:, 0:n], ob, dr=dstb, load=False)


def load_fm(K, tile, tb, src, srcb, nchunks=2):
    for cc in range(nchunks):
        K.S.dma("sp", tile[:, cc, :], src[cc * 128:(cc + 1) * 128, :], tb, dr=srcb)


def head_norm_finish(K, po, pob, n, gt, gtb, out, outb, sq_rot, rs_rot, wcol=None, wb=None):
    S = K.S
    sq, sqb = sq_rot.get()
    sqh = sq[:].bitcast(BF16)
    S.op("act", lambda h: h.activation(out=sqh[0:64, 0:n], in_=po[0:64, 0:n], func=ACT.Square), r=[pob], w=[sqb])
    pss, pssb = K.bank()
    S.op("pe", lambda h: h.matmul(pss[0:64, 0:n], lhsT=K.onesb[0:64, 0:64], rhs=sqh[0:64, 0:n], start=True, stop=True), r=[sqb, K.onesb_b], w=[pssb])
    rs, rsb = rs_rot.get()
    S.op("act", lambda h: h.activation(out=rs[0:64, 0:n], in_=pss[0:64, 0:n], func=ACT.Sqrt, scale=1.0 / 64, bias=K.epsc[0:64, 0:1]), r=[pssb, K.epsc_b], w=[rsb])
    S.op("dve", lambda h: h.reciprocal(out=rs[0:64, 0:n], in_=rs[0:64, 0:n]), r=[rsb], w=[rsb])
    if wcol is None:
        S.op("dve", lambda h: h.tensor_tensor(out=sq[0:64, 0:n], in0=po[0:64, 0:n], in1=rs[0:64, 0:n], op=ALU.mult), r=[pob, rsb], w=[sqb])
    else:
        S.op("dve", lambda h: h.scalar_tensor_tensor(out=sq[0:64, 0:n], in0=po[0:64, 0:n], scalar=wcol, in1=rs[0:64, 0:n], op0=ALU.mult, op1=ALU.mult),
             r=[pob, rsb], w=[sqb], rs=[wb])
    S.op("pool", lambda h: h.tensor_tensor(out=out, in0=sq[0:64, 0:n], in1=gt, op=ALU.mult), r=[sqb, gtb], w=[outb])


def mm_k64(K, ps, psb, c0, n, lhsT, rhs, r0, rb, start=True, stop=True):
    S = K.S
    if r0 == 0:
        S.op("pe", lambda h: h.matmul(ps[:, c0:c0 + n], lhsT=lhsT, rhs=rhs, start=start, stop=stop), r=rb, w=[psb])
    else:
        for hf in range(2):
            S.op("pe", lambda h: h.matmul(ps[hf * 64:(hf + 1) * 64, c0:c0 + n], lhsT=lhsT[:, hf * 64:(hf + 1) * 64], rhs=rhs, start=start, stop=stop), r=rb, w=[psb])


def load_tm(K, tile, tb, src, srcb, c0=0, cw=256):
    v = src.rearrange("(n p) c -> p n c", p=128)
    for n0 in range(0, NT, 6):
        n1 = min(NT, n0 + 6)
        K.S.dma("sp", tile[:, n0:n1, :], v[:, n0:n1, c0:c0 + cw], tb, dr=srcb)


def hv(dr):
    return dr.rearrange("(h v) t -> v h t", v=64)


def phase_ret(K, li, P, O):
    nc, S = K.nc, K.S
    dlin = K.inp("ret_bc", [DEPTH, 128, 8])
    cE0 = K.inp("c_epos0", [128, 128]); cE1 = K.inp("c_epos1", [128, 128])
    cM0 = K.inp("c_mk0", [128, 128]); cM1 = K.inp("c_mk1", [128, 128])
    cI1 = K.inp("c_iota1", [128, 128]); cIr = K.inp("c_iotar", [128, 128])
    cPc = K.inp("c_pcol", [128, 2])
    Od, Odb = O
    with phase(K):
        cst = K.sb("rt_cst", [128, 6, 128]); cst_b = Buf("rt_cst")
        for i, c in enumerate((cE0, cE1, cM0, cM1, cI1, cIr)):
            S.dma("sp", cst[:, i, :], c, cst_b)
        pcol = K.sb("rt_pcol", [128, 2]); pcol_b = Buf("rt_pcol")
        S.dma("sp", pcol[:], cPc, pcol_b)
        dl = K.sb("rt_dl", [128, 8]); dl_b = Buf("rt_dl")
        S.dma("sp", dl[:], dlin[li], dl_b)
        y = K.sb("rt_y", [128, 8]); ta = K.sb("rt_ta", [128, 8]); lg = K.sb("rt_lg", [128, 8]); lg_b = Buf("rt_lg")
        S.op("act", lambda h: h.activation(out=y[:], in_=dl[:], func=ACT.Exp, scale=-1.0), r=[dl_b], w=[lg_b])
        S.op("dve", lambda h: h.tensor_scalar(out=ta[:], in0=y[:], scalar1=-0.25, scalar2=1.0 / 3.0, op0=ALU.mult, op1=ALU.add), r=[lg_b], w=[lg_b])
        S.op("dve", lambda h: h.tensor_tensor(out=ta[:], in0=ta[:], in1=y[:], op=ALU.mult), r=[lg_b], w=[lg_b])
        S.op("dve", lambda h: h.tensor_scalar(out=ta[:], in0=ta[:], scalar1=-0.5, scalar2=None, op0=ALU.add), r=[lg_b], w=[lg_b])
        S.op("dve", lambda h: h.tensor_tensor(out=ta[:], in0=ta[:], in1=y[:], op=ALU.mult), r=[lg_b], w=[lg_b])
        S.op("dve", lambda h: h.tensor_scalar(out=ta[:], in0=ta[:], scalar1=1.0, scalar2=None, op0=ALU.add), r=[lg_b], w=[lg_b])
        S.op("dve", lambda h: h.tensor_tensor(out=ta[:], in0=ta[:], in1=y[:], op=ALU.mult), r=[lg_b], w=[lg_b])
        S.op("dve", lambda h: h.tensor_scalar(out=lg[:], in0=ta[:], scalar1=-1.0, scalar2=None, op0=ALU.mult), r=[lg_b], w=[lg_b])
        lgp = K.sb("rt_lgp", [128, 2, 2]); lgp_b = Buf("rt_lgp")
        for cc in range(2):
            for d in range(2):
                for hp in range(2):
                    col = d * 4 + 2 * cc + hp
                    S.op("dve", lambda h: h.tensor_copy(out=lgp[hp * 64:(hp + 1) * 64, cc, d:d + 1], in_=lg[hp * 64:(hp + 1) * 64, col:col + 1]), r=[lg_b], w=[lgp_b])
        M = K.sb("rt_M", [128, 4, 128]); M_b = Buf("rt_M")
        e0 = K.sb("rt_e0", [128, 128]); e1 = K.sb("rt_e1", [128, 128]); e_b = Buf("rt_e")
        for hh in range(4):
            S.op("act", lambda h: h.activation(out=e0[:], in_=cst[:, 0, :], func=ACT.Exp, scale=lg[:, hh:hh + 1]), r=[cst_b], w=[e_b], rs=[lg_b])
            S.op("act", lambda h: h.activation(out=e1[:], in_=cst[:, 1, :], func=ACT.Exp, scale=lg[:, 4 + hh:5 + hh]), r=[cst_b], w=[e_b], rs=[lg_b])
            S.op("dve", lambda h: h.tensor_tensor(out=e0[:], in0=e0[:], in1=cst[:, 2, :], op=ALU.mult), r=[e_b, cst_b], w=[e_b])
            S.op("dve", lambda h: h.tensor_tensor(out=e1[:], in0=e1[:], in1=cst[:, 3, :], op=ALU.mult), r=[e_b, cst_b], w=[e_b])
            S.op("dve", lambda h: h.tensor_tensor(out=M[:, hh, :], in0=e0[:], in1=e1[:], op=ALU.add), r=[e_b], w=[M_b])
        XI = K.sb("rt_XI", [128, 2, 2, 128]); XI_b = Buf("rt_XI")
        for d in range(2):
            for cc in range(2):
                S.op("act", lambda h: h.activation(out=XI[:, d, cc, :], in_=cst[:, 4 + d, :], func=ACT.Exp, scale=lgp[:, cc, d:d + 1]), r=[cst_b], w=[XI_b], rs=[lgp_b])
        ZT = K.sb("rt_ZT", [128, 2, 4]); ZT_b = Buf("rt_ZT")
        for d in range(2):
            S.op("act", lambda h: h.activation(out=ZT[:, d, :], in_=lg[:, d * 4:(d + 1) * 4], func=ACT.Exp, scale=pcol[:, d:d + 1]), r=[lg_b, pcol_b], w=[ZT_b], rs=[pcol_b])
        G128 = K.sb("rt_G", [128, 2, 2]); G_b = Buf("rt_G")
        S.op("act", lambda h: h.activation(out=G128[:], in_=lgp[:], func=ACT.Exp, scale=128.0), r=[lgp_b], w=[G_b])
        qT = K.sb("rt_qT", [128, 2, T], BF16); qT_b = Buf("rt_qT")
        kT = K.sb("rt_kT", [128, 2, T], BF16); kT_b = Buf("rt_kT")
        load_fm(K, qT, qT_b, *P["rtq"])
        load_fm(K, kT, kT_b, *P["rtk"])
        vall = K.sb("rt_v", [128, NT, 256], BF16); v_b = Buf("rt_v")
        load_tm(K, vall, v_b, *P["rtv"])
        qx = K.sb("rt_qx", [128, 2, 2, T], BF16); qx_b = Buf("rt_qx")
        for d in range(2):
            for cc in range(2):
                eng = "dve" if cc == 0 else "pool"
                S.op(eng, lambda h: h.tensor_tensor(out=qx[:, d, cc, :].rearrange("p (n a) -> p n a", a=128),
                                                    in0=qT[:, cc, :].rearrange("p (n a) -> p n a", a=128),
                                                    in1=XI[:, d, cc, :].unsqueeze(1).to_broadcast([128, NT, 128]), op=ALU.mult),
                     r=[qT_b, XI_b], w=[qx_b])
        kz = K.sb("rt_kz", [128, 2, NT, 256], BF16); kz_b = Buf("rt_kz")
        for t in range(NT):
            ps, psb = K.bank()
            pv = ps[:].bitcast(BF16)
            for cc in range(2):
                S.op("pe", lambda h: h.transpose(out=pv[:, cc * 128:(cc + 1) * 128], in_=kT[:, cc, t * 128:(t + 1) * 128], identity=K.identb[:]),
                     r=[kT_b, K.identb_b], w=[psb])
            for d in range(2):
                eng = "dve" if d == 0 else "pool"
                if eng == "pool":
                    eng = "dve"
                S.op(eng, lambda h: h.tensor_tensor(out=kz[:, d, t, :].rearrange("p (h k) -> p h k", h=4),
                                                    in0=pv[:, 0:256].rearrange("p (h k) -> p h k", h=4),
                                                    in1=ZT[:, d, :].unsqueeze(2).to_broadcast([128, 4, 64]), op=ALU.mult),
                     r=[psb, ZT_b], w=[kz_b])
        Sst = K.sb("rt_S", [128, 2, 2, 64]); Sst_b = [Buf("rt_S0"), Buf("rt_S1")]
        Sall = K.sb("rt_Sall", [128, 2, NT, 2, 64], BF16); Sall_b = Buf("rt_Sall")
        S.op("dve", lambda h: h.memset(Sst[:], 0.0), w=Sst_b)
        order = [list(range(NT)), [1, 0] + list(range(NT - 1, 1, -1))]
        for step in range(NT):
            for d in range(2):
                t = order[d][step]
                pk, pkb = K.bank()
                for hh in range(4):
                    cc = hh // 2
                    S.op("pe", lambda h: h.matmul(pk[:, hh * 64:(hh + 1) * 64], lhsT=kz[:, d, t, cc * 128:(cc + 1) * 128], rhs=vall[:, t, hh * 64:(hh + 1) * 64],
                                                  start=True, stop=True), r=[kz_b, v_b], w=[pkb])
                S.op("act", lambda h: h.activation(out=Sall[:, d, t, :, :], in_=Sst[:, d, :, :], func=ACT.Copy), r=[Sst_b[d]], w=[Sall_b])
                S.op("dve", lambda h: h.tensor_tensor(out=Sst[:, d, :, :], in0=Sst[:, d, :, :], in1=G128[:, :, d:d + 1].to_broadcast([128, 2, 64]), op=ALU.mult),
                     r=[Sst_b[d], G_b], w=[Sst_b[d]])
                pkv = pk[:, 0:256].rearrange("p (c q v) -> p c q v", c=2, q=2)
                for hp in range(2):
                    S.op("dve", lambda h: h.tensor_tensor(out=Sst[hp * 64:(hp + 1) * 64, d, :, :], in0=Sst[hp * 64:(hp + 1) * 64, d, :, :],
                                                          in1=pkv[hp * 64:(hp + 1) * 64, :, hp, :], op=ALU.add), r=[Sst_b[d], pkb], w=[Sst_b[d]])
        A_r = Rot(K, "rt_A", [128, 4, 128], BF16, 3)
        sq_r = Rot(K, "rt_sq", [64, 512], F32, 2)
        rs_r = Rot(K, "rt_rs", [64, 512], F32, 2)
        g_r = Rot(K, "rt_g", [64, 4, 128], BF16, 3)
        o_r = Rot(K, "rt_o", [64, 4, 128], BF16, 3)
        gsrc, gsrcb = P["rtg"]
        for t in range(NT):
            tk = slice(t * 128, (t + 1) * 128)
            pa, pab = K.bank()
            for hh in range(4):
                cc, r0 = hh // 2, (hh % 2) * 64
                mm_k64(K, pa, pab, hh * 128, 128, kT[r0:r0 + 64, cc, tk], qT[r0:r0 + 64, cc, tk], r0, [kT_b, qT_b])
            A, Ab = A_r.get()
            S.op("dve", lambda h: h.tensor_tensor(out=A[:], in0=pa[:].rearrange("p (h t) -> p h t", h=4), in1=M[:], op=ALU.mult), r=[pab, M_b], w=[Ab])
            po, pob = K.bank()
            for hh in range(4):
                cc, r0 = hh // 2, (hh % 2) * 64
                oo = po[0:64, hh * 128:(hh + 1) * 128]
                S.op("pe", lambda h: h.matmul(oo, lhsT=vall[:, t, hh * 64:(hh + 1) * 64], rhs=A[:, hh, :], start=True, stop=False), r=[v_b, Ab], w=[pob])
                for d in range(2):
                    S.op("pe", lambda h: h.matmul(oo, lhsT=Sall[r0:r0 + 64, d, t, cc, :], rhs=qx[r0:r0 + 64, d, cc, tk], start=False, stop=(d == 1)),
                         r=[Sall_b, qx_b], w=[pob])
            g, gb = g_r.get()
            S.dma("sp", g[:], hv(gsrc)[:, :, tk], gb, dr=gsrcb)
            o, ob = o_r.get()
            head_norm_finish(K, po, pob, 512, g[:].rearrange("p h t -> p (h t)"), gb, o[:].rearrange("p h t -> p (h t)"), ob, sq_r, rs_r)
            S.dma("sp", hv(Od)[:, :, tk], o[:], ob, dr=Odb, load=False)


def _band_start(r):
    return min(max(r - 4, 0), 56)


def phase_na(K, li, P, O):
    nc, S = K.nc, K.S
    rpb = K.inp("na_rpbT2", [DEPTH, 128, 4, 15, 64])
    nmask = K.inp("c_namask", [128, 64])
    nw = K.inp("na_qk_normT", [DEPTH, 128, 2])
    blk64 = K.inp("c_blk64", [128, 128])
    Od, Odb = O
    NEG = -240000.0
    with phase(K):
        qT = K.sb("na_qT", [128, 2, T], BF16); qT_b = Buf("na_qT")
        kT = K.sb("na_kT", [128, 2, T], BF16); kT_b = Buf("na_kT")
        load_fm(K, qT, qT_b, *P["naq"])
        load_fm(K, kT, kT_b, *P["nak"])
        vall = K.sb("na_v", [128, NT, 256], BF16); v_b = Buf("na_v")
        load_tm(K, vall, v_b, *P["nav"])
        wq = K.sb("na_w", [128, 2]); wq_b = Buf("na_w")
        S.dma("sp", wq[:], nw[li], wq_b)
        b64 = K.sb("na_b64", [128, 128]); b64_b = Buf("na_b64")
        S.dma("sp", b64[:], blk64, b64_b)
        sq_r = Rot(K, "na_sq", [128, 512], F32, 2)
        rs_r = Rot(K, "na_rs", [128, 512], F32, 2)
        for wi, (X, Xb) in enumerate(((qT, qT_b), (kT, kT_b))):
            for cc in range(2):
                for (t0, n) in TB:
                    xs = X[:, cc, t0:t0 + n]
                    sq, sqb = sq_r.get()
                    S.op("act", lambda h: h.activation(out=sq[:, 0:n], in_=xs, func=ACT.Square), r=[Xb], w=[sqb])
                    ps, psb = K.bank()
                    S.op("pe", lambda h: h.matmul(ps[:, 0:n], lhsT=b64[:], rhs=sq[:, 0:n], start=True, stop=True), r=[b64_b, sqb], w=[psb])
                    rs, rsb = rs_r.get()
                    S.op("act", lambda h: h.activation(out=rs[:, 0:n], in_=ps[:, 0:n], func=ACT.Sqrt, scale=1.0 / 64, bias=K.epsc[:, 0:1]), r=[psb, K.epsc_b], w=[rsb])
                    S.op("dve", lambda h: h.reciprocal(out=rs[:, 0:n], in_=rs[:, 0:n]), r=[rsb], w=[rsb])
                    S.op("dve", lambda h: h.scalar_tensor_tensor(out=xs, in0=xs, scalar=wq[:, wi:wi + 1], in1=rs[:, 0:n], op0=ALU.mult, op1=ALU.mult),
                         r=[Xb, rsb], w=[Xb], rs=[wq_b])
        rp = K.sb("na_rp", [128, 4, 15, 64]); rp_b = Buf("na_rp")
        S.dma("sp", rp[:], rpb[li], rp_b)
        mk = K.sb("na_mk", [128, 64]); mk_b = Buf("na_mk")
        S.dma("sp", mk[:], nmask, mk_b)
        Bt = K.sb("na_Bt", [128, 4, 15, 64], BF16); Bt_b = Buf("na_Bt")
        S.op("dve", lambda h: h.scalar_tensor_tensor(out=Bt[:].rearrange("p h r q -> p (h r) q"), in0=rp[:].rearrange("p h r q -> p (h r) q"), scalar=8.0,
                                                     in1=mk[:].unsqueeze(1).to_broadcast([128, 60, 64]), op0=ALU.mult, op1=ALU.add),
             r=[rp_b, mk_b], w=[Bt_b])
        NPAT = 30
        pat_t = K.sb("na_pat", [128, NPAT, 4, 128], BF16)
        pat_b = [Buf(f"na_pat{i}") for i in range(NPAT)]
        pats = {}

        def bias_tile(tq, m):
            key = []
            for kr in range(2):
                for qr in range(2):
                    krow, qrow = 2 * m + kr, 2 * tq + qr
                    bs = _band_start(qrow)
                    key.append(krow - qrow if bs <= krow < bs + 8 else None)
            key = tuple(key)
            if key not in pats:
                idx = len(pats)
                assert idx < NPAT
                pats[key] = idx
                i = 0
                for kr in range(2):
                    for qr in range(2):
                        dr = key[i]; i += 1
                        dst = pat_t[kr * 64:(kr + 1) * 64, idx, :, qr * 64:(qr + 1) * 64]
                        if dr is None:
                            S.op("pool", lambda h: h.memset(dst, NEG), w=[pat_b[idx]])
                        else:
                            S.op("pool", lambda h: h.tensor_copy(out=dst, in_=Bt[kr * 64:(kr + 1) * 64, :, dr + 7, :]), r=[Bt_b], w=[pat_b[idx]])
            idx = pats[key]
            return pat_t[:, idx, :, :], pat_b[idx]

        ones64 = K.sb("na_ones", [128, 64], BF16); ones64_b = Buf("na_ones")
        S.op("dve", lambda h: h.memset(ones64[:], 1.0), w=[ones64_b])
        pm_r = Rot(K, "na_pm", [128, 7, 512], BF16, 2)
        rd_r = Rot(K, "na_rd", [64, 512], F32, 2)
        o_r = Rot(K, "na_o", [64, 4, 128], BF16, 3)
        for tt in range(NT):
            tk = slice(tt * 128, (tt + 1) * 128)
            if tt < 2:
                keys = [(0, None), (1, None)]
            else:
                tq = tt - 2
                m0 = _band_start(2 * tq) // 2
                m1 = (_band_start(2 * tq + 1) + 7) // 2
                keys = [(2 + m, m) for m in range(m0, m1 + 1)] + [(0, None), (1, None)]
            pm, pmb = pm_r.get()
            for ki, (kt, m) in enumerate(keys):
                kk = slice(kt * 128, (kt + 1) * 128)
                ps, psb = K.bank()
                if m is not None:
                    bt, btb = bias_tile(tt - 2, m)
                for hh in range(4):
                    cc, r0 = hh // 2, (hh % 2) * 64
                    mm_k64(K, ps, psb, hh * 128, 128, kT[r0:r0 + 64, cc, kk], qT[r0:r0 + 64, cc, tk], r0, [kT_b, qT_b], start=True, stop=(m is None))
                    if m is not None:
                        S.op("pe", lambda h: h.matmul(ps[:, hh * 128:(hh + 1) * 128], lhsT=K.identb[:], rhs=bt[:, hh, :], start=False, stop=True),
                             r=[K.identb_b, btb], w=[psb])
                S.op("act", lambda h: h.activation(out=pm[:, ki, :], in_=ps[:], func=ACT.Exp, scale=0.125), r=[psb], w=[pmb])
            nk = len(keys)
            po, pob = K.bank()
            for hh in range(4):
                for ki, (kt, m) in enumerate(keys):
                    S.op("pe", lambda h: h.matmul(po[0:64, hh * 128:(hh + 1) * 128], lhsT=vall[:, kt, hh * 64:(hh + 1) * 64], rhs=pm[:, ki, hh * 128:(hh + 1) * 128],
                                                  start=(ki == 0), stop=(ki == nk - 1)), r=[v_b, pmb], w=[pob])
            pd, pdb = K.bank()
            for ki in range(nk):
                S.op("pe", lambda h: h.matmul(pd[0:64, :], lhsT=ones64[:], rhs=pm[:, ki, :], start=(ki == 0), stop=(ki == nk - 1)), r=[ones64_b, pmb], w=[pdb])
            rd, rdb = rd_r.get()
            S.op("dve", lambda h: h.reciprocal(out=rd[:], in_=pd[0:64, :]), r=[pdb], w=[rdb])
            o, ob = o_r.get()
            S.op("dve", lambda h: h.tensor_tensor(out=o[:].rearrange("p h t -> p (h t)"), in0=po[0:64, :], in1=rd[:], op=ALU.mult), r=[pob, rdb], w=[ob])
            S.dma("sp", hv(Od)[:, :, tk], o[:], ob, dr=Odb, load=False)


NCH = T // 16
HB = 256


def _hg_spos(d, n):
    if d == 0:
        return n
    return 15 - n if n < 16 else 16 + (271 - n)


def phase_hg(K, li, P, O):
    nc, S = K.nc, K.S
    lbin = K.inp("hg_lbT", [128, 2, 2])
    nwin = K.inp("hg_norm_wT", [DEPTH, 64, 1])
    crst = K.inp("c_rst", [128, HB])
    cm = [K.inp("c_hgm0", [128, 128]), K.inp("c_hgm1", [128, 128])]
    cbm = K.inp("c_blkm", [128, 2, 8])
    Od, Odb = O
    NC16 = HB // 16
    with phase(K):
        lbr = K.sb("hg_lbr", [128, 2, 2]); lb_b = Buf("hg_lb")
        S.dma("sp", lbr[:], lbin, lb_b)
        lb = K.sb("hg_lbv", [128, 2]); oml = K.sb("hg_oml", [128, 2])
        if li == 0:
            S.op("dve", lambda h: h.memset(lb[:], 0.0), w=[lb_b])
        else:
            S.op("dve", lambda h: h.tensor_tensor(out=lb[:], in0=lbr[:, 1, :], in1=lbr[:, 0, :], op=ALU.subtract), r=[lb_b], w=[lb_b])
            S.op("act", lambda h: h.activation(out=lb[:], in_=lb[:], func=ACT.Sigmoid), r=[lb_b], w=[lb_b])
        S.op("dve", lambda h: h.tensor_scalar(out=oml[:], in0=lb[:], scalar1=-1.0, scalar2=1.0, op0=ALU.mult, op1=ALU.add), r=[lb_b], w=[lb_b])
        nwt = K.sb("hg_nw", [64, 1]); nw_b = Buf("hg_nw")
        S.dma("sp", nwt[:], nwin[li], nw_b)
        rst = K.sb("hg_rst", [128, HB]); rst_b = Buf("hg_rst")
        S.dma("sp", rst[:], crst, rst_b)
        msk = K.sb("hg_msk", [128, 2, 128]); msk_b = Buf("hg_msk")
        for d in range(2):
            S.dma("sp", msk[:, d, :], cm[d], msk_b)
        bm = K.sb("hg_bm", [128, 2, 8]); bm_b = Buf("hg_bm")
        S.dma("sp", bm[:], cbm, bm_b)
        vall = K.sb("hg_v", [128, NT, 128], BF16); v_b = Buf("hg_v")
        qd = K.sb("hg_qd", [128, T], BF16); kd = K.sb("hg_kd", [128, T], BF16)
        qd_b, kd_b = Buf("hg_qd"), Buf("hg_kd")
        dec = K.sb("hg_dec", [128, NCH]); dec_b = Buf("hg_dec")
        decs = K.sb("hg_decs", [128, NCH]); decs_b = Buf("hg_decs")
        kvb = K.sb("hg_kvb", [128, NCH, 64], BF16); kvb_b = Buf("hg_kvb")
        Sb = K.sb("hg_Sb", [128, NCH, 64], BF16); Sb_b = Buf("hg_Sb")
        Oacc = K.sb("hg_Oacc", [64, 2, T]); Oacc_b = Buf("hg_Oacc")
        fl_r = Rot(K, "hg_fl", [128, HB], F32, 3)
        qs_r = Rot(K, "hg_qs", [128, HB], BF16, 3)
        ex_r = Rot(K, "hg_ex", [128, HB], F32, 6)
        ke_r = Rot(K, "hg_ke", [128, HB], BF16, 3)
        tA_r = Rot(K, "hg_tA", [128, HB], F32, 3); tB_r = Rot(K, "hg_tB", [128, HB], F32, 3)
        tC_r = Rot(K, "hg_tC", [128, HB], F32, 3); tD_r = Rot(K, "hg_tD", [128, HB], F32, 3)
        keT_r = Rot(K, "hg_keT", [128, 128], BF16, 3)
        vb_r = Rot(K, "hg_vb", [128, 2, 8, 64], BF16, 3)
        A_r = Rot(K, "hg_A", [128, 2, 128], BF16, 3)
        sq_r = Rot(K, "hg_sq", [64, 512], F32, 2)
        rs_r = Rot(K, "hg_rs", [64, 512], F32, 2)
        g_r = Rot(K, "hg_g", [64, 2, 256], BF16, 2)
        o_r = Rot(K, "hg_o", [64, 2, 256], BF16, 2)
        fsrc = [P["hgff"], P["hgfb"]]
        c3 = lambda ap: ap.rearrange("p (n c) -> p n c", c=16)
        for cc in range(2):
            load_tm(K, vall, v_b, *P["hgi"], c0=cc * 128, cw=128)
            for d in range(2):
                for bi in range(T // HB):
                    tb = slice(bi * HB, (bi + 1) * HB)
                    cb = slice(bi * NC16, (bi + 1) * NC16)
                    fl, flb = fl_r.get()
                    S.dma("sp", fl[:], fsrc[d][0][cc * 128:(cc + 1) * 128, tb], flb, dr=fsrc[d][1])
                    qs, qsb = qs_r.get()
                    S.dma("sp", qs[:], P["hgq"][0][cc * 128:(cc + 1) * 128, tb], qsb, dr=P["hgq"][1])
                    tA, tAb = tA_r.get(); tB, tBb = tB_r.get(); tC, tCb = tC_r.get(); tD, tDb = tD_r.get()
                    S.op("act", lambda h: h.activation(out=tA[:], in_=fl[:], func=ACT.Sigmoid), r=[flb], w=[tAb])
                    S.op("dve", lambda h: h.tensor_scalar(out=tA[:], in0=tA[:], scalar1=oml[:, cc:cc + 1], scalar2=lb[:, cc:cc + 1], op0=ALU.mult, op1=ALU.add),
                         r=[tAb], w=[tAb], rs=[lb_b])
                    S.op("act", lambda h: h.activation(out=tB[:], in_=tA[:], func=ACT.Ln), r=[tAb], w=[tBb])
                    S.op("pool", lambda h: h.tensor_scalar(out=tC[:], in0=tA[:], scalar1=-1.0, scalar2=1.0, op0=ALU.mult, op1=ALU.add), r=[tAb], w=[tCb])
                    S.op("dve", lambda h: h.tensor_tensor_scan(out=tA[:], data0=rst[:], data1=tB[:], initial=0.0, op0=ALU.mult, op1=ALU.add),
                         r=[tBb, rst_b], w=[tAb])
                    Pv = c3(tA[:])
                    totb = Pv[:, :, 15:16].to_broadcast([128, NC16, 16])
                    if d == 0:
                        Eq, Eqb = tA, tAb
                        S.op("dve", lambda h: h.tensor_tensor(out=c3(tD[:]), in0=totb, in1=Pv, op=ALU.subtract), r=[tAb], w=[tDb])
                    else:
                        S.op("dve", lambda h: h.tensor_tensor(out=tD[:], in0=tA[:], in1=tB[:], op=ALU.subtract), r=[tAb, tBb], w=[tDb])
                        S.op("dve", lambda h: h.tensor_tensor(out=c3(tB[:]), in0=totb, in1=c3(tD[:]), op=ALU.subtract), r=[tAb, tDb], w=[tBb])
                        Eq, Eqb = tB, tBb
                    S.op("act", lambda h: h.activation(out=dec[:, cb], in_=Pv[:, :, 15], func=ACT.Exp), r=[tAb], w=[dec_b])
                    e1, e1b = ex_r.get()
                    S.op("act", lambda h: h.activation(out=e1[:], in_=Eq[:], func=ACT.Exp), r=[Eqb], w=[e1b])
                    S.op("dve", lambda h: h.tensor_tensor(out=qd[:, tb], in0=qs[:], in1=e1[:], op=ALU.mult), r=[qsb, e1b], w=[qd_b])
                    e2, e2b = ex_r.get()
                    S.op("act", lambda h: h.activation(out=e2[:], in_=Eq[:], func=ACT.Exp, scale=-1.0), r=[Eqb], w=[e2b])
                    S.op("pool", lambda h: h.tensor_tensor(out=kd[:, tb], in0=tC[:], in1=e2[:], op=ALU.mult), r=[tCb, e2b], w=[kd_b])
                    e3, e3b = ex_r.get()
                    S.op("act", lambda h: h.activation(out=e3[:], in_=tD[:], func=ACT.Exp), r=[tDb], w=[e3b])
                    ke, keb = ke_r.get()
                    S.op("dve", lambda h: h.tensor_tensor(out=ke[:], in0=tC[:], in1=e3[:], op=ALU.mult), r=[tCb, e3b], w=[keb])
                    for tl in range(HB // 128):
                        t = bi * (HB // 128) + tl
                        ps, psb = K.bank()
                        pv = ps[:].bitcast(BF16)
                        S.op("pe", lambda h: h.transpose(out=pv[:, 0:128], in_=ke[:, tl * 128:(tl + 1) * 128], identity=K.identb[:]), r=[keb, K.identb_b], w=[psb])
                        keT, keTb = keT_r.get()
                        S.op("act", lambda h: h.activation(out=keT[:], in_=pv[:, 0:128], func=ACT.Copy), r=[psb], w=[keTb])
                        vb, vbb = vb_r.get()
                        S.op("pool", lambda h: h.tensor_tensor(out=vb[:], in0=vall[:, t, :].rearrange("p (h v) -> p h v", h=2).unsqueeze(2).to_broadcast([128, 2, 8, 64]),
                                                               in1=bm[:, d, :].unsqueeze(1).unsqueeze(3).to_broadcast([128, 2, 8, 64]), op=ALU.mult), r=[v_b, bm_b], w=[vbb])
                        if d == 0:
                            s0 = 8 * t
                        else:
                            s0 = 8 * (1 - t) if t < 2 else 280 - 8 * t
                        for hp in range(2):
                            pk, pkb = K.bank()
                            S.op("pe", lambda h: h.matmul(pk[:], lhsT=keT[:], rhs=vb[:, hp, :, :].rearrange("p c v -> p (c v)"), start=True, stop=True),
                                 r=[keTb, vbb], w=[pkb])
                            evac(K, K.ev(), kvb[hp * 64:(hp + 1) * 64, s0:s0 + 8, :], pk[hp * 64:(hp + 1) * 64, :].rearrange("p (c v) -> p c v", c=8), r=[pkb], w=[kvb_b])
                if d == 0:
                    S.op("dve", lambda h: h.tensor_copy(out=decs[:], in_=dec[:]), r=[dec_b], w=[decs_b])
                else:
                    da = dec[:]
                    r1 = bass.AP(da.tensor, da.offset + 15, [list(da.ap[0]), [-1, 16]])
                    r2 = bass.AP(da.tensor, da.offset + 271, [list(da.ap[0]), [-1, 256]])
                    S.op("dve", lambda h: h.tensor_copy(out=decs[:, 0:16], in_=r1), r=[dec_b], w=[decs_b])
                    S.op("dve", lambda h: h.tensor_copy(out=decs[:, 16:272], in_=r2), r=[dec_b], w=[decs_b])
                for v in range(64):
                    S.op("dve", lambda h: h.tensor_tensor_scan(out=Sb[:, :, v], data0=decs[:], data1=kvb[:, :, v], initial=0.0, op0=ALU.mult, op1=ALU.add),
                         r=[decs_b, kvb_b], w=[Sb_b])
                for t in range(NT):
                    tk = slice(t * 128, (t + 1) * 128)
                    pa, pab = K.bank()
                    for hp in range(2):
                        r0 = hp * 64
                        mm_k64(K, pa, pab, hp * 128, 128, kd[r0:r0 + 64, tk], qd[r0:r0 + 64, tk], r0, [kd_b, qd_b])
                    A, Ab = A_r.get()
                    S.op("dve", lambda h: h.tensor_tensor(out=A[:], in0=pa[:, 0:256].rearrange("p (h t) -> p h t", h=2),
                                                          in1=msk[:, d, :].unsqueeze(1).to_broadcast([128, 2, 128]), op=ALU.mult), r=[pab, msk_b], w=[Ab])
                    po, pob = K.bank()
                    for hp in range(2):
                        r0 = hp * 64
                        mm = []
                        for c in range(8):
                            sp = _hg_spos(d, 8 * t + c)
                            if sp >= 1:
                                mm.append((c, sp))
                        S.op("pe", lambda h: h.matmul(po[0:64, hp * 128:(hp + 1) * 128], lhsT=vall[:, t, hp * 64:(hp + 1) * 64], rhs=A[:, hp, :], start=True, stop=(len(mm) == 0)),
                             r=[v_b, Ab], w=[pob])
                        for i, (c, sp) in enumerate(mm):
                            S.op("pe", lambda h: h.matmul(po[0:64, hp * 128 + 16 * c:hp * 128 + 16 * c + 16], lhsT=Sb[r0:r0 + 64, sp - 1, :],
                                                          rhs=qd[r0:r0 + 64, t * 128 + 16 * c:t * 128 + 16 * c + 16], start=False, stop=(i == len(mm) - 1)),
                                 r=[Sb_b, qd_b], w=[pob])
                    ov = Oacc[:, :, tk]
                    pov = po[0:64, 0:256].rearrange("p (h t) -> p h t", h=2)
                    if d == 0:
                        S.op("act", lambda h: h.activation(out=ov, in_=pov, func=ACT.Copy), r=[pob], w=[Oacc_b])
                    else:
                        S.op("dve", lambda h: h.tensor_tensor(out=ov, in0=ov, in1=pov, op=ALU.add), r=[pob, Oacc_b], w=[Oacc_b])
            gsrc, gsrcb = P["hgg"]
            for t2 in range(T // 256):
                tk = slice(t2 * 256, (t2 + 1) * 256)
                g, gb = g_r.get()
                S.dma("sp", g[:], hv(gsrc)[:, 2 * cc:2 * cc + 2, tk], gb, dr=gsrcb)
                o, ob = o_r.get()
                sq, sqb = sq_r.get()
                h2 = lambda ap: ap.rearrange("p (h t) -> p h t", h=2)
                sqh = sq[:].bitcast(BF16)
                S.op("act", lambda h: h.activation(out=h2(sqh[:, 0:512]), in_=Oacc[:, :, tk], func=ACT.Square), r=[Oacc_b], w=[sqb])
                pss, pssb = K.bank()
                S.op("pe", lambda h: h.matmul(pss[0:64, :], lhsT=K.onesb[0:64, 0:64], rhs=sqh[:, 0:512], start=True, stop=True), r=[sqb, K.onesb_b], w=[pssb])
                rs, rsb = rs_r.get()
                S.op("act", lambda h: h.activation(out=rs[:], in_=pss[0:64, :], func=ACT.Sqrt, scale=1.0 / 64, bias=K.epsc[0:64, 0:1]), r=[pssb, K.epsc_b], w=[rsb])
                S.op("dve", lambda h: h.reciprocal(out=rs[:], in_=rs[:]), r=[rsb], w=[rsb])
                S.op("dve", lambda h: h.scalar_tensor_tensor(out=h2(sq[:]), in0=Oacc[:, :, tk], scalar=nwt[:, 0:1], in1=h2(rs[:]), op0=ALU.mult, op1=ALU.mult),
                     r=[Oacc_b, rsb], w=[sqb], rs=[nw_b])
                S.op("pool", lambda h: h.tensor_tensor(out=o[:], in0=h2(sq[:]), in1=g[:], op=ALU.mult), r=[sqb, gb], w=[ob])
                S.dma("sp", hv(Od)[:, 2 * cc:2 * cc + 2, tk], o[:], ob, dr=Odb, load=False)


TWO_PI_LO = 6.28318


def _s5_spos(m):
    return 15 - m if m < 16 else 287 - m


def phase_s5(K, li, P, O):
    nc, S = K.nc, K.S
    lamin = K.inp("s5_lamT", [DEPTH, 128, 2, 32])
    stepin = K.inp("s5_stepT", [DEPTH, 128, 32])
    Bin = K.inp("s5_BT", [DEPTH, 128, 2, 32, 16])
    Cin = K.inp("s5_CT", [DEPTH, 128, 2, 32, 16])
    din = K.inp("s5_dT", [DEPTH, 128, 2])
    gluin = K.inp("s5_glu_w", [DEPTH, 256, 256])
    selin = K.inp("c_sel", [128, 64, 128], BF16)
    selTin = K.inp("c_selT", [128, 64, 128], BF16)
    m01in = [K.inp("c_s5m0", [128, 2, 256]), K.inp("c_s5m1", [128, 2, 256])]
    kin = [K.inp("c_kA", [128, 32]), K.inp("c_kB", [128, 32])]
    sel3in = K.inp("c_sel3", [128, 3])
    Od, Odb = O
    with phase(K):
        Gl = K.sb("s5_Gl", [128, 32, 2, 128], BF16); Gl_b = Buf("s5_Gl")
        Aw = K.sb("s5_Aw", [128, 16, 2, 256], BF16); Aw_b = Buf("s5_Aw")
        Hs = K.sb("s5_H", [128, 2, 16, 256], BF16); Hs_b = Buf("s5_H")
        MUA = K.sb("s5_MUA", [128, 2, 16]); MUB = K.sb("s5_MUB", [128, 2, 16]); MU_b = Buf("s5_MU")
        with phase(K):
            lam = K.sb("s5_lam", [128, 2, 32]); lam_b = Buf("s5_lam")
            stp = K.sb("s5_stp", [128, 32]); stp_b = Buf("s5_stp")
            Bt = K.sb("s5_Bt", [128, 2, 32, 16]); Bt_b = Buf("s5_Bt")
            Ct = K.sb("s5_Ct", [128, 2, 32, 16]); Ct_b = Buf("s5_Ct")
            kk = K.sb("s5_kk", [128, 2, 32]); kk_b = Buf("s5_kk")
            sel3 = K.sb("s5_sel3", [128, 3]); sel3_b = Buf("s5_sel3")
            m01 = K.sb("s5_m01", [128, 2, 2, 256]); m01_b = Buf("s5_m01")
            S.dma("sp", lam[:], lamin[li], lam_b)
            S.dma("sp", stp[:], stepin[li], stp_b)
            S.dma("sp", Bt[:], Bin[li], Bt_b)
            S.dma("sp", Ct[:], Cin[li], Ct_b)
            for i in range(2):
                S.dma("sp", kk[:, i, :], kin[i], kk_b)
                S.dma("sp", m01[:, i, :, :], m01in[i], m01_b)
            S.dma("sp", sel3[:], sel3in, sel3_b)
            sm = K.sb("s5_sm", [128, 12, 32]); sm_b = Buf("s5_sm")
            DT, EA, TH, DEN, AM1, ZR, ZI, T1, T2 = [sm[:, i, :] for i in range(9)]
            lr, lim = lam[:, 0, :], lam[:, 1, :]
            dv = lambda f, r=(), w=(), rs=(): S.op("dve", f, r=list(r) + [sm_b], w=list(w) + [sm_b], rs=rs)
            S.op("act", lambda h: h.activation(out=DT, in_=stp[:], func=ACT.Exp), r=[stp_b], w=[sm_b])
            dv(lambda h: h.tensor_tensor(out=EA, in0=lr, in1=DT, op=ALU.mult), r=[lam_b])
            dv(lambda h: h.tensor_tensor(out=TH, in0=lim, in1=DT, op=ALU.mult), r=[lam_b])
            PW = K.sb("s5_PW", [128, 2, 2, 32, 32]); PW_b = Buf("s5_PW")
            big = K.sb("s5_big", [128, 4, 32, 32]); big_b = Buf("s5_big")
            bigi = K.sb("s5_bigi", [128, 32, 32], I32)
            bg = lambda f, r=(), w=(): S.op("dve", f, r=list(r) + [big_b, sm_b], w=list(w) + [big_b])
            for ab in range(2):
                kb3 = kk[:, ab, :].unsqueeze(1).to_broadcast([128, 32, 32])
                bg(lambda h: h.tensor_tensor(out=big[:, 0], in0=EA.unsqueeze(2).to_broadcast([128, 32, 32]), in1=kb3, op=ALU.mult), r=[kk_b])
                S.op("act", lambda h: h.activation(out=big[:, 1], in_=big[:, 0], func=ACT.Exp), r=[big_b], w=[big_b])
                bg(lambda h: h.tensor_tensor(out=big[:, 0], in0=TH.unsqueeze(2).to_broadcast([128, 32, 32]), in1=kb3, op=ALU.mult), r=[kk_b])
                for ri in range(2):
                    bg(lambda h: h.tensor_scalar(out=big[:, 2], in0=big[:, 0], scalar1=1.0 / (2 * np.pi), scalar2=(0.25 if ri == 0 else 0.0), op0=ALU.mult, op1=ALU.add))
                    bg(lambda h: h.tensor_copy(out=bigi[:], in_=big[:, 2]))
                    bg(lambda h: h.tensor_copy(out=big[:, 3], in_=bigi[:]))
                    bg(lambda h: h.tensor_tensor(out=big[:, 2], in0=big[:, 2], in1=big[:, 3], op=ALU.subtract))
                    bg(lambda h: h.tensor_scalar(out=big[:, 3], in0=big[:, 2], scalar1=0.5, scalar2=None, op0=ALU.is_gt))
                    bg(lambda h: h.tensor_tensor(out=big[:, 2], in0=big[:, 2], in1=big[:, 3], op=ALU.subtract))
                    bg(lambda h: h.tensor_scalar(out=big[:, 3], in0=big[:, 2], scalar1=-0.5, scalar2=None, op0=ALU.is_lt))
                    bg(lambda h: h.tensor_tensor(out=big[:, 2], in0=big[:, 2], in1=big[:, 3], op=ALU.add))
                    S.op("act", lambda h: h.activation(out=big[:, 3], in_=big[:, 2], func=ACT.Sin, scale=TWO_PI_LO), r=[big_b], w=[big_b])
                    bg(lambda h: h.tensor_tensor(out=PW[:, ab, ri], in0=big[:, 3], in1=big[:, 1], op=ALU.mult), w=[PW_b])
            AR, AI = PW[:, 0, 0, :, 16], PW[:, 0, 1, :, 16]
            dv(lambda h: h.tensor_tensor(out=DEN, in0=lr, in1=lr, op=ALU.mult), r=[lam_b])
            dv(lambda h: h.tensor_tensor(out=T1, in0=lim, in1=lim, op=ALU.mult), r=[lam_b])
            dv(lambda h: h.tensor_tensor(out=DEN, in0=DEN, in1=T1, op=ALU.add))
            dv(lambda h: h.reciprocal(out=DEN, in_=DEN))
            dv(lambda h: h.tensor_scalar(out=AM1, in0=AR, scalar1=-1.0, scalar2=None, op0=ALU.add), r=[PW_b])
            dv(lambda h: h.tensor_tensor(out=T1, in0=AM1, in1=lr, op=ALU.mult), r=[lam_b])
            dv(lambda h: h.tensor_tensor(out=T2, in0=AI, in1=lim, op=ALU.mult), r=[lam_b, PW_b])
            dv(lambda h: h.tensor_tensor(out=ZR, in0=T1, in1=T2, op=ALU.add))
            dv(lambda h: h.tensor_tensor(out=ZR, in0=ZR, in1=DEN, op=ALU.mult))
            dv(lambda h: h.tensor_tensor(out=T1, in0=AI, in1=lr, op=ALU.mult), r=[lam_b, PW_b])
            dv(lambda h: h.tensor_tensor(out=T2, in0=AM1, in1=lim, op=ALU.mult), r=[lam_b])
            dv(lambda h: h.tensor_tensor(out=ZI, in0=T1, in1=T2, op=ALU.subtract))
            dv(lambda h: h.tensor_tensor(out=ZI, in0=ZI, in1=DEN, op=ALU.mult))
            BB = K.sb("s5_BB", [128, 2, 32, 16]); BB_b = Buf("s5_BB")
            tq = K.sb("s5_tq", [128, 2, 32, 16]); tq_b = Buf("s5_tq")
            zr3 = ZR.unsqueeze(2).to_broadcast([128, 32, 16]); zi3 = ZI.unsqueeze(2).to_broadcast([128, 32, 16])
            bq = lambda f: S.op("dve", f, r=[sm_b, Bt_b, tq_b, BB_b], w=[tq_b, BB_b])
            bq(lambda h: h.tensor_tensor(out=tq[:, 0], in0=Bt[:, 0], in1=zr3, op=ALU.mult))
            bq(lambda h: h.tensor_tensor(out=tq[:, 1], in0=Bt[:, 1], in1=zi3, op=ALU.mult))
            bq(lambda h: h.tensor_tensor(out=BB[:, 0], in0=tq[:, 0], in1=tq[:, 1], op=ALU.subtract))
            bq(lambda h: h.tensor_tensor(out=tq[:, 0], in0=Bt[:, 1], in1=zr3, op=ALU.mult))
            bq(lambda h: h.tensor_tensor(out=tq[:, 1], in0=Bt[:, 0], in1=zi3, op=ALU.mult))
            bq(lambda h: h.tensor_tensor(out=BB[:, 1], in0=tq[:, 0], in1=tq[:, 1], op=ALU.add))
            for d in range(2):
                rows = slice(d * 64, (d + 1) * 64)
                mur = PW[rows, 0, 0, d * 16:(d + 1) * 16, 31]; mui = PW[rows, 0, 1, d * 16:(d + 1) * 16, 31]
                S.op("dve", lambda h: h.tensor_copy(out=MUA[rows, 0, :], in_=mur), r=[PW_b], w=[MU_b])
                S.op("dve", lambda h: h.tensor_copy(out=MUA[rows, 1, :], in_=mur), r=[PW_b], w=[MU_b])
                S.op("dve", lambda h: h.tensor_scalar(out=MUB[rows, 0, :], in0=mui, scalar1=-1.0, scalar2=None, op0=ALU.mult), r=[PW_b], w=[MU_b])
                S.op("dve", lambda h: h.tensor_copy(out=MUB[rows, 1, :], in_=mui), r=[PW_b], w=[MU_b])
            cp = K.sb("s5_cp", [128, 6, 8, 256]); cp_b = Buf("s5_cp")
            XS = K.sb("s5_XS", [128, 2, 8, 256], BF16); YS = K.sb("s5_YS", [128, 2, 8, 256], BF16); XY_b = Buf("s5_XY")
            GTs = K.sb("s5_GTs", [128, 8, 256], BF16); GTs_b = Buf("s5_GTs")
            atmp = Rot(K, "s5_at", [128, 2, 256], F32, 2)
            v4 = lambda ap: ap.rearrange("p g (a b) -> p g a b", a=16)

            def cprod(ab, k0, coef, coef_b, dgs):
                pr = PW[:, ab, 0, dgs, k0:k0 + 16].unsqueeze(3).to_broadcast([128, 8, 16, 16])
                pi = PW[:, ab, 1, dgs, k0:k0 + 16].unsqueeze(3).to_broadcast([128, 8, 16, 16])
                cr = coef[:, 0, dgs, :].unsqueeze(2).to_broadcast([128, 8, 16, 16])
                ci = coef[:, 1, dgs, :].unsqueeze(2).to_broadcast([128, 8, 16, 16])
                rr = [PW_b, coef_b, cp_b]
                S.op("dve", lambda h: h.tensor_tensor(out=v4(cp[:, 0]), in0=pr, in1=cr, op=ALU.mult), r=rr, w=[cp_b])
                S.op("pool", lambda h: h.tensor_tensor(out=v4(cp[:, 1]), in0=pi, in1=ci, op=ALU.mult), r=rr, w=[cp_b])
                S.op("dve", lambda h: h.tensor_tensor(out=v4(cp[:, 2]), in0=pr, in1=ci, op=ALU.mult), r=rr, w=[cp_b])
                S.op("pool", lambda h: h.tensor_tensor(out=v4(cp[:, 3]), in0=pi, in1=cr, op=ALU.mult), r=rr, w=[cp_b])
                S.op("dve", lambda h: h.tensor_tensor(out=cp[:, 4], in0=cp[:, 0], in1=cp[:, 1], op=ALU.subtract), r=[cp_b], w=[cp_b])
                S.op("pool", lambda h: h.tensor_tensor(out=cp[:, 5], in0=cp[:, 2], in1=cp[:, 3], op=ALU.add), r=[cp_b], w=[cp_b])

            def stack(out, outb, imcol):
                S.op("dve", lambda h: h.tensor_scalar(out=cp[:, 0], in0=cp[:, 4], scalar1=sel3[:, 0:1], scalar2=None, op0=ALU.mult), r=[cp_b, sel3_b], w=[cp_b])
                S.op("dve", lambda h: h.scalar_tensor_tensor(out=out, in0=cp[:, 5], scalar=sel3[:, imcol:imcol + 1], in1=cp[:, 0], op0=ALU.mult, op1=ALU.add),
                     r=[cp_b, sel3_b], w=[outb])

            for gb in range(2):
                for d in range(2):
                    dgs = slice(d * 16 + gb * 8, d * 16 + gb * 8 + 8)
                    cprod(1 if d == 0 else 0, 1 if d == 0 else 15, BB, BB_b, dgs)
                    stack(GTs[:], GTs_b, 1)
                    if d == 1:
                        S.op("pool", lambda h: h.tensor_copy(out=XS[:, 1], in_=GTs[:]), r=[GTs_b], w=[XY_b])
                    for gl in range(8):
                        g = gb * 8 + gl
                        for jh in range(2):
                            ps, psb = K.bank()
                            pv = ps[:].bitcast(BF16)
                            S.op("pe", lambda h: h.transpose(out=pv[:, 0:128], in_=GTs[:, gl, jh * 128:(jh + 1) * 128], identity=K.identb[:]), r=[GTs_b, K.identb_b], w=[psb])
                            evac(K, K.ev(), Gl[:, d * 16 + g, jh, :], pv[:, 0:128], r=[psb], w=[Gl_b])
                    if d == 0:
                        cprod(1, 16, BB, BB_b, dgs)
                        stack(XS[:, 0], XY_b, 1)
                    cprod(0 if d == 0 else 1, 15 if d == 0 else 16, Ct, Ct_b, dgs)
                    stack(YS[:, d], XY_b, 2)
                    cprod(0 if d == 0 else 1, 16 if d == 0 else 0, Ct, Ct_b, dgs)
                    rows = slice(d * 64, (d + 1) * 64)
                    S.op("act", lambda h: h.activation(out=Hs[rows, 0, gb * 8:gb * 8 + 8, :], in_=cp[rows, 4], func=ACT.Copy), r=[cp_b], w=[Hs_b])
                    S.op("act", lambda h: h.activation(out=Hs[rows, 1, gb * 8:gb * 8 + 8, :], in_=cp[rows, 5], func=ACT.Copy, scale=-1.0), r=[cp_b], w=[Hs_b])
                for gl in range(8):
                    g = gb * 8 + gl
                    for jh in range(2):
                        pss = []
                        for d in range(2):
                            ps, psb = K.bank()
                            S.op("pe", lambda h: h.matmul(ps[:, 0:256], lhsT=XS[:, d, gl, jh * 128:(jh + 1) * 128], rhs=YS[:, d, gl, :], start=True, stop=True), r=[XY_b], w=[psb])
                            pss.append((ps, psb))
                        at, atb = atmp.get()
                        for d in range(2):
                            S.op("dve", lambda h: h.tensor_tensor(out=at[:, d, :], in0=pss[d][0][:, 0:256], in1=m01[:, d, jh, :], op=ALU.mult), r=[pss[d][1], m01_b], w=[atb])
                        S.op("pool", lambda h: h.tensor_tensor(out=Aw[:, g, jh, :], in0=at[:, 0, :], in1=at[:, 1, :], op=ALU.add), r=[atb], w=[Aw_b])
        with phase(K):
            uT = K.sb("s5_uT", [128, 2, T], BF16); uT_b = Buf("s5_uT")
            load_fm(K, uT, uT_b, *P["s5u"])
            U = K.sb("s5_U", [128, 16, 2, NCH], BF16); U_b = Buf("s5_U")
            SN = K.sb("s5_SN", [128, 2, 16, NCH], BF16); SN_b = Buf("s5_SN")
            with phase(K):
                sel = K.sb("s5_sel", [128, 64, 128], BF16); sel_b = Buf("s5_sel")
                for q4 in range(4):
                    S.dma("sp", sel[:, q4 * 16:(q4 + 1) * 16, :], selin[:, q4 * 16:(q4 + 1) * 16, :], sel_b)
                E = K.sb("s5_E", [128, 2, 16, NCH]); E_b = Buf("s5_E")
                ua = uT[:]
                for g in range(16):
                    cc, gl = g // 8, g % 8
                    for jh in range(2):
                        ps, psb = K.bank()
                        for jl in range(8):
                            rhs = bass.AP(ua.tensor, ua.offset + cc * T + 8 * jh + jl, [list(ua.ap[0]), [16, NCH]])
                            S.op("pe", lambda h: h.matmul(ps[:, 0:NCH], lhsT=sel[:, gl * 8 + jl, :], rhs=rhs, start=(jl == 0), stop=(jl == 7)), r=[sel_b, uT_b], w=[psb])
                        evac(K, K.ev(), U[:, g, jh, :], ps[:, 0:NCH], r=[psb], w=[U_b])
                for g in range(16):
                    for ri in range(2):
                        ps, psb = K.bank()
                        for d in range(2):
                            for jh in range(2):
                                S.op("pe", lambda h: h.matmul(ps[d * 64:(d + 1) * 64, 0:NCH], lhsT=Gl[:, d * 16 + g, jh, ri * 64:(ri + 1) * 64], rhs=U[:, g, jh, :],
                                                              start=(jh == 0), stop=(jh == 1)), r=[Gl_b, U_b], w=[psb])
                        evac(K, K.ev(), E[0:64, ri, g, :], ps[0:64, 0:NCH], r=[psb], w=[E_b])
                        pa = ps[64:128, 0:NCH]
                        r1 = bass.AP(pa.tensor, pa.offset + 15, [list(pa.ap[0]), [-1, 16]])
                        r2 = bass.AP(pa.tensor, pa.offset + 271, [list(pa.ap[0]), [-1, 256]])
                        S.op("dve", lambda h: h.tensor_copy(out=E[64:128, ri, g, 0:16], in_=r1), r=[psb], w=[E_b])
                        S.op("dve", lambda h: h.tensor_copy(out=E[64:128, ri, g, 16:NCH], in_=r2), r=[psb], w=[E_b])
                w1 = K.sb("s5_w1", [128, 2, 16]); w2 = K.sb("s5_w2", [128, 2, 16]); w_b = Buf("s5_w")
                ea_ = E[:]
                rstride = 16 * NCH
                for pos in range(1, NCH):
                    prev = bass.AP(ea_.tensor, ea_.offset + pos - 1, [list(ea_.ap[0]), [rstride, 2], [NCH, 16]])
                    prev_sw = bass.AP(ea_.tensor, ea_.offset + rstride + pos - 1, [list(ea_.ap[0]), [-rstride, 2], [NCH, 16]])
                    cur = bass.AP(ea_.tensor, ea_.offset + pos, [list(ea_.ap[0]), [rstride, 2], [NCH, 16]])
                    S.op("dve", lambda h: h.tensor_tensor(out=w1[:], in0=prev, in1=MUA[:], op=ALU.mult), r=[E_b, MU_b], w=[w_b])
                    S.op("dve", lambda h: h.tensor_tensor(out=w2[:], in0=prev_sw, in1=MUB[:], op=ALU.mult), r=[E_b, MU_b], w=[w_b])
                    S.op("dve", lambda h: h.tensor_tensor(out=w1[:], in0=w1[:], in1=w2[:], op=ALU.add), r=[w_b], w=[w_b])
                    S.op("dve", lambda h: h.tensor_tensor(out=cur, in0=cur, in1=w1[:], op=ALU.add), r=[w_b, E_b], w=[E_b])
                S.op("pool", lambda h: h.memset(SN[:], 0.0), w=[SN_b])
                S.op("act", lambda h: h.activation(out=SN[0:64, :, :, 1:NCH], in_=E[0:64, :, :, 0:NCH - 1], func=ACT.Copy), r=[E_b], w=[SN_b])
                eh = E[64:128, :, :, :]
                rv1 = bass.AP(eh.tensor, eh.offset + 14, [list(eh.ap[0]), [rstride, 2], [NCH, 16], [-1, 15]])
                rv2 = bass.AP(eh.tensor, eh.offset + 270, [list(eh.ap[0]), [rstride, 2], [NCH, 16], [-1, 256]])
                S.op("dve", lambda h: h.tensor_copy(out=SN[64:128, :, :, 0:15], in_=rv1), r=[E_b], w=[SN_b])
                S.op("dve", lambda h: h.tensor_copy(out=SN[64:128, :, :, 16:NCH], in_=rv2), r=[E_b], w=[SN_b])
            with phase(K):
                selT = K.sb("s5_selT", [128, 64, 128], BF16); selT_b = Buf("s5_selT")
                for q4 in range(4):
                    S.dma("sp", selT[:, q4 * 16:(q4 + 1) * 16, :], selTin[:, q4 * 16:(q4 + 1) * 16, :], selT_b)
                Ysb = K.sb("s5_Ysb", [128, 16, 2, NCH], BF16); Ysb_b = Buf("s5_Ysb")
                yT = K.sb("s5_yT", [128, T]); yT_b = Buf("s5_yT")
                dcol = K.sb("s5_dcol", [128, 2]); dcol_b = Buf("s5_dcol")
                S.dma("sp", dcol[:], din[li], dcol_b)
                wgs = K.sb("s5_wgs", [128, 2, 256]); wgs_b = Buf("s5_wgs")
                S.dma("sp", wgs[:], gluin[li].rearrange("(c p) n -> p c n", p=128), wgs_b)
                wg = K.sb("s5_wg", [128, 2, 256], BF16); wg_b = Buf("s5_wg")
                S.op("pool", lambda h: h.tensor_copy(out=wg[:], in_=wgs[:]), r=[wgs_b], w=[wg_b])
                for g in range(16):
                    for th in range(2):
                        ps, psb = K.bank()
                        cols = slice(th * 128, (th + 1) * 128)
                        for jh in range(2):
                            S.op("pe", lambda h: h.matmul(ps[:, 0:NCH], lhsT=Aw[:, g, jh, cols], rhs=U[:, g, jh, :], start=(jh == 0), stop=False), r=[Aw_b, U_b], w=[psb])
                        for ri in range(2):
                            S.op("pe", lambda h: h.matmul(ps[:, 0:NCH], lhsT=Hs[0:64, ri, g, cols], rhs=SN[0:64, ri, g, :], start=False, stop=False), r=[Hs_b, SN_b], w=[psb])
                        for ri in range(2):
                            for hf in range(2):
                                c2 = slice(th * 128 + hf * 64, th * 128 + (hf + 1) * 64)
                                S.op("pe", lambda h: h.matmul(ps[hf * 64:(hf + 1) * 64, 0:NCH], lhsT=Hs[64:128, ri, g, c2], rhs=SN[64:128, ri, g, :], start=False, stop=(ri == 1)),
                                     r=[Hs_b, SN_b], w=[psb])
                        evac(K, K.ev(), Ysb[:, g, th, :], ps[:, 0:NCH], r=[psb], w=[Ysb_b])
                zT = K.sb("s5_zT", [128, 2, T], BF16); zT_b = Buf("s5_zT")
                ft = Rot(K, "s5_ft", [128, 512], F32, 3)
                ya = yT[:]
                for cc in range(2):
                    for t in range(16):
                        th, tl = t // 8, t % 8
                        ps, psb = K.bank()
                        for gl in range(8):
                            S.op("pe", lambda h: h.matmul(ps[:, 0:NCH], lhsT=selT[:, gl * 8 + tl, :], rhs=Ysb[:, cc * 8 + gl, th, :], start=(gl == 0), stop=(gl == 7)),
                                 r=[selT_b, Ysb_b], w=[psb])
                        dst = bass.AP(ya.tensor, ya.offset + t, [list(ya.ap[0]), [16, NCH]])
                        evac(K, K.ev(), dst, ps[:, 0:NCH], r=[psb], w=[yT_b])
                    for (t0, n) in TB:
                        yv = yT[:, t0:t0 + n]
                        S.op("dve", lambda h: h.scalar_tensor_tensor(out=yv, in0=uT[:, cc, t0:t0 + n], scalar=dcol[:, cc:cc + 1], in1=yv, op0=ALU.mult, op1=ALU.add),
                             r=[uT_b, yT_b], w=[yT_b], rs=[dcol_b])
                        a, ab_ = ft.get()
                        S.op("pool", lambda h: h.tensor_tensor(out=a[:, 0:n], in0=yv, in1=yv, op=ALU.mult), r=[yT_b], w=[ab_])
                        S.op("dve", lambda h: h.tensor_scalar(out=a[:, 0:n], in0=a[:, 0:n], scalar1=0.044715, scalar2=1.0, op0=ALU.mult, op1=ALU.add), r=[ab_], w=[ab_])
                        S.op("pool", lambda h: h.tensor_tensor(out=a[:, 0:n], in0=a[:, 0:n], in1=yv, op=ALU.mult), r=[ab_, yT_b], w=[ab_])
                        S.op("act", lambda h: h.activation(out=a[:, 0:n], in_=a[:, 0:n], func=ACT.Sigmoid, scale=1.5957691216), r=[ab_], w=[ab_])
                        S.op("dve", lambda h: h.tensor_tensor(out=zT[:, cc, t0:t0 + n], in0=a[:, 0:n], in1=yv, op=ALU.mult), r=[ab_, yT_b], w=[zT_b])
                ob = Rot(K, "s5_ob", [128, 512], BF16, 3)
                for co in range(2):
                    for (t0, n) in TB:
                        ps, psb = K.bank()
                        for ci in range(2):
                            S.op("pe", lambda h: h.matmul(ps[:, 0:n], lhsT=wg[:, ci, co * 128:(co + 1) * 128], rhs=zT[:, ci, t0:t0 + n], start=(ci == 0), stop=(ci == 1)),
                                 r=[wg_b, zT_b], w=[psb])
                        a, ab_ = ft.get()
                        S.op("act", lambda h: h.activation(out=a[:, 0:n], in_=ps[:, 0:n], func=ACT.Sigmoid), r=[psb], w=[ab_])
                        o, obb = ob.get()
                        S.op("dve", lambda h: h.tensor_tensor(out=o[:, 0:n], in0=a[:, 0:n], in1=zT[:, co, t0:t0 + n], op=ALU.mult), r=[ab_, zT_b], w=[obb])
                        S.dma("sp", Od[co * 128:(co + 1) * 128, t0:t0 + n], o[:, 0:n], obb, dr=Odb, load=False)


def phase_merge(K, li, P, O, xsrc, xsrc_b, xdst, xdst_b, last):
    nc, S = K.nc, K.S
    wbr_in = K.inp("w_branch", [DEPTH, 4, 256, D])
    wout_in = K.inp("w_out", [DEPTH, D, D])
    with phase(K):
        stg = Rot(K, "mg_stg", [128, D], F32, 2)
        wbr = K.sb("mg_wbr", [128, 4, 2, D], BF16); wbr_b = Buf("mg_wbr")
        wog = K.sb("mg_wog", [128, 2, 8, D], BF16); wog_b = Buf("mg_wog")
        for br in range(4):
            for cc in range(2):
                st, stb = stg.get()
                S.dma("sp", st[:], wbr_in[li, br, cc * 128:(cc + 1) * 128, :], stb)
                S.op("pool", lambda h: h.tensor_copy(out=wbr[:, br, cc, :], in_=st[:]), r=[stb], w=[wbr_b])
        for kc in range(8):
            st, stb = stg.get()
            S.dma("sp", st[:], wout_in[li, kc * 128:(kc + 1) * 128, :], stb)
            for s_ in range(2):
                S.op("dve", lambda h: h.tensor_tensor(out=wog[:, s_, kc, :], in0=st[:], in1=K.gbc[:, s_, :], op=ALU.mult), r=[stb, K.gbc_b], w=[wog_b])
        ob_r = Rot(K, "mg_ob", [128, 4, 2, 512], BF16, 2)
        gt_r = Rot(K, "mg_gt", [128, 32, 512], BF16, 2)
        tm_r = Rot(K, "mg_tm", [128, 4, 512], F32, 2)
        mT_r = Rot(K, "mg_mT", [128, 8, 512], BF16, 2)
        x_r = Rot(K, "mg_x", [128, D], F32, 3)
        names = ("s5", "na", "hg", "rt")
        gsrc, gsrcb = P["gate"]
        gview = gsrc.rearrange("(j p) t -> p j t", p=128)
        for (t0, n) in TB:
            ob, obb = ob_r.get()
            for br in range(4):
                for cc in range(2):
                    S.dma("sp", ob[:, br, cc, 0:n], O[names[br]][0][cc * 128:(cc + 1) * 128, t0:t0 + n], obb, dr=O[names[br]][1])
            gt, gtb = gt_r.get()
            for br in range(4):
                S.dma("sp", gt[:, br * 8:(br + 1) * 8, 0:n], gview[:, br * 8:(br + 1) * 8, t0:t0 + n], gtb, dr=gsrcb)
            mT, mTb = mT_r.get()
            for dc in range(8):
                tm, tmb = tm_r.get()
                for br in range(4):
                    ps, psb = K.bank()
                    for cc in range(2):
                        S.op("pe", lambda h: h.matmul(ps[:, 0:n], lhsT=wbr[:, br, cc, dc * 128:(dc + 1) * 128], rhs=ob[:, br, cc, 0:n], start=(cc == 0), stop=(cc == 1)),
                             r=[wbr_b, obb], w=[psb])
                    S.op("dve", lambda h: h.tensor_tensor(out=tm[:, br, 0:n], in0=ps[:, 0:n], in1=gt[:, br * 8 + dc, 0:n], op=ALU.mult), r=[psb, gtb], w=[tmb])
                S.op("pool", lambda h: h.tensor_tensor(out=tm[:, 0, 0:n], in0=tm[:, 0, 0:n], in1=tm[:, 1, 0:n], op=ALU.add), r=[tmb], w=[tmb])
                S.op("pool", lambda h: h.tensor_tensor(out=tm[:, 2, 0:n], in0=tm[:, 2, 0:n], in1=tm[:, 3, 0:n], op=ALU.add), r=[tmb], w=[tmb])
                S.op("pool", lambda h: h.tensor_tensor(out=mT[:, dc, 0:n], in0=tm[:, 0, 0:n], in1=tm[:, 2, 0:n], op=ALU.add), r=[tmb], w=[mTb])
            for i in range(n // 128):
                t = t0 // 128 + i
                s_ = 1 if t < 2 else 0
                if last and t < 2:
                    continue
                x, xb = x_r.get()
                S.dma("sp", x[:], xsrc[t * 128:(t + 1) * 128, :], xb, dr=xsrc_b)
                for half in range(2):
                    ps, psb = K.bank()
                    for kc in range(8):
                        S.op("pe", lambda h: h.matmul(ps[:], lhsT=mT[:, kc, i * 128:(i + 1) * 128], rhs=wog[:, s_, kc, half * 512:(half + 1) * 512], start=(kc == 0), stop=(kc == 7)),
                             r=[mTb, wog_b], w=[psb])
                    S.op("dve", lambda h: h.tensor_tensor(out=x[:, half * 512:(half + 1) * 512], in0=x[:, half * 512:(half + 1) * 512], in1=ps[:], op=ALU.add), r=[psb, xb], w=[xb])
                r0 = t * 128 - (CTX if last else 0)
                S.dma("sp", xdst[r0:r0 + 128, :], x[:], xb, dr=xdst_b, load=False)


def phase_route(K, li, xsrc, xsrc_b, row_off, xn2, xn2_b, R, with_ctx):
    nc, S = K.nc, K.S
    rwin = K.inp("router_wT", [DEPTH, 128, 8, 16])
    groups = ([(0, 2, 1)] if with_ctx else []) + [(2 + 4 * i, 4, 0) for i in range(8)]
    with phase(K):
        rw = K.sb("rt_rw", [128, 8, 16]); rw_b = Buf("rt_rw")
        S.dma("sp", rw[:], rwin[li], rw_b)
        xr = Rot(K, "r_xr", [128, D], F32, 3)
        xnr = Rot(K, "r_xnr", [128, D], F32, 8)
        junk = K.sb("r_junk", [128, D], BF16); junk_b = Buf("r_junk")
        ssr = Rot(K, "r_ssr", [128, 8], F32, 4)
        xnb = Rot(K, "r_xnb", [128, D], BF16, 3)
        hg_r = Rot(K, "r_hg", [128, 8, 512], F32, 2)
        affT = K.sb("r_affT", [16, T]); affT_b = Buf("r_affT")
        codeT = K.sb("r_codeT", [16, T]); codeT_b = Buf("r_codeT")
        S.op("pool", lambda h: h.memset(R.aff[:], 0.0), w=[R.aff_b])
        S.op("pool", lambda h: h.memset(R.code[:], -1.0), w=[R.code_b])
        for (t0, n, s) in groups:
            xns = []
            for i in range(n):
                t = t0 + i
                x, xb = xr.get()
                S.dma("sp", x[:], xsrc[t * 128 - row_off:(t + 1) * 128 - row_off, :], xb, dr=xsrc_b)
                ss, ssb = ssr.get()
                S.op("pool", lambda h: h.memset(ss[:], 0.0), w=[ssb])
                S.op("act", lambda h: h.activation(out=junk[:], in_=x[:], func=ACT.Square, accum_out=ss[:, 0:1]), r=[xb], w=[junk_b, ssb])
                S.op("act", lambda h: h.activation(out=ss[:, 1:2], in_=ss[:, 0:1], func=ACT.Sqrt, scale=1.0 / D, bias=K.epsc[:, 0:1]), r=[ssb, K.epsc_b], w=[ssb])
                S.op("dve", lambda h: h.reciprocal(out=ss[:, 2:3], in_=ss[:, 1:2]), r=[ssb], w=[ssb])
                xn, xnbuf = xnr.get()
                S.op("act", lambda h: h.activation(out=xn[:], in_=x[:], func=ACT.Copy, scale=ss[:, 2:3]), r=[xb], w=[xnbuf], rs=[ssb])
                xns.append((xn, xnbuf))
                o, ob = xnb.get()
                S.op("pool", lambda h: h.tensor_copy(out=o[:], in_=xn[:]), r=[xnbuf], w=[ob])
                S.dma("sp", xn2[t * 128:(t + 1) * 128, :], o[:], ob, dr=xn2_b, load=False)
            hgt, hgb = hg_r.get()
            for j in range(8):
                ps, psb = K.bank()
                for i, (xn, xnbuf) in enumerate(xns):
                    S.op("pe", lambda h: h.transpose(out=ps[:, i * 128:(i + 1) * 128], in_=xn[:, j * 128:(j + 1) * 128], identity=K.ident[:]),
                         r=[xnbuf, K.ident_b], w=[psb])
                evac(K, K.ev(), hgt[:, j, 0:n * 128], ps[:, 0:n * 128], r=[psb, K.A_b, K.mv_b], w=[hgb],
                     scale=K.A2[:, j, s:s + 1], bias=K.mv[:, 3, j, s:s + 1])
            for i in range(n):
                t = t0 + i
                pl, plb = K.bank()
                for j in range(8):
                    S.op("pe", lambda h: h.matmul(pl[:, 0:16], lhsT=hgt[:, j, i * 128:(i + 1) * 128], rhs=rw[:, j, :], start=(j == 0), stop=(j == 7)),
                         r=[hgb, rw_b], w=[plb])
                ss, ssb = ssr.get()
                S.op("dve", lambda h: h.reduce_max(out=ss[:, 0:1], in_=pl[:, 0:16], axis=AX.X), r=[plb], w=[ssb])
                S.op("dve", lambda h: h.tensor_scalar(out=ss[:, 1:2], in0=ss[:, 0:1], scalar1=-1.0, scalar2=None, op0=ALU.mult), r=[ssb], w=[ssb])
                S.op("pool", lambda h: h.memset(ss[:, 2:3], 0.0), w=[ssb])
                S.op("act", lambda h: h.activation(out=R.aff[:, t, :], in_=pl[:, 0:16], func=ACT.Exp, bias=ss[:, 1:2], accum_out=ss[:, 2:3]), r=[plb], w=[R.aff_b, ssb], rs=[ssb])
                S.op("dve", lambda h: h.reciprocal(out=ss[:, 3:4], in_=ss[:, 2:3]), r=[ssb], w=[ssb])
                S.op("dve", lambda h: h.tensor_scalar(out=R.aff[:, t, :], in0=R.aff[:, t, :], scalar1=ss[:, 3:4], scalar2=None, op0=ALU.mult), r=[R.aff_b], w=[R.aff_b], rs=[ssb])
                pt, ptb = K.bank()
                S.op("pe", lambda h: h.transpose(out=pt[0:16, 0:128], in_=R.aff[:, t, :], identity=K.ident[:]), r=[R.aff_b, K.ident_b], w=[ptb])
                evac(K, K.ev(), affT[:, t * 128:(t + 1) * 128], pt[0:16, 0:128], r=[ptb], w=[affT_b])
        bs = K.sb("r_bs", [16, 8]); bs_b = Buf("r_bs")
        cj = K.sb("r_cj", [16, SEQ]); cj_b = Buf("r_cj")
        onesT = K.sb("r_onesT", [16, SEQ]); onesT_b = Buf("r_onesT")
        S.op("pool", lambda h: h.memset(onesT[:], 1.0), w=[onesT_b])
        sets = ([(0, CTX, 2 * CTX // NE)] if with_ctx else []) + [(CTX, SEQ, 2 * SEQ // NE)]
        for (c0, n, cap) in sets:
            av = affT[:, c0:c0 + n]
            LO, HI, MID, CNT, GE, D1 = [bs[:, i:i + 1] for i in range(6)]
            b1 = lambda f, rs_=True: S.op("dve", f, r=[bs_b], w=[bs_b], rs=[bs_b] if rs_ else [])
            b1(lambda h: h.memset(bs[:], 0.0), False)
            b1(lambda h: h.memset(HI, 1.0), False)
            for it in range(34):
                b1(lambda h: h.tensor_tensor(out=MID, in0=LO, in1=HI, op=ALU.add), False)
                b1(lambda h: h.tensor_scalar(out=MID, in0=MID, scalar1=0.5, scalar2=None, op0=ALU.mult), False)
                S.op("dve", lambda h: h.tensor_scalar(out=cj[:, 0:n], in0=av, scalar1=MID, scalar2=0.0, op0=ALU.is_ge, op1=ALU.add, accum_out=CNT),
                     r=[affT_b], w=[cj_b, bs_b], rs=[bs_b])
                b1(lambda h: h.tensor_scalar(out=GE, in0=CNT, scalar1=float(cap) - 0.5, scalar2=None, op0=ALU.is_ge), False)
                b1(lambda h: h.tensor_tensor(out=D1, in0=MID, in1=LO, op=ALU.subtract), False)
                b1(lambda h: h.scalar_tensor_tensor(out=LO, in0=D1, scalar=GE, in1=LO, op0=ALU.mult, op1=ALU.add))
                b1(lambda h: h.tensor_tensor(out=D1, in0=HI, in1=MID, op=ALU.subtract), False)
                b1(lambda h: h.scalar_tensor_tensor(out=HI, in0=D1, scalar=GE, in1=MID, op0=ALU.mult, op1=ALU.add))
            S.op("dve", lambda h: h.tensor_scalar(out=cj[:, 0:n], in0=av, scalar1=LO, scalar2=None, op0=ALU.is_ge), r=[affT_b], w=[cj_b], rs=[bs_b])
            S.op("dve", lambda h: h.tensor_tensor_scan(out=codeT[:, c0:c0 + n], data0=onesT[:, 0:n], data1=cj[:, 0:n], initial=0.0, op0=ALU.mult, op1=ALU.add),
                 r=[cj_b, onesT_b], w=[codeT_b])
            S.op("dve", lambda h: h.tensor_tensor(out=codeT[:, c0:c0 + n], in0=codeT[:, c0:c0 + n], in1=cj[:, 0:n], op=ALU.mult), r=[cj_b, codeT_b], w=[codeT_b])
            S.op("dve", lambda h: h.tensor_scalar(out=codeT[:, c0:c0 + n], in0=codeT[:, c0:c0 + n], scalar1=-1.0, scalar2=None, op0=ALU.add), r=[codeT_b], w=[codeT_b])
            for t in range(c0 // 128, (c0 + n) // 128):
                pt, ptb = K.bank()
                S.op("pe", lambda h: h.transpose(out=pt[:, 0:16], in_=codeT[0:16, t * 128:(t + 1) * 128], identity=K.ident[0:16, 0:16]), r=[codeT_b, K.ident_b], w=[ptb])
                evac(K, K.ev(), R.code[:, t, :], pt[:, 0:16], r=[ptb], w=[R.code_b])
        dump(K, "dbg_aff", R.aff[:], R.aff_b, [128, NT, 16], F32)
        dump(K, "dbg_code", R.code[:], R.code_b, [128, NT, 16], F32)


NF = FE // 128


def phase_moe(K, li, xn2, xn2_b, R, xacc, xacc_b, row_off, with_ctx):
    nc, S = K.nc, K.S
    wg_in = K.inp("ex_w_gate", [DEPTH, NE, D, FE])
    wu_in = K.inp("ex_w_up", [DEPTH, NE, D, FE])
    wd_in = K.inp("ex_w_down", [DEPTH, NE, FE, D])
    iot_in = K.inp("c_iota512", [128, 512])
    pt_in = K.inp("c_ptidx", [128, 1 + NT])
    NS = 544 if with_ctx else 512
    CAPC = 2 * CTX // NE
    nst = 5 if with_ctx else 4
    with phase(K):
        iot = K.sb("mo_iot", [128, 512]); iot_b = Buf("mo_iot")
        S.dma("sp", iot[:], iot_in, iot_b)
        ptx = K.sb("mo_ptx", [128, 1 + NT]); ptx_b = Buf("mo_ptx")
        S.dma("sp", ptx[:], pt_in, ptx_b)
        TA = K.sb("mo_TA", [128, NT, NE, 4], BF16); TA_b = Buf("mo_TA")
        tf = K.sb("mo_tf", [128, NT, NE]); tf_b = Buf("mo_tf")
        S.op("dve", lambda h: h.tensor_copy(out=TA[:, :, :, 0], in_=ptx[:, 0:1].unsqueeze(2).to_broadcast([128, NT, NE])), r=[ptx_b], w=[TA_b])
        S.op("dve", lambda h: h.tensor_copy(out=TA[:, :, :, 1], in_=ptx[:, 1:1 + NT].unsqueeze(2).to_broadcast([128, NT, NE])), r=[ptx_b], w=[TA_b])
        S.op("dve", lambda h: h.tensor_copy(out=TA[:, :, :, 2], in_=R.aff[:]), r=[R.aff_b], w=[TA_b])
        S.op("dve", lambda h: h.tensor_tensor(out=tf[:], in0=R.aff[:], in1=TA[:, :, :, 2], op=ALU.subtract), r=[R.aff_b, TA_b], w=[tf_b])
        S.op("dve", lambda h: h.tensor_copy(out=TA[:, :, :, 3], in_=tf[:]), r=[tf_b], w=[TA_b])
        sel_r = Rot(K, "mo_sel", [128, 512], BF16, 4)
        ix_r = Rot(K, "mo_ix", [128, 5, 8], F32, 2)
        idx_r = Rot(K, "mo_idx", [128, 2, 8], I32, 2)
        xe_r = Rot(K, "mo_xe", [128, 5, D], BF16, 2)
        actT = K.sb("mo_actT", [128, NF, NS], BF16); actT_b = Buf("mo_actT")
        wst = Rot(K, "mo_wst", [128, 8, 128], F32, 6)
        wbf = Rot(K, "mo_wbf", [128, 8, 128], BF16, 4)
        dst_r = Rot(K, "mo_dst", [128, 512], F32, 3)
        dbf = Rot(K, "mo_dbf", [128, 2, 512], BF16, 3)
        sl_r = Rot(K, "mo_sl", [128, 512], F32, 3)
        ye_r = Rot(K, "mo_ye", [128, 5, D], F32, 2)
        xsc_b = Buf("xacc_sc")
        xeT_r = Rot(K, "mo_xeTr", [128, 8, NS], BF16, 2)

        def pro1(e):
            pidx = [K.bank() for _ in range(4)]
            for t in range(2, NT):
                sel, selb = sel_r.get()
                S.op("dve", lambda h: h.tensor_scalar(out=sel[:], in0=iot[:], scalar1=R.code[:, t, e:e + 1], scalar2=None, op0=ALU.is_equal), r=[iot_b], w=[selb], rs=[R.code_b])
                for st in range(4):
                    S.op("pe", lambda h: h.matmul(pidx[st][0][:, 0:4], lhsT=sel[:, st * 128:(st + 1) * 128], rhs=TA[:, t, e, :], start=(t == 2), stop=(t == NT - 1)),
                         r=[selb, TA_b], w=[pidx[st][1]])
            ix, ixb = ix_r.get()
            for st in range(4):
                evac(K, K.ev(), ix[:, st, 0:4], pidx[st][0][:, 0:4], r=[pidx[st][1]], w=[ixb])
            if with_ctx:
                pic, picb = K.bank()
                for t in range(2):
                    sel, selb = sel_r.get()
                    S.op("dve", lambda h: h.tensor_scalar(out=sel[:, 0:CAPC], in0=iot[:, 0:CAPC], scalar1=R.code[:, t, e:e + 1], scalar2=None, op0=ALU.is_equal), r=[iot_b], w=[selb], rs=[R.code_b])
                    S.op("pe", lambda h: h.matmul(pic[0:CAPC, 0:4], lhsT=sel[:, 0:CAPC], rhs=TA[:, t, e, :], start=(t == 0), stop=(t == 1)), r=[selb, TA_b], w=[picb])
                S.op("dve", lambda h: h.memset(ix[:, 4, :], 0.0), w=[ixb])
                evac(K, K.ev(), ix[0:CAPC, 4, 0:4], pic[0:CAPC, 0:4], r=[picb], w=[ixb])
            S.op("dve", lambda h: h.scalar_tensor_tensor(out=ix[:, :, 4], in0=ix[:, :, 1], scalar=128.0, in1=ix[:, :, 0], op0=ALU.mult, op1=ALU.add), r=[ixb], w=[ixb])
            S.op("dve", lambda h: h.tensor_tensor(out=ix[:, :, 5], in0=ix[:, :, 2], in1=ix[:, :, 3], op=ALU.add), r=[ixb], w=[ixb])
            S.op("dve", lambda h: h.tensor_scalar(out=ix[:, :, 6], in0=ix[:, :, 4], scalar1=-float(row_off), scalar2=None, op0=ALU.add), r=[ixb], w=[ixb])
            idx, idxb = idx_r.get()
            S.op("dve", lambda h: h.tensor_copy(out=idx[:, 0, 0:5], in_=ix[:, :, 4]), r=[ixb], w=[idxb])
            S.op("dve", lambda h: h.tensor_copy(out=idx[:, 1, 0:5], in_=ix[:, :, 6]), r=[ixb], w=[idxb])
            xe, xeb = xe_r.get()
            for st in range(nst):
                ns_ = 128 if st < 4 else CAPC
                S.idma(xe[0:ns_, st, :], xn2[:, :], xeb, idxb, in_off=bass.IndirectOffsetOnAxis(ap=idx[0:ns_, 0, st:st + 1], axis=0), dr=xn2_b, gather=True)
            return ix, ixb, idx, idxb, xe, xeb

        def pro2(e, st8):
            ix, ixb, idx, idxb, xe, xeb = st8
            xeT, xeT_b = xeT_r.get()
            for kc in range(8):
                pt, ptb = K.bank()
                pv = pt[:].bitcast(BF16)
                for st in range(nst):
                    ns_ = 128 if st < 4 else CAPC
                    S.op("pe", lambda h: h.transpose(out=pv[:, st * 128:st * 128 + ns_], in_=xe[0:ns_, st, kc * 128:(kc + 1) * 128], identity=K.identb[0:ns_, 0:ns_]),
                         r=[xeb, K.identb_b], w=[ptb])
                evac(K, "act", xeT[:, kc, 0:512], pv[:, 0:512], r=[ptb, K.A_b, K.mv_b], w=[xeT_b], scale=K.A2[:, kc, 0:1], bias=K.mv[:, 3, kc, 0:1])
                if with_ctx:
                    evac(K, "dve", xeT[:, kc, 512:NS], pv[:, 512:NS], r=[ptb, K.A_b, K.mv_b], w=[xeT_b], scale=K.A2[:, kc, 1:2], bias=K.mv[:, 3, kc, 1:2])
            return xeT, xeT_b

        def gate_up(e, xeT, xeT_b):
            def load_gu(f):
                res = []
                for w_in in (wg_in, wu_in):
                    ws, wsb = wst.get()
                    S.dma("sp", ws[:], w_in[li, e].rearrange("(k p) f -> p k f", p=128)[:, :, f * 128:(f + 1) * 128], wsb)
                    res.append((ws, wsb))
                return res
            pre = [load_gu(0), load_gu(1)]
            for f in range(NF):
                cur = pre.pop(0)
                wbs = []
                for i, (ws, wsb) in enumerate(cur):
                    wb, wbb = wbf.get()
                    if i == 0:
                        S.op("act", lambda h: h.activation(out=wb[:], in_=ws[:], func=ACT.Copy), r=[wsb], w=[wbb])
                    else:
                        S.op("dve", lambda h: h.tensor_copy(out=wb[:], in_=ws[:]), r=[wsb], w=[wbb])
                    wbs.append((wb, wbb))
                if f + 2 < NF:
                    pre.append(load_gu(f + 2))
                segs = [(0, 512)] + ([(512, NS)] if with_ctx else [])
                for (a0, a1) in segs:
                    pg, pgb = K.bank()
                    pu, pub = K.bank()
                    for (pp, ppb, (wb, wbb)) in ((pg, pgb, wbs[0]), (pu, pub, wbs[1])):
                        for kc in range(8):
                            S.op("pe", lambda h: h.matmul(pp[:, 0:a1 - a0], lhsT=wb[:, kc, :], rhs=xeT[:, kc, a0:a1], start=(kc == 0), stop=(kc == 7)), r=[wbb, xeT_b], w=[ppb])
                    sl, slb = sl_r.get()
                    S.op("act", lambda h: h.activation(out=sl[:, 0:a1 - a0], in_=pg[:, 0:a1 - a0], func=ACT.Silu), r=[pgb], w=[slb])
                    S.op("dve", lambda h: h.tensor_tensor(out=actT[:, f, a0:a1], in0=sl[:, 0:a1 - a0], in1=pu[:, 0:a1 - a0], op=ALU.mult), r=[slb, pub], w=[actT_b])

        def down_scatter(e, st8):
            ix, ixb, idx, idxb, xe, xeb = st8
            ye, yeb = ye_r.get()
            for half in range(2):
                hs = slice(half * 512, (half + 1) * 512)
                pys = [K.bank() for _ in range(nst)]
                for f in range(NF):
                    ds_, dsb = dst_r.get()
                    S.dma("sp", ds_[:], wd_in[li, e, f * 128:(f + 1) * 128, hs], dsb)
                    db, dbb = dbf.get()
                    S.op("dve", lambda h: h.tensor_tensor(out=db[:, 0, :], in0=ds_[:], in1=K.gbc[:, 2, hs], op=ALU.mult), r=[dsb, K.gbc_b], w=[dbb])
                    if with_ctx:
                        S.op("dve", lambda h: h.tensor_tensor(out=db[:, 1, :], in0=ds_[:], in1=K.gbc[:, 3, hs], op=ALU.mult), r=[dsb, K.gbc_b], w=[dbb])
                    for st in range(nst):
                        ns_ = 128 if st < 4 else CAPC
                        S.op("pe", lambda h: h.matmul(pys[st][0][0:ns_, :], lhsT=actT[:, f, st * 128:st * 128 + ns_], rhs=db[:, 1 if st == 4 else 0, :], start=(f == 0), stop=(f == NF - 1)),
                             r=[actT_b, dbb], w=[pys[st][1]])
                for st in range(nst):
                    ns_ = 128 if st < 4 else CAPC
                    evac(K, "act", ye[0:ns_, st, hs], pys[st][0][0:ns_, :], r=[pys[st][1]], w=[yeb], scale=ix[0:ns_, st, 5:6], rs=[ixb])
            for st in range(nst):
                ns_ = 128 if st < 4 else CAPC
                S.idma(xacc[:, :], ye[0:ns_, st, :], yeb, idxb, out_off=bass.IndirectOffsetOnAxis(ap=idx[0:ns_, 1, st:st + 1], axis=0), dr=xsc_b, gather=False,
                       compute_op=ALU.add)

        cur = pro1(0)
        xt = pro2(0, cur)
        for e in range(NE):
            gate_up(e, *xt)
            nxt = pro1(e + 1) if e + 1 < NE else None
            down_scatter(e, cur)
            if nxt is not None:
                xt = pro2(e + 1, nxt)
            cur = nxt


def setup_consts(K):
    nc, S = K.nc, K.S
    K.ident = K.sb("ident", [128, 128]); K.ident_b = Buf("ident")
    K.ones = K.sb("ones", [128, 128]); K.ones_b = Buf("ones")
    K.epsc = K.sb("epsc", [128, 1]); K.epsc_b = Buf("epsc")
    K.cond = K.sb("cond", [128, 8, 2]); K.cond_b = Buf("cond")
    K.mv = K.sb("mv", [128, 6, 8, 2]); K.mv_b = Buf("mv")
    K.A1 = K.sb("A1", [128, 8, 2]); K.A2 = K.sb("A2", [128, 8, 2]); K.A_b = Buf("A")
    K.gbc = K.sb("gbc", [128, 4, D]); K.gbc_b = Buf("gbc")
    S.dma("sp", K.ident[:], K.inp("c_ident", [128, 128]), K.ident_b)
    K.identb = K.sb("identb", [128, 128], BF16); K.identb_b = Buf("identb")
    S.op("pool", lambda h: h.tensor_copy(out=K.identb[:], in_=K.ident[:]), r=[K.ident_b], w=[K.identb_b])
    S.op("dve", lambda h: h.memset(K.ones[:], 1.0), w=[K.ones_b])
    K.onesb = K.sb("onesb", [128, 128], BF16); K.onesb_b = Buf("onesb")
    S.op("dve", lambda h: h.memset(K.onesb[:], 1.0), w=[K.onesb_b])
    S.op("dve", lambda h: h.memset(K.epsc[:], EPS), w=[K.epsc_b])
    craw = K.sb("craw", [128, 2, 8]); craw_b = Buf("craw")
    S.dma("sp", craw[:, 0, :], K.inp("cT", [128, 8]), craw_b)
    S.dma("sp", craw[:, 1, :], K.inp("c_ctxT", [128, 8]), craw_b)
    for s in range(2):
        S.op("act", lambda h: h.activation(out=K.cond[:, :, s], in_=craw[:, s, :], func=ACT.Silu), r=[craw_b], w=[K.cond_b])
    S.scope_ents = []


def build_program(debug=None, upto="all", only=None, p_external=False, o_external=False, layers=DEPTH):
    nc = bass.Bass("TRN2", target_bir_lowering=False)
    with ExitStack() as stack:
        K = mk_ctx(nc, stack, debug)
        setup_consts(K)
        R = Ctx()
        R.aff = K.sb("R_aff", [128, NT, 16]); R.aff_b = Buf("R_aff")
        R.code = K.sb("R_code", [128, NT, 16]); R.code_b = Buf("R_code")
        xs0 = K.inp("xs0", [T, D]); xs0_b = Buf("xs0")
        kind1 = "ExternalOutput" if "xs1" in K.debug else "Internal"
        xs1 = nc.dram_tensor("xs1", [T, D], F32, kind=kind1).ap(); xs1_b = Buf("xs1")
        out = nc.dram_tensor("out", [SEQ, D], F32, kind="ExternalOutput").ap(); out_b = Buf("out")
        xn2, xn2_b = K.dram("xn2", [T, D], BF16)
        P = alloc_proj_scratch(K, p_external)
        if o_external:
            O = {n: (K.inp("O_" + n, [256, T], BF16), Buf("O_" + n)) for n in ("s5", "na", "hg", "rt")}
        else:
            O = {n: K.dram("O_" + n, [256, T], BF16) for n in ("s5", "na", "hg", "rt")}
        xcur, xcur_b = xs0, xs0_b
        for li in range(layers):
            last = li == DEPTH - 1
            if not p_external or upto in ("all", "merge", "route"):
                phase0(K, li)
            if not p_external:
                with phase(K):
                    hT = K.sb("hT", [128, 8, T], BF16); hT_b = Buf("hT")
                    norm_to_fm(K, xcur, xcur_b, hT, hT_b, K.A1, 0)
                    dump(K, "dbg_hT", hT[:], hT_b, [128, 8, T], BF16)
                    phase1b(K, li, hT, hT_b, P)
            if upto == "p1":
                break
            if not o_external:
                if only is None or "rt" in only:
                    phase_ret(K, li, P, O["rt"])
                if only is None or "na" in only:
                    phase_na(K, li, P, O["na"])
                if only is None or "hg" in only:
                    phase_hg(K, li, P, O["hg"])
                if only is None or "s5" in only:
                    phase_s5(K, li, P, O["s5"])
            if upto == "mix":
                break
            xnext, xnext_b = (out, out_b) if last else (xs1, xs1_b)
            phase_merge(K, li, P, O, xcur, xcur_b, xnext, xnext_b, last)
            if upto == "merge":
                break
            phase_route(K, li, xnext, xnext_b, CTX if last else 0, xn2, xn2_b, R, not last)
            if upto == "route":
                break
            phase_moe(K, li, xn2, xn2_b, R, xnext, xnext_b, CTX if last else 0, not last)
            xcur, xcur_b = xnext, xnext_b
        K.S.barrier()
    return nc, K


def _pk(v):
    v = np.asarray(v, np.float32)
    return np.ascontiguousarray(v.reshape(-1, 128).T)


def rope_tables():
    t = np.arange(SEQ)
    quarter = 16
    inv = 10000.0 ** (-np.arange(quarter) / quarter)
    ang = np.concatenate([(t // 64)[:, None] * inv, (t % 64)[:, None] * inv], axis=1)
    cos = np.ones((128, T), np.float32)
    sin = np.zeros((128, T), np.float32)
    for p in range(128):
        i = p % 32
        cos[p, CTX:] = np.cos(ang[:, i])
        sin[p, CTX:] = np.sin(ang[:, i])
    return cos, sin


def host_consts():
    g = {}
    g["c_ident"] = np.eye(128, dtype=np.float32)
    j = np.arange(128)[:, None]; t = np.arange(128)[None, :]
    g["c_epos0"] = np.maximum(t - j, 0).astype(np.float32)
    g["c_epos1"] = np.maximum(j - t, 0).astype(np.float32)
    g["c_mk0"] = (t >= j).astype(np.float32)
    g["c_mk1"] = (j >= t).astype(np.float32)
    g["c_iota1"] = np.broadcast_to((np.arange(128) + 1.0)[None, :], (128, 128)).astype(np.float32)
    g["c_iotar"] = np.broadcast_to((128.0 - np.arange(128))[None, :], (128, 128)).astype(np.float32)
    g["c_pcol"] = np.stack([127.0 - np.arange(128), np.arange(128) * 1.0], axis=1).astype(np.float32)
    g["c_blk64"] = ((j // 64) == (t // 64)).astype(np.float32)
    same = (j // 16) == (t // 16)
    g["c_hgm0"] = (same & (j <= t)).astype(np.float32)
    g["c_hgm1"] = (same & (j >= t)).astype(np.float32)
    c = np.arange(8)[None, :]
    tt = np.arange(128)[:, None]
    g["c_blkm"] = np.stack([(tt // 16 == c), (tt // 16 == 7 - c)], axis=1).astype(np.float32)
    rst = np.ones((128, HB), np.float32); rst[:, ::16] = 0.0
    g["c_rst"] = rst
    kcol = np.arange(64)[:, None]; qcol = np.arange(64)[None, :]
    cs = np.clip(qcol - 8, 0, 48)
    ok = (kcol >= cs) & (kcol < cs + 16)
    m = np.where(ok, 0.0, -240000.0).astype(np.float32)
    g["c_namask"] = np.concatenate([m, m], axis=0)
    cos, sin = rope_tables()
    g["rope_cos"] = cos
    g["rope_sin"] = sin
    sel = np.zeros((128, 8, 8, 128), np.float32)
    for gl in range(8):
        for jl in range(8):
            for q in range(16):
                sel[16 * gl + q, gl, jl, 16 * jl + q] = 1.0
    g["c_sel"] = sel.reshape(128, 64, 128).astype(ml_dtypes.bfloat16)
    g["c_selT"] = np.ascontiguousarray(np.transpose(sel, (3, 1, 2, 0))).reshape(128, 64, 128).astype(ml_dtypes.bfloat16)
    jj = (np.arange(2)[None, :, None] * 8 + (np.arange(128) // 16)[:, None, None])
    tt2 = (np.arange(256) // 16)[None, None, :]
    g["c_s5m0"] = (jj <= tt2).astype(np.float32)
    g["c_s5m1"] = (jj >= tt2).astype(np.float32)
    g["c_kA"] = np.broadcast_to((np.arange(32) - 15.0)[None, :], (128, 32)).astype(np.float32)
    g["c_kB"] = np.broadcast_to((16.0 - np.arange(32))[None, :], (128, 32)).astype(np.float32)
    g["c_iota512"] = np.broadcast_to(np.arange(512, dtype=np.float32)[None, :], (128, 512))
    g["c_ptidx"] = np.concatenate([np.arange(128, dtype=np.float32)[:, None], np.broadcast_to(np.arange(NT, dtype=np.float32)[None, :], (128, NT))], axis=1)
    g["c_iotap"] = (np.arange(128)[:, None] + 128.0 * np.arange(4)[None, :]).astype(np.float32)
    e16 = np.zeros((128, 32, 128), np.float32)
    for j in range(16):
        e16[j, j, :] = 1.0
        e16[32 + j, 16 + j, :] = 1.0
    g["c_e16"] = e16
    p = np.arange(128)
    g["c_sel3"] = np.stack([(p < 64) * 1.0, (p >= 64) * 1.0, (p >= 64) * -1.0], axis=1).astype(np.float32)
    return g


def prep_core_inputs(inputs, b, names):
    g = host_consts()
    f32 = lambda a: np.asarray(a, np.float32)
    g["xs0"] = np.concatenate([inputs["ctx"][b], inputs["x"][b]], axis=0).astype(np.float32)
    g["cT"] = _pk(inputs["c"][b])
    g["c_ctxT"] = _pk(inputs["c_ctx"])
    g["ada_w"] = inputs["ada_w"]
    g["ada_bT"] = np.stack([_pk(inputs["ada_b"][l]) for l in range(DEPTH)])
    g["norm_mix_wT"] = np.stack([_pk(inputs["norm_mix_w"][l]) for l in range(DEPTH)])
    g["norm_ffn_wT"] = np.stack([_pk(inputs["norm_ffn_w"][l]) for l in range(DEPTH)])
    g["w_in"] = inputs["w_in"]
    g["ret_bc"] = np.broadcast_to(f32(inputs["ret_decay_logit"]).reshape(DEPTH, 1, 8), (DEPTH, 128, 8))
    qn, kn = f32(inputs["na_q_norm"]), f32(inputs["na_k_norm"])
    g["na_qk_normT"] = np.stack([np.stack([np.tile(qn[l], 2), np.tile(kn[l], 2)], axis=1) for l in range(DEPTH)])
    kcol = np.arange(64)[:, None]; qcol = np.arange(64)[None, :]
    dc = np.clip(kcol - qcol + 15, 0, 30)
    rp = f32(inputs["na_rpb"])[:, :, :, dc]
    rp = np.transpose(rp, (0, 3, 1, 2, 4))
    g["na_rpbT2"] = np.concatenate([rp, rp], axis=1)
    g["w_branch"] = inputs["w_branch"]
    g["w_out"] = inputs["w_out"]
    g["router_wT"] = np.ascontiguousarray(np.transpose(f32(inputs["router_w"]).reshape(DEPTH, 8, 128, NE), (0, 2, 1, 3)))
    g["ex_w_gate"] = inputs["ex_w_gate"]
    g["ex_w_up"] = inputs["ex_w_up"]
    g["ex_w_down"] = inputs["ex_w_down"]
    def nfirst(a):
        a = f32(a)
        a = np.moveaxis(a, 3, 1)
        a = a.reshape(a.shape[0], 64, 32, *a.shape[4:])
        return np.concatenate([a, a], axis=1)
    g["s5_lamT"] = np.stack([nfirst(inputs["s5_lam_re"]), nfirst(inputs["s5_lam_im"])], axis=2)
    g["s5_stepT"] = np.broadcast_to(f32(inputs["s5_log_step"]).reshape(DEPTH, 1, 32), (DEPTH, 128, 32))
    g["s5_BT"] = np.stack([nfirst(inputs["s5_b_re"]), nfirst(inputs["s5_b_im"])], axis=2)
    cre = np.swapaxes(f32(inputs["s5_c_re"]), 3, 4); cim = np.swapaxes(f32(inputs["s5_c_im"]), 3, 4)
    g["s5_CT"] = np.stack([nfirst(cre), nfirst(cim)], axis=2)
    g["s5_dT"] = np.ascontiguousarray(np.transpose(f32(inputs["s5_d"]).reshape(DEPTH, 2, 128), (0, 2, 1)))
    g["s5_glu_w"] = f32(inputs["s5_glu_w"])
    lbp = f32(inputs["hg_lower_bounds"])
    g["hg_lbT"] = np.ascontiguousarray(np.transpose(lbp.reshape(DEPTH, 2, 128), (2, 0, 1)))
    g["hg_norm_wT"] = f32(inputs["hg_norm_w"]).reshape(DEPTH, 64, 1)
    return {k: np.ascontiguousarray(v) for k, v in g.items() if k in names}


def kernel(**inputs):
    nc, K = build_program()
    names = set(K.inputs.keys())
    in_maps = [prep_core_inputs(inputs, b, names) for b in range(8)]
    res = run_bass_kernel_spmd(nc, in_maps, core_ids=list(range(8)))
    return np.stack([np.asarray(r["out"], np.float32) for r in res.results], axis=0)
```
